# Optimizing a Trainium2 kernel written in Bass

```python
import jax, jax.numpy as jnp
from jax import lax
import numpy as np

D_MODEL = 1024
BATCH = 2
SEQ = 8192
DEPTH = 1

HEAD_DIM = 64
N_NSA_HEADS = 8
N_NSA_KV = 2
NSA_GROUP = N_NSA_HEADS // N_NSA_KV
N_FOX_HEADS = 8
D_NSA = N_NSA_HEADS * HEAD_DIM
D_FOX = N_FOX_HEADS * HEAD_DIM
D_MIX = D_NSA + D_FOX
D_KV = N_NSA_KV * HEAD_DIM
D_PROJ = D_NSA + 6 * D_KV + 3 * N_NSA_HEADS + 3 * D_FOX + N_FOX_HEADS
CMP_BLOCK = 32
CMP_STRIDE = 16
CMP_HIDDEN = 128
SEL_BLOCK = 64
SEL_TOPK = 16
WINDOW = 512
Q_BLOCK = 128
N_EXPERTS = 32
TOP_K = 4
D_FF = D_MODEL
SWIGLU_LIMIT = 7.0
SWIGLU_ALPHA = 1.702
PLE_DIM = 256
EPS = 1e-6
NEG = -1e30
FORCED_SCORE = 1e9

kernel_name = "hybrid_nsa_fox_moe_block"


def rmsnorm(x, g):
    xf = x.astype(jnp.float32)
    y = xf * lax.rsqrt(jnp.mean(xf * xf, axis=-1, keepdims=True) + EPS)
    return (y * g.astype(jnp.float32)).astype(x.dtype)


def masked_softmax(logits, mask):
    logits = jnp.where(mask, logits.astype(jnp.float32), NEG)
    m = jnp.max(logits, axis=-1, keepdims=True)
    e = jnp.exp(logits - m) * mask
    return e / jnp.maximum(jnp.sum(e, axis=-1, keepdims=True), 1e-30)


def alibi_slopes(n):
    return jnp.exp2(-8.0 * jnp.arange(1, n + 1, dtype=jnp.float32) / n)


def proj_split_points():
    sizes = [D_NSA, D_KV, D_KV, D_KV, D_KV, D_KV, D_KV, 3 * N_NSA_HEADS,
             D_FOX, D_FOX, D_FOX, N_FOX_HEADS]
    pts, acc = [], 0
    for s in sizes[:-1]:
        acc += s
        pts.append(acc)
    return pts


def compress(kv, w1, w2, pe):
    B, T, G, Dh = kv.shape
    nc = (T - CMP_BLOCK) // CMP_STRIDE + 1
    idx = jnp.arange(nc)[:, None] * CMP_STRIDE + jnp.arange(CMP_BLOCK)[None, :]
    blocks = kv[:, idx] + pe[None, None, :, None, :]
    blocks = jnp.swapaxes(blocks, 2, 3).reshape(B, nc, G, CMP_BLOCK * Dh)
    return jax.nn.gelu(blocks @ w1) @ w2


def nsa_attention(q, k_c, v_c, k_s, v_s, k_w, v_w, gate_logits, slopes):
    B, T, H, Dh = q.shape
    G = k_s.shape[2]
    R = H // G
    scale = Dh ** -0.5
    nc = k_c.shape[1]
    ns = T // SEL_BLOCK
    topk = min(SEL_TOPK, ns)
    cmp_start = jnp.arange(nc) * CMP_STRIDE
    cmp_end = cmp_start + CMP_BLOCK - 1
    sel_start = jnp.arange(ns) * SEL_BLOCK
    overlap = ((cmp_start[:, None] < sel_start[None, :] + SEL_BLOCK)
               & (cmp_end[:, None] >= sel_start[None, :])).astype(jnp.float32)
    ks_blocks = k_s.reshape(B, ns, SEL_BLOCK, G, Dh).transpose(0, 3, 1, 2, 4)
    vs_blocks = v_s.reshape(B, ns, SEL_BLOCK, G, Dh).transpose(0, 3, 1, 2, 4)
    pad = jnp.zeros((B, WINDOW, G, Dh), k_w.dtype)
    kw_pad = jnp.concatenate([pad, k_w], axis=1)
    vw_pad = jnp.concatenate([pad, v_w], axis=1)
    qg = q.reshape(B, T, G, R, Dh)
    gates = jax.nn.sigmoid(gate_logits.reshape(B, T, G, R, 3))
    sl = slopes.reshape(G, R)
    b_ix = jnp.arange(B)[:, None, None, None]
    g_ix = jnp.arange(G)[None, None, :, None]
    j_sel = jnp.arange(ns)
    l_sel = jnp.arange(SEL_BLOCK)

    def block(start):
        qb = lax.dynamic_slice_in_dim(qg, start, Q_BLOCK, 1)
        gb = lax.dynamic_slice_in_dim(gates, start, Q_BLOCK, 1)
        t = start + jnp.arange(Q_BLOCK)
        lc = jnp.einsum('bqgrd,bcgd->bqgrc', qb, k_c).astype(jnp.float32) * scale
        dist_c = (t[:, None] - cmp_end[None, :]).astype(jnp.float32)
        lc = lc - sl[None, None, :, :, None] * dist_c[None, :, None, None, :]
        mask_c = (cmp_end[None, :] <= t[:, None])[None, :, None, None, :]
        p_c = masked_softmax(lc, mask_c)
        o_c = jnp.einsum('bqgrc,bcgd->bqgrd', p_c.astype(v_c.dtype), v_c)
        imp = jnp.einsum('bqgrc,cn->bqgn', p_c, overlap)
        cur = t // SEL_BLOCK
        valid = j_sel[None, :] <= cur[:, None]
        forced = valid & ((j_sel[None, :] == 0) | (j_sel[None, :] == cur[:, None])
                          | (j_sel[None, :] == cur[:, None] - 1))
        score = jnp.where(forced[None, :, None, :], FORCED_SCORE,
                          jnp.where(valid[None, :, None, :], imp, -1.0))
        _, idx = lax.top_k(score, topk)
        ks = ks_blocks[b_ix, g_ix, idx]
        vs = vs_blocks[b_ix, g_ix, idx]
        ls = jnp.einsum('bqgrd,bqgkld->bqgrkl', qb, ks).astype(jnp.float32) * scale
        pos = idx[..., None] * SEL_BLOCK + l_sel
        dist_s = (t[None, :, None, None, None] - pos)[:, :, :, None]
        ls = ls - sl[None, None, :, :, None, None] * dist_s.astype(jnp.float32)
        mask_s = jnp.broadcast_to(dist_s >= 0, ls.shape)
        flat = (B, Q_BLOCK, G, R, topk * SEL_BLOCK)
        p_s = masked_softmax(ls.reshape(flat), mask_s.reshape(flat)).reshape(ls.shape)
        o_s = jnp.einsum('bqgrkl,bqgkld->bqgrd', p_s.astype(vs.dtype), vs)
        kw = lax.dynamic_slice_in_dim(kw_pad, start, Q_BLOCK + WINDOW, 1)
        vw = lax.dynamic_slice_in_dim(vw_pad, start, Q_BLOCK + WINDOW, 1)
        s = start - WINDOW + jnp.arange(Q_BLOCK + WINDOW)
        dist_w = t[:, None] - s[None, :]
        lw = jnp.einsum('bqgrd,bsgd->bqgrs', qb, kw).astype(jnp.float32) * scale
        lw = lw - sl[None, None, :, :, None] * dist_w.astype(jnp.float32)[None, :, None, None, :]
        mask_w = ((dist_w >= 0) & (dist_w < WINDOW) & (s[None, :] >= 0))[None, :, None, None, :]
        p_w = masked_softmax(lw, mask_w)
        o_w = jnp.einsum('bqgrs,bsgd->bqgrd', p_w.astype(vw.dtype), vw)
        o = gb[..., 0:1] * o_c + gb[..., 1:2] * o_s + gb[..., 2:3] * o_w
        return o.reshape(B, Q_BLOCK, H * Dh)

    starts = jnp.arange(T // Q_BLOCK) * Q_BLOCK
    out = lax.map(block, starts)
    return out.transpose(1, 0, 2, 3).reshape(B, T, H * Dh)


def fox_attention(q, k, v, f_logit):
    B, T, H, Dh = q.shape
    scale = Dh ** -0.5
    c = jnp.cumsum(jax.nn.log_sigmoid(f_logit.astype(jnp.float32)), axis=1).transpose(0, 2, 1)
    kpos = jnp.arange(T)

    def block(start):
        qb = lax.dynamic_slice_in_dim(q, start, Q_BLOCK, 1)
        cb = lax.dynamic_slice_in_dim(c, start, Q_BLOCK, 2)
        t = start + jnp.arange(Q_BLOCK)
        l = jnp.einsum('bqhd,bshd->bhqs', qb, k).astype(jnp.float32) * scale
        l = l + (cb[..., None] - c[:, :, None, :])
        mask = (kpos[None, :] <= t[:, None])[None, None]
        pr = masked_softmax(l, mask)
        o = jnp.einsum('bhqs,bshd->bqhd', pr.astype(v.dtype), v)
        return o.reshape(B, Q_BLOCK, H * Dh)

    starts = jnp.arange(T // Q_BLOCK) * Q_BLOCK
    out = lax.map(block, starts)
    return out.transpose(1, 0, 2, 3).reshape(B, T, H * Dh)


def moe(x, w_router, b_router, w_up, b_up, w_down, b_down):
    B, T, D = x.shape
    xf = x.reshape(-1, D)
    n = xf.shape[0]
    logits = (xf @ w_router + b_router).astype(jnp.float32)
    top_v, top_i = lax.top_k(logits, TOP_K)
    w = jax.nn.softmax(top_v, axis=-1)
    flat_e = top_i.reshape(-1)
    order = jnp.argsort(flat_e)
    sorted_e = flat_e[order]
    tok = order // TOP_K
    sizes = jnp.bincount(flat_e, length=N_EXPERTS).astype(jnp.int32)
    xs = xf[tok]
    h = lax.ragged_dot(xs, w_up, sizes) + b_up[sorted_e]
    g, lin = jnp.split(h, 2, axis=-1)
    g = jnp.minimum(g, SWIGLU_LIMIT)
    lin = jnp.clip(lin, -SWIGLU_LIMIT, SWIGLU_LIMIT)
    act = g * jax.nn.sigmoid(SWIGLU_ALPHA * g) * (lin + 1.0)
    y = lax.ragged_dot(act, w_down, sizes) + b_down[sorted_e]
    y = y * w.reshape(-1)[order][:, None].astype(y.dtype)
    return jax.ops.segment_sum(y, tok, num_segments=n).reshape(B, T, D)


def setup_inputs(seed: int = 0) -> dict:
    key = jax.random.key(seed)
    ks = jax.random.split(key, 32)
    L, D, E, F = DEPTH, D_MODEL, N_EXPERTS, D_FF
    nrm = lambda k, shape, fan: jax.random.normal(k, shape, jnp.float32) * (fan ** -0.5)
    gain = lambda k, shape: 1.0 + 0.02 * jax.random.normal(k, shape, jnp.float32)
    small = lambda k, shape, s: s * jax.random.normal(k, shape, jnp.float32)
    return {
        "x": jax.random.normal(ks[0], (BATCH, SEQ, D), jnp.float32),
        "p": jax.random.normal(ks[1], (L, BATCH, SEQ, PLE_DIM), jnp.float32),
        "ln1": gain(ks[2], (L, D)),
        "w_in": nrm(ks[3], (L, D, D_PROJ), D),
        "b_fg": jax.random.uniform(ks[4], (L, N_FOX_HEADS), jnp.float32, 1.0, 5.0),
        "w_cmp1_k": nrm(ks[5], (L, CMP_BLOCK * HEAD_DIM, CMP_HIDDEN), CMP_BLOCK * HEAD_DIM),
        "w_cmp2_k": nrm(ks[6], (L, CMP_HIDDEN, HEAD_DIM), CMP_HIDDEN),
        "pe_cmp_k": small(ks[7], (L, CMP_BLOCK, HEAD_DIM), 0.1),
        "w_cmp1_v": nrm(ks[8], (L, CMP_BLOCK * HEAD_DIM, CMP_HIDDEN), CMP_BLOCK * HEAD_DIM),
        "w_cmp2_v": nrm(ks[9], (L, CMP_HIDDEN, HEAD_DIM), CMP_HIDDEN),
        "pe_cmp_v": small(ks[10], (L, CMP_BLOCK, HEAD_DIM), 0.1),
        "gn_nsa": gain(ks[11], (L, D_NSA)),
        "gn_fox": gain(ks[12], (L, D_FOX)),
        "w_out": nrm(ks[13], (L, D_MIX, D), D_MIX),
        "ln2": gain(ks[14], (L, D)),
        "w_router": nrm(ks[15], (L, D, E), D),
        "b_router": small(ks[16], (L, E), 0.01),
        "w_up": nrm(ks[17], (L, E, D, 2 * F), D),
        "b_up": small(ks[18], (L, E, 2 * F), 0.01),
        "w_down": nrm(ks[19], (L, E, F, D), F),
        "b_down": small(ks[20], (L, E, D), 0.01),
        "ln_ple": gain(ks[21], (L, D)),
        "w_ple": nrm(ks[22], (L, PLE_DIM, D), PLE_DIM),
        "w_ple_gate": nrm(ks[23], (L, D, D), D),
        "ln_f": gain(ks[24], (D,)),
    }


def reference(x, p, ln1, w_in, b_fg, w_cmp1_k, w_cmp2_k, pe_cmp_k, w_cmp1_v, w_cmp2_v,
              pe_cmp_v, gn_nsa, gn_fox, w_out, ln2, w_router, b_router, w_up, b_up,
              w_down, b_down, ln_ple, w_ple, w_ple_gate, ln_f):
    B, T, _ = x.shape
    G, Dh = N_NSA_KV, HEAD_DIM
    slopes = alibi_slopes(N_NSA_HEADS)
    pts = proj_split_points()
    h = x
    for i in range(DEPTH):
        u = rmsnorm(h, ln1[i])
        proj = u @ w_in[i]
        (q_n, k_c, v_c, k_s, v_s, k_w, v_w, g_n,
         q_f, k_f, v_f, f_f) = jnp.split(proj, pts, axis=-1)
        kc = compress(k_c.reshape(B, T, G, Dh), w_cmp1_k[i], w_cmp2_k[i], pe_cmp_k[i])
        vc = compress(v_c.reshape(B, T, G, Dh), w_cmp1_v[i], w_cmp2_v[i], pe_cmp_v[i])
        o_nsa = nsa_attention(q_n.reshape(B, T, N_NSA_HEADS, Dh), kc, vc,
                              k_s.reshape(B, T, G, Dh), v_s.reshape(B, T, G, Dh),
                              k_w.reshape(B, T, G, Dh), v_w.reshape(B, T, G, Dh),
                              g_n.reshape(B, T, N_NSA_HEADS, 3), slopes)
        o_fox = fox_attention(q_f.reshape(B, T, N_FOX_HEADS, Dh),
                              k_f.reshape(B, T, N_FOX_HEADS, Dh),
                              v_f.reshape(B, T, N_FOX_HEADS, Dh),
                              f_f + b_fg[i])
        mix = jnp.concatenate([rmsnorm(o_nsa, gn_nsa[i]), rmsnorm(o_fox, gn_fox[i])], axis=-1)
        h = h + mix @ w_out[i]
        h = h + moe(rmsnorm(h, ln2[i]), w_router[i], b_router[i], w_up[i], b_up[i],
                    w_down[i], b_down[i])
        h = h + (p[i] @ w_ple[i]) * jax.nn.sigmoid(rmsnorm(h, ln_ple[i]) @ w_ple_gate[i])
    return rmsnorm(h, ln_f)
```

```python
from contextlib import ExitStack
import numpy as np
import ml_dtypes
import concourse.bass as bass
import concourse.mybir as mybir
from concourse.bass_utils import run_bass_kernel_spmd

F32 = mybir.dt.float32
BF16 = mybir.dt.bfloat16
AF = mybir.ActivationFunctionType
ALU = mybir.AluOpType
AX = mybir.AxisListType

NCORES = 8
T = 8192
D = 1024
NEGM = -30000.0
EPS = 1e-6
NE = 32
MOE_EXPERTS = 32
CAP = 512


class Dep:
    __slots__ = ("w", "r")

    def __init__(self):
        self.w = None
        self.r = []


class Sched:
    ROLL = 30000

    def __init__(self, nc, n_dma=40):
        self.nc = nc
        self.E = {"pe": nc.tensor, "act": nc.scalar, "dve": nc.vector, "pool": nc.gpsimd, "sp": nc.sync}
        self.csem = {}
        self.cnt = {}
        self.nsem = 0
        for k in ("pe", "act", "dve", "pool"):
            self._new_csem(k)
        self.seen = {k: {} for k in self.E}
        self.dsem = [nc.alloc_semaphore(name=f"dq{i}") for i in range(n_dma)]
        self.dval = [0] * n_dma
        self.dnext = 0
        self.mute = False
        self.ninst = 0

    def _new_csem(self, k):
        self.csem[k] = self.nc.alloc_semaphore(name=f"c{k}{self.nsem}")
        self.nsem += 1
        self.cnt[k] = 0

    def _collect(self, e, R, W):
        evs = []
        for d in R:
            if d.w is not None:
                evs.append(d.w)
        for d in W:
            if d.w is not None:
                evs.append(d.w)
            evs.extend(d.r)
        return evs

    def _wait(self, e, evs):
        eng = self.E[e]
        seen = self.seen[e]
        need = {}
        for (s, v, src) in evs:
            if src == "pe" and e == "pe":
                continue
            if src == e and s is self.csem.get(e) and self.cnt[e] - v >= 3:
                continue
            key = s.num
            if seen.get(key, 0) >= v:
                continue
            if key not in need or need[key][1] < v:
                need[key] = (s, v)
        for key, (s, v) in need.items():
            eng.wait_ge(s, v)
            seen[key] = v
            self.ninst += 1

    def _mark(self, ev, R, W):
        for d in R:
            d.r.append(ev)
            if len(d.r) > 64:
                d.r = d.r[-64:]
        for d in W:
            d.w = ev
            d.r = []

    def op(self, e, fn, R=(), W=()):
        if self.mute:
            return None
        self._wait(e, self._collect(e, R, W))
        ins = fn(self.E[e])
        if self.cnt[e] >= self.ROLL:
            self._new_csem(e)
        self.cnt[e] += 1
        ins.then_inc(self.csem[e], 1)
        ev = (self.csem[e], self.cnt[e], e)
        self._mark(ev, R, W)
        self.ninst += 1
        return ev

    def dma(self, out, in_, R=(), W=(), e="sp"):
        if self.mute:
            return None
        k = self.dnext
        self.dnext = (k + 1) % len(self.dsem)
        if self.dval[k] >= self.ROLL:
            self._wait(e, [(self.dsem[k], self.dval[k], "dma")])
            self.dsem[k] = self.nc.alloc_semaphore(name=f"dq{k}_{self.nsem}")
            self.nsem += 1
            self.dval[k] = 0
        s = self.dsem[k]
        evs = self._collect(e, R, W)
        if self.dval[k] > 0:
            evs.append((s, self.dval[k], "dma"))
        self._wait(e, evs)
        self.E[e].dma_start(out=out, in_=in_).then_inc(s, 16)
        self.dval[k] += 16
        ev = (s, self.dval[k], "dma")
        self._mark(ev, R, W)
        self.ninst += 1
        return ev

    def barrier(self):
        evs = [(self.csem[k], self.cnt[k], k + "_b") for k in self.csem if self.cnt[k] > 0]
        evs += [(self.dsem[k], self.dval[k], "dma") for k in range(len(self.dsem)) if self.dval[k] > 0]
        for e in self.E:
            self._wait(e, [x for x in evs])


def _bf(a):
    return np.ascontiguousarray(a).astype(ml_dtypes.bfloat16)


def build_program(stage=99, n_experts=MOE_EXPERTS, skip123=False, p4stop=99, nblk4=16, skip5=False, skip6=False):
    nc = bass.Bass("TRN2", target_bir_lowering=False)
    S = Sched(nc)

    def din(name, shape, dt=F32):
        return nc.dram_tensor(name, list(shape), dt, kind="ExternalInput").ap()

    dbg = stage < 99

    def dscr(name, shape, dt):
        return nc.dram_tensor(name, list(shape), dt, kind=("ExternalOutput" if dbg else "Internal")).ap()

    xT_d = din("xT", [8, 128, T])
    xTo_d = din("xTo", [8, 128, 2048])
    xo_d = din("xo", [2048, D])
    pTo_d = din("pTo", [2, 128, 2048])
    wA_d = din("wA", [D, 1024])
    wB_d = din("wB", [D, 800])
    wQ_d = din("wQ", [D, 1024])
    ln1c_d = din("ln1c", [128, 8])
    bfg_d = din("bfg", [128, 8])
    w1k_d = din("w1k", [128, 32, 128])
    w1v_d = din("w1v", [128, 32, 128])
    w2k_d = din("w2k", [128, 128])
    w2v_d = din("w2v", [128, 64])
    pek_d = din("pek", [128, 32, 2])
    pev_d = din("pev", [128, 32, 2])
    gn_d = din("gnb", [128, 1024])
    wout_d = din("wout", [D, D])
    ln2_d = din("ln2b", [128, D])
    wr_d = din("wr", [D, 32])
    br_d = din("brb", [128, 32])
    wup_d = din("wup", [n_experts, D, 2048])
    bup_d = din("bupc", [128, NE, 16])
    wdn_d = din("wdn", [n_experts, D, D])
    bdn_d = din("bdn", [NE, D])
    lnp_d = din("lnpb", [128, D])
    wple_d = din("wple", [256, D])
    wpg_d = din("wpg", [D, D])
    lnf_d = din("lnfb", [128, D])
    identb_d = din("identb", [128, 128], BF16)
    identf_d = din("identf", [128, 128])
    tri_d = din("trif", [128, 128])
    diagm_d = din("diagm", [128, 4, 128], BF16)
    winm_d = din("winm", [128, 8, 128], BF16)
    cmask_d = din("cmask", [128, 5, 128], BF16)
    ab_d = din("ab", [128, 8, 64])
    cab_d = din("cab", [128, 8, 16])
    selA_d = din("selA", [128, 16, 128], BF16)
    selB_d = din("selB", [128, 16, 128], BF16)
    wsel_d = din("wsel", [128, 16, 64])
    R_d = din("Rexp", [128, T], BF16)
    ovl_d = din("ovl", [128, 4, 128], BF16)
    iota_d = din("iotac", [128, CAP])

    out_d = nc.dram_tensor("out", [2048, D], F32, kind="ExternalOutput").ap()

    fmS = dscr("fmS", [8, 128, T], BF16)
    tmS = dscr("tmS", [T, 780], BF16)
    qS = dscr("qS", [16, 16, 128, 128], BF16)
    fmS_dep = [Dep() for _ in range(8)]
    tmS_dep = Dep()
    qS_dep = Dep()

    es_all = ExitStack()

    def sb(es, name, shape, dt):
        return es.enter_context(nc.sbuf_tensor("s_" + name, list(shape), dt))

    ps = [es_all.enter_context(nc.psum_tensor(f"ps{i}", [128, 512], F32)) for i in range(7)]
    psd = [Dep() for _ in range(7)]
    psb = es_all.enter_context(nc.psum_tensor("psb", [128, 1024], BF16))
    psb_dep = Dep()

    identb = sb(es_all, "identb", [128, 128], BF16)
    identf = sb(es_all, "identf", [128, 128], F32)
    trif = sb(es_all, "trif", [128, 128], F32)
    onesb = sb(es_all, "onesb", [128, 128], BF16)
    onesf = sb(es_all, "onesf", [128, 128], F32)
    epsc = sb(es_all, "epsc", [128, 1], F32)
    onec = sb(es_all, "onec", [128, 1], F32)
    cst = Dep()
    S.dma(identb[:], identb_d, W=[cst])
    S.dma(identf[:], identf_d, W=[cst])
    S.dma(trif[:], tri_d, W=[cst])
    S.op("dve", lambda e: e.memset(onesb[:], 1.0), W=[cst])
    S.op("dve", lambda e: e.memset(onesf[:], 1.0), W=[cst])
    S.op("dve", lambda e: e.memset(epsc[:], EPS), W=[cst])
    S.op("dve", lambda e: e.memset(onec[:], 1.0), W=[cst])

    es_attn = ExitStack()
    ffall = sb(es_attn, "ffall", [128, 64, 8], F32)
    ffall_dep = Dep()
    gown = sb(es_attn, "gown", [128, 16, 24], F32)
    gown_dep = Dep()

    rr = {"cast": 0, "evac": 0}

    def cast_eng():
        rr["cast"] += 1
        return ("act", "dve", "pool")[rr["cast"] % 3]

    def copy_on(e, out, in_, R, W):
        if e == "act":
            S.op("act", lambda g: g.copy(out=out, in_=in_), R, W)
        elif e == "dve":
            S.op("dve", lambda g: g.tensor_copy(out=out, in_=in_), R, W)
        else:
            S.op("pool", lambda g: g.tensor_copy(out=out, in_=in_), R, W)

    def evac_eng():
        rr["evac"] += 1
        return ("act", "dve")[rr["evac"] % 2]

    def mm(out, lhsT, rhs, start, stop, R, W):
        S.op("pe", lambda g: g.matmul(out, lhsT=lhsT, rhs=rhs, start=start, stop=stop), R, W)

    pctr = [0]
    LAG = 2

    def run_pipe(steps, LAG=1):
        n = len(steps)
        for k in range(n + LAG):
            if k < n:
                steps[k][0]()
            if k - LAG >= 0:
                steps[k - LAG][1]()

    S.mute = skip123
    with ExitStack() as es:
        wA = sb(es, "wA", [128, 8, 1024], BF16)
        wB = sb(es, "wB", [128, 8, 800], BF16)
        wQz = sb(es, "wQz", [128, 8, 16, 128], BF16)
        stg = [sb(es, f"stg{i}", [128, 1024], F32) for i in range(2)]
        stg_dep = [Dep() for _ in range(2)]
        ln1c = sb(es, "ln1c", [128, 8], F32)
        xt = [sb(es, f"xt{i}", [128, 8, 512], F32) for i in range(2)]
        xt_dep = [Dep() for _ in range(2)]
        sq = sb(es, "sq", [128, 8, 512], BF16)
        sq_dep = Dep()
        rstd = sb(es, "rstd", [128, 512], F32)
        rstd_dep = Dep()
        uT = sb(es, "uT", [128, 8, 512], BF16)
        uT_dep = Dep()
        fmo = [sb(es, f"fmo{i}", [128, 8, 512], BF16) for i in range(2)]
        fmo_dep = [Dep() for _ in range(2)]
        tmv = [sb(es, f"tmv{i}", [128, 4, 780], BF16) for i in range(2)]
        tmv_dep = [Dep() for _ in range(2)]
        qo = [sb(es, f"qo{i}", [128, 4, 16, 128], BF16) for i in range(2)]
        qo_dep = [Dep() for _ in range(2)]
        w_dep = Dep()

        S.dma(ln1c[:], ln1c_d, W=[w_dep])
        S.op("pool", lambda e: e.memset(wQz[:], 0.0), W=[w_dep])
        for k in range(2):
            S.op("pool", lambda e: e.memset(tmv[k][:], 1.0), W=[tmv_dep[k]])
        sc = 0
        for dc in range(8):
            for (src, dst, ncol) in ((wA_d, wA, 1024), (wB_d, wB, 800)):
                k = sc % 2
                sc += 1
                S.dma(stg[k][:, 0:ncol], src[dc * 128:(dc + 1) * 128, :], W=[stg_dep[k]])
                copy_on(cast_eng(), dst[:, dc, :], stg[k][:, 0:ncol], [stg_dep[k]], [w_dep])
            k = sc % 2
            sc += 1
            S.dma(stg[k][:, :], wQ_d[dc * 128:(dc + 1) * 128, :], W=[stg_dep[k]])
            copy_on(cast_eng(), wQz[:, dc, 0:4, 0:64],
                    stg[k][:, 0:256].rearrange("p (h e) -> p h e", e=64), [stg_dep[k]], [w_dep])
            copy_on(cast_eng(), wQz[:, dc, 4:8, 64:128],
                    stg[k][:, 256:512].rearrange("p (h e) -> p h e", e=64), [stg_dep[k]], [w_dep])
            fx = stg[k][:, 512:1024].rearrange("p (h two e) -> p h two e", two=2, e=64)
            wz = wQz[:, dc, 8:16, :].rearrange("p (h two) e -> p h two e", two=2)
            copy_on(cast_eng(), wz[:, :, 0, 0:64], fx[:, :, 0, :], [stg_dep[k]], [w_dep])
            copy_on(cast_eng(), wz[:, :, 1, 64:128], fx[:, :, 1, :], [stg_dep[k]], [w_dep])

        def norm_tile(src_ap, k):
            S.dma(xt[k][:], src_ap, W=[xt_dep[k]])
            S.op("act", lambda e: e.activation(out=sq[:], in_=xt[k][:], func=AF.Square), [xt_dep[k]], [sq_dep])
            for dc in range(8):
                mm(ps[0][:, :], onesb[:], sq[:, dc, :], dc == 0, dc == 7, [sq_dep, cst], [psd[0]])
            S.op("act", lambda e: e.activation(out=rstd[:], in_=ps[0][:, :], func=AF.Sqrt,
                                               bias=epsc[:, 0:1], scale=1.0 / D), [psd[0], cst], [rstd_dep])
            S.op("dve", lambda e: e.reciprocal(out=rstd[:], in_=rstd[:]), [rstd_dep], [rstd_dep])
            for dc in range(8):
                S.op("dve", lambda e: e.scalar_tensor_tensor(
                    out=uT[:, dc, :], in0=xt[k][:, dc, :], scalar=ln1c[:, dc:dc + 1], in1=rstd[:],
                    op0=ALU.mult, op1=ALU.mult), [xt_dep[k], rstd_dep, w_dep], [uT_dep])

        xT_v = xT_d.rearrange("c p s -> p c s")
        xTo_v = xTo_d.rearrange("c p s -> p c s")
        fmS_v = fmS.rearrange("o p s -> p o s")
        for Tt in range(16):
            k = Tt % 2
            norm_tile(xT_v[:, :, Tt * 512:(Tt + 1) * 512], k)
            for oc in range(8):
                b = 1 + oc % 2
                for dc in range(8):
                    mm(ps[b][:, :], wA[:, dc, oc * 128:(oc + 1) * 128], uT[:, dc, :], dc == 0, dc == 7,
                       [uT_dep, w_dep], [psd[b]])
                copy_on(evac_eng(), fmo[k][:, oc, :], ps[b][:, :], [psd[b]], [fmo_dep[k]])
            S.dma(fmS_v[:, :, Tt * 512:(Tt + 1) * 512], fmo[k][:], R=[fmo_dep[k]], W=fmS_dep)
            for sub in range(4):
                bA = 3 + (sub % 2) * 2
                bB = bA + 1
                for dc in range(8):
                    mm(ps[bA][:, 0:512], uT[:, dc, sub * 128:(sub + 1) * 128], wB[:, dc, 0:512], dc == 0, dc == 7,
                       [uT_dep, w_dep], [psd[bA]])
                for dc in range(8):
                    mm(ps[bB][:, 0:288], uT[:, dc, sub * 128:(sub + 1) * 128], wB[:, dc, 512:800], dc == 0, dc == 7,
                       [uT_dep, w_dep], [psd[bB]])
                copy_on("act", tmv[k][:, sub, 0:520].rearrange("p (h e) -> p h e", e=65)[:, :, 0:64],
                        ps[bA][:, 0:512].rearrange("p (h e) -> p h e", e=64), [psd[bA]], [tmv_dep[k]])
                copy_on("dve", tmv[k][:, sub, 520:780].rearrange("p (h e) -> p h e", e=65)[:, :, 0:64],
                        ps[bB][:, 0:256].rearrange("p (h e) -> p h e", e=64), [psd[bB]], [tmv_dep[k]])
                copy_on("dve", ffall[:, Tt * 4 + sub, :], ps[bB][:, 256:264], [psd[bB]], [ffall_dep])
            S.dma(tmS[Tt * 512:(Tt + 1) * 512, :].rearrange("(s p) c -> p s c", p=128), tmv[k][:],
                  R=[tmv_dep[k]], W=[tmS_dep])

        qS_v = qS.rearrange("i h p t -> i p h t")
        for T4 in range(4):
            norm_tile(xTo_v[:, :, T4 * 512:(T4 + 1) * 512], T4 % 2)
            k = T4 % 2
            for hd in range(16):
                b = 1 + hd % 2
                for dc in range(8):
                    mm(ps[b][:, :], wQz[:, dc, hd, :], uT[:, dc, :], dc == 0, dc == 7, [uT_dep, w_dep], [psd[b]])
                qdst = qo[k][:, :, hd, :]
                qsrc = ps[b][:, :].rearrange("p (b t) -> p b t", t=128)
                if hd % 2 == 0:
                    S.op("act", lambda e: e.activation(out=qdst, in_=qsrc, func=AF.Copy, scale=0.125), [psd[b]], [qo_dep[k]])
                else:
                    S.op("dve", lambda e: e.tensor_scalar(out=qdst, in0=qsrc, scalar1=0.125, scalar2=None, op0=ALU.mult),
                         [psd[b]], [qo_dep[k]])
            for bi in range(4):
                i = T4 * 4 + bi
                for dc in range(8):
                    mm(ps[3][:, 0:24], uT[:, dc, bi * 128:(bi + 1) * 128], wB[:, dc, 776:800], dc == 0, dc == 7,
                       [uT_dep, w_dep], [psd[3]])
                copy_on("dve", gown[:, i, :], ps[3][:, 0:24], [psd[3]], [gown_dep])
                S.dma(qS_v[i], qo[k][:, bi, :, :], R=[qo_dep[k]], W=[qS_dep])
        S.barrier()

    if stage <= 1:
        dbg_f = nc.dram_tensor("dbg_ff", [128, 64 * 8 + 16 * 24], F32, kind="ExternalOutput").ap()
        S.dma(dbg_f[:, 0:512], ffall[:].rearrange("p a b -> p (a b)"))
        S.dma(dbg_f[:, 512:896], gown[:].rearrange("p a b -> p (a b)"))
        S.barrier()
        es_attn.close()
        es_all.close()
        return nc, S

    mixS = dscr("mixS", [2048, D], BF16)
    mixS_dep = Dep()
    diagm = sb(es_attn, "diagm", [128, 4, 128], BF16)
    S.dma(diagm[:], diagm_d, W=[cst])
    pT = [sb(es_attn, f"pT{i}", [128, 512], BF16) for i in range(4)]
    pT_dep = [Dep() for _ in range(4)]
    zc = [sb(es_attn, f"zc{i}", [128, 4], F32) for i in range(2)]
    zc_dep = [Dep() for _ in range(2)]

    with ExitStack() as es:
        bfg = sb(es, "bfg", [128, 8], F32)
        wsel = sb(es, "wsel", [128, 16, 64], F32)
        lsp = sb(es, "lsp", [128, 64, 8], F32)
        cpcol = sb(es, "cpcol", [128, 64, 8], F32)
        tot = sb(es, "tot", [128, 64, 8], F32)
        pre = sb(es, "pre", [128, 64, 8], F32)
        cpref = sb(es, "cpref", [128, 16, 8], F32)
        tmpw = sb(es, "tmpw", [128, 8, 64], F32)
        cd = Dep()
        S.dma(bfg[:], bfg_d, W=[cd])
        S.dma(wsel[:], wsel_d, W=[cd])
        S.op("dve", lambda e: e.tensor_tensor(out=lsp[:], in0=ffall[:], in1=bfg[:, :].unsqueeze(1).to_broadcast([128, 64, 8]),
                                              op=ALU.add), [ffall_dep, cd], [cd])
        S.op("act", lambda e: e.activation(out=lsp[:], in_=lsp[:], func=AF.Exp, scale=-1.0), [cd], [cd])
        S.op("act", lambda e: e.activation(out=lsp[:], in_=lsp[:], func=AF.Ln, bias=onec[:, 0:1], scale=1.0), [cd, cst], [cd])
        lflat = lsp[:].rearrange("p a b -> p (a b)")
        mm(ps[0][:, :], trif[:], lflat, True, True, [cd, cst], [psd[0]])
        mm(ps[1][:, :], onesf[:], lflat, True, True, [cd, cst], [psd[1]])
        S.op("dve", lambda e: e.tensor_copy(out=tot[:].rearrange("p a b -> p (a b)"), in_=ps[1][:, :]), [psd[1]], [cd])
        S.op("dve", lambda e: e.memset(pre[:, 0, :], 0.0), [], [cd])
        for k in range(1, 64):
            S.op("dve", lambda e: e.tensor_tensor(out=pre[:, k, :], in0=pre[:, k - 1, :], in1=tot[:, k - 1, :], op=ALU.add), [cd], [cd])
        S.op("dve", lambda e: e.tensor_tensor(out=cpcol[:].rearrange("p a b -> p (a b)"), in0=ps[0][:, :],
                                              in1=pre[:].rearrange("p a b -> p (a b)"), op=ALU.add), [psd[0], cd], [cd])
        for i in range(16):
            S.op("dve", lambda e: e.tensor_tensor(out=tmpw[:], in0=tot[:].rearrange("p k h -> p h k"),
                                                  in1=wsel[:, i, :].unsqueeze(1).to_broadcast([128, 8, 64]), op=ALU.mult), [cd], [cd])
            S.op("dve", lambda e: e.reduce_sum(out=cpref[:, i, :], in_=tmpw[:], axis=AX.X), [cd], [cd])

        kT = [sb(es, f"kT{i}", [128, T], BF16) for i in range(2)]
        vP = [sb(es, f"vP{i}", [128, 64, 130], BF16) for i in range(2)]
        qP = [sb(es, f"qP{i}", [128, 16, 2, 128], BF16) for i in range(2)]
        kvq_dep = [Dep() for _ in range(2)]
        wF = [sb(es, f"wF{i}", [128, 64], F32) for i in range(2)]
        wF_dep = [Dep() for _ in range(2)]
        Vp = [sb(es, f"Vp{i}", [128, 64, 65], BF16) for i in range(2)]
        Vp_dep = [Dep() for _ in range(2)]
        mo = [sb(es, f"mo{i}", [128, 128], BF16) for i in range(2)]
        mo_dep = [Dep() for _ in range(2)]
        tmS_v = tmS.rearrange("(k p) c -> p k c", p=128)
        qS_p = qS.rearrange("i h p t -> p i h t")
        items = [(hp, i, hh) for hp in range(4) for i in range(16) for hh in range(2)]

        def fox_prep(n):
            hp, i, hh = items[n]
            kb = hp % 2
            bb = n % 2
            h = 2 * hp + hh
            nk = 4 * i + 4
            if i == 0 and hh == 0:
                S.dma(kT[kb][:], fmS[hp], R=[fmS_dep[hp]], W=[kvq_dep[kb]])
                for q4 in range(4):
                    S.dma(vP[kb][:, q4 * 16:(q4 + 1) * 16, :], tmS_v[:, q4 * 16:(q4 + 1) * 16, hp * 130:(hp + 1) * 130],
                          R=[tmS_dep], W=[kvq_dep[kb]])
                for q2 in range(2):
                    S.dma(qP[kb][:, :, q2, :], qS_p[:, :, 8 + 2 * hp + q2, :], R=[qS_dep], W=[kvq_dep[kb]])
            S.op("dve", lambda e: e.tensor_scalar(out=wF[bb][:, 0:nk], in0=cpcol[:, 0:nk, h], scalar1=cpref[:, i, h:h + 1],
                                                  scalar2=0.0, op0=ALU.subtract, op1=ALU.min), [cd], [wF_dep[bb]])
            S.op("act", lambda e: e.activation(out=wF[bb][:, 0:nk], in_=wF[bb][:, 0:nk], func=AF.Exp), [wF_dep[bb]], [wF_dep[bb]])
            eng = "dve" if n % 2 == 0 else "pool"
            S.op(eng, lambda e: e.tensor_tensor(out=Vp[bb][:, 0:nk, :], in0=vP[kb][:, 0:nk, hh * 65:(hh + 1) * 65],
                                                in1=wF[bb][:, 0:nk].unsqueeze(2).to_broadcast([128, nk, 65]), op=ALU.mult),
                 [kvq_dep[kb], wF_dep[bb]], [Vp_dep[bb]])

        def fox_run(n):
            hp, i, hh = items[n]
            kb = hp % 2
            bb = n % 2
            ob = 4 + n % 2
            mb = i % 2
            nk = 4 * i + 4
            steps = []
            for gq in range(nk // 4):
                bank = pctr[0] % 4
                pctr[0] += 1

                def qk(gq=gq, bank=bank):
                    for q in range(4):
                        k = 4 * gq + q
                        diag = k >= 4 * i
                        mm(ps[bank][:, q * 128:(q + 1) * 128], kT[kb][:, k * 128:(k + 1) * 128], qP[kb][:, i, hh, :], True, not diag,
                           [kvq_dep[kb]], [psd[bank]])
                        if diag:
                            mm(ps[bank][:, q * 128:(q + 1) * 128], identb[:], diagm[:, k - 4 * i, :], False, True, [cst], [psd[bank]])
                    S.op("act", lambda e: e.activation(out=pT[bank][:], in_=ps[bank][:, :], func=AF.Exp), [psd[bank]], [pT_dep[bank]])

                def pv(gq=gq, bank=bank):
                    for q in range(4):
                        k = 4 * gq + q
                        mm(ps[ob][:, 0:65], pT[bank][:, q * 128:(q + 1) * 128], Vp[bb][:, k, :], k == 0, k == nk - 1,
                           [pT_dep[bank], Vp_dep[bb]], [psd[ob]])
                steps.append((qk, pv))
            run_pipe(steps)
            z = zc[bb]
            S.op("dve", lambda e: e.tensor_scalar_max(out=z[:, 0:1], in0=ps[ob][:, 64:65], scalar1=1e-30), [psd[ob]], [zc_dep[bb]])
            S.op("dve", lambda e: e.reciprocal(out=z[:, 0:1], in_=z[:, 0:1]), [zc_dep[bb]], [zc_dep[bb]])
            S.op("dve", lambda e: e.tensor_scalar(out=mo[mb][:, hh * 64:(hh + 1) * 64], in0=ps[ob][:, 0:64],
                                                  scalar1=z[:, 0:1], scalar2=None, op0=ALU.mult),
                 [psd[ob], zc_dep[bb]], [mo_dep[mb]])
            if hh == 1:
                S.dma(mixS[i * 128:(i + 1) * 128, 512 + hp * 128:512 + (hp + 1) * 128], mo[mb][:], R=[mo_dep[mb]], W=[mixS_dep])

        fox_prep(0)
        for n in range(len(items)):
            if n + 1 < len(items):
                fox_prep(n + 1)
            fox_run(n)
        S.barrier()

    if stage <= 2:
        es_attn.close()
        es_all.close()
        return nc, S

    with ExitStack() as es:
        kcT = sb(es, "kcT", [128, 512], BF16)
        vc = sb(es, "vc", [128, 4, 130], BF16)
        kc_dep = Dep()
        S.op("pool", lambda e: e.memset(vc[:], 1.0), [], [kc_dep])
        with ExitStack() as es2:
            kraw = sb(es2, "kraw", [128, T], BF16)
            w1f = sb(es2, "w1f", [128, 32, 128], F32)
            w1b = sb(es2, "w1b", [128, 32, 128], BF16)
            pef = sb(es2, "pef", [128, 32, 2], F32)
            peb = sb(es2, "peb", [128, 32, 2], BF16)
            w2f = sb(es2, "w2f", [128, 128], F32)
            w2b = sb(es2, "w2b", [128, 128], BF16)
            hx = sb(es2, "hx", [128, 512], F32)
            hu = sb(es2, "hu", [128, 512], F32)
            hidT = sb(es2, "hidT", [128, 512], BF16)
            cbias = sb(es2, "cbias", [128, 1], F32)
            cpd = Dep()
            for which in range(2):
                S.dma(kraw[:], fmS[6 + which], R=[fmS_dep[6 + which]], W=[cpd])
                S.dma(w1f[:], (w1k_d, w1v_d)[which], W=[cpd])
                S.dma(pef[:], (pek_d, pev_d)[which], W=[cpd])
                if which == 0:
                    S.dma(w2f[:, :], w2k_d, W=[cpd])
                else:
                    S.dma(w2f[:, 0:64], w2v_d, W=[cpd])
                S.op("dve", lambda e: e.tensor_copy(out=w1b[:], in_=w1f[:]), [cpd], [cpd])
                S.op("dve", lambda e: e.tensor_copy(out=peb[:], in_=pef[:]), [cpd], [cpd])
                S.op("dve", lambda e: e.tensor_copy(out=w2b[:], in_=w2f[:]), [cpd], [cpd])
                for g in range(2):
                    r0, r1 = g * 64, g * 64 + 64
                    for l in range(32):
                        mm(ps[0][:, 0:511], w1b[r0:r1, l, :], kraw[r0:r1, l:l + 16 * 510 + 1:16], l == 0, l == 31, [cpd], [psd[0]])
                    for l in range(32):
                        mm(ps[1][:, 0:2], w1b[r0:r1, l, :], peb[r0:r1, l, :], l == 0, l == 31, [cpd], [psd[1]])
                    S.op("dve", lambda e: e.tensor_copy(out=cbias[:], in_=ps[1][:, 0:1]), [psd[1]], [cpd])
                    S.op("dve", lambda e: e.memset(hx[:], 0.0), [], [cpd])
                    S.op("dve", lambda e: e.tensor_scalar(out=hx[:, 0:511], in0=ps[0][:, 0:511], scalar1=cbias[:, 0:1],
                                                          scalar2=None, op0=ALU.add), [psd[0], cpd], [cpd])
                    S.op("dve", lambda e: e.tensor_tensor(out=hu[:], in0=hx[:], in1=hx[:], op=ALU.mult), [cpd], [cpd])
                    S.op("dve", lambda e: e.tensor_scalar(out=hu[:], in0=hu[:], scalar1=0.044715, scalar2=1.0,
                                                          op0=ALU.mult, op1=ALU.add), [cpd], [cpd])
                    S.op("dve", lambda e: e.tensor_tensor(out=hu[:], in0=hu[:], in1=hx[:], op=ALU.mult), [cpd], [cpd])
                    S.op("act", lambda e: e.activation(out=hu[:], in_=hu[:], func=AF.Exp, scale=-1.5957691216057308), [cpd], [cpd])
                    S.op("dve", lambda e: e.tensor_scalar(out=hu[:], in0=hu[:], scalar1=1.0, scalar2=None, op0=ALU.add), [cpd], [cpd])
                    S.op("dve", lambda e: e.reciprocal(out=hu[:], in_=hu[:]), [cpd], [cpd])
                    S.op("dve", lambda e: e.tensor_tensor(out=hidT[:], in0=hu[:], in1=hx[:], op=ALU.mult), [cpd], [cpd])
                    if which == 0:
                        mm(ps[2][:, 0:512], w2b[:, :], hidT[:], True, True, [cpd], [psd[2]])
                        S.op("dve", lambda e: e.tensor_copy(out=kcT[r0:r1, :], in_=ps[2][r0:r1, 0:512]), [psd[2]], [kc_dep])
                    else:
                        for m in range(4):
                            mm(ps[2][:, m * 64:(m + 1) * 64], hidT[:, m * 128:(m + 1) * 128], w2b[:, 0:64], True, True, [cpd], [psd[2]])
                        S.op("dve", lambda e: e.tensor_copy(out=vc[:, :, g * 65:g * 65 + 64],
                                                            in_=ps[2][:, 0:256].rearrange("p (m e) -> p m e", e=64)), [psd[2]], [kc_dep])
            S.barrier()

        KsT = sb(es, "KsT", [128, T], BF16)
        KwT = sb(es, "KwT", [128, T], BF16)
        Vs = sb(es, "Vs", [128, 64, 130], BF16)
        Vw = sb(es, "Vw", [128, 64, 130], BF16)
        Rx = sb(es, "Rx", [128, T], BF16)
        ovl = sb(es, "ovl", [128, 4, 128], BF16)
        ab = sb(es, "ab", [128, 8, 64], F32)
        cab = sb(es, "cab", [128, 8, 16], F32)
        selA = sb(es, "selA", [128, 16, 128], BF16)
        selB = sb(es, "selB", [128, 16, 128], BF16)
        cmask = sb(es, "cmask", [128, 5, 128], BF16)
        winm = sb(es, "winm", [128, 8, 128], BF16)
        nd = Dep()
        tmS_v = tmS.rearrange("(k p) c -> p k c", p=128)
        S.dma(KsT[:], fmS[4], R=[fmS_dep[4]], W=[nd])
        S.dma(KwT[:], fmS[5], R=[fmS_dep[5]], W=[nd])
        for q4 in range(4):
            S.dma(Vs[:, q4 * 16:(q4 + 1) * 16, :], tmS_v[:, q4 * 16:(q4 + 1) * 16, 520:650], R=[tmS_dep], W=[nd])
            S.dma(Vw[:, q4 * 16:(q4 + 1) * 16, :], tmS_v[:, q4 * 16:(q4 + 1) * 16, 650:780], R=[tmS_dep], W=[nd])
        for (dst, src) in ((Rx, R_d), (ovl, ovl_d), (ab, ab_d), (cab, cab_d), (selA, selA_d), (selB, selB_d),
                           (cmask, cmask_d), (winm, winm_d)):
            S.dma(dst[:], src, W=[nd])
        wab = sb(es, "wab", [128, 8, 64], F32)
        wab_dep = Dep()
        S.op("dve", lambda e: e.tensor_scalar(out=wab[:], in0=ab[:], scalar1=0.0, scalar2=None, op0=ALU.min), [nd], [wab_dep])
        S.op("act", lambda e: e.activation(out=wab[:], in_=wab[:], func=AF.Exp), [wab_dep], [wab_dep])
        Vsp = [sb(es, f"Vsp{i}", [128, 64, 65], BF16) for i in range(2)]
        Vwp = [sb(es, f"Vwp{i}", [128, 8, 65], BF16) for i in range(2)]
        Vxp_dep = [Dep() for _ in range(2)]
        qN = [sb(es, f"qN{i}", [128, 8, 128], BF16) for i in range(2)]
        qN_dep = [Dep() for _ in range(2)]
        eC = [sb(es, f"eC{i}", [128, 4, 128], BF16) for i in range(4)]
        eC_dep = [Dep() for _ in range(4)]
        Ocs = sb(es, "Ocs", [128, 4, 64], F32)
        Ocs_dep = Dep()
        rzc = sb(es, "rzc", [128, 4], F32)
        impacc = sb(es, "impacc", [128, 128], F32)
        score = sb(es, "score", [128, 128], F32)
        sc2 = sb(es, "sc2", [128, 128], F32)
        mx8 = sb(es, "mx8", [128, 8], F32)
        mx8b = sb(es, "mx8b", [128, 8], F32)
        MnegB = sb(es, "MnegB", [128, 128], BF16)
        MnegT = sb(es, "MnegT", [128, 128], BF16)
        MnegT_dep = Dep()
        tk = Dep()
        sgate = sb(es, "sgate", [128, 24], F32)
        sg_dep = Dep()
        coef = sb(es, "coef", [128, 4], F32)
        t1 = sb(es, "t1", [128, 64], F32)
        fin = Dep()
        mon = [sb(es, f"mon{i}", [128, 512], BF16) for i in range(2)]
        mon_dep = [Dep() for _ in range(2)]
        qS_p = qS.rearrange("i h p t -> i p h t")
        sctr = 0

        def nsa_prep(nidx):
            r_ = nidx % 4
            g_ = (nidx // 4) % 2
            i_ = nidx // 8
            h_ = 4 * g_ + r_
            vb_ = nidx % 2
            nk_ = 4 * i_ + 4
            k0_ = max(0, 4 * i_ - 4)
            e1, e2 = ("dve", "pool") if nidx % 2 == 0 else ("pool", "dve")
            S.op(e1, lambda e: e.tensor_tensor(out=Vsp[vb_][:, 0:nk_, :], in0=Vs[:, 0:nk_, g_ * 65:(g_ + 1) * 65],
                                               in1=wab[:, h_, 60 - 4 * i_:64].unsqueeze(2).to_broadcast([128, nk_, 65]), op=ALU.mult),
                 [nd, wab_dep], [Vxp_dep[vb_]])
            S.op(e2, lambda e: e.tensor_tensor(out=Vwp[vb_][:, 0:nk_ - k0_, :], in0=Vw[:, k0_:nk_, g_ * 65:(g_ + 1) * 65],
                                               in1=wab[:, h_, 60 - 4 * i_ + k0_:64].unsqueeze(2).to_broadcast([128, nk_ - k0_, 65]), op=ALU.mult),
                 [nd, wab_dep], [Vxp_dep[vb_]])
        for i in range(16):
            qb = i % 2
            S.dma(qN[qb][:], qS_p[i][:, 0:8, :], R=[qS_dep], W=[qN_dep[qb]])
            S.op("act", lambda e: e.activation(out=sgate[:], in_=gown[:, i, :], func=AF.Exp, scale=-1.0), [gown_dep], [sg_dep])
            S.op("dve", lambda e: e.tensor_scalar(out=sgate[:], in0=sgate[:], scalar1=1.0, scalar2=None, op0=ALU.add), [sg_dep], [sg_dep])
            S.op("dve", lambda e: e.reciprocal(out=sgate[:], in_=sgate[:]), [sg_dep], [sg_dep])
            ncm = i // 4 + 1
            nk = 4 * i + 4
            for g in range(2):
                for r in range(4):
                    h = 4 * g + r
                    for m in range(ncm):
                        d = 4 * m - i
                        partial = d >= -4
                        sbk = sctr % 4
                        sctr += 1
                        mm(ps[sbk][:, 0:128], kcT[:, m * 128:(m + 1) * 128], qN[qb][:, h, :], True, not partial,
                           [kc_dep, qN_dep[qb]], [psd[sbk]])
                        if partial:
                            mm(ps[sbk][:, 0:128], identb[:], cmask[:, d + 4, :], False, True, [cst, nd], [psd[sbk]])
                        S.op("act", lambda e: e.activation(out=eC[r][:, m, :], in_=ps[sbk][:, 0:128], func=AF.Exp,
                                                           bias=cab[:, h, d + 15:d + 16], scale=1.0), [psd[sbk], nd], [eC_dep[r]])
                    for m in range(ncm):
                        mm(ps[4][:, 0:65], eC[r][:, m, :], vc[:, m, g * 65:(g + 1) * 65], m == 0, m == ncm - 1,
                           [eC_dep[r], kc_dep], [psd[4]])
                    for m in range(ncm):
                        mm(ps[5][:, 0:128], eC[r][:, m, :], ovl[:, m, :], m == 0, m == ncm - 1, [eC_dep[r], nd], [psd[5]])
                    S.op("dve", lambda e: e.tensor_scalar_max(out=rzc[:, r:r + 1], in0=ps[4][:, 64:65], scalar1=1e-30), [psd[4]], [tk, fin])
                    S.op("dve", lambda e: e.reciprocal(out=rzc[:, r:r + 1], in_=rzc[:, r:r + 1]), [tk], [tk])
                    S.op("dve", lambda e: e.tensor_copy(out=Ocs[:, r, :], in_=ps[4][:, 0:64]), [psd[4]], [Ocs_dep, fin])
                    if r == 0:
                        S.op("dve", lambda e: e.tensor_scalar(out=impacc[:], in0=ps[5][:, 0:128], scalar1=rzc[:, r:r + 1],
                                                              scalar2=None, op0=ALU.mult), [psd[5], tk], [tk])
                    else:
                        S.op("dve", lambda e: e.scalar_tensor_tensor(out=impacc[:], in0=ps[5][:, 0:128], scalar=rzc[:, r:r + 1],
                                                                     in1=impacc[:], op0=ALU.mult, op1=ALU.add), [psd[5], tk], [tk])
                S.op("dve", lambda e: e.tensor_tensor(out=score[:], in0=impacc[:], in1=selA[:, i, :], op=ALU.mult), [tk, nd], [tk])
                S.op("dve", lambda e: e.tensor_tensor(out=score[:], in0=score[:], in1=selB[:, i, :], op=ALU.add), [tk, nd], [tk])
                S.op("dve", lambda e: e.max(out=mx8[:], in_=score[:]), [tk], [tk])
                S.op("dve", lambda e: e.match_replace(out=sc2[:], in_to_replace=mx8[:], in_values=score[:], imm_value=-2.0), [tk], [tk])
                S.op("dve", lambda e: e.max(out=mx8b[:], in_=sc2[:]), [tk], [tk])
                S.op("dve", lambda e: e.tensor_scalar(out=MnegB[:], in0=score[:], scalar1=mx8b[:, 7:8], scalar2=NEGM,
                                                      op0=ALU.is_lt, op1=ALU.mult), [tk], [tk])
                S.op("pe", lambda e: e.transpose(out=psb[:, 0:128], in_=MnegB[:], identity=identb[:]), [tk, cst], [psb_dep])
                S.op("dve", lambda e: e.tensor_copy(out=MnegT[:], in_=psb[:, 0:128]), [psb_dep], [MnegT_dep])
                for r in range(4):
                    h = 4 * g + r
                    osb = 6 if r % 2 == 0 else 5
                    nidx = (i * 2 + g) * 4 + r
                    vb = nidx % 2
                    if nidx == 0:
                        nsa_prep(0)
                    if nidx + 1 < 128:
                        nsa_prep(nidx + 1)
                    k0 = max(0, 4 * i - 4)
                    steps = []
                    for gq in range(nk // 4):
                        bank = pctr[0] % 4
                        pctr[0] += 1

                        def qk(gq=gq, bank=bank, i=i, h=h, qb=qb):
                            for q in range(4):
                                k = 4 * gq + q
                                diag = k >= 4 * i
                                o = ps[bank][:, q * 128:(q + 1) * 128]
                                mm(o, KsT[:, k * 128:(k + 1) * 128], qN[qb][:, h, :], True, False, [nd, qN_dep[qb]], [psd[bank]])
                                mm(o, Rx[:, k * 128:(k + 1) * 128], MnegT[:], False, not diag, [nd, MnegT_dep], [psd[bank]])
                                if diag:
                                    mm(o, identb[:], diagm[:, k - 4 * i, :], False, True, [cst], [psd[bank]])
                            S.op("act", lambda e: e.activation(out=pT[bank][:], in_=ps[bank][:, :], func=AF.Exp), [psd[bank]], [pT_dep[bank]])

                        def pv(gq=gq, bank=bank, osb=osb, nk=nk, vb=vb):
                            for q in range(4):
                                k = 4 * gq + q
                                mm(ps[osb][:, 0:65], pT[bank][:, q * 128:(q + 1) * 128], Vsp[vb][:, k, :], k == 0, k == nk - 1,
                                   [pT_dep[bank], Vxp_dep[vb]], [psd[osb]])
                        steps.append((qk, pv))
                    for gq in range((nk - k0) // 4):
                        bank = pctr[0] % 4
                        pctr[0] += 1

                        def qk(gq=gq, bank=bank, i=i, h=h, qb=qb, k0=k0):
                            for q in range(4):
                                k = k0 + 4 * gq + q
                                wk = k - (4 * i - 4)
                                o = ps[bank][:, q * 128:(q + 1) * 128]
                                mm(o, KwT[:, k * 128:(k + 1) * 128], qN[qb][:, h, :], True, False, [nd, qN_dep[qb]], [psd[bank]])
                                mm(o, identb[:], winm[:, wk, :], False, True, [cst, nd], [psd[bank]])
                            S.op("act", lambda e: e.activation(out=pT[bank][:], in_=ps[bank][:, :], func=AF.Exp), [psd[bank]], [pT_dep[bank]])

                        def pv(gq=gq, bank=bank, k0=k0, nk=nk, vb=vb):
                            for q in range(4):
                                k = k0 + 4 * gq + q
                                mm(ps[4][:, 0:65], pT[bank][:, q * 128:(q + 1) * 128], Vwp[vb][:, k - k0, :], k == k0, k == nk - 1,
                                   [pT_dep[bank], Vxp_dep[vb]], [psd[4]])
                        steps.append((qk, pv))
                    run_pipe(steps)
                    S.op("dve", lambda e: e.tensor_scalar_max(out=coef[:, 1:2], in0=ps[osb][:, 64:65], scalar1=1e-30), [psd[osb]], [fin])
                    S.op("dve", lambda e: e.tensor_scalar_max(out=coef[:, 2:3], in0=ps[4][:, 64:65], scalar1=1e-30), [psd[4]], [fin])
                    S.op("dve", lambda e: e.reciprocal(out=coef[:, 1:3], in_=coef[:, 1:3]), [fin], [fin])
                    S.op("dve", lambda e: e.tensor_copy(out=coef[:, 0:1], in_=rzc[:, r:r + 1]), [fin, tk], [fin])
                    S.op("dve", lambda e: e.tensor_tensor(out=coef[:, 0:3], in0=coef[:, 0:3], in1=sgate[:, 3 * h:3 * h + 3], op=ALU.mult), [fin, sg_dep], [fin])
                    S.op("dve", lambda e: e.tensor_scalar(out=t1[:], in0=Ocs[:, r, :], scalar1=coef[:, 0:1], scalar2=None, op0=ALU.mult), [fin, Ocs_dep], [fin])
                    S.op("dve", lambda e: e.scalar_tensor_tensor(out=t1[:], in0=ps[osb][:, 0:64], scalar=coef[:, 1:2], in1=t1[:],
                                                                 op0=ALU.mult, op1=ALU.add), [fin, psd[osb]], [fin])
                    S.op("dve", lambda e: e.scalar_tensor_tensor(out=mon[qb][:, h * 64:(h + 1) * 64], in0=ps[4][:, 0:64], scalar=coef[:, 2:3],
                                                                 in1=t1[:], op0=ALU.mult, op1=ALU.add), [fin, psd[4]], [mon_dep[qb], fin])
            S.dma(mixS[i * 128:(i + 1) * 128, 0:512], mon[qb][:], R=[mon_dep[qb]], W=[mixS_dep])
        S.barrier()

    if stage <= 3:
        es_attn.close()
        es_all.close()
        return nc, S

    S.mute = False
    es_attn.close()
    es_tail = ExitStack()
    hres = sb(es_tail, "hres", [128, 16, D], F32)
    hres_dep = [Dep() for _ in range(16)]
    xnT = sb(es_tail, "xntok", [128, 16, D], BF16)
    xnT_dep = Dep()
    gate = sb(es_tail, "gate", [128, 16, NE], F32)
    gate_dep = Dep()
    ssc = sb(es_tail, "ssc", [128, 4], F32)
    nrm = Dep()
    sqt_box = [None]

    def rms_rstd(src, n, col, R):
        sqt = sqt_box[0]
        S.op("dve", lambda e: e.tensor_tensor(out=sqt[:, 0:n], in0=src, in1=src, op=ALU.mult), list(R) + [nrm], [nrm])
        S.op("dve", lambda e: e.reduce_sum(out=ssc[:, col:col + 1], in_=sqt[:, 0:n], axis=AX.X), [nrm], [nrm])
        S.op("act", lambda e: e.activation(out=ssc[:, col:col + 1], in_=ssc[:, col:col + 1], func=AF.Sqrt,
                                           bias=epsc[:, 0:1], scale=1.0 / n), [nrm, cst], [nrm])
        S.op("dve", lambda e: e.reciprocal(out=ssc[:, col:col + 1], in_=ssc[:, col:col + 1]), [nrm], [nrm])

    def load_w_bf16(dst, src, nchunk, ncol, stg, stg_dep, wdep, ctr):
        for dc in range(nchunk):
            k = ctr[0] % len(stg)
            ctr[0] += 1
            S.dma(stg[k][:, 0:ncol], src[dc * 128:(dc + 1) * 128, :], W=[stg_dep[k]])
            copy_on(cast_eng(), dst[:, dc, :], stg[k][:, 0:ncol], [stg_dep[k]], [wdep])

    with ExitStack() as es:
        sqt_box[0] = sb(es, "sqt4", [128, D], F32)
        woutb = sb(es, "woutb", [128, 8, D], BF16)
        stg = [sb(es, f"stg4{i}", [128, D], F32) for i in range(2)]
        stg_dep = [Dep() for _ in range(2)]
        gnb = sb(es, "gnb", [128, D], F32)
        ln2b = sb(es, "ln2b", [128, D], F32)
        wrf = sb(es, "wrf", [128, 8, NE], F32)
        brb = sb(es, "brb", [128, NE], F32)
        bdnf = sb(es, "bdnf", [NE, D], F32)
        mixb = [sb(es, f"mixb{i}", [128, D], BF16) for i in range(2)]
        mixb_dep = [Dep() for _ in range(2)]
        mixn = sb(es, "mixn", [128, D], BF16)
        mixT = sb(es, "mixT", [128, 8, 128], BF16)
        xn = sb(es, "xn", [128, D], F32)
        xnTf = sb(es, "xnTf", [128, 8, 128], F32)
        lg = sb(es, "lg", [128, NE], F32)
        ex = sb(es, "ex", [128, NE], F32)
        mxr = sb(es, "mxr", [128, 8], F32)
        gT = sb(es, "gT", [NE, 128], F32)
        wd = Dep()
        p4 = Dep()
        ctr = [0]
        load_w_bf16(woutb, wout_d, 8, D, stg, stg_dep, wd, ctr)
        S.dma(gnb[:], gn_d, W=[wd])
        S.dma(ln2b[:], ln2_d, W=[wd])
        S.dma(wrf[:], wr_d.rearrange("(c p) e -> p c e", p=128), W=[wd])
        S.dma(brb[:], br_d, W=[wd])
        S.dma(bdnf[:], bdn_d, W=[wd])
        def g4(n):
            if p4stop <= n:
                S.mute = True
        for i in range(nblk4):
            mb = i % 2
            S.mute = False
            S.dma(mixb[mb][:], mixS[i * 128:(i + 1) * 128, :], R=[mixS_dep], W=[mixb_dep[mb]])
            S.dma(hres[:, i, :], xo_d[i * 128:(i + 1) * 128, :], W=[hres_dep[i]])
            g4(1)
            for half in range(2):
                hs = slice(half * 512, (half + 1) * 512)
                rms_rstd(mixb[mb][:, hs], 512, half, [mixb_dep[mb]])
                S.op("dve", lambda e: e.scalar_tensor_tensor(out=mixn[:, hs], in0=mixb[mb][:, hs], scalar=ssc[:, half:half + 1],
                                                             in1=gnb[:, hs], op0=ALU.mult, op1=ALU.mult), [mixb_dep[mb], nrm, wd], [p4])
            g4(2)
            for c in range(8):
                S.op("pe", lambda e: e.transpose(out=psb[:, c * 128:(c + 1) * 128], in_=mixn[:, c * 128:(c + 1) * 128],
                                                 identity=identb[:]), [p4, cst], [psb_dep])
            S.op("act", lambda e: e.copy(out=mixT[:].rearrange("p c t -> p (c t)"), in_=psb[:, :]), [psb_dep], [p4])
            g4(3)
            for half in range(2):
                hs = slice(half * 512, (half + 1) * 512)
                for c in range(8):
                    mm(ps[half][:, :], mixT[:, c, :], woutb[:, c, hs], c == 0, c == 7, [p4, wd], [psd[half]])
                S.op("dve", lambda e: e.tensor_tensor(out=hres[:, i, hs], in0=ps[half][:, :], in1=hres[:, i, hs], op=ALU.add),
                     [psd[half], hres_dep[i]], [hres_dep[i]])
            rms_rstd(hres[:, i, :], D, 2, [hres_dep[i]])
            g4(4)
            S.op("dve", lambda e: e.scalar_tensor_tensor(out=xn[:], in0=hres[:, i, :], scalar=ssc[:, 2:3], in1=ln2b[:],
                                                         op0=ALU.mult, op1=ALU.mult), [hres_dep[i], nrm, wd], [p4])
            S.op("pool", lambda e: e.tensor_copy(out=xnT[:, i, :], in_=xn[:]), [p4], [xnT_dep])
            for c in range(8):
                b = 2 + c // 4
                g4(5)
                S.op("pe", lambda e: e.transpose(out=ps[b][:, (c % 4) * 128:(c % 4 + 1) * 128], in_=xn[:, c * 128:(c + 1) * 128],
                                                 identity=identf[:]), [p4, cst], [psd[b]])
            for b2 in range(2):
                S.op("act", lambda e: e.copy(out=xnTf[:, b2 * 4:(b2 + 1) * 4, :], in_=ps[2 + b2][:, :].rearrange("p (c t) -> p c t", t=128)),
                     [psd[2 + b2]], [p4])
            for c in range(8):
                mm(ps[4][:, 0:NE], xnTf[:, c, :], wrf[:, c, :], c == 0, c == 7, [p4, wd], [psd[4]])
            g4(7)
            S.op("dve", lambda e: e.tensor_tensor(out=lg[:], in0=ps[4][:, 0:NE], in1=brb[:], op=ALU.add), [psd[4], wd], [p4])
            S.op("dve", lambda e: e.max(out=mxr[:], in_=lg[:]), [p4], [p4])
            S.op("dve", lambda e: e.tensor_scalar(out=mxr[:, 4:5], in0=mxr[:, 0:1], scalar1=-1.0, scalar2=None, op0=ALU.mult), [p4], [p4])
            S.op("act", lambda e: e.activation(out=ex[:], in_=lg[:], func=AF.Exp, bias=mxr[:, 4:5], scale=1.0), [p4], [p4])
            S.op("dve", lambda e: e.scalar_tensor_tensor(out=ex[:], in0=lg[:], scalar=mxr[:, 3:4], in1=ex[:],
                                                         op0=ALU.is_ge, op1=ALU.mult), [p4], [p4])
            S.op("dve", lambda e: e.reduce_sum(out=mxr[:, 5:6], in_=ex[:], axis=AX.X), [p4], [p4])
            S.op("dve", lambda e: e.reciprocal(out=mxr[:, 5:6], in_=mxr[:, 5:6]), [p4], [p4])
            S.op("dve", lambda e: e.tensor_scalar(out=gate[:, i, :], in0=ex[:], scalar1=mxr[:, 5:6], scalar2=None, op0=ALU.mult),
                 [p4], [gate_dep])
            g4(8)
        S.mute = False
        S.barrier()

    if stage == 4:
        dbg_h = nc.dram_tensor("dbg_h", [128, 16, D], F32, kind="ExternalOutput").ap()
        dbg_g = nc.dram_tensor("dbg_g", [128, 16, NE], F32, kind="ExternalOutput").ap()
        dbg_x = nc.dram_tensor("dbg_x", [128, 16, D], BF16, kind="ExternalOutput").ap()
        S.dma(dbg_h, hres[:])
        S.dma(dbg_g, gate[:])
        S.dma(dbg_x, xnT[:])
        S.barrier()
        es_tail.close()
        es_all.close()
        return nc, S

    S.mute = skip5
    with ExitStack() as es:
        C = CAP
        NSC = C // 128
        wupb = sb(es, "wupb", [128, 8, 2048], BF16)
        wdnb = sb(es, "wdnb", [128, 8, D], BF16)
        wup_dep = Dep()
        wdn_dep = Dep()
        NSTG5 = 4
        stg = [sb(es, f"stg5{i}", [128, 512], F32) for i in range(NSTG5)]
        stg_dep = [Dep() for _ in range(NSTG5)]
        bupc = sb(es, "bupc", [128, NE, 16], F32)
        browb = sb(es, "browb", [1, D], BF16)
        brow_dep = Dep()
        bd = Dep()
        S.dma(bupc[:], bup_d, W=[bd])
        iotac = sb(es, "iotac", [128, C], F32)
        S.dma(iotac[:], iota_d, W=[bd])
        Mf = sb(es, "Mf", [128, 16, NE], F32)
        pos = sb(es, "pos", [128, 16, NE], F32)
        dsp = Dep()
        with ExitStack() as est:
            trisb = sb(est, "trisb", [128, 128], BF16)
            Mb = sb(est, "Mb", [128, 16, NE], BF16)
            tot5 = sb(est, "tot5", [128, 16, NE], F32)
            pre5 = sb(est, "pre5", [128, 16, NE], F32)
            S.op("dve", lambda g: g.tensor_tensor(out=trisb[:], in0=trif[:], in1=identf[:], op=ALU.subtract), [cst], [dsp])
            S.op("dve", lambda g: g.tensor_scalar(out=Mf[:], in0=gate[:], scalar1=0.0, scalar2=None, op0=ALU.is_gt), [gate_dep], [dsp])
            S.op("dve", lambda g: g.tensor_copy(out=Mb[:], in_=Mf[:]), [dsp], [dsp])
            mflat = Mb[:].rearrange("p a b -> p (a b)")
            mm(ps[0][:, :], trisb[:], mflat, True, True, [dsp], [psd[0]])
            mm(ps[1][:, :], onesb[:], mflat, True, True, [dsp, cst], [psd[1]])
            S.op("dve", lambda g: g.tensor_copy(out=tot5[:].rearrange("p a b -> p (a b)"), in_=ps[1][:, :]), [psd[1]], [dsp])
            S.op("dve", lambda g: g.memset(pre5[:, 0, :], 0.0), [], [dsp])
            for k in range(1, 16):
                S.op("dve", lambda g: g.tensor_tensor(out=pre5[:, k, :], in0=pre5[:, k - 1, :], in1=tot5[:, k - 1, :], op=ALU.add), [dsp], [dsp])
            S.op("dve", lambda g: g.tensor_tensor(out=pos[:].rearrange("p a b -> p (a b)"), in0=ps[0][:, :],
                                                  in1=pre5[:].rearrange("p a b -> p (a b)"), op=ALU.add), [psd[0], dsp], [dsp])
            S.barrier()

        Sel = sb(es, "Sel", [128, 16, C], BF16)
        Sel_dep = Dep()
        SelTb = [sb(es, f"SelTb{i}", [128, NSC, 128], BF16) for i in range(2)]
        SelTb_dep = [Dep() for _ in range(2)]
        XeT = sb(es, "XeT", [128, 8, C], BF16)
        XeT_dep = Dep()
        actT = sb(es, "actT5", [128, 8, C], BF16)
        actT_dep = Dep()
        assert NSC * D == 8 * C
        ye = XeT[:].rearrange("p (s two) c -> p s (two c)", two=2)
        ye_dep = XeT_dep
        gcs = [sb(es, f"gcs{i}", [128, C], F32) for i in range(1)] * 2
        sgs = [sb(es, f"sgs{i}", [128, C], F32) for i in range(1)] * 2
        lcs = [sb(es, f"lcs{i}", [128, C], F32) for i in range(1)] * 2
        gc_dep = [Dep()] * 2
        sg_dep5 = [Dep()] * 2
        lc_dep = [Dep()] * 2
        sctr = [0]

        def load_up(e):
            for dc in range(8):
                for half in range(4):
                    k = sctr[0] % NSTG5
                    sctr[0] += 1
                    S.dma(stg[k][:], wup_d[e, dc * 128:(dc + 1) * 128, half * 512:(half + 1) * 512], W=[stg_dep[k]])
                    copy_on("pool" if k % 2 == 0 else "act", wupb[:, dc, half * 512:(half + 1) * 512], stg[k][:], [stg_dep[k]], [wup_dep])

        def load_dn(e):
            for fc in range(8):
                for half in range(2):
                    k = sctr[0] % NSTG5
                    sctr[0] += 1
                    S.dma(stg[k][:], wdn_d[e, fc * 128:(fc + 1) * 128, half * 512:(half + 1) * 512], W=[stg_dep[k]])
                    copy_on("pool" if k % 2 == 0 else "act", wdnb[:, fc, half * 512:(half + 1) * 512], stg[k][:], [stg_dep[k]], [wdn_dep])

        load_up(0)
        load_dn(0)
        uc = 0
        dcn = 0
        tcn = 0
        for e_ in range(n_experts):
            for half in range(2):
                kk = sctr[0] % NSTG5
                sctr[0] += 1
                S.dma(stg[kk][0:1, :], bdn_d[e_:e_ + 1, half * 512:(half + 1) * 512], W=[stg_dep[kk]])
                S.op("act", lambda g: g.copy(out=browb[0:1, half * 512:(half + 1) * 512], in_=stg[kk][0:1, :]), [stg_dep[kk]], [brow_dep])
            for blk in range(16):
                S.op("dve", lambda g: g.tensor_scalar(out=Sel[:, blk, :], in0=iotac[:], scalar1=pos[:, blk, e_:e_ + 1],
                                                      scalar2=Mf[:, blk, e_:e_ + 1], op0=ALU.is_equal, op1=ALU.mult), [dsp, bd], [Sel_dep])
            for dc in range(8):
                bX = 4 + dcn % 3
                dcn += 1
                for blk in range(16):
                    mm(ps[bX][:, 0:C], xnT[:, blk, dc * 128:(dc + 1) * 128], Sel[:, blk, :], blk == 0, blk == 15,
                       [xnT_dep, Sel_dep], [psd[bX]])
                S.op("act", lambda g: g.copy(out=XeT[:, dc, :], in_=ps[bX][:, 0:C]), [psd[bX]], [XeT_dep])
            for fc in range(8):
                bG = (uc % 2) * 2
                bL = bG + 1
                tb = uc % 2
                uc += 1
                for dc in range(8):
                    mm(ps[bG][:, 0:C], wupb[:, dc, fc * 128:(fc + 1) * 128], XeT[:, dc, :], dc == 0, dc == 7, [wup_dep, XeT_dep], [psd[bG]])
                for dc in range(8):
                    mm(ps[bL][:, 0:C], wupb[:, dc, 1024 + fc * 128:1024 + (fc + 1) * 128], XeT[:, dc, :], dc == 0, dc == 7,
                       [wup_dep, XeT_dep], [psd[bL]])
                S.op("dve", lambda g: g.tensor_scalar(out=gcs[tb][:], in0=ps[bG][:, 0:C], scalar1=bupc[:, e_, fc:fc + 1], scalar2=7.0,
                                                      op0=ALU.add, op1=ALU.min), [psd[bG], bd], [gc_dep[tb]])
                S.op("act", lambda g: g.activation(out=sgs[tb][:], in_=gcs[tb][:], func=AF.Sigmoid, scale=1.702), [gc_dep[tb]], [sg_dep5[tb]])
                S.op("dve", lambda g: g.tensor_scalar(out=lcs[tb][:], in0=ps[bL][:, 0:C], scalar1=bupc[:, e_, 8 + fc:9 + fc], scalar2=7.0,
                                                      op0=ALU.add, op1=ALU.min), [psd[bL], bd], [lc_dep[tb]])
                S.op("dve", lambda g: g.tensor_scalar(out=lcs[tb][:], in0=lcs[tb][:], scalar1=-7.0, scalar2=1.0,
                                                      op0=ALU.max, op1=ALU.add), [lc_dep[tb]], [lc_dep[tb]])
                S.op("pool", lambda g: g.tensor_tensor(out=gcs[tb][:], in0=gcs[tb][:], in1=sgs[tb][:], op=ALU.mult), [sg_dep5[tb]], [gc_dep[tb]])
                S.op("pool", lambda g: g.tensor_tensor(out=actT[:, fc, :], in0=gcs[tb][:], in1=lcs[tb][:], op=ALU.mult),
                     [gc_dep[tb], lc_dep[tb]], [actT_dep])
            if e_ + 1 < n_experts:
                load_up(e_ + 1)
            for sc in range(NSC):
                for half in range(2):
                    hs = slice(half * 512, (half + 1) * 512)
                    bD = 4 + dcn % 3
                    dcn += 1
                    for fc in range(8):
                        mm(ps[bD][:, :], actT[:, fc, sc * 128:(sc + 1) * 128], wdnb[:, fc, hs], fc == 0, False,
                           [actT_dep, wdn_dep], [psd[bD]])
                    mm(ps[bD][:, :], onesb[0:1, :], browb[0:1, hs], False, True, [brow_dep, cst], [psd[bD]])
                    S.op("act", lambda g: g.copy(out=ye[:, sc, hs], in_=ps[bD][:, :]), [psd[bD]], [ye_dep])
            if e_ + 1 < n_experts:
                load_dn(e_ + 1)
            for blk in range(16):
                tbf = tcn % 2
                tcn += 1
                for sc in range(NSC):
                    S.op("pe", lambda g: g.transpose(out=psb[:, sc * 128:(sc + 1) * 128], in_=Sel[:, blk, sc * 128:(sc + 1) * 128],
                                                     identity=identb[:]), [Sel_dep, cst], [psb_dep])
                S.op("act", lambda g: g.copy(out=SelTb[tbf][:].rearrange("p c t -> p (c t)"), in_=psb[:, 0:NSC * 128]), [psb_dep], [SelTb_dep[tbf]])
                for half in range(2):
                    hs = slice(half * 512, (half + 1) * 512)
                    bY = 4 + dcn % 3
                    dcn += 1
                    for sc in range(NSC):
                        mm(ps[bY][:, :], SelTb[tbf][:, sc, :], ye[:, sc, hs], sc == 0, sc == NSC - 1, [SelTb_dep[tbf], ye_dep], [psd[bY]])
                    S.op("dve", lambda g: g.scalar_tensor_tensor(out=hres[:, blk, hs], in0=ps[bY][:, :], scalar=gate[:, blk, e_:e_ + 1],
                                                                 in1=hres[:, blk, hs], op0=ALU.mult, op1=ALU.add),
                         [psd[bY], gate_dep, hres_dep[blk]], [hres_dep[blk]])
        S.barrier()

    S.mute = skip6
    with ExitStack() as es:
        sqt_box[0] = sb(es, "sqt6", [128, D], F32)
        wpgb = sb(es, "wpgb", [128, 8, D], BF16)
        wpleb = sb(es, "wpleb", [128, 2, D], BF16)
        pTb = sb(es, "pTb", [128, 2, 2048], BF16)
        stg = [sb(es, f"stg6{i}", [128, 2048], F32) for i in range(2)]
        stg_dep = [Dep() for _ in range(2)]
        lnpb = sb(es, "lnpb", [128, D], F32)
        lnfb = sb(es, "lnfb", [128, D], F32)
        hn = sb(es, "hn", [128, D], BF16)
        hnT = sb(es, "hnT", [128, 8, 128], BF16)
        sig = [sb(es, f"sig{i}", [128, 512], F32) for i in range(2)]
        outt = [sb(es, f"outt{i}", [128, D], F32) for i in range(2)]
        outt_dep = [Dep() for _ in range(2)]
        wd = Dep()
        p6 = Dep()
        out_dep = Dep()
        ctr = [0]
        load_w_bf16(wpgb, wpg_d, 8, D, stg, stg_dep, wd, ctr)
        load_w_bf16(wpleb, wple_d, 2, D, stg, stg_dep, wd, ctr)
        for c2 in range(2):
            k = ctr[0] % 2
            ctr[0] += 1
            S.dma(stg[k][:], pTo_d[c2], W=[stg_dep[k]])
            copy_on(cast_eng(), pTb[:, c2, :], stg[k][:], [stg_dep[k]], [wd])
        S.dma(lnpb[:], lnp_d, W=[wd])
        S.dma(lnfb[:], lnf_d, W=[wd])
        for i in range(16):
            ob = i % 2
            rms_rstd(hres[:, i, :], D, 0, [hres_dep[i]])
            S.op("dve", lambda e: e.scalar_tensor_tensor(out=hn[:], in0=hres[:, i, :], scalar=ssc[:, 0:1], in1=lnpb[:],
                                                         op0=ALU.mult, op1=ALU.mult), [hres_dep[i], nrm, wd], [p6])
            for c in range(8):
                S.op("pe", lambda e: e.transpose(out=psb[:, c * 128:(c + 1) * 128], in_=hn[:, c * 128:(c + 1) * 128],
                                                 identity=identb[:]), [p6, cst], [psb_dep])
            S.op("act", lambda e: e.copy(out=hnT[:].rearrange("p c t -> p (c t)"), in_=psb[:, :]), [psb_dep], [p6])
            for half in range(2):
                hs = slice(half * 512, (half + 1) * 512)
                for c in range(8):
                    mm(ps[half][:, :], hnT[:, c, :], wpgb[:, c, hs], c == 0, c == 7, [p6, wd], [psd[half]])
                for c2 in range(2):
                    mm(ps[2 + half][:, :], pTb[:, c2, i * 128:(i + 1) * 128], wpleb[:, c2, hs], c2 == 0, c2 == 1, [wd], [psd[2 + half]])
                S.op("act", lambda e: e.activation(out=sig[half][:], in_=ps[half][:, :], func=AF.Exp, scale=-1.0), [psd[half]], [p6])
                S.op("dve", lambda e: e.tensor_scalar(out=sig[half][:], in0=sig[half][:], scalar1=1.0, scalar2=None, op0=ALU.add), [p6], [p6])
                S.op("dve", lambda e: e.reciprocal(out=sig[half][:], in_=sig[half][:]), [p6], [p6])
                S.op("dve", lambda e: e.tensor_tensor(out=sig[half][:], in0=ps[2 + half][:, :], in1=sig[half][:], op=ALU.mult),
                     [psd[2 + half], p6], [p6])
                S.op("dve", lambda e: e.tensor_tensor(out=hres[:, i, hs], in0=sig[half][:], in1=hres[:, i, hs], op=ALU.add),
                     [p6, hres_dep[i], nrm], [hres_dep[i]])
            rms_rstd(hres[:, i, :], D, 1, [hres_dep[i]])
            S.op("dve", lambda e: e.scalar_tensor_tensor(out=outt[ob][:], in0=hres[:, i, :], scalar=ssc[:, 1:2], in1=lnfb[:],
                                                         op0=ALU.mult, op1=ALU.mult), [hres_dep[i], nrm, wd], [outt_dep[ob]])
            S.dma(out_d[i * 128:(i + 1) * 128, :], outt[ob][:], R=[outt_dep[ob]], W=[out_dep])
        S.barrier()
    es_tail.close()

    if stage <= 3:
        dbg_f = nc.dram_tensor("dbg_ff", [128, 64 * 8 + 16 * 24], F32, kind="ExternalOutput").ap()
        S.dma(dbg_f[:, 0:512], ffall[:].rearrange("p a b -> p (a b)"))
        S.dma(dbg_f[:, 512:896], gown[:].rearrange("p a b -> p (a b)"))
        S.barrier()
        es_all.close()
        return nc, S

    es_all.close()
    return nc, S


def own_tokens(j):
    return np.concatenate([np.arange(512 * i + 128 * j, 512 * i + 128 * j + 128) for i in range(16)])


def const_tables(j):
    p = np.arange(128)
    c = {}
    c["identb"] = _bf(np.eye(128, dtype=np.float32))
    c["identf"] = np.eye(128, dtype=np.float32)
    c["trif"] = (p[:, None] <= p[None, :]).astype(np.float32)
    sl = p[:, None]
    tl = p[None, :]
    dm = np.zeros((128, 4, 128), np.float32)
    for kk in range(4):
        dist = 128 * (j - kk) + tl - sl
        dm[:, kk, :] = np.where(dist >= 0, 0.0, NEGM)
    c["diagm"] = _bf(dm)
    wm = np.zeros((128, 8, 128), np.float32)
    for wk in range(8):
        dist = 128 * (j + 4 - wk) + tl - sl
        wm[:, wk, :] = np.where((dist >= 0) & (dist < 512), 0.0, NEGM)
    c["winm"] = _bf(wm)
    cm = np.zeros((128, 5, 128), np.float32)
    for dd in range(5):
        d = dd - 4
        cond = (512 * d + 16 * sl - tl - 128 * j + 31) <= 0
        cm[:, dd, :] = np.where(cond, 0.0, NEGM)
    c["cmask"] = _bf(cm)
    slopes = np.exp2(-8.0 * np.arange(1, 9, dtype=np.float32) / 8).astype(np.float32)
    rel = np.arange(64)
    ab = slopes[None, :, None] * (p[:, None, None] - 127 - 128 * (rel[None, None, :] + j - 3))
    c["ab"] = np.ascontiguousarray(ab[:, :, ::-1]).astype(np.float32)
    dd = np.arange(16) - 15
    cab = slopes[None, :, None] * (16 * p[:, None, None] + 512 * dd[None, None, :] - 128 * j - 96)
    c["cab"] = cab.astype(np.float32)
    n = np.arange(128)
    selA = np.zeros((128, 16, 128), np.float32)
    selB = np.zeros((128, 16, 128), np.float32)
    for i in range(16):
        cur = (512 * i + 128 * j + p) // 64
        valid = n[None, :] <= cur[:, None]
        forced = valid & ((n[None, :] == 0) | (n[None, :] == cur[:, None]) | (n[None, :] == cur[:, None] - 1))
        selA[:, i, :] = (valid & ~forced)
        selB[:, i, :] = np.where(forced, 1e9, np.where(valid, 0.0, -1.0))
    c["selA"] = _bf(selA)
    c["selB"] = _bf(selB)
    ws = np.zeros((128, 16, 64), np.float32)
    for i in range(16):
        ws[:, i, :] = (np.arange(64)[None, :] <= 4 * i + j)
    c["wsel"] = ws
    s = np.arange(T)
    c["Rexp"] = _bf((n[:, None] == (s[None, :] // 64)).astype(np.float32))
    cc = np.arange(512)
    ov = ((cc[:, None] * 16 < n[None, :] * 64 + 64) & (cc[:, None] * 16 + 31 >= n[None, :] * 64)).astype(np.float32)
    ov[511, :] = 0.0
    c["ovl"] = _bf(ov.reshape(4, 128, 128).transpose(1, 0, 2))
    c["iotac"] = np.ascontiguousarray(np.broadcast_to(np.arange(CAP, dtype=np.float32)[None, :], (128, CAP)))
    return c


def make_in_maps(x, p, ln1, w_in, b_fg, w_cmp1_k, w_cmp2_k, pe_cmp_k, w_cmp1_v, w_cmp2_v, pe_cmp_v, gn_nsa, gn_fox,
                 w_out, ln2, w_router, b_router, w_up, b_up, w_down, b_down, ln_ple, w_ple, w_ple_gate, ln_f, ne=NE):
    f = lambda a: np.ascontiguousarray(np.asarray(a, dtype=np.float32))
    x = f(x); p = f(p); w = f(w_in)[0]
    q_n = w[:, 0:512]; k_c = w[:, 512:640]; v_c = w[:, 640:768]; k_s = w[:, 768:896]; v_s = w[:, 896:1024]
    k_w = w[:, 1024:1152]; v_w = w[:, 1152:1280]; g_n = w[:, 1280:1304]; q_f = w[:, 1304:1816]
    k_f = w[:, 1816:2328]; v_f = w[:, 2328:2840]; f_f = w[:, 2840:2848]
    bc = lambda v: f(np.broadcast_to(np.asarray(v, np.float32).reshape(1, -1), (128, np.asarray(v).size)))
    shared = {
        "wA": f(np.concatenate([k_f, k_s, k_w, k_c, v_c], 1)),
        "wB": f(np.concatenate([v_f, v_s, v_w, f_f, g_n], 1)),
        "wQ": f(np.concatenate([q_n, q_f], 1)),
        "ln1c": f(np.asarray(ln1, np.float32)[0].reshape(8, 128).T),
        "bfg": bc(np.asarray(b_fg)[0]),
        "gnb": bc(np.concatenate([np.asarray(gn_nsa)[0], np.asarray(gn_fox)[0]])),
        "wout": f(w_out)[0], "ln2b": bc(np.asarray(ln2)[0]), "wr": f(w_router)[0], "brb": bc(np.asarray(b_router)[0]),
        "wup": f(np.asarray(w_up)[0, :ne]), "wdn": f(np.asarray(w_down)[0, :ne]), "bdn": f(np.asarray(b_down)[0]),
        "bupc": f(np.asarray(b_up, np.float32)[0].reshape(NE, 16, 128).transpose(2, 0, 1)),
        "lnpb": bc(np.asarray(ln_ple)[0]), "wple": f(w_ple)[0], "wpg": f(w_ple_gate)[0], "lnfb": bc(np.asarray(ln_f)),
    }
    for nm, w1, w2, pe in (("k", w_cmp1_k, w_cmp2_k, pe_cmp_k), ("v", w_cmp1_v, w_cmp2_v, pe_cmp_v)):
        w1r = np.asarray(w1, np.float32)[0].reshape(32, 64, 128).transpose(1, 0, 2)
        shared["w1" + nm] = f(np.concatenate([w1r, w1r], 0))
        peT = np.asarray(pe, np.float32)[0].T
        peT = np.concatenate([peT, peT], 0)
        shared["pe" + nm] = f(np.stack([peT, peT], -1))
    w2k = np.asarray(w_cmp2_k, np.float32)[0]
    shared["w2k"] = f(np.concatenate([w2k, w2k], 1))
    shared["w2v"] = f(np.asarray(w_cmp2_v, np.float32)[0])
    maps = []
    for c in range(NCORES):
        b, j = c // 4, c % 4
        tok = own_tokens(j)
        m = dict(shared)
        m["xT"] = f(x[b].T.reshape(8, 128, T))
        m["xTo"] = f(x[b][tok].T.reshape(8, 128, 2048))
        m["xo"] = f(x[b][tok])
        m["pTo"] = f(p[0, b][tok].T.reshape(2, 128, 2048))
        m.update(const_tables(j))
        maps.append(m)
    return maps


_CACHE = {}


def kernel(**inputs):
    maps = make_in_maps(**inputs)
    if "nc" not in _CACHE:
        _CACHE["nc"] = build_program()[0]
    nc = _CACHE["nc"]
    res = run_bass_kernel_spmd(nc, maps, core_ids=list(range(NCORES)))
    out = np.zeros((2, T, D), np.float32)
    for c in range(NCORES):
        b, j = c // 4, c % 4
        out[b, own_tokens(j)] = np.asarray(res.results[c]["out"], np.float32).reshape(2048, D)
    return out
```

```python
from contextlib import ExitStack
import numpy as np
import ml_dtypes
import concourse.bass as bass
import concourse.mybir as mybir
from concourse.bass_utils import run_bass_kernel_spmd

F32 = mybir.dt.float32
BF16 = mybir.dt.bfloat16
AF = mybir.ActivationFunctionType
ALU = mybir.AluOpType
AX = mybir.AxisListType

NCORES = 8
T = 8192
D = 1024
NEGM = -30000.0
EPS = 1e-6
NE = 32
MOE_EXPERTS = 32
CAP = 512


class Dep:
    __slots__ = ("w", "r")

    def __init__(self):
        self.w = None
        self.r = []


class Sched:
    ROLL = 30000

    def __init__(self, nc, n_dma=40):
        self.nc = nc
        self.E = {"pe": nc.tensor, "act": nc.scalar, "dve": nc.vector, "pool": nc.gpsimd, "sp": nc.sync}
        self.csem = {}
        self.cnt = {}
        self.nsem = 0
        for k in ("pe", "act", "dve", "pool"):
            self._new_csem(k)
        self.seen = {k: {} for k in self.E}
        self.dsem = [nc.alloc_semaphore(name=f"dq{i}") for i in range(n_dma)]
        self.dval = [0] * n_dma
        self.dnext = 0
        self.mute = False
        self.ninst = 0

    def _new_csem(self, k):
        self.csem[k] = self.nc.alloc_semaphore(name=f"c{k}{self.nsem}")
        self.nsem += 1
        self.cnt[k] = 0

    def _collect(self, e, R, W):
        evs = []
        for d in R:
            if d.w is not None:
                evs.append(d.w)
        for d in W:
            if d.w is not None:
                evs.append(d.w)
            evs.extend(d.r)
        return evs

    def _wait(self, e, evs):
        eng = self.E[e]
        seen = self.seen[e]
        need = {}
        for (s, v, src) in evs:
            if src == "pe" and e == "pe":
                continue
            if src == e and s is self.csem.get(e) and self.cnt[e] - v >= 3:
                continue
            key = s.num
            if seen.get(key, 0) >= v:
                continue
            if key not in need or need[key][1] < v:
                need[key] = (s, v)
        for key, (s, v) in need.items():
            eng.wait_ge(s, v)
            seen[key] = v
            self.ninst += 1

    def _mark(self, ev, R, W):
        for d in R:
            d.r.append(ev)
            if len(d.r) > 64:
                d.r = d.r[-64:]
        for d in W:
            d.w = ev
            d.r = []

    def op(self, e, fn, R=(), W=()):
        if self.mute:
            return None
        self._wait(e, self._collect(e, R, W))
        ins = fn(self.E[e])
        if self.cnt[e] >= self.ROLL:
            self._new_csem(e)
        self.cnt[e] += 1
        ins.then_inc(self.csem[e], 1)
        ev = (self.csem[e], self.cnt[e], e)
        self._mark(ev, R, W)
        self.ninst += 1
        return ev

    def dma(self, out, in_, R=(), W=(), e="sp"):
        if self.mute:
            return None
        k = self.dnext
        self.dnext = (k + 1) % len(self.dsem)
        if self.dval[k] >= self.ROLL:
            self._wait(e, [(self.dsem[k], self.dval[k], "dma")])
            self.dsem[k] = self.nc.alloc_semaphore(name=f"dq{k}_{self.nsem}")
            self.nsem += 1
            self.dval[k] = 0
        s = self.dsem[k]
        evs = self._collect(e, R, W)
        if self.dval[k] > 0:
            evs.append((s, self.dval[k], "dma"))
        self._wait(e, evs)
        self.E[e].dma_start(out=out, in_=in_).then_inc(s, 16)
        self.dval[k] += 16
        ev = (s, self.dval[k], "dma")
        self._mark(ev, R, W)
        self.ninst += 1
        return ev

    def barrier(self):
        evs = [(self.csem[k], self.cnt[k], k + "_b") for k in self.csem if self.cnt[k] > 0]
        evs += [(self.dsem[k], self.dval[k], "dma") for k in range(len(self.dsem)) if self.dval[k] > 0]
        for e in self.E:
            self._wait(e, [x for x in evs])


def _bf(a):
    return np.ascontiguousarray(a).astype(ml_dtypes.bfloat16)


def build_program(stage=99, n_experts=MOE_EXPERTS, skip123=False, p4stop=99, nblk4=16, skip5=False, skip6=False):
    nc = bass.Bass("TRN2", target_bir_lowering=False)
    S = Sched(nc)

    def din(name, shape, dt=F32):
        return nc.dram_tensor(name, list(shape), dt, kind="ExternalInput").ap()

    dbg = stage < 99

    def dscr(name, shape, dt):
        return nc.dram_tensor(name, list(shape), dt, kind=("ExternalOutput" if dbg else "Internal")).ap()

    xT_d = din("xT", [8, 128, T])
    xTo_d = din("xTo", [8, 128, 2048])
    xo_d = din("xo", [2048, D])
    pTo_d = din("pTo", [2, 128, 2048])
    wA_d = din("wA", [D, 1024])
    wB_d = din("wB", [D, 800])
    wQ_d = din("wQ", [D, 1024])
    ln1c_d = din("ln1c", [128, 8])
    bfg_d = din("bfg", [128, 8])
    w1k_d = din("w1k", [128, 32, 128])
    w1v_d = din("w1v", [128, 32, 128])
    w2k_d = din("w2k", [128, 128])
    w2v_d = din("w2v", [128, 64])
    pek_d = din("pek", [128, 32, 2])
    pev_d = din("pev", [128, 32, 2])
    gn_d = din("gnb", [128, 1024])
    wout_d = din("wout", [D, D])
    ln2_d = din("ln2b", [128, D])
    wr_d = din("wr", [D, 32])
    br_d = din("brb", [128, 32])
    wup_d = din("wup", [n_experts, D, 2048])
    bup_d = din("bupc", [128, NE, 16])
    wdn_d = din("wdn", [n_experts, D, D])
    bdn_d = din("bdn", [NE, D])
    lnp_d = din("lnpb", [128, D])
    wple_d = din("wple", [256, D])
    wpg_d = din("wpg", [D, D])
    lnf_d = din("lnfb", [128, D])
    identb_d = din("identb", [128, 128], BF16)
    identf_d = din("identf", [128, 128])
    tri_d = din("trif", [128, 128])
    diagm_d = din("diagm", [128, 4, 128], BF16)
    winm_d = din("winm", [128, 8, 128], BF16)
    cmask_d = din("cmask", [128, 5, 128], BF16)
    ab_d = din("ab", [128, 8, 64])
    cab_d = din("cab", [128, 8, 16])
    selA_d = din("selA", [128, 16, 128], BF16)
    selB_d = din("selB", [128, 16, 128], BF16)
    wsel_d = din("wsel", [128, 16, 64])
    R_d = din("Rexp", [128, T], BF16)
    ovl_d = din("ovl", [128, 4, 128], BF16)
    iota_d = din("iotac", [128, CAP])

    out_d = nc.dram_tensor("out", [2048, D], F32, kind="ExternalOutput").ap()

    fmS = dscr("fmS", [8, 128, T], BF16)
    tmS = dscr("tmS", [T, 780], BF16)
    qS = dscr("qS", [16, 16, 128, 128], BF16)
    fmS_dep = [Dep() for _ in range(8)]
    tmS_dep = Dep()
    qS_dep = Dep()

    es_all = ExitStack()

    def sb(es, name, shape, dt):
        return es.enter_context(nc.sbuf_tensor("s_" + name, list(shape), dt))

    ps = [es_all.enter_context(nc.psum_tensor(f"ps{i}", [128, 512], F32)) for i in range(7)]
    psd = [Dep() for _ in range(7)]
    psb = es_all.enter_context(nc.psum_tensor("psb", [128, 1024], BF16))
    psb_dep = Dep()

    identb = sb(es_all, "identb", [128, 128], BF16)
    identf = sb(es_all, "identf", [128, 128], F32)
    trif = sb(es_all, "trif", [128, 128], F32)
    onesb = sb(es_all, "onesb", [128, 128], BF16)
    onesf = sb(es_all, "onesf", [128, 128], F32)
    epsc = sb(es_all, "epsc", [128, 1], F32)
    onec = sb(es_all, "onec", [128, 1], F32)
    cst = Dep()
    S.dma(identb[:], identb_d, W=[cst])
    S.dma(identf[:], identf_d, W=[cst])
    S.dma(trif[:], tri_d, W=[cst])
    S.op("dve", lambda e: e.memset(onesb[:], 1.0), W=[cst])
    S.op("dve", lambda e: e.memset(onesf[:], 1.0), W=[cst])
    S.op("dve", lambda e: e.memset(epsc[:], EPS), W=[cst])
    S.op("dve", lambda e: e.memset(onec[:], 1.0), W=[cst])

    es_attn = ExitStack()
    ffall = sb(es_attn, "ffall", [128, 64, 8], F32)
    ffall_dep = Dep()
    gown = sb(es_attn, "gown", [128, 16, 24], F32)
    gown_dep = Dep()

    rr = {"cast": 0, "evac": 0}

    def cast_eng():
        rr["cast"] += 1
        return ("act", "dve", "pool")[rr["cast"] % 3]

    def copy_on(e, out, in_, R, W):
        if e == "act":
            S.op("act", lambda g: g.copy(out=out, in_=in_), R, W)
        elif e == "dve":
            S.op("dve", lambda g: g.tensor_copy(out=out, in_=in_), R, W)
        else:
            S.op("pool", lambda g: g.tensor_copy(out=out, in_=in_), R, W)

    def evac_eng():
        rr["evac"] += 1
        return ("act", "dve")[rr["evac"] % 2]

    def mm(out, lhsT, rhs, start, stop, R, W):
        S.op("pe", lambda g: g.matmul(out, lhsT=lhsT, rhs=rhs, start=start, stop=stop), R, W)

    pctr = [0]
    LAG = 2

    def run_pipe(steps, LAG=1):
        n = len(steps)
        for k in range(n + LAG):
            if k < n:
                steps[k][0]()
            if k - LAG >= 0:
                steps[k - LAG][1]()

    S.mute = skip123
    with ExitStack() as es:
        wA = sb(es, "wA", [128, 8, 1024], BF16)
        wB = sb(es, "wB", [128, 8, 800], BF16)
        wQz = sb(es, "wQz", [128, 8, 16, 128], BF16)
        stg = [sb(es, f"stg{i}", [128, 1024], F32) for i in range(2)]
        stg_dep = [Dep() for _ in range(2)]
        ln1c = sb(es, "ln1c", [128, 8], F32)
        xt = [sb(es, f"xt{i}", [128, 8, 512], F32) for i in range(2)]
        xt_dep = [Dep() for _ in range(2)]
        sq = sb(es, "sq", [128, 8, 512], BF16)
        sq_dep = Dep()
        rstd = sb(es, "rstd", [128, 512], F32)
        rstd_dep = Dep()
        uT = sb(es, "uT", [128, 8, 512], BF16)
        uT_dep = Dep()
        fmo = [sb(es, f"fmo{i}", [128, 8, 512], BF16) for i in range(2)]
        fmo_dep = [Dep() for _ in range(2)]
        tmv = [sb(es, f"tmv{i}", [128, 4, 780], BF16) for i in range(2)]
        tmv_dep = [Dep() for _ in range(2)]
        qo = [sb(es, f"qo{i}", [128, 4, 16, 128], BF16) for i in range(2)]
        qo_dep = [Dep() for _ in range(2)]
        w_dep = Dep()

        S.dma(ln1c[:], ln1c_d, W=[w_dep])
        S.op("pool", lambda e: e.memset(wQz[:], 0.0), W=[w_dep])
        for k in range(2):
            S.op("pool", lambda e: e.memset(tmv[k][:], 1.0), W=[tmv_dep[k]])
        sc = 0
        for dc in range(8):
            for (src, dst, ncol) in ((wA_d, wA, 1024), (wB_d, wB, 800)):
                k = sc % 2
                sc += 1
                S.dma(stg[k][:, 0:ncol], src[dc * 128:(dc + 1) * 128, :], W=[stg_dep[k]])
                copy_on(cast_eng(), dst[:, dc, :], stg[k][:, 0:ncol], [stg_dep[k]], [w_dep])
            k = sc % 2
            sc += 1
            S.dma(stg[k][:, :], wQ_d[dc * 128:(dc + 1) * 128, :], W=[stg_dep[k]])
            copy_on(cast_eng(), wQz[:, dc, 0:4, 0:64],
                    stg[k][:, 0:256].rearrange("p (h e) -> p h e", e=64), [stg_dep[k]], [w_dep])
            copy_on(cast_eng(), wQz[:, dc, 4:8, 64:128],
                    stg[k][:, 256:512].rearrange("p (h e) -> p h e", e=64), [stg_dep[k]], [w_dep])
            fx = stg[k][:, 512:1024].rearrange("p (h two e) -> p h two e", two=2, e=64)
            wz = wQz[:, dc, 8:16, :].rearrange("p (h two) e -> p h two e", two=2)
            copy_on(cast_eng(), wz[:, :, 0, 0:64], fx[:, :, 0, :], [stg_dep[k]], [w_dep])
            copy_on(cast_eng(), wz[:, :, 1, 64:128], fx[:, :, 1, :], [stg_dep[k]], [w_dep])

        def norm_tile(src_ap, k):
            S.dma(xt[k][:], src_ap, W=[xt_dep[k]])
            S.op("act", lambda e: e.activation(out=sq[:], in_=xt[k][:], func=AF.Square), [xt_dep[k]], [sq_dep])
            for dc in range(8):
                mm(ps[0][:, :], onesb[:], sq[:, dc, :], dc == 0, dc == 7, [sq_dep, cst], [psd[0]])
            S.op("act", lambda e: e.activation(out=rstd[:], in_=ps[0][:, :], func=AF.Sqrt,
                                               bias=epsc[:, 0:1], scale=1.0 / D), [psd[0], cst], [rstd_dep])
            S.op("dve", lambda e: e.reciprocal(out=rstd[:], in_=rstd[:]), [rstd_dep], [rstd_dep])
            for dc in range(8):
                S.op("dve", lambda e: e.scalar_tensor_tensor(
                    out=uT[:, dc, :], in0=xt[k][:, dc, :], scalar=ln1c[:, dc:dc + 1], in1=rstd[:],
                    op0=ALU.mult, op1=ALU.mult), [xt_dep[k], rstd_dep, w_dep], [uT_dep])

        xT_v = xT_d.rearrange("c p s -> p c s")
        xTo_v = xTo_d.rearrange("c p s -> p c s")
        fmS_v = fmS.rearrange("o p s -> p o s")
        for Tt in range(16):
            k = Tt % 2
            norm_tile(xT_v[:, :, Tt * 512:(Tt + 1) * 512], k)
            for oc in range(8):
                b = 1 + oc % 2
                for dc in range(8):
                    mm(ps[b][:, :], wA[:, dc, oc * 128:(oc + 1) * 128], uT[:, dc, :], dc == 0, dc == 7,
                       [uT_dep, w_dep], [psd[b]])
                copy_on(evac_eng(), fmo[k][:, oc, :], ps[b][:, :], [psd[b]], [fmo_dep[k]])
            S.dma(fmS_v[:, :, Tt * 512:(Tt + 1) * 512], fmo[k][:], R=[fmo_dep[k]], W=fmS_dep)
            for sub in range(4):
                bA = 3 + (sub % 2) * 2
                bB = bA + 1
                for dc in range(8):
                    mm(ps[bA][:, 0:512], uT[:, dc, sub * 128:(sub + 1) * 128], wB[:, dc, 0:512], dc == 0, dc == 7,
                       [uT_dep, w_dep], [psd[bA]])
                for dc in range(8):
                    mm(ps[bB][:, 0:288], uT[:, dc, sub * 128:(sub + 1) * 128], wB[:, dc, 512:800], dc == 0, dc == 7,
                       [uT_dep, w_dep], [psd[bB]])
                copy_on("act", tmv[k][:, sub, 0:520].rearrange("p (h e) -> p h e", e=65)[:, :, 0:64],
                        ps[bA][:, 0:512].rearrange("p (h e) -> p h e", e=64), [psd[bA]], [tmv_dep[k]])
                copy_on("dve", tmv[k][:, sub, 520:780].rearrange("p (h e) -> p h e", e=65)[:, :, 0:64],
                        ps[bB][:, 0:256].rearrange("p (h e) -> p h e", e=64), [psd[bB]], [tmv_dep[k]])
                copy_on("dve", ffall[:, Tt * 4 + sub, :], ps[bB][:, 256:264], [psd[bB]], [ffall_dep])
            S.dma(tmS[Tt * 512:(Tt + 1) * 512, :].rearrange("(s p) c -> p s c", p=128), tmv[k][:],
                  R=[tmv_dep[k]], W=[tmS_dep])

        qS_v = qS.rearrange("i h p t -> i p h t")
        for T4 in range(4):
            norm_tile(xTo_v[:, :, T4 * 512:(T4 + 1) * 512], T4 % 2)
            k = T4 % 2
            for hd in range(16):
                b = 1 + hd % 2
                for dc in range(8):
                    mm(ps[b][:, :], wQz[:, dc, hd, :], uT[:, dc, :], dc == 0, dc == 7, [uT_dep, w_dep], [psd[b]])
                qdst = qo[k][:, :, hd, :]
                qsrc = ps[b][:, :].rearrange("p (b t) -> p b t", t=128)
                if hd % 2 == 0:
                    S.op("act", lambda e: e.activation(out=qdst, in_=qsrc, func=AF.Copy, scale=0.125), [psd[b]], [qo_dep[k]])
                else:
                    S.op("dve", lambda e: e.tensor_scalar(out=qdst, in0=qsrc, scalar1=0.125, scalar2=None, op0=ALU.mult),
                         [psd[b]], [qo_dep[k]])
            for bi in range(4):
                i = T4 * 4 + bi
                for dc in range(8):
                    mm(ps[3][:, 0:24], uT[:, dc, bi * 128:(bi + 1) * 128], wB[:, dc, 776:800], dc == 0, dc == 7,
                       [uT_dep, w_dep], [psd[3]])
                copy_on("dve", gown[:, i, :], ps[3][:, 0:24], [psd[3]], [gown_dep])
                S.dma(qS_v[i], qo[k][:, bi, :, :], R=[qo_dep[k]], W=[qS_dep])
        S.barrier()

    if stage <= 1:
        dbg_f = nc.dram_tensor("dbg_ff", [128, 64 * 8 + 16 * 24], F32, kind="ExternalOutput").ap()
        S.dma(dbg_f[:, 0:512], ffall[:].rearrange("p a b -> p (a b)"))
        S.dma(dbg_f[:, 512:896], gown[:].rearrange("p a b -> p (a b)"))
        S.barrier()
        es_attn.close()
        es_all.close()
        return nc, S

    mixS = dscr("mixS", [2048, D], BF16)
    mixS_dep = Dep()
    diagm = sb(es_attn, "diagm", [128, 4, 128], BF16)
    S.dma(diagm[:], diagm_d, W=[cst])
    pT = [sb(es_attn, f"pT{i}", [128, 512], BF16) for i in range(4)]
    pT_dep = [Dep() for _ in range(4)]
    zc = [sb(es_attn, f"zc{i}", [128, 4], F32) for i in range(2)]
    zc_dep = [Dep() for _ in range(2)]

    with ExitStack() as es:
        bfg = sb(es, "bfg", [128, 8], F32)
        wsel = sb(es, "wsel", [128, 16, 64], F32)
        lsp = sb(es, "lsp", [128, 64, 8], F32)
        cpcol = sb(es, "cpcol", [128, 64, 8], F32)
        tot = sb(es, "tot", [128, 64, 8], F32)
        pre = sb(es, "pre", [128, 64, 8], F32)
        cpref = sb(es, "cpref", [128, 16, 8], F32)
        tmpw = sb(es, "tmpw", [128, 8, 64], F32)
        cd = Dep()
        S.dma(bfg[:], bfg_d, W=[cd])
        S.dma(wsel[:], wsel_d, W=[cd])
        S.op("dve", lambda e: e.tensor_tensor(out=lsp[:], in0=ffall[:], in1=bfg[:, :].unsqueeze(1).to_broadcast([128, 64, 8]),
                                              op=ALU.add), [ffall_dep, cd], [cd])
        S.op("act", lambda e: e.activation(out=lsp[:], in_=lsp[:], func=AF.Exp, scale=-1.0), [cd], [cd])
        S.op("act", lambda e: e.activation(out=lsp[:], in_=lsp[:], func=AF.Ln, bias=onec[:, 0:1], scale=1.0), [cd, cst], [cd])
        lflat = lsp[:].rearrange("p a b -> p (a b)")
        mm(ps[0][:, :], trif[:], lflat, True, True, [cd, cst], [psd[0]])
        mm(ps[1][:, :], onesf[:], lflat, True, True, [cd, cst], [psd[1]])
        S.op("dve", lambda e: e.tensor_copy(out=tot[:].rearrange("p a b -> p (a b)"), in_=ps[1][:, :]), [psd[1]], [cd])
        S.op("dve", lambda e: e.memset(pre[:, 0, :], 0.0), [], [cd])
        for k in range(1, 64):
            S.op("dve", lambda e: e.tensor_tensor(out=pre[:, k, :], in0=pre[:, k - 1, :], in1=tot[:, k - 1, :], op=ALU.add), [cd], [cd])
        S.op("dve", lambda e: e.tensor_tensor(out=cpcol[:].rearrange("p a b -> p (a b)"), in0=ps[0][:, :],
                                              in1=pre[:].rearrange("p a b -> p (a b)"), op=ALU.add), [psd[0], cd], [cd])
        for i in range(16):
            S.op("dve", lambda e: e.tensor_tensor(out=tmpw[:], in0=tot[:].rearrange("p k h -> p h k"),
                                                  in1=wsel[:, i, :].unsqueeze(1).to_broadcast([128, 8, 64]), op=ALU.mult), [cd], [cd])
            S.op("dve", lambda e: e.reduce_sum(out=cpref[:, i, :], in_=tmpw[:], axis=AX.X), [cd], [cd])

        kT = [sb(es, f"kT{i}", [128, T], BF16) for i in range(2)]
        vP = [sb(es, f"vP{i}", [128, 64, 130], BF16) for i in range(2)]
        qP = [sb(es, f"qP{i}", [128, 16, 2, 128], BF16) for i in range(2)]
        kvq_dep = [Dep() for _ in range(2)]
        wF = [sb(es, f"wF{i}", [128, 64], F32) for i in range(2)]
        wF_dep = [Dep() for _ in range(2)]
        Vp = [sb(es, f"Vp{i}", [128, 64, 65], BF16) for i in range(2)]
        Vp_dep = [Dep() for _ in range(2)]
        mo = [sb(es, f"mo{i}", [128, 128], BF16) for i in range(2)]
        mo_dep = [Dep() for _ in range(2)]
        tmS_v = tmS.rearrange("(k p) c -> p k c", p=128)
        qS_p = qS.rearrange("i h p t -> p i h t")
        items = [(hp, i, hh) for hp in range(4) for i in range(16) for hh in range(2)]

        def fox_prep(n):
            hp, i, hh = items[n]
            kb = hp % 2
            bb = n % 2
            h = 2 * hp + hh
            nk = 4 * i + 4
            if i == 0 and hh == 0:
                S.dma(kT[kb][:], fmS[hp], R=[fmS_dep[hp]], W=[kvq_dep[kb]])
                for q4 in range(4):
                    S.dma(vP[kb][:, q4 * 16:(q4 + 1) * 16, :], tmS_v[:, q4 * 16:(q4 + 1) * 16, hp * 130:(hp + 1) * 130],
                          R=[tmS_dep], W=[kvq_dep[kb]])
                for q2 in range(2):
                    S.dma(qP[kb][:, :, q2, :], qS_p[:, :, 8 + 2 * hp + q2, :], R=[qS_dep], W=[kvq_dep[kb]])
            S.op("dve", lambda e: e.tensor_scalar(out=wF[bb][:, 0:nk], in0=cpcol[:, 0:nk, h], scalar1=cpref[:, i, h:h + 1],
                                                  scalar2=0.0, op0=ALU.subtract, op1=ALU.min), [cd], [wF_dep[bb]])
            S.op("act", lambda e: e.activation(out=wF[bb][:, 0:nk], in_=wF[bb][:, 0:nk], func=AF.Exp), [wF_dep[bb]], [wF_dep[bb]])
            eng = "dve" if n % 2 == 0 else "pool"
            S.op(eng, lambda e: e.tensor_tensor(out=Vp[bb][:, 0:nk, :], in0=vP[kb][:, 0:nk, hh * 65:(hh + 1) * 65],
                                                in1=wF[bb][:, 0:nk].unsqueeze(2).to_broadcast([128, nk, 65]), op=ALU.mult),
                 [kvq_dep[kb], wF_dep[bb]], [Vp_dep[bb]])

        def fox_run(n):
            hp, i, hh = items[n]
            kb = hp % 2
            bb = n % 2
            ob = 4 + n % 2
            mb = i % 2
            nk = 4 * i + 4
            steps = []
            for gq in range(nk // 4):
                bank = pctr[0] % 4
                pctr[0] += 1

                def qk(gq=gq, bank=bank):
                    for q in range(4):
                        k = 4 * gq + q
                        diag = k >= 4 * i
                        mm(ps[bank][:, q * 128:(q + 1) * 128], kT[kb][:, k * 128:(k + 1) * 128], qP[kb][:, i, hh, :], True, not diag,
                           [kvq_dep[kb]], [psd[bank]])
                        if diag:
                            mm(ps[bank][:, q * 128:(q + 1) * 128], identb[:], diagm[:, k - 4 * i, :], False, True, [cst], [psd[bank]])
                    S.op("act", lambda e: e.activation(out=pT[bank][:], in_=ps[bank][:, :], func=AF.Exp), [psd[bank]], [pT_dep[bank]])

                def pv(gq=gq, bank=bank):
                    for q in range(4):
                        k = 4 * gq + q
                        mm(ps[ob][:, 0:65], pT[bank][:, q * 128:(q + 1) * 128], Vp[bb][:, k, :], k == 0, k == nk - 1,
                           [pT_dep[bank], Vp_dep[bb]], [psd[ob]])
                steps.append((qk, pv))
            run_pipe(steps)
            z = zc[bb]
            S.op("dve", lambda e: e.tensor_scalar_max(out=z[:, 0:1], in0=ps[ob][:, 64:65], scalar1=1e-30), [psd[ob]], [zc_dep[bb]])
            S.op("dve", lambda e: e.reciprocal(out=z[:, 0:1], in_=z[:, 0:1]), [zc_dep[bb]], [zc_dep[bb]])
            S.op("dve", lambda e: e.tensor_scalar(out=mo[mb][:, hh * 64:(hh + 1) * 64], in0=ps[ob][:, 0:64],
                                                  scalar1=z[:, 0:1], scalar2=None, op0=ALU.mult),
                 [psd[ob], zc_dep[bb]], [mo_dep[mb]])
            if hh == 1:
                S.dma(mixS[i * 128:(i + 1) * 128, 512 + hp * 128:512 + (hp + 1) * 128], mo[mb][:], R=[mo_dep[mb]], W=[mixS_dep])

        fox_prep(0)
        for n in range(len(items)):
            if n + 1 < len(items):
                fox_prep(n + 1)
            fox_run(n)
        S.barrier()

    if stage <= 2:
        es_attn.close()
        es_all.close()
        return nc, S

    with ExitStack() as es:
        kcT = sb(es, "kcT", [128, 512], BF16)
        vc = sb(es, "vc", [128, 4, 130], BF16)
        kc_dep = Dep()
        S.op("pool", lambda e: e.memset(vc[:], 1.0), [], [kc_dep])
        with ExitStack() as es2:
            kraw = sb(es2, "kraw", [128, T], BF16)
            w1f = sb(es2, "w1f", [128, 32, 128], F32)
            w1b = sb(es2, "w1b", [128, 32, 128], BF16)
            pef = sb(es2, "pef", [128, 32, 2], F32)
            peb = sb(es2, "peb", [128, 32, 2], BF16)
            w2f = sb(es2, "w2f", [128, 128], F32)
            w2b = sb(es2, "w2b", [128, 128], BF16)
            hx = sb(es2, "hx", [128, 512], F32)
            hu = sb(es2, "hu", [128, 512], F32)
            hidT = sb(es2, "hidT", [128, 512], BF16)
            cbias = sb(es2, "cbias", [128, 1], F32)
            cpd = Dep()
            for which in range(2):
                S.dma(kraw[:], fmS[6 + which], R=[fmS_dep[6 + which]], W=[cpd])
                S.dma(w1f[:], (w1k_d, w1v_d)[which], W=[cpd])
                S.dma(pef[:], (pek_d, pev_d)[which], W=[cpd])
                if which == 0:
                    S.dma(w2f[:, :], w2k_d, W=[cpd])
                else:
                    S.dma(w2f[:, 0:64], w2v_d, W=[cpd])
                S.op("dve", lambda e: e.tensor_copy(out=w1b[:], in_=w1f[:]), [cpd], [cpd])
                S.op("dve", lambda e: e.tensor_copy(out=peb[:], in_=pef[:]), [cpd], [cpd])
                S.op("dve", lambda e: e.tensor_copy(out=w2b[:], in_=w2f[:]), [cpd], [cpd])
                for g in range(2):
                    r0, r1 = g * 64, g * 64 + 64
                    for l in range(32):
                        mm(ps[0][:, 0:511], w1b[r0:r1, l, :], kraw[r0:r1, l:l + 16 * 510 + 1:16], l == 0, l == 31, [cpd], [psd[0]])
                    for l in range(32):
                        mm(ps[1][:, 0:2], w1b[r0:r1, l, :], peb[r0:r1, l, :], l == 0, l == 31, [cpd], [psd[1]])
                    S.op("dve", lambda e: e.tensor_copy(out=cbias[:], in_=ps[1][:, 0:1]), [psd[1]], [cpd])
                    S.op("dve", lambda e: e.memset(hx[:], 0.0), [], [cpd])
                    S.op("dve", lambda e: e.tensor_scalar(out=hx[:, 0:511], in0=ps[0][:, 0:511], scalar1=cbias[:, 0:1],
                                                          scalar2=None, op0=ALU.add), [psd[0], cpd], [cpd])
                    S.op("dve", lambda e: e.tensor_tensor(out=hu[:], in0=hx[:], in1=hx[:], op=ALU.mult), [cpd], [cpd])
                    S.op("dve", lambda e: e.tensor_scalar(out=hu[:], in0=hu[:], scalar1=0.044715, scalar2=1.0,
                                                          op0=ALU.mult, op1=ALU.add), [cpd], [cpd])
                    S.op("dve", lambda e: e.tensor_tensor(out=hu[:], in0=hu[:], in1=hx[:], op=ALU.mult), [cpd], [cpd])
                    S.op("act", lambda e: e.activation(out=hu[:], in_=hu[:], func=AF.Exp, scale=-1.5957691216057308), [cpd], [cpd])
                    S.op("dve", lambda e: e.tensor_scalar(out=hu[:], in0=hu[:], scalar1=1.0, scalar2=None, op0=ALU.add), [cpd], [cpd])
                    S.op("dve", lambda e: e.reciprocal(out=hu[:], in_=hu[:]), [cpd], [cpd])
                    S.op("dve", lambda e: e.tensor_tensor(out=hidT[:], in0=hu[:], in1=hx[:], op=ALU.mult), [cpd], [cpd])
                    if which == 0:
                        mm(ps[2][:, 0:512], w2b[:, :], hidT[:], True, True, [cpd], [psd[2]])
                        S.op("dve", lambda e: e.tensor_copy(out=kcT[r0:r1, :], in_=ps[2][r0:r1, 0:512]), [psd[2]], [kc_dep])
                    else:
                        for m in range(4):
                            mm(ps[2][:, m * 64:(m + 1) * 64], hidT[:, m * 128:(m + 1) * 128], w2b[:, 0:64], True, True, [cpd], [psd[2]])
                        S.op("dve", lambda e: e.tensor_copy(out=vc[:, :, g * 65:g * 65 + 64],
                                                            in_=ps[2][:, 0:256].rearrange("p (m e) -> p m e", e=64)), [psd[2]], [kc_dep])
            S.barrier()

        KsT = sb(es, "KsT", [128, T], BF16)
        KwT = sb(es, "KwT", [128, T], BF16)
        Vs = sb(es, "Vs", [128, 64, 130], BF16)
        Vw = sb(es, "Vw", [128, 64, 130], BF16)
        Rx = sb(es, "Rx", [128, T], BF16)
        ovl = sb(es, "ovl", [128, 4, 128], BF16)
        ab = sb(es, "ab", [128, 8, 64], F32)
        cab = sb(es, "cab", [128, 8, 16], F32)
        selA = sb(es, "selA", [128, 16, 128], BF16)
        selB = sb(es, "selB", [128, 16, 128], BF16)
        cmask = sb(es, "cmask", [128, 5, 128], BF16)
        winm = sb(es, "winm", [128, 8, 128], BF16)
        nd = Dep()
        tmS_v = tmS.rearrange("(k p) c -> p k c", p=128)
        S.dma(KsT[:], fmS[4], R=[fmS_dep[4]], W=[nd])
        S.dma(KwT[:], fmS[5], R=[fmS_dep[5]], W=[nd])
        for q4 in range(4):
            S.dma(Vs[:, q4 * 16:(q4 + 1) * 16, :], tmS_v[:, q4 * 16:(q4 + 1) * 16, 520:650], R=[tmS_dep], W=[nd])
            S.dma(Vw[:, q4 * 16:(q4 + 1) * 16, :], tmS_v[:, q4 * 16:(q4 + 1) * 16, 650:780], R=[tmS_dep], W=[nd])
        for (dst, src) in ((Rx, R_d), (ovl, ovl_d), (ab, ab_d), (cab, cab_d), (selA, selA_d), (selB, selB_d),
                           (cmask, cmask_d), (winm, winm_d)):
            S.dma(dst[:], src, W=[nd])
        wab = sb(es, "wab", [128, 8, 64], F32)
        wab_dep = Dep()
        S.op("dve", lambda e: e.tensor_scalar(out=wab[:], in0=ab[:], scalar1=0.0, scalar2=None, op0=ALU.min), [nd], [wab_dep])
        S.op("act", lambda e: e.activation(out=wab[:], in_=wab[:], func=AF.Exp), [wab_dep], [wab_dep])
        Vsp = [sb(es, f"Vsp{i}", [128, 64, 65], BF16) for i in range(2)]
        Vwp = [sb(es, f"Vwp{i}", [128, 8, 65], BF16) for i in range(2)]
        Vxp_dep = [Dep() for _ in range(2)]
        qN = [sb(es, f"qN{i}", [128, 8, 128], BF16) for i in range(2)]
        qN_dep = [Dep() for _ in range(2)]
        eC = [sb(es, f"eC{i}", [128, 4, 128], BF16) for i in range(4)]
        eC_dep = [Dep() for _ in range(4)]
        Ocs = sb(es, "Ocs", [128, 4, 64], F32)
        Ocs_dep = Dep()
        rzc = sb(es, "rzc", [128, 4], F32)
        impacc = sb(es, "impacc", [128, 128], F32)
        score = sb(es, "score", [128, 128], F32)
        sc2 = sb(es, "sc2", [128, 128], F32)
        mx8 = sb(es, "mx8", [128, 8], F32)
        mx8b = sb(es, "mx8b", [128, 8], F32)
        MnegB = sb(es, "MnegB", [128, 128], BF16)
        MnegT = sb(es, "MnegT", [128, 128], BF16)
        MnegT_dep = Dep()
        tk = Dep()
        sgate = sb(es, "sgate", [128, 24], F32)
        sg_dep = Dep()
        coef = sb(es, "coef", [128, 4], F32)
        t1 = sb(es, "t1", [128, 64], F32)
        fin = Dep()
        mon = [sb(es, f"mon{i}", [128, 512], BF16) for i in range(2)]
        mon_dep = [Dep() for _ in range(2)]
        qS_p = qS.rearrange("i h p t -> i p h t")
        sctr = 0

        def nsa_prep(nidx):
            r_ = nidx % 4
            g_ = (nidx // 4) % 2
            i_ = nidx // 8
            h_ = 4 * g_ + r_
            vb_ = nidx % 2
            nk_ = 4 * i_ + 4
            k0_ = max(0, 4 * i_ - 4)
            e1, e2 = ("dve", "pool") if nidx % 2 == 0 else ("pool", "dve")
            S.op(e1, lambda e: e.tensor_tensor(out=Vsp[vb_][:, 0:nk_, :], in0=Vs[:, 0:nk_, g_ * 65:(g_ + 1) * 65],
                                               in1=wab[:, h_, 60 - 4 * i_:64].unsqueeze(2).to_broadcast([128, nk_, 65]), op=ALU.mult),
                 [nd, wab_dep], [Vxp_dep[vb_]])
            S.op(e2, lambda e: e.tensor_tensor(out=Vwp[vb_][:, 0:nk_ - k0_, :], in0=Vw[:, k0_:nk_, g_ * 65:(g_ + 1) * 65],
                                               in1=wab[:, h_, 60 - 4 * i_ + k0_:64].unsqueeze(2).to_broadcast([128, nk_ - k0_, 65]), op=ALU.mult),
                 [nd, wab_dep], [Vxp_dep[vb_]])
        for i in range(16):
            qb = i % 2
            S.dma(qN[qb][:], qS_p[i][:, 0:8, :], R=[qS_dep], W=[qN_dep[qb]])
            S.op("act", lambda e: e.activation(out=sgate[:], in_=gown[:, i, :], func=AF.Exp, scale=-1.0), [gown_dep], [sg_dep])
            S.op("dve", lambda e: e.tensor_scalar(out=sgate[:], in0=sgate[:], scalar1=1.0, scalar2=None, op0=ALU.add), [sg_dep], [sg_dep])
            S.op("dve", lambda e: e.reciprocal(out=sgate[:], in_=sgate[:]), [sg_dep], [sg_dep])
            ncm = i // 4 + 1
            nk = 4 * i + 4
            for g in range(2):
                for r in range(4):
                    h = 4 * g + r
                    for m in range(ncm):
                        d = 4 * m - i
                        partial = d >= -4
                        sbk = sctr % 4
                        sctr += 1
                        mm(ps[sbk][:, 0:128], kcT[:, m * 128:(m + 1) * 128], qN[qb][:, h, :], True, not partial,
                           [kc_dep, qN_dep[qb]], [psd[sbk]])
                        if partial:
                            mm(ps[sbk][:, 0:128], identb[:], cmask[:, d + 4, :], False, True, [cst, nd], [psd[sbk]])
                        S.op("act", lambda e: e.activation(out=eC[r][:, m, :], in_=ps[sbk][:, 0:128], func=AF.Exp,
                                                           bias=cab[:, h, d + 15:d + 16], scale=1.0), [psd[sbk], nd], [eC_dep[r]])
                    for m in range(ncm):
                        mm(ps[4][:, 0:65], eC[r][:, m, :], vc[:, m, g * 65:(g + 1) * 65], m == 0, m == ncm - 1,
                           [eC_dep[r], kc_dep], [psd[4]])
                    for m in range(ncm):
                        mm(ps[5][:, 0:128], eC[r][:, m, :], ovl[:, m, :], m == 0, m == ncm - 1, [eC_dep[r], nd], [psd[5]])
                    S.op("dve", lambda e: e.tensor_scalar_max(out=rzc[:, r:r + 1], in0=ps[4][:, 64:65], scalar1=1e-30), [psd[4]], [tk, fin])
                    S.op("dve", lambda e: e.reciprocal(out=rzc[:, r:r + 1], in_=rzc[:, r:r + 1]), [tk], [tk])
                    S.op("dve", lambda e: e.tensor_copy(out=Ocs[:, r, :], in_=ps[4][:, 0:64]), [psd[4]], [Ocs_dep, fin])
                    if r == 0:
                        S.op("dve", lambda e: e.tensor_scalar(out=impacc[:], in0=ps[5][:, 0:128], scalar1=rzc[:, r:r + 1],
                                                              scalar2=None, op0=ALU.mult), [psd[5], tk], [tk])
                    else:
                        S.op("dve", lambda e: e.scalar_tensor_tensor(out=impacc[:], in0=ps[5][:, 0:128], scalar=rzc[:, r:r + 1],
                                                                     in1=impacc[:], op0=ALU.mult, op1=ALU.add), [psd[5], tk], [tk])
                S.op("dve", lambda e: e.tensor_tensor(out=score[:], in0=impacc[:], in1=selA[:, i, :], op=ALU.mult), [tk, nd], [tk])
                S.op("dve", lambda e: e.tensor_tensor(out=score[:], in0=score[:], in1=selB[:, i, :], op=ALU.add), [tk, nd], [tk])
                S.op("dve", lambda e: e.max(out=mx8[:], in_=score[:]), [tk], [tk])
                S.op("dve", lambda e: e.match_replace(out=sc2[:], in_to_replace=mx8[:], in_values=score[:], imm_value=-2.0), [tk], [tk])
                S.op("dve", lambda e: e.max(out=mx8b[:], in_=sc2[:]), [tk], [tk])
                S.op("dve", lambda e: e.tensor_scalar(out=MnegB[:], in0=score[:], scalar1=mx8b[:, 7:8], scalar2=NEGM,
                                                      op0=ALU.is_lt, op1=ALU.mult), [tk], [tk])
                S.op("pe", lambda e: e.transpose(out=psb[:, 0:128], in_=MnegB[:], identity=identb[:]), [tk, cst], [psb_dep])
                S.op("dve", lambda e: e.tensor_copy(out=MnegT[:], in_=psb[:, 0:128]), [psb_dep], [MnegT_dep])
                for r in range(4):
                    h = 4 * g + r
                    osb = 6 if r % 2 == 0 else 5
                    nidx = (i * 2 + g) * 4 + r
                    vb = nidx % 2
                    if nidx == 0:
                        nsa_prep(0)
                    if nidx + 1 < 128:
                        nsa_prep(nidx + 1)
                    k0 = max(0, 4 * i - 4)
                    steps = []
                    for gq in range(nk // 4):
                        bank = pctr[0] % 4
                        pctr[0] += 1

                        def qk(gq=gq, bank=bank, i=i, h=h, qb=qb):
                            for q in range(4):
                                k = 4 * gq + q
                                diag = k >= 4 * i
                                o = ps[bank][:, q * 128:(q + 1) * 128]
                                mm(o, KsT[:, k * 128:(k + 1) * 128], qN[qb][:, h, :], True, False, [nd, qN_dep[qb]], [psd[bank]])
                                mm(o, Rx[:, k * 128:(k + 1) * 128], MnegT[:], False, not diag, [nd, MnegT_dep], [psd[bank]])
                                if diag:
                                    mm(o, identb[:], diagm[:, k - 4 * i, :], False, True, [cst], [psd[bank]])
                            S.op("act", lambda e: e.activation(out=pT[bank][:], in_=ps[bank][:, :], func=AF.Exp), [psd[bank]], [pT_dep[bank]])

                        def pv(gq=gq, bank=bank, osb=osb, nk=nk, vb=vb):
                            for q in range(4):
                                k = 4 * gq + q
                                mm(ps[osb][:, 0:65], pT[bank][:, q * 128:(q + 1) * 128], Vsp[vb][:, k, :], k == 0, k == nk - 1,
                                   [pT_dep[bank], Vxp_dep[vb]], [psd[osb]])
                        steps.append((qk, pv))
                    for gq in range((nk - k0) // 4):
                        bank = pctr[0] % 4
                        pctr[0] += 1

                        def qk(gq=gq, bank=bank, i=i, h=h, qb=qb, k0=k0):
                            for q in range(4):
                                k = k0 + 4 * gq + q
                                wk = k - (4 * i - 4)
                                o = ps[bank][:, q * 128:(q + 1) * 128]
                                mm(o, KwT[:, k * 128:(k + 1) * 128], qN[qb][:, h, :], True, False, [nd, qN_dep[qb]], [psd[bank]])
                                mm(o, identb[:], winm[:, wk, :], False, True, [cst, nd], [psd[bank]])
                            S.op("act", lambda e: e.activation(out=pT[bank][:], in_=ps[bank][:, :], func=AF.Exp), [psd[bank]], [pT_dep[bank]])

                        def pv(gq=gq, bank=bank, k0=k0, nk=nk, vb=vb):
                            for q in range(4):
                                k = k0 + 4 * gq + q
                                mm(ps[4][:, 0:65], pT[bank][:, q * 128:(q + 1) * 128], Vwp[vb][:, k - k0, :], k == k0, k == nk - 1,
                                   [pT_dep[bank], Vxp_dep[vb]], [psd[4]])
                        steps.append((qk, pv))
                    run_pipe(steps)
                    S.op("dve", lambda e: e.tensor_scalar_max(out=coef[:, 1:2], in0=ps[osb][:, 64:65], scalar1=1e-30), [psd[osb]], [fin])
                    S.op("dve", lambda e: e.tensor_scalar_max(out=coef[:, 2:3], in0=ps[4][:, 64:65], scalar1=1e-30), [psd[4]], [fin])
                    S.op("dve", lambda e: e.reciprocal(out=coef[:, 1:3], in_=coef[:, 1:3]), [fin], [fin])
                    S.op("dve", lambda e: e.tensor_copy(out=coef[:, 0:1], in_=rzc[:, r:r + 1]), [fin, tk], [fin])
                    S.op("dve", lambda e: e.tensor_tensor(out=coef[:, 0:3], in0=coef[:, 0:3], in1=sgate[:, 3 * h:3 * h + 3], op=ALU.mult), [fin, sg_dep], [fin])
                    S.op("dve", lambda e: e.tensor_scalar(out=t1[:], in0=Ocs[:, r, :], scalar1=coef[:, 0:1], scalar2=None, op0=ALU.mult), [fin, Ocs_dep], [fin])
                    S.op("dve", lambda e: e.scalar_tensor_tensor(out=t1[:], in0=ps[osb][:, 0:64], scalar=coef[:, 1:2], in1=t1[:],
                                                                 op0=ALU.mult, op1=ALU.add), [fin, psd[osb]], [fin])
                    S.op("dve", lambda e: e.scalar_tensor_tensor(out=mon[qb][:, h * 64:(h + 1) * 64], in0=ps[4][:, 0:64], scalar=coef[:, 2:3],
                                                                 in1=t1[:], op0=ALU.mult, op1=ALU.add), [fin, psd[4]], [mon_dep[qb], fin])
            S.dma(mixS[i * 128:(i + 1) * 128, 0:512], mon[qb][:], R=[mon_dep[qb]], W=[mixS_dep])
        S.barrier()

    if stage <= 3:
        es_attn.close()
        es_all.close()
        return nc, S

    S.mute = False
    es_attn.close()
    es_tail = ExitStack()
    hres = sb(es_tail, "hres", [128, 16, D], F32)
    hres_dep = [Dep() for _ in range(16)]
    xnT = sb(es_tail, "xntok", [128, 16, D], BF16)
    xnT_dep = Dep()
    gate = sb(es_tail, "gate", [128, 16, NE], F32)
    gate_dep = Dep()
    ssc = sb(es_tail, "ssc", [128, 4], F32)
    nrm = Dep()
    sqt_box = [None]

    def rms_rstd(src, n, col, R):
        sqt = sqt_box[0]
        S.op("dve", lambda e: e.tensor_tensor(out=sqt[:, 0:n], in0=src, in1=src, op=ALU.mult), list(R) + [nrm], [nrm])
        S.op("dve", lambda e: e.reduce_sum(out=ssc[:, col:col + 1], in_=sqt[:, 0:n], axis=AX.X), [nrm], [nrm])
        S.op("act", lambda e: e.activation(out=ssc[:, col:col + 1], in_=ssc[:, col:col + 1], func=AF.Sqrt,
                                           bias=epsc[:, 0:1], scale=1.0 / n), [nrm, cst], [nrm])
        S.op("dve", lambda e: e.reciprocal(out=ssc[:, col:col + 1], in_=ssc[:, col:col + 1]), [nrm], [nrm])

    def load_w_bf16(dst, src, nchunk, ncol, stg, stg_dep, wdep, ctr):
        for dc in range(nchunk):
            k = ctr[0] % len(stg)
            ctr[0] += 1
            S.dma(stg[k][:, 0:ncol], src[dc * 128:(dc + 1) * 128, :], W=[stg_dep[k]])
            copy_on(cast_eng(), dst[:, dc, :], stg[k][:, 0:ncol], [stg_dep[k]], [wdep])

    with ExitStack() as es:
        sqt_box[0] = sb(es, "sqt4", [128, D], F32)
        woutb = sb(es, "woutb", [128, 8, D], BF16)
        stg = [sb(es, f"stg4{i}", [128, D], F32) for i in range(2)]
        stg_dep = [Dep() for _ in range(2)]
        gnb = sb(es, "gnb", [128, D], F32)
        ln2b = sb(es, "ln2b", [128, D], F32)
        wrf = sb(es, "wrf", [128, 8, NE], F32)
        brb = sb(es, "brb", [128, NE], F32)
        bdnf = sb(es, "bdnf", [NE, D], F32)
        mixb = [sb(es, f"mixb{i}", [128, D], BF16) for i in range(2)]
        mixb_dep = [Dep() for _ in range(2)]
        mixn = sb(es, "mixn", [128, D], BF16)
        mixT = sb(es, "mixT", [128, 8, 128], BF16)
        xn = sb(es, "xn", [128, D], F32)
        xnTf = sb(es, "xnTf", [128, 8, 128], F32)
        lg = sb(es, "lg", [128, NE], F32)
        ex = sb(es, "ex", [128, NE], F32)
        mxr = sb(es, "mxr", [128, 8], F32)
        gT = sb(es, "gT", [NE, 128], F32)
        wd = Dep()
        p4 = Dep()
        ctr = [0]
        load_w_bf16(woutb, wout_d, 8, D, stg, stg_dep, wd, ctr)
        S.dma(gnb[:], gn_d, W=[wd])
        S.dma(ln2b[:], ln2_d, W=[wd])
        S.dma(wrf[:], wr_d.rearrange("(c p) e -> p c e", p=128), W=[wd])
        S.dma(brb[:], br_d, W=[wd])
        S.dma(bdnf[:], bdn_d, W=[wd])
        def g4(n):
            if p4stop <= n:
                S.mute = True
        for i in range(nblk4):
            mb = i % 2
            S.mute = False
            S.dma(mixb[mb][:], mixS[i * 128:(i + 1) * 128, :], R=[mixS_dep], W=[mixb_dep[mb]])
            S.dma(hres[:, i, :], xo_d[i * 128:(i + 1) * 128, :], W=[hres_dep[i]])
            g4(1)
            for half in range(2):
                hs = slice(half * 512, (half + 1) * 512)
                rms_rstd(mixb[mb][:, hs], 512, half, [mixb_dep[mb]])
                S.op("dve", lambda e: e.scalar_tensor_tensor(out=mixn[:, hs], in0=mixb[mb][:, hs], scalar=ssc[:, half:half + 1],
                                                             in1=gnb[:, hs], op0=ALU.mult, op1=ALU.mult), [mixb_dep[mb], nrm, wd], [p4])
            g4(2)
            for c in range(8):
                S.op("pe", lambda e: e.transpose(out=psb[:, c * 128:(c + 1) * 128], in_=mixn[:, c * 128:(c + 1) * 128],
                                                 identity=identb[:]), [p4, cst], [psb_dep])
            S.op("act", lambda e: e.copy(out=mixT[:].rearrange("p c t -> p (c t)"), in_=psb[:, :]), [psb_dep], [p4])
            g4(3)
            for half in range(2):
                hs = slice(half * 512, (half + 1) * 512)
                for c in range(8):
                    mm(ps[half][:, :], mixT[:, c, :], woutb[:, c, hs], c == 0, c == 7, [p4, wd], [psd[half]])
                S.op("dve", lambda e: e.tensor_tensor(out=hres[:, i, hs], in0=ps[half][:, :], in1=hres[:, i, hs], op=ALU.add),
                     [psd[half], hres_dep[i]], [hres_dep[i]])
            rms_rstd(hres[:, i, :], D, 2, [hres_dep[i]])
            g4(4)
            S.op("dve", lambda e: e.scalar_tensor_tensor(out=xn[:], in0=hres[:, i, :], scalar=ssc[:, 2:3], in1=ln2b[:],
                                                         op0=ALU.mult, op1=ALU.mult), [hres_dep[i], nrm, wd], [p4])
            S.op("pool", lambda e: e.tensor_copy(out=xnT[:, i, :], in_=xn[:]), [p4], [xnT_dep])
            for c in range(8):
                b = 2 + c // 4
                g4(5)
                S.op("pe", lambda e: e.transpose(out=ps[b][:, (c % 4) * 128:(c % 4 + 1) * 128], in_=xn[:, c * 128:(c + 1) * 128],
                                                 identity=identf[:]), [p4, cst], [psd[b]])
            for b2 in range(2):
                S.op("act", lambda e: e.copy(out=xnTf[:, b2 * 4:(b2 + 1) * 4, :], in_=ps[2 + b2][:, :].rearrange("p (c t) -> p c t", t=128)),
                     [psd[2 + b2]], [p4])
            for c in range(8):
                mm(ps[4][:, 0:NE], xnTf[:, c, :], wrf[:, c, :], c == 0, c == 7, [p4, wd], [psd[4]])
            g4(7)
            S.op("dve", lambda e: e.tensor_tensor(out=lg[:], in0=ps[4][:, 0:NE], in1=brb[:], op=ALU.add), [psd[4], wd], [p4])
            S.op("dve", lambda e: e.max(out=mxr[:], in_=lg[:]), [p4], [p4])
            S.op("dve", lambda e: e.tensor_scalar(out=mxr[:, 4:5], in0=mxr[:, 0:1], scalar1=-1.0, scalar2=None, op0=ALU.mult), [p4], [p4])
            S.op("act", lambda e: e.activation(out=ex[:], in_=lg[:], func=AF.Exp, bias=mxr[:, 4:5], scale=1.0), [p4], [p4])
            S.op("dve", lambda e: e.scalar_tensor_tensor(out=ex[:], in0=lg[:], scalar=mxr[:, 3:4], in1=ex[:],
                                                         op0=ALU.is_ge, op1=ALU.mult), [p4], [p4])
            S.op("dve", lambda e: e.reduce_sum(out=mxr[:, 5:6], in_=ex[:], axis=AX.X), [p4], [p4])
            S.op("dve", lambda e: e.reciprocal(out=mxr[:, 5:6], in_=mxr[:, 5:6]), [p4], [p4])
            S.op("dve", lambda e: e.tensor_scalar(out=gate[:, i, :], in0=ex[:], scalar1=mxr[:, 5:6], scalar2=None, op0=ALU.mult),
                 [p4], [gate_dep])
            g4(8)
        S.mute = False
        S.barrier()

    if stage == 4:
        dbg_h = nc.dram_tensor("dbg_h", [128, 16, D], F32, kind="ExternalOutput").ap()
        dbg_g = nc.dram_tensor("dbg_g", [128, 16, NE], F32, kind="ExternalOutput").ap()
        dbg_x = nc.dram_tensor("dbg_x", [128, 16, D], BF16, kind="ExternalOutput").ap()
        S.dma(dbg_h, hres[:])
        S.dma(dbg_g, gate[:])
        S.dma(dbg_x, xnT[:])
        S.barrier()
        es_tail.close()
        es_all.close()
        return nc, S

    S.mute = skip5
    with ExitStack() as es:
        C = CAP
        NSC = C // 128
        wupb = sb(es, "wupb", [128, 8, 2048], BF16)
        wdnb = sb(es, "wdnb", [128, 8, D], BF16)
        wup_dep = Dep()
        wdn_dep = Dep()
        NSTG5 = 4
        stg = [sb(es, f"stg5{i}", [128, 512], F32) for i in range(NSTG5)]
        stg_dep = [Dep() for _ in range(NSTG5)]
        bupc = sb(es, "bupc", [128, NE, 16], F32)
        browb = sb(es, "browb", [1, D], BF16)
        brow_dep = Dep()
        bd = Dep()
        S.dma(bupc[:], bup_d, W=[bd])
        iotac = sb(es, "iotac", [128, C], F32)
        S.dma(iotac[:], iota_d, W=[bd])
        Mf = sb(es, "Mf", [128, 16, NE], F32)
        pos = sb(es, "pos", [128, 16, NE], F32)
        dsp = Dep()
        with ExitStack() as est:
            trisb = sb(est, "trisb", [128, 128], BF16)
            Mb = sb(est, "Mb", [128, 16, NE], BF16)
            tot5 = sb(est, "tot5", [128, 16, NE], F32)
            pre5 = sb(est, "pre5", [128, 16, NE], F32)
            S.op("dve", lambda g: g.tensor_tensor(out=trisb[:], in0=trif[:], in1=identf[:], op=ALU.subtract), [cst], [dsp])
            S.op("dve", lambda g: g.tensor_scalar(out=Mf[:], in0=gate[:], scalar1=0.0, scalar2=None, op0=ALU.is_gt), [gate_dep], [dsp])
            S.op("dve", lambda g: g.tensor_copy(out=Mb[:], in_=Mf[:]), [dsp], [dsp])
            mflat = Mb[:].rearrange("p a b -> p (a b)")
            mm(ps[0][:, :], trisb[:], mflat, True, True, [dsp], [psd[0]])
            mm(ps[1][:, :], onesb[:], mflat, True, True, [dsp, cst], [psd[1]])
            S.op("dve", lambda g: g.tensor_copy(out=tot5[:].rearrange("p a b -> p (a b)"), in_=ps[1][:, :]), [psd[1]], [dsp])
            S.op("dve", lambda g: g.memset(pre5[:, 0, :], 0.0), [], [dsp])
            for k in range(1, 16):
                S.op("dve", lambda g: g.tensor_tensor(out=pre5[:, k, :], in0=pre5[:, k - 1, :], in1=tot5[:, k - 1, :], op=ALU.add), [dsp], [dsp])
            S.op("dve", lambda g: g.tensor_tensor(out=pos[:].rearrange("p a b -> p (a b)"), in0=ps[0][:, :],
                                                  in1=pre5[:].rearrange("p a b -> p (a b)"), op=ALU.add), [psd[0], dsp], [dsp])
            S.barrier()

        Sel = sb(es, "Sel", [128, 16, C], BF16)
        Sel_dep = Dep()
        SelTb = [sb(es, f"SelTb{i}", [128, NSC, 128], BF16) for i in range(2)]
        SelTb_dep = [Dep() for _ in range(2)]
        XeT = sb(es, "XeT", [128, 8, C], BF16)
        XeT_dep = Dep()
        actT = sb(es, "actT5", [128, 8, C], BF16)
        actT_dep = Dep()
        assert NSC * D == 8 * C
        ye = XeT[:].rearrange("p (s two) c -> p s (two c)", two=2)
        ye_dep = XeT_dep
        gcs = [sb(es, f"gcs{i}", [128, C], F32) for i in range(1)] * 2
        sgs = [sb(es, f"sgs{i}", [128, C], F32) for i in range(1)] * 2
        lcs = [sb(es, f"lcs{i}", [128, C], F32) for i in range(1)] * 2
        gc_dep = [Dep()] * 2
        sg_dep5 = [Dep()] * 2
        lc_dep = [Dep()] * 2
        sctr = [0]

        def load_up(e):
            for dc in range(8):
                for half in range(4):
                    k = sctr[0] % NSTG5
                    sctr[0] += 1
                    S.dma(stg[k][:], wup_d[e, dc * 128:(dc + 1) * 128, half * 512:(half + 1) * 512], W=[stg_dep[k]])
                    copy_on("pool", wupb[:, dc, half * 512:(half + 1) * 512], stg[k][:], [stg_dep[k]], [wup_dep])

        def load_dn(e):
            for fc in range(8):
                for half in range(2):
                    k = sctr[0] % NSTG5
                    sctr[0] += 1
                    S.dma(stg[k][:], wdn_d[e, fc * 128:(fc + 1) * 128, half * 512:(half + 1) * 512], W=[stg_dep[k]])
                    copy_on("pool", wdnb[:, fc, half * 512:(half + 1) * 512], stg[k][:], [stg_dep[k]], [wdn_dep])

        load_up(0)
        load_dn(0)
        uc = 0
        dcn = 0
        tcn = 0
        for e_ in range(n_experts):
            for half in range(2):
                kk = sctr[0] % NSTG5
                sctr[0] += 1
                S.dma(stg[kk][0:1, :], bdn_d[e_:e_ + 1, half * 512:(half + 1) * 512], W=[stg_dep[kk]])
                S.op("act", lambda g: g.copy(out=browb[0:1, half * 512:(half + 1) * 512], in_=stg[kk][0:1, :]), [stg_dep[kk]], [brow_dep])
            for blk in range(16):
                S.op("dve", lambda g: g.tensor_scalar(out=Sel[:, blk, :], in0=iotac[:], scalar1=pos[:, blk, e_:e_ + 1],
                                                      scalar2=Mf[:, blk, e_:e_ + 1], op0=ALU.is_equal, op1=ALU.mult), [dsp, bd], [Sel_dep])
            for dc in range(8):
                bX = 4 + dcn % 3
                dcn += 1
                for blk in range(16):
                    mm(ps[bX][:, 0:C], xnT[:, blk, dc * 128:(dc + 1) * 128], Sel[:, blk, :], blk == 0, blk == 15,
                       [xnT_dep, Sel_dep], [psd[bX]])
                S.op("act", lambda g: g.copy(out=XeT[:, dc, :], in_=ps[bX][:, 0:C]), [psd[bX]], [XeT_dep])
            for fc in range(8):
                bG = (uc % 2) * 2
                bL = bG + 1
                tb = uc % 2
                uc += 1
                for dc in range(8):
                    mm(ps[bG][:, 0:C], wupb[:, dc, fc * 128:(fc + 1) * 128], XeT[:, dc, :], dc == 0, dc == 7, [wup_dep, XeT_dep], [psd[bG]])
                for dc in range(8):
                    mm(ps[bL][:, 0:C], wupb[:, dc, 1024 + fc * 128:1024 + (fc + 1) * 128], XeT[:, dc, :], dc == 0, dc == 7,
                       [wup_dep, XeT_dep], [psd[bL]])
                S.op("dve", lambda g: g.tensor_scalar(out=gcs[tb][:], in0=ps[bG][:, 0:C], scalar1=bupc[:, e_, fc:fc + 1], scalar2=7.0,
                                                      op0=ALU.add, op1=ALU.min), [psd[bG], bd], [gc_dep[tb]])
                S.op("act", lambda g: g.activation(out=sgs[tb][:], in_=gcs[tb][:], func=AF.Sigmoid, scale=1.702), [gc_dep[tb]], [sg_dep5[tb]])
                S.op("dve", lambda g: g.tensor_scalar(out=lcs[tb][:], in0=ps[bL][:, 0:C], scalar1=bupc[:, e_, 8 + fc:9 + fc], scalar2=7.0,
                                                      op0=ALU.add, op1=ALU.min), [psd[bL], bd], [lc_dep[tb]])
                S.op("dve", lambda g: g.tensor_scalar(out=lcs[tb][:], in0=lcs[tb][:], scalar1=-7.0, scalar2=1.0,
                                                      op0=ALU.max, op1=ALU.add), [lc_dep[tb]], [lc_dep[tb]])
                S.op("pool", lambda g: g.tensor_tensor(out=gcs[tb][:], in0=gcs[tb][:], in1=sgs[tb][:], op=ALU.mult), [sg_dep5[tb]], [gc_dep[tb]])
                S.op("pool", lambda g: g.tensor_tensor(out=actT[:, fc, :], in0=gcs[tb][:], in1=lcs[tb][:], op=ALU.mult),
                     [gc_dep[tb], lc_dep[tb]], [actT_dep])
            if e_ + 1 < n_experts:
                load_up(e_ + 1)
            for sc in range(NSC):
                for half in range(2):
                    hs = slice(half * 512, (half + 1) * 512)
                    bD = 4 + dcn % 3
                    dcn += 1
                    for fc in range(8):
                        mm(ps[bD][:, :], actT[:, fc, sc * 128:(sc + 1) * 128], wdnb[:, fc, hs], fc == 0, False,
                           [actT_dep, wdn_dep], [psd[bD]])
                    mm(ps[bD][:, :], onesb[0:1, :], browb[0:1, hs], False, True, [brow_dep, cst], [psd[bD]])
                    S.op("act", lambda g: g.copy(out=ye[:, sc, hs], in_=ps[bD][:, :]), [psd[bD]], [ye_dep])
            if e_ + 1 < n_experts:
                load_dn(e_ + 1)
            for blk in range(16):
                tbf = tcn % 2
                tcn += 1
                for sc in range(NSC):
                    S.op("pe", lambda g: g.transpose(out=psb[:, sc * 128:(sc + 1) * 128], in_=Sel[:, blk, sc * 128:(sc + 1) * 128],
                                                     identity=identb[:]), [Sel_dep, cst], [psb_dep])
                S.op("act", lambda g: g.copy(out=SelTb[tbf][:].rearrange("p c t -> p (c t)"), in_=psb[:, 0:NSC * 128]), [psb_dep], [SelTb_dep[tbf]])
                for half in range(2):
                    hs = slice(half * 512, (half + 1) * 512)
                    bY = 4 + dcn % 3
                    dcn += 1
                    for sc in range(NSC):
                        mm(ps[bY][:, :], SelTb[tbf][:, sc, :], ye[:, sc, hs], sc == 0, sc == NSC - 1, [SelTb_dep[tbf], ye_dep], [psd[bY]])
                    S.op("dve", lambda g: g.scalar_tensor_tensor(out=hres[:, blk, hs], in0=ps[bY][:, :], scalar=gate[:, blk, e_:e_ + 1],
                                                                 in1=hres[:, blk, hs], op0=ALU.mult, op1=ALU.add),
                         [psd[bY], gate_dep, hres_dep[blk]], [hres_dep[blk]])
        S.barrier()

    S.mute = skip6
    with ExitStack() as es:
        sqt_box[0] = sb(es, "sqt6", [128, D], F32)
        wpgb = sb(es, "wpgb", [128, 8, D], BF16)
        wpleb = sb(es, "wpleb", [128, 2, D], BF16)
        pTb = sb(es, "pTb", [128, 2, 2048], BF16)
        stg = [sb(es, f"stg6{i}", [128, 2048], F32) for i in range(2)]
        stg_dep = [Dep() for _ in range(2)]
        lnpb = sb(es, "lnpb", [128, D], F32)
        lnfb = sb(es, "lnfb", [128, D], F32)
        hn = sb(es, "hn", [128, D], BF16)
        hnT = sb(es, "hnT", [128, 8, 128], BF16)
        sig = [sb(es, f"sig{i}", [128, 512], F32) for i in range(2)]
        outt = [sb(es, f"outt{i}", [128, D], F32) for i in range(2)]
        outt_dep = [Dep() for _ in range(2)]
        wd = Dep()
        p6 = Dep()
        out_dep = Dep()
        ctr = [0]
        load_w_bf16(wpgb, wpg_d, 8, D, stg, stg_dep, wd, ctr)
        load_w_bf16(wpleb, wple_d, 2, D, stg, stg_dep, wd, ctr)
        for c2 in range(2):
            k = ctr[0] % 2
            ctr[0] += 1
            S.dma(stg[k][:], pTo_d[c2], W=[stg_dep[k]])
            copy_on(cast_eng(), pTb[:, c2, :], stg[k][:], [stg_dep[k]], [wd])
        S.dma(lnpb[:], lnp_d, W=[wd])
        S.dma(lnfb[:], lnf_d, W=[wd])
        for i in range(16):
            ob = i % 2
            rms_rstd(hres[:, i, :], D, 0, [hres_dep[i]])
            S.op("dve", lambda e: e.scalar_tensor_tensor(out=hn[:], in0=hres[:, i, :], scalar=ssc[:, 0:1], in1=lnpb[:],
                                                         op0=ALU.mult, op1=ALU.mult), [hres_dep[i], nrm, wd], [p6])
            for c in range(8):
                S.op("pe", lambda e: e.transpose(out=psb[:, c * 128:(c + 1) * 128], in_=hn[:, c * 128:(c + 1) * 128],
                                                 identity=identb[:]), [p6, cst], [psb_dep])
            S.op("act", lambda e: e.copy(out=hnT[:].rearrange("p c t -> p (c t)"), in_=psb[:, :]), [psb_dep], [p6])
            for half in range(2):
                hs = slice(half * 512, (half + 1) * 512)
                for c in range(8):
                    mm(ps[half][:, :], hnT[:, c, :], wpgb[:, c, hs], c == 0, c == 7, [p6, wd], [psd[half]])
                for c2 in range(2):
                    mm(ps[2 + half][:, :], pTb[:, c2, i * 128:(i + 1) * 128], wpleb[:, c2, hs], c2 == 0, c2 == 1, [wd], [psd[2 + half]])
                S.op("act", lambda e: e.activation(out=sig[half][:], in_=ps[half][:, :], func=AF.Exp, scale=-1.0), [psd[half]], [p6])
                S.op("dve", lambda e: e.tensor_scalar(out=sig[half][:], in0=sig[half][:], scalar1=1.0, scalar2=None, op0=ALU.add), [p6], [p6])
                S.op("dve", lambda e: e.reciprocal(out=sig[half][:], in_=sig[half][:]), [p6], [p6])
                S.op("dve", lambda e: e.tensor_tensor(out=sig[half][:], in0=ps[2 + half][:, :], in1=sig[half][:], op=ALU.mult),
                     [psd[2 + half], p6], [p6])
                S.op("dve", lambda e: e.tensor_tensor(out=hres[:, i, hs], in0=sig[half][:], in1=hres[:, i, hs], op=ALU.add),
                     [p6, hres_dep[i], nrm], [hres_dep[i]])
            rms_rstd(hres[:, i, :], D, 1, [hres_dep[i]])
            S.op("dve", lambda e: e.scalar_tensor_tensor(out=outt[ob][:], in0=hres[:, i, :], scalar=ssc[:, 1:2], in1=lnfb[:],
                                                         op0=ALU.mult, op1=ALU.mult), [hres_dep[i], nrm, wd], [outt_dep[ob]])
            S.dma(out_d[i * 128:(i + 1) * 128, :], outt[ob][:], R=[outt_dep[ob]], W=[out_dep])
        S.barrier()
    es_tail.close()

    if stage <= 3:
        dbg_f = nc.dram_tensor("dbg_ff", [128, 64 * 8 + 16 * 24], F32, kind="ExternalOutput").ap()
        S.dma(dbg_f[:, 0:512], ffall[:].rearrange("p a b -> p (a b)"))
        S.dma(dbg_f[:, 512:896], gown[:].rearrange("p a b -> p (a b)"))
        S.barrier()
        es_all.close()
        return nc, S

    es_all.close()
    return nc, S


def own_tokens(j):
    return np.concatenate([np.arange(512 * i + 128 * j, 512 * i + 128 * j + 128) for i in range(16)])


def const_tables(j):
    p = np.arange(128)
    c = {}
    c["identb"] = _bf(np.eye(128, dtype=np.float32))
    c["identf"] = np.eye(128, dtype=np.float32)
    c["trif"] = (p[:, None] <= p[None, :]).astype(np.float32)
    sl = p[:, None]
    tl = p[None, :]
    dm = np.zeros((128, 4, 128), np.float32)
    for kk in range(4):
        dist = 128 * (j - kk) + tl - sl
        dm[:, kk, :] = np.where(dist >= 0, 0.0, NEGM)
    c["diagm"] = _bf(dm)
    wm = np.zeros((128, 8, 128), np.float32)
    for wk in range(8):
        dist = 128 * (j + 4 - wk) + tl - sl
        wm[:, wk, :] = np.where((dist >= 0) & (dist < 512), 0.0, NEGM)
    c["winm"] = _bf(wm)
    cm = np.zeros((128, 5, 128), np.float32)
    for dd in range(5):
        d = dd - 4
        cond = (512 * d + 16 * sl - tl - 128 * j + 31) <= 0
        cm[:, dd, :] = np.where(cond, 0.0, NEGM)
    c["cmask"] = _bf(cm)
    slopes = np.exp2(-8.0 * np.arange(1, 9, dtype=np.float32) / 8).astype(np.float32)
    rel = np.arange(64)
    ab = slopes[None, :, None] * (p[:, None, None] - 127 - 128 * (rel[None, None, :] + j - 3))
    c["ab"] = np.ascontiguousarray(ab[:, :, ::-1]).astype(np.float32)
    dd = np.arange(16) - 15
    cab = slopes[None, :, None] * (16 * p[:, None, None] + 512 * dd[None, None, :] - 128 * j - 96)
    c["cab"] = cab.astype(np.float32)
    n = np.arange(128)
    selA = np.zeros((128, 16, 128), np.float32)
    selB = np.zeros((128, 16, 128), np.float32)
    for i in range(16):
        cur = (512 * i + 128 * j + p) // 64
        valid = n[None, :] <= cur[:, None]
        forced = valid & ((n[None, :] == 0) | (n[None, :] == cur[:, None]) | (n[None, :] == cur[:, None] - 1))
        selA[:, i, :] = (valid & ~forced)
        selB[:, i, :] = np.where(forced, 1e9, np.where(valid, 0.0, -1.0))
    c["selA"] = _bf(selA)
    c["selB"] = _bf(selB)
    ws = np.zeros((128, 16, 64), np.float32)
    for i in range(16):
        ws[:, i, :] = (np.arange(64)[None, :] <= 4 * i + j)
    c["wsel"] = ws
    s = np.arange(T)
    c["Rexp"] = _bf((n[:, None] == (s[None, :] // 64)).astype(np.float32))
    cc = np.arange(512)
    ov = ((cc[:, None] * 16 < n[None, :] * 64 + 64) & (cc[:, None] * 16 + 31 >= n[None, :] * 64)).astype(np.float32)
    ov[511, :] = 0.0
    c["ovl"] = _bf(ov.reshape(4, 128, 128).transpose(1, 0, 2))
    c["iotac"] = np.ascontiguousarray(np.broadcast_to(np.arange(CAP, dtype=np.float32)[None, :], (128, CAP)))
    return c


def make_in_maps(x, p, ln1, w_in, b_fg, w_cmp1_k, w_cmp2_k, pe_cmp_k, w_cmp1_v, w_cmp2_v, pe_cmp_v, gn_nsa, gn_fox,
                 w_out, ln2, w_router, b_router, w_up, b_up, w_down, b_down, ln_ple, w_ple, w_ple_gate, ln_f, ne=NE):
    f = lambda a: np.ascontiguousarray(np.asarray(a, dtype=np.float32))
    x = f(x); p = f(p); w = f(w_in)[0]
    q_n = w[:, 0:512]; k_c = w[:, 512:640]; v_c = w[:, 640:768]; k_s = w[:, 768:896]; v_s = w[:, 896:1024]
    k_w = w[:, 1024:1152]; v_w = w[:, 1152:1280]; g_n = w[:, 1280:1304]; q_f = w[:, 1304:1816]
    k_f = w[:, 1816:2328]; v_f = w[:, 2328:2840]; f_f = w[:, 2840:2848]
    bc = lambda v: f(np.broadcast_to(np.asarray(v, np.float32).reshape(1, -1), (128, np.asarray(v).size)))
    shared = {
        "wA": f(np.concatenate([k_f, k_s, k_w, k_c, v_c], 1)),
        "wB": f(np.concatenate([v_f, v_s, v_w, f_f, g_n], 1)),
        "wQ": f(np.concatenate([q_n, q_f], 1)),
        "ln1c": f(np.asarray(ln1, np.float32)[0].reshape(8, 128).T),
        "bfg": bc(np.asarray(b_fg)[0]),
        "gnb": bc(np.concatenate([np.asarray(gn_nsa)[0], np.asarray(gn_fox)[0]])),
        "wout": f(w_out)[0], "ln2b": bc(np.asarray(ln2)[0]), "wr": f(w_router)[0], "brb": bc(np.asarray(b_router)[0]),
        "wup": f(np.asarray(w_up)[0, :ne]), "wdn": f(np.asarray(w_down)[0, :ne]), "bdn": f(np.asarray(b_down)[0]),
        "bupc": f(np.asarray(b_up, np.float32)[0].reshape(NE, 16, 128).transpose(2, 0, 1)),
        "lnpb": bc(np.asarray(ln_ple)[0]), "wple": f(w_ple)[0], "wpg": f(w_ple_gate)[0], "lnfb": bc(np.asarray(ln_f)),
    }
    for nm, w1, w2, pe in (("k", w_cmp1_k, w_cmp2_k, pe_cmp_k), ("v", w_cmp1_v, w_cmp2_v, pe_cmp_v)):
        w1r = np.asarray(w1, np.float32)[0].reshape(32, 64, 128).transpose(1, 0, 2)
        shared["w1" + nm] = f(np.concatenate([w1r, w1r], 0))
        peT = np.asarray(pe, np.float32)[0].T
        peT = np.concatenate([peT, peT], 0)
        shared["pe" + nm] = f(np.stack([peT, peT], -1))
    w2k = np.asarray(w_cmp2_k, np.float32)[0]
    shared["w2k"] = f(np.concatenate([w2k, w2k], 1))
    shared["w2v"] = f(np.asarray(w_cmp2_v, np.float32)[0])
    maps = []
    for c in range(NCORES):
        b, j = c // 4, c % 4
        tok = own_tokens(j)
        m = dict(shared)
        m["xT"] = f(x[b].T.reshape(8, 128, T))
        m["xTo"] = f(x[b][tok].T.reshape(8, 128, 2048))
        m["xo"] = f(x[b][tok])
        m["pTo"] = f(p[0, b][tok].T.reshape(2, 128, 2048))
        m.update(const_tables(j))
        maps.append(m)
    return maps


_CACHE = {}


def kernel(**inputs):
    maps = make_in_maps(**inputs)
    if "nc" not in _CACHE:
        _CACHE["nc"] = build_program()[0]
    nc = _CACHE["nc"]
    res = run_bass_kernel_spmd(nc, maps, core_ids=list(range(NCORES)))
    out = np.zeros((2, T, D), np.float32)
    for c in range(NCORES):
        b, j = c // 4, c % 4
        out[b, own_tokens(j)] = np.asarray(res.results[c]["out"], np.float32).reshape(2048, D)
    return out
```

```python
from contextlib import ExitStack
import numpy as np
import ml_dtypes
import concourse.bass as bass
import concourse.mybir as mybir
from concourse.bass_utils import run_bass_kernel_spmd

F32 = mybir.dt.float32
BF16 = mybir.dt.bfloat16
AF = mybir.ActivationFunctionType
ALU = mybir.AluOpType
AX = mybir.AxisListType

NCORES = 8
T = 8192
D = 1024
NEGM = -30000.0
EPS = 1e-6
NE = 32
MOE_EXPERTS = 32
CAP = 512


class Dep:
    __slots__ = ("w", "r")

    def __init__(self):
        self.w = None
        self.r = []


class Sched:
    ROLL = 30000

    def __init__(self, nc, n_dma=40):
        self.nc = nc
        self.E = {"pe": nc.tensor, "act": nc.scalar, "dve": nc.vector, "pool": nc.gpsimd, "sp": nc.sync}
        self.csem = {}
        self.cnt = {}
        self.nsem = 0
        for k in ("pe", "act", "dve", "pool"):
            self._new_csem(k)
        self.seen = {k: {} for k in self.E}
        self.dsem = [nc.alloc_semaphore(name=f"dq{i}") for i in range(n_dma)]
        self.dval = [0] * n_dma
        self.dnext = 0
        self.mute = False
        self.ninst = 0

    def _new_csem(self, k):
        self.csem[k] = self.nc.alloc_semaphore(name=f"c{k}{self.nsem}")
        self.nsem += 1
        self.cnt[k] = 0

    def _collect(self, e, R, W):
        evs = []
        for d in R:
            if d.w is not None:
                evs.append(d.w)
        for d in W:
            if d.w is not None:
                evs.append(d.w)
            evs.extend(d.r)
        return evs

    def _wait(self, e, evs):
        eng = self.E[e]
        seen = self.seen[e]
        need = {}
        for (s, v, src) in evs:
            if src == "pe" and e == "pe":
                continue
            if src == e and s is self.csem.get(e) and self.cnt[e] - v >= 3:
                continue
            key = s.num
            if seen.get(key, 0) >= v:
                continue
            if key not in need or need[key][1] < v:
                need[key] = (s, v)
        for key, (s, v) in need.items():
            eng.wait_ge(s, v)
            seen[key] = v
            self.ninst += 1

    def _mark(self, ev, R, W):
        for d in R:
            d.r.append(ev)
            if len(d.r) > 64:
                d.r = d.r[-64:]
        for d in W:
            d.w = ev
            d.r = []

    def op(self, e, fn, R=(), W=()):
        if self.mute:
            return None
        self._wait(e, self._collect(e, R, W))
        ins = fn(self.E[e])
        if self.cnt[e] >= self.ROLL:
            self._new_csem(e)
        self.cnt[e] += 1
        ins.then_inc(self.csem[e], 1)
        ev = (self.csem[e], self.cnt[e], e)
        self._mark(ev, R, W)
        self.ninst += 1
        return ev

    def dma(self, out, in_, R=(), W=(), e="sp"):
        if self.mute:
            return None
        k = self.dnext
        self.dnext = (k + 1) % len(self.dsem)
        if self.dval[k] >= self.ROLL:
            self._wait(e, [(self.dsem[k], self.dval[k], "dma")])
            self.dsem[k] = self.nc.alloc_semaphore(name=f"dq{k}_{self.nsem}")
            self.nsem += 1
            self.dval[k] = 0
        s = self.dsem[k]
        evs = self._collect(e, R, W)
        if self.dval[k] > 0:
            evs.append((s, self.dval[k], "dma"))
        self._wait(e, evs)
        self.E[e].dma_start(out=out, in_=in_).then_inc(s, 16)
        self.dval[k] += 16
        ev = (s, self.dval[k], "dma")
        self._mark(ev, R, W)
        self.ninst += 1
        return ev

    def barrier(self):
        evs = [(self.csem[k], self.cnt[k], k + "_b") for k in self.csem if self.cnt[k] > 0]
        evs += [(self.dsem[k], self.dval[k], "dma") for k in range(len(self.dsem)) if self.dval[k] > 0]
        for e in self.E:
            self._wait(e, [x for x in evs])


def _bf(a):
    return np.ascontiguousarray(a).astype(ml_dtypes.bfloat16)


def build_program(stage=99, n_experts=MOE_EXPERTS, skip123=False, p4stop=99, nblk4=16, skip5=False, skip6=False):
    nc = bass.Bass("TRN2", target_bir_lowering=False)
    S = Sched(nc)

    def din(name, shape, dt=F32):
        return nc.dram_tensor(name, list(shape), dt, kind="ExternalInput").ap()

    dbg = stage < 99

    def dscr(name, shape, dt):
        return nc.dram_tensor(name, list(shape), dt, kind=("ExternalOutput" if dbg else "Internal")).ap()

    xT_d = din("xT", [8, 128, T])
    xTo_d = din("xTo", [8, 128, 2048])
    xo_d = din("xo", [2048, D])
    pTo_d = din("pTo", [2, 128, 2048])
    wA_d = din("wA", [D, 1024])
    wB_d = din("wB", [D, 800])
    wQ_d = din("wQ", [D, 1024])
    ln1c_d = din("ln1c", [128, 8])
    bfg_d = din("bfg", [128, 8])
    w1k_d = din("w1k", [128, 32, 128])
    w1v_d = din("w1v", [128, 32, 128])
    w2k_d = din("w2k", [128, 128])
    w2v_d = din("w2v", [128, 64])
    pek_d = din("pek", [128, 32, 2])
    pev_d = din("pev", [128, 32, 2])
    gn_d = din("gnb", [128, 1024])
    wout_d = din("wout", [D, D])
    ln2_d = din("ln2b", [128, D])
    wr_d = din("wr", [D, 32])
    br_d = din("brb", [128, 32])
    wup_d = din("wup", [n_experts, D, 2048])
    bup_d = din("bupc", [128, NE, 16])
    wdn_d = din("wdn", [n_experts, D, D])
    bdn_d = din("bdn", [NE, D])
    lnp_d = din("lnpb", [128, D])
    wple_d = din("wple", [256, D])
    wpg_d = din("wpg", [D, D])
    lnf_d = din("lnfb", [128, D])
    identb_d = din("identb", [128, 128], BF16)
    identf_d = din("identf", [128, 128])
    tri_d = din("trif", [128, 128])
    diagm_d = din("diagm", [128, 4, 128], BF16)
    winm_d = din("winm", [128, 8, 128], BF16)
    cmask_d = din("cmask", [128, 5, 128], BF16)
    ab_d = din("ab", [128, 8, 64])
    cab_d = din("cab", [128, 8, 16])
    selA_d = din("selA", [128, 16, 128], BF16)
    selB_d = din("selB", [128, 16, 128], BF16)
    wsel_d = din("wsel", [128, 16, 64])
    R_d = din("Rexp", [128, T], BF16)
    ovl_d = din("ovl", [128, 4, 128], BF16)
    iota_d = din("iotac", [128, CAP])

    out_d = nc.dram_tensor("out", [2048, D], F32, kind="ExternalOutput").ap()

    fmS = dscr("fmS", [8, 128, T], BF16)
    tmS = dscr("tmS", [T, 780], BF16)
    qS = dscr("qS", [16, 16, 128, 128], BF16)
    fmS_dep = [Dep() for _ in range(8)]
    tmS_dep = Dep()
    qS_dep = Dep()

    es_all = ExitStack()

    def sb(es, name, shape, dt):
        return es.enter_context(nc.sbuf_tensor("s_" + name, list(shape), dt))

    ps = [es_all.enter_context(nc.psum_tensor(f"ps{i}", [128, 512], F32)) for i in range(7)]
    psd = [Dep() for _ in range(7)]
    psb = es_all.enter_context(nc.psum_tensor("psb", [128, 1024], BF16))
    psb_dep = Dep()

    identb = sb(es_all, "identb", [128, 128], BF16)
    identf = sb(es_all, "identf", [128, 128], F32)
    trif = sb(es_all, "trif", [128, 128], F32)
    onesb = sb(es_all, "onesb", [128, 128], BF16)
    onesf = sb(es_all, "onesf", [128, 128], F32)
    epsc = sb(es_all, "epsc", [128, 1], F32)
    onec = sb(es_all, "onec", [128, 1], F32)
    cst = Dep()
    S.dma(identb[:], identb_d, W=[cst])
    S.dma(identf[:], identf_d, W=[cst])
    S.dma(trif[:], tri_d, W=[cst])
    S.op("dve", lambda e: e.memset(onesb[:], 1.0), W=[cst])
    S.op("dve", lambda e: e.memset(onesf[:], 1.0), W=[cst])
    S.op("dve", lambda e: e.memset(epsc[:], EPS), W=[cst])
    S.op("dve", lambda e: e.memset(onec[:], 1.0), W=[cst])

    es_attn = ExitStack()
    ffall = sb(es_attn, "ffall", [128, 64, 8], F32)
    ffall_dep = Dep()
    gown = sb(es_attn, "gown", [128, 16, 24], F32)
    gown_dep = Dep()

    rr = {"cast": 0, "evac": 0}

    def cast_eng():
        rr["cast"] += 1
        return ("act", "dve", "pool")[rr["cast"] % 3]

    def copy_on(e, out, in_, R, W):
        if e == "act":
            S.op("act", lambda g: g.copy(out=out, in_=in_), R, W)
        elif e == "dve":
            S.op("dve", lambda g: g.tensor_copy(out=out, in_=in_), R, W)
        else:
            S.op("pool", lambda g: g.tensor_copy(out=out, in_=in_), R, W)

    def evac_eng():
        rr["evac"] += 1
        return ("act", "dve")[rr["evac"] % 2]

    def mm(out, lhsT, rhs, start, stop, R, W):
        S.op("pe", lambda g: g.matmul(out, lhsT=lhsT, rhs=rhs, start=start, stop=stop), R, W)

    pctr = [0]
    LAG = 2

    def run_pipe(steps, LAG=1):
        n = len(steps)
        for k in range(n + LAG):
            if k < n:
                steps[k][0]()
            if k - LAG >= 0:
                steps[k - LAG][1]()

    S.mute = skip123
    with ExitStack() as es:
        wA = sb(es, "wA", [128, 8, 1024], BF16)
        wB = sb(es, "wB", [128, 8, 800], BF16)
        wQz = sb(es, "wQz", [128, 8, 16, 128], BF16)
        stg = [sb(es, f"stg{i}", [128, 1024], F32) for i in range(2)]
        stg_dep = [Dep() for _ in range(2)]
        ln1c = sb(es, "ln1c", [128, 8], F32)
        xt = [sb(es, f"xt{i}", [128, 8, 512], F32) for i in range(2)]
        xt_dep = [Dep() for _ in range(2)]
        sq = sb(es, "sq", [128, 8, 512], BF16)
        sq_dep = Dep()
        rstd = sb(es, "rstd", [128, 512], F32)
        rstd_dep = Dep()
        uT = sb(es, "uT", [128, 8, 512], BF16)
        uT_dep = Dep()
        fmo = [sb(es, f"fmo{i}", [128, 8, 512], BF16) for i in range(2)]
        fmo_dep = [Dep() for _ in range(2)]
        tmv = [sb(es, f"tmv{i}", [128, 4, 780], BF16) for i in range(2)]
        tmv_dep = [Dep() for _ in range(2)]
        qo = [sb(es, f"qo{i}", [128, 4, 16, 128], BF16) for i in range(2)]
        qo_dep = [Dep() for _ in range(2)]
        w_dep = Dep()

        S.dma(ln1c[:], ln1c_d, W=[w_dep])
        S.op("pool", lambda e: e.memset(wQz[:], 0.0), W=[w_dep])
        for k in range(2):
            S.op("pool", lambda e: e.memset(tmv[k][:], 1.0), W=[tmv_dep[k]])
        sc = 0
        for dc in range(8):
            for (src, dst, ncol) in ((wA_d, wA, 1024), (wB_d, wB, 800)):
                k = sc % 2
                sc += 1
                S.dma(stg[k][:, 0:ncol], src[dc * 128:(dc + 1) * 128, :], W=[stg_dep[k]])
                copy_on(cast_eng(), dst[:, dc, :], stg[k][:, 0:ncol], [stg_dep[k]], [w_dep])
            k = sc % 2
            sc += 1
            S.dma(stg[k][:, :], wQ_d[dc * 128:(dc + 1) * 128, :], W=[stg_dep[k]])
            copy_on(cast_eng(), wQz[:, dc, 0:4, 0:64],
                    stg[k][:, 0:256].rearrange("p (h e) -> p h e", e=64), [stg_dep[k]], [w_dep])
            copy_on(cast_eng(), wQz[:, dc, 4:8, 64:128],
                    stg[k][:, 256:512].rearrange("p (h e) -> p h e", e=64), [stg_dep[k]], [w_dep])
            fx = stg[k][:, 512:1024].rearrange("p (h two e) -> p h two e", two=2, e=64)
            wz = wQz[:, dc, 8:16, :].rearrange("p (h two) e -> p h two e", two=2)
            copy_on(cast_eng(), wz[:, :, 0, 0:64], fx[:, :, 0, :], [stg_dep[k]], [w_dep])
            copy_on(cast_eng(), wz[:, :, 1, 64:128], fx[:, :, 1, :], [stg_dep[k]], [w_dep])

        def norm_tile(src_ap, k):
            S.dma(xt[k][:], src_ap, W=[xt_dep[k]])
            S.op("act", lambda e: e.activation(out=sq[:], in_=xt[k][:], func=AF.Square), [xt_dep[k]], [sq_dep])
            for dc in range(8):
                mm(ps[0][:, :], onesb[:], sq[:, dc, :], dc == 0, dc == 7, [sq_dep, cst], [psd[0]])
            S.op("act", lambda e: e.activation(out=rstd[:], in_=ps[0][:, :], func=AF.Sqrt,
                                               bias=epsc[:, 0:1], scale=1.0 / D), [psd[0], cst], [rstd_dep])
            S.op("dve", lambda e: e.reciprocal(out=rstd[:], in_=rstd[:]), [rstd_dep], [rstd_dep])
            for dc in range(8):
                S.op("dve", lambda e: e.scalar_tensor_tensor(
                    out=uT[:, dc, :], in0=xt[k][:, dc, :], scalar=ln1c[:, dc:dc + 1], in1=rstd[:],
                    op0=ALU.mult, op1=ALU.mult), [xt_dep[k], rstd_dep, w_dep], [uT_dep])

        xT_v = xT_d.rearrange("c p s -> p c s")
        xTo_v = xTo_d.rearrange("c p s -> p c s")
        fmS_v = fmS.rearrange("o p s -> p o s")
        for Tt in range(16):
            k = Tt % 2
            norm_tile(xT_v[:, :, Tt * 512:(Tt + 1) * 512], k)
            for oc in range(8):
                b = 1 + oc % 2
                for dc in range(8):
                    mm(ps[b][:, :], wA[:, dc, oc * 128:(oc + 1) * 128], uT[:, dc, :], dc == 0, dc == 7,
                       [uT_dep, w_dep], [psd[b]])
                copy_on(evac_eng(), fmo[k][:, oc, :], ps[b][:, :], [psd[b]], [fmo_dep[k]])
            S.dma(fmS_v[:, :, Tt * 512:(Tt + 1) * 512], fmo[k][:], R=[fmo_dep[k]], W=fmS_dep)
            for sub in range(4):
                bA = 3 + (sub % 2) * 2
                bB = bA + 1
                for dc in range(8):
                    mm(ps[bA][:, 0:512], uT[:, dc, sub * 128:(sub + 1) * 128], wB[:, dc, 0:512], dc == 0, dc == 7,
                       [uT_dep, w_dep], [psd[bA]])
                for dc in range(8):
                    mm(ps[bB][:, 0:288], uT[:, dc, sub * 128:(sub + 1) * 128], wB[:, dc, 512:800], dc == 0, dc == 7,
                       [uT_dep, w_dep], [psd[bB]])
                copy_on("act", tmv[k][:, sub, 0:520].rearrange("p (h e) -> p h e", e=65)[:, :, 0:64],
                        ps[bA][:, 0:512].rearrange("p (h e) -> p h e", e=64), [psd[bA]], [tmv_dep[k]])
                copy_on("dve", tmv[k][:, sub, 520:780].rearrange("p (h e) -> p h e", e=65)[:, :, 0:64],
                        ps[bB][:, 0:256].rearrange("p (h e) -> p h e", e=64), [psd[bB]], [tmv_dep[k]])
                copy_on("dve", ffall[:, Tt * 4 + sub, :], ps[bB][:, 256:264], [psd[bB]], [ffall_dep])
            S.dma(tmS[Tt * 512:(Tt + 1) * 512, :].rearrange("(s p) c -> p s c", p=128), tmv[k][:],
                  R=[tmv_dep[k]], W=[tmS_dep])

        qS_v = qS.rearrange("i h p t -> i p h t")
        for T4 in range(4):
            norm_tile(xTo_v[:, :, T4 * 512:(T4 + 1) * 512], T4 % 2)
            k = T4 % 2
            for hd in range(16):
                b = 1 + hd % 2
                for dc in range(8):
                    mm(ps[b][:, :], wQz[:, dc, hd, :], uT[:, dc, :], dc == 0, dc == 7, [uT_dep, w_dep], [psd[b]])
                qdst = qo[k][:, :, hd, :]
                qsrc = ps[b][:, :].rearrange("p (b t) -> p b t", t=128)
                if hd % 2 == 0:
                    S.op("act", lambda e: e.activation(out=qdst, in_=qsrc, func=AF.Copy, scale=0.125), [psd[b]], [qo_dep[k]])
                else:
                    S.op("dve", lambda e: e.tensor_scalar(out=qdst, in0=qsrc, scalar1=0.125, scalar2=None, op0=ALU.mult),
                         [psd[b]], [qo_dep[k]])
            for bi in range(4):
                i = T4 * 4 + bi
                for dc in range(8):
                    mm(ps[3][:, 0:24], uT[:, dc, bi * 128:(bi + 1) * 128], wB[:, dc, 776:800], dc == 0, dc == 7,
                       [uT_dep, w_dep], [psd[3]])
                copy_on("dve", gown[:, i, :], ps[3][:, 0:24], [psd[3]], [gown_dep])
                S.dma(qS_v[i], qo[k][:, bi, :, :], R=[qo_dep[k]], W=[qS_dep])
        S.barrier()

    if stage <= 1:
        dbg_f = nc.dram_tensor("dbg_ff", [128, 64 * 8 + 16 * 24], F32, kind="ExternalOutput").ap()
        S.dma(dbg_f[:, 0:512], ffall[:].rearrange("p a b -> p (a b)"))
        S.dma(dbg_f[:, 512:896], gown[:].rearrange("p a b -> p (a b)"))
        S.barrier()
        es_attn.close()
        es_all.close()
        return nc, S

    mixS = dscr("mixS", [2048, D], BF16)
    mixS_dep = Dep()
    diagm = sb(es_attn, "diagm", [128, 4, 128], BF16)
    S.dma(diagm[:], diagm_d, W=[cst])
    pT = [sb(es_attn, f"pT{i}", [128, 512], BF16) for i in range(4)]
    pT_dep = [Dep() for _ in range(4)]
    zc = [sb(es_attn, f"zc{i}", [128, 4], F32) for i in range(2)]
    zc_dep = [Dep() for _ in range(2)]

    with ExitStack() as es:
        bfg = sb(es, "bfg", [128, 8], F32)
        wsel = sb(es, "wsel", [128, 16, 64], F32)
        lsp = sb(es, "lsp", [128, 64, 8], F32)
        cpcol = sb(es, "cpcol", [128, 64, 8], F32)
        tot = sb(es, "tot", [128, 64, 8], F32)
        pre = sb(es, "pre", [128, 64, 8], F32)
        cpref = sb(es, "cpref", [128, 16, 8], F32)
        tmpw = sb(es, "tmpw", [128, 8, 64], F32)
        cd = Dep()
        S.dma(bfg[:], bfg_d, W=[cd])
        S.dma(wsel[:], wsel_d, W=[cd])
        S.op("dve", lambda e: e.tensor_tensor(out=lsp[:], in0=ffall[:], in1=bfg[:, :].unsqueeze(1).to_broadcast([128, 64, 8]),
                                              op=ALU.add), [ffall_dep, cd], [cd])
        S.op("act", lambda e: e.activation(out=lsp[:], in_=lsp[:], func=AF.Exp, scale=-1.0), [cd], [cd])
        S.op("act", lambda e: e.activation(out=lsp[:], in_=lsp[:], func=AF.Ln, bias=onec[:, 0:1], scale=1.0), [cd, cst], [cd])
        lflat = lsp[:].rearrange("p a b -> p (a b)")
        mm(ps[0][:, :], trif[:], lflat, True, True, [cd, cst], [psd[0]])
        mm(ps[1][:, :], onesf[:], lflat, True, True, [cd, cst], [psd[1]])
        S.op("dve", lambda e: e.tensor_copy(out=tot[:].rearrange("p a b -> p (a b)"), in_=ps[1][:, :]), [psd[1]], [cd])
        S.op("dve", lambda e: e.memset(pre[:, 0, :], 0.0), [], [cd])
        for k in range(1, 64):
            S.op("dve", lambda e: e.tensor_tensor(out=pre[:, k, :], in0=pre[:, k - 1, :], in1=tot[:, k - 1, :], op=ALU.add), [cd], [cd])
        S.op("dve", lambda e: e.tensor_tensor(out=cpcol[:].rearrange("p a b -> p (a b)"), in0=ps[0][:, :],
                                              in1=pre[:].rearrange("p a b -> p (a b)"), op=ALU.add), [psd[0], cd], [cd])
        for i in range(16):
            S.op("dve", lambda e: e.tensor_tensor(out=tmpw[:], in0=tot[:].rearrange("p k h -> p h k"),
                                                  in1=wsel[:, i, :].unsqueeze(1).to_broadcast([128, 8, 64]), op=ALU.mult), [cd], [cd])
            S.op("dve", lambda e: e.reduce_sum(out=cpref[:, i, :], in_=tmpw[:], axis=AX.X), [cd], [cd])

        kT = [sb(es, f"kT{i}", [128, T], BF16) for i in range(2)]
        vP = [sb(es, f"vP{i}", [128, 64, 130], BF16) for i in range(2)]
        qP = [sb(es, f"qP{i}", [128, 16, 2, 128], BF16) for i in range(2)]
        kvq_dep = [Dep() for _ in range(2)]
        wF = [sb(es, f"wF{i}", [128, 64], F32) for i in range(2)]
        wF_dep = [Dep() for _ in range(2)]
        Vp = [sb(es, f"Vp{i}", [128, 64, 65], BF16) for i in range(2)]
        Vp_dep = [Dep() for _ in range(2)]
        mo = [sb(es, f"mo{i}", [128, 128], BF16) for i in range(2)]
        mo_dep = [Dep() for _ in range(2)]
        tmS_v = tmS.rearrange("(k p) c -> p k c", p=128)
        qS_p = qS.rearrange("i h p t -> p i h t")
        items = [(hp, i, hh) for hp in range(4) for i in range(16) for hh in range(2)]

        def fox_prep(n):
            hp, i, hh = items[n]
            kb = hp % 2
            bb = n % 2
            h = 2 * hp + hh
            nk = 4 * i + 4
            if i == 0 and hh == 0:
                S.dma(kT[kb][:], fmS[hp], R=[fmS_dep[hp]], W=[kvq_dep[kb]])
                for q4 in range(4):
                    S.dma(vP[kb][:, q4 * 16:(q4 + 1) * 16, :], tmS_v[:, q4 * 16:(q4 + 1) * 16, hp * 130:(hp + 1) * 130],
                          R=[tmS_dep], W=[kvq_dep[kb]])
                for q2 in range(2):
                    S.dma(qP[kb][:, :, q2, :], qS_p[:, :, 8 + 2 * hp + q2, :], R=[qS_dep], W=[kvq_dep[kb]])
            S.op("dve", lambda e: e.tensor_scalar(out=wF[bb][:, 0:nk], in0=cpcol[:, 0:nk, h], scalar1=cpref[:, i, h:h + 1],
                                                  scalar2=0.0, op0=ALU.subtract, op1=ALU.min), [cd], [wF_dep[bb]])
            S.op("act", lambda e: e.activation(out=wF[bb][:, 0:nk], in_=wF[bb][:, 0:nk], func=AF.Exp), [wF_dep[bb]], [wF_dep[bb]])
            eng = "dve" if n % 2 == 0 else "pool"
            S.op(eng, lambda e: e.tensor_tensor(out=Vp[bb][:, 0:nk, :], in0=vP[kb][:, 0:nk, hh * 65:(hh + 1) * 65],
                                                in1=wF[bb][:, 0:nk].unsqueeze(2).to_broadcast([128, nk, 65]), op=ALU.mult),
                 [kvq_dep[kb], wF_dep[bb]], [Vp_dep[bb]])

        def fox_run(n):
            hp, i, hh = items[n]
            kb = hp % 2
            bb = n % 2
            ob = 4 + n % 2
            mb = i % 2
            nk = 4 * i + 4
            steps = []
            for gq in range(nk // 4):
                bank = pctr[0] % 4
                pctr[0] += 1

                def qk(gq=gq, bank=bank):
                    for q in range(4):
                        k = 4 * gq + q
                        diag = k >= 4 * i
                        mm(ps[bank][:, q * 128:(q + 1) * 128], kT[kb][:, k * 128:(k + 1) * 128], qP[kb][:, i, hh, :], True, not diag,
                           [kvq_dep[kb]], [psd[bank]])
                        if diag:
                            mm(ps[bank][:, q * 128:(q + 1) * 128], identb[:], diagm[:, k - 4 * i, :], False, True, [cst], [psd[bank]])
                    S.op("act", lambda e: e.activation(out=pT[bank][:], in_=ps[bank][:, :], func=AF.Exp), [psd[bank]], [pT_dep[bank]])

                def pv(gq=gq, bank=bank):
                    for q in range(4):
                        k = 4 * gq + q
                        mm(ps[ob][:, 0:65], pT[bank][:, q * 128:(q + 1) * 128], Vp[bb][:, k, :], k == 0, k == nk - 1,
                           [pT_dep[bank], Vp_dep[bb]], [psd[ob]])
                steps.append((qk, pv))
            run_pipe(steps)
            z = zc[bb]
            S.op("dve", lambda e: e.tensor_scalar_max(out=z[:, 0:1], in0=ps[ob][:, 64:65], scalar1=1e-30), [psd[ob]], [zc_dep[bb]])
            S.op("dve", lambda e: e.reciprocal(out=z[:, 0:1], in_=z[:, 0:1]), [zc_dep[bb]], [zc_dep[bb]])
            S.op("dve", lambda e: e.tensor_scalar(out=mo[mb][:, hh * 64:(hh + 1) * 64], in0=ps[ob][:, 0:64],
                                                  scalar1=z[:, 0:1], scalar2=None, op0=ALU.mult),
                 [psd[ob], zc_dep[bb]], [mo_dep[mb]])
            if hh == 1:
                S.dma(mixS[i * 128:(i + 1) * 128, 512 + hp * 128:512 + (hp + 1) * 128], mo[mb][:], R=[mo_dep[mb]], W=[mixS_dep])

        fox_prep(0)
        for n in range(len(items)):
            if n + 1 < len(items):
                fox_prep(n + 1)
            fox_run(n)
        S.barrier()

    if stage <= 2:
        es_attn.close()
        es_all.close()
        return nc, S

    with ExitStack() as es:
        kcT = sb(es, "kcT", [128, 512], BF16)
        vc = sb(es, "vc", [128, 4, 130], BF16)
        kc_dep = Dep()
        S.op("pool", lambda e: e.memset(vc[:], 1.0), [], [kc_dep])
        KsT = sb(es, "KsT", [128, T], BF16)
        KwT = sb(es, "KwT", [128, T], BF16)
        Vs = sb(es, "Vs", [128, 64, 130], BF16)
        Vw = sb(es, "Vw", [128, 64, 130], BF16)
        Rx = sb(es, "Rx", [128, T], BF16)
        ovl = sb(es, "ovl", [128, 4, 128], BF16)
        ab = sb(es, "ab", [128, 8, 64], F32)
        cab = sb(es, "cab", [128, 8, 16], F32)
        selA = sb(es, "selA", [128, 16, 128], BF16)
        selB = sb(es, "selB", [128, 16, 128], BF16)
        cmask = sb(es, "cmask", [128, 5, 128], BF16)
        winm = sb(es, "winm", [128, 8, 128], BF16)
        nd = Dep()
        tmS_v = tmS.rearrange("(k p) c -> p k c", p=128)
        S.dma(KsT[:], fmS[4], R=[fmS_dep[4]], W=[nd])
        S.dma(KwT[:], fmS[5], R=[fmS_dep[5]], W=[nd])
        for q4 in range(4):
            S.dma(Vs[:, q4 * 16:(q4 + 1) * 16, :], tmS_v[:, q4 * 16:(q4 + 1) * 16, 520:650], R=[tmS_dep], W=[nd])
            S.dma(Vw[:, q4 * 16:(q4 + 1) * 16, :], tmS_v[:, q4 * 16:(q4 + 1) * 16, 650:780], R=[tmS_dep], W=[nd])
        for (dst, src) in ((Rx, R_d), (ovl, ovl_d), (ab, ab_d), (cab, cab_d), (selA, selA_d), (selB, selB_d),
                           (cmask, cmask_d), (winm, winm_d)):
            S.dma(dst[:], src, W=[nd])
        wab = sb(es, "wab", [128, 8, 64], F32)
        wab_dep = Dep()
        Vsp = [sb(es, f"Vsp{i}", [128, 64, 65], BF16) for i in range(2)]
        Vwp = [sb(es, f"Vwp{i}", [128, 8, 65], BF16) for i in range(2)]
        Vxp_dep = [Dep() for _ in range(2)]
        with ExitStack() as es2:
            kraw = sb(es2, "kraw", [128, T], BF16)
            w1f = sb(es2, "w1f", [128, 32, 128], F32)
            w1b = sb(es2, "w1b", [128, 32, 128], BF16)
            pef = sb(es2, "pef", [128, 32, 2], F32)
            peb = sb(es2, "peb", [128, 32, 2], BF16)
            w2f = sb(es2, "w2f", [128, 128], F32)
            w2b = sb(es2, "w2b", [128, 128], BF16)
            hx = sb(es2, "hx", [128, 512], F32)
            hu = sb(es2, "hu", [128, 512], F32)
            hidT = sb(es2, "hidT", [128, 512], BF16)
            cbias = sb(es2, "cbias", [128, 1], F32)
            cpd = Dep()
            for which in range(2):
                S.dma(kraw[:], fmS[6 + which], R=[fmS_dep[6 + which]], W=[cpd])
                S.dma(w1f[:], (w1k_d, w1v_d)[which], W=[cpd])
                S.dma(pef[:], (pek_d, pev_d)[which], W=[cpd])
                if which == 0:
                    S.dma(w2f[:, :], w2k_d, W=[cpd])
                else:
                    S.dma(w2f[:, 0:64], w2v_d, W=[cpd])
                S.op("dve", lambda e: e.tensor_copy(out=w1b[:], in_=w1f[:]), [cpd], [cpd])
                S.op("dve", lambda e: e.tensor_copy(out=peb[:], in_=pef[:]), [cpd], [cpd])
                S.op("dve", lambda e: e.tensor_copy(out=w2b[:], in_=w2f[:]), [cpd], [cpd])
                for g in range(2):
                    r0, r1 = g * 64, g * 64 + 64
                    for l in range(32):
                        mm(ps[0][:, 0:511], w1b[r0:r1, l, :], kraw[r0:r1, l:l + 16 * 510 + 1:16], l == 0, l == 31, [cpd], [psd[0]])
                    for l in range(32):
                        mm(ps[1][:, 0:2], w1b[r0:r1, l, :], peb[r0:r1, l, :], l == 0, l == 31, [cpd], [psd[1]])
                    S.op("dve", lambda e: e.tensor_copy(out=cbias[:], in_=ps[1][:, 0:1]), [psd[1]], [cpd])
                    S.op("dve", lambda e: e.memset(hx[:], 0.0), [], [cpd])
                    S.op("dve", lambda e: e.tensor_scalar(out=hx[:, 0:511], in0=ps[0][:, 0:511], scalar1=cbias[:, 0:1],
                                                          scalar2=None, op0=ALU.add), [psd[0], cpd], [cpd])
                    S.op("dve", lambda e: e.tensor_tensor(out=hu[:], in0=hx[:], in1=hx[:], op=ALU.mult), [cpd], [cpd])
                    S.op("dve", lambda e: e.tensor_scalar(out=hu[:], in0=hu[:], scalar1=0.044715, scalar2=1.0,
                                                          op0=ALU.mult, op1=ALU.add), [cpd], [cpd])
                    S.op("dve", lambda e: e.tensor_tensor(out=hu[:], in0=hu[:], in1=hx[:], op=ALU.mult), [cpd], [cpd])
                    S.op("act", lambda e: e.activation(out=hu[:], in_=hu[:], func=AF.Exp, scale=-1.5957691216057308), [cpd], [cpd])
                    S.op("dve", lambda e: e.tensor_scalar(out=hu[:], in0=hu[:], scalar1=1.0, scalar2=None, op0=ALU.add), [cpd], [cpd])
                    S.op("dve", lambda e: e.reciprocal(out=hu[:], in_=hu[:]), [cpd], [cpd])
                    S.op("dve", lambda e: e.tensor_tensor(out=hidT[:], in0=hu[:], in1=hx[:], op=ALU.mult), [cpd], [cpd])
                    if which == 0:
                        mm(ps[2][:, 0:512], w2b[:, :], hidT[:], True, True, [cpd], [psd[2]])
                        S.op("dve", lambda e: e.tensor_copy(out=kcT[r0:r1, :], in_=ps[2][r0:r1, 0:512]), [psd[2]], [kc_dep])
                    else:
                        for m in range(4):
                            mm(ps[2][:, m * 64:(m + 1) * 64], hidT[:, m * 128:(m + 1) * 128], w2b[:, 0:64], True, True, [cpd], [psd[2]])
                        S.op("dve", lambda e: e.tensor_copy(out=vc[:, :, g * 65:g * 65 + 64],
                                                            in_=ps[2][:, 0:256].rearrange("p (m e) -> p m e", e=64)), [psd[2]], [kc_dep])
            S.barrier()

        S.op("dve", lambda e: e.tensor_scalar(out=wab[:], in0=ab[:], scalar1=0.0, scalar2=None, op0=ALU.min), [nd], [wab_dep])
        S.op("act", lambda e: e.activation(out=wab[:], in_=wab[:], func=AF.Exp), [wab_dep], [wab_dep])
        qN = [sb(es, f"qN{i}", [128, 8, 128], BF16) for i in range(2)]
        qN_dep = [Dep() for _ in range(2)]
        eC = [sb(es, f"eC{i}", [128, 4, 128], BF16) for i in range(4)]
        eC_dep = [Dep() for _ in range(4)]
        Ocs = sb(es, "Ocs", [128, 4, 64], F32)
        Ocs_dep = Dep()
        rzc = sb(es, "rzc", [128, 4], F32)
        impacc = sb(es, "impacc", [128, 128], F32)
        score = sb(es, "score", [128, 128], F32)
        sc2 = sb(es, "sc2", [128, 128], F32)
        mx8 = sb(es, "mx8", [128, 8], F32)
        mx8b = sb(es, "mx8b", [128, 8], F32)
        MnegB = sb(es, "MnegB", [128, 128], BF16)
        MnegT = sb(es, "MnegT", [128, 128], BF16)
        MnegT_dep = Dep()
        tk = Dep()
        sgate = sb(es, "sgate", [128, 24], F32)
        sg_dep = Dep()
        coef = sb(es, "coef", [128, 4], F32)
        t1 = sb(es, "t1", [128, 64], F32)
        fin = Dep()
        mon = [sb(es, f"mon{i}", [128, 512], BF16) for i in range(2)]
        mon_dep = [Dep() for _ in range(2)]
        qS_p = qS.rearrange("i h p t -> i p h t")
        sctr = 0

        def nsa_prep(nidx):
            r_ = nidx % 4
            g_ = (nidx // 4) % 2
            i_ = nidx // 8
            h_ = 4 * g_ + r_
            vb_ = nidx % 2
            nk_ = 4 * i_ + 4
            k0_ = max(0, 4 * i_ - 4)
            e1, e2 = ("dve", "pool") if nidx % 2 == 0 else ("pool", "dve")
            S.op(e1, lambda e: e.tensor_tensor(out=Vsp[vb_][:, 0:nk_, :], in0=Vs[:, 0:nk_, g_ * 65:(g_ + 1) * 65],
                                               in1=wab[:, h_, 60 - 4 * i_:64].unsqueeze(2).to_broadcast([128, nk_, 65]), op=ALU.mult),
                 [nd, wab_dep], [Vxp_dep[vb_]])
            S.op(e2, lambda e: e.tensor_tensor(out=Vwp[vb_][:, 0:nk_ - k0_, :], in0=Vw[:, k0_:nk_, g_ * 65:(g_ + 1) * 65],
                                               in1=wab[:, h_, 60 - 4 * i_ + k0_:64].unsqueeze(2).to_broadcast([128, nk_ - k0_, 65]), op=ALU.mult),
                 [nd, wab_dep], [Vxp_dep[vb_]])
        for i in range(16):
            qb = i % 2
            S.dma(qN[qb][:], qS_p[i][:, 0:8, :], R=[qS_dep], W=[qN_dep[qb]])
            S.op("act", lambda e: e.activation(out=sgate[:], in_=gown[:, i, :], func=AF.Exp, scale=-1.0), [gown_dep], [sg_dep])
            S.op("dve", lambda e: e.tensor_scalar(out=sgate[:], in0=sgate[:], scalar1=1.0, scalar2=None, op0=ALU.add), [sg_dep], [sg_dep])
            S.op("dve", lambda e: e.reciprocal(out=sgate[:], in_=sgate[:]), [sg_dep], [sg_dep])
            ncm = i // 4 + 1
            nk = 4 * i + 4
            for g in range(2):
                for r in range(4):
                    h = 4 * g + r
                    for m in range(ncm):
                        d = 4 * m - i
                        partial = d >= -4
                        sbk = sctr % 4
                        sctr += 1
                        mm(ps[sbk][:, 0:128], kcT[:, m * 128:(m + 1) * 128], qN[qb][:, h, :], True, not partial,
                           [kc_dep, qN_dep[qb]], [psd[sbk]])
                        if partial:
                            mm(ps[sbk][:, 0:128], identb[:], cmask[:, d + 4, :], False, True, [cst, nd], [psd[sbk]])
                        S.op("act", lambda e: e.activation(out=eC[r][:, m, :], in_=ps[sbk][:, 0:128], func=AF.Exp,
                                                           bias=cab[:, h, d + 15:d + 16], scale=1.0), [psd[sbk], nd], [eC_dep[r]])
                    for m in range(ncm):
                        mm(ps[4][:, 0:65], eC[r][:, m, :], vc[:, m, g * 65:(g + 1) * 65], m == 0, m == ncm - 1,
                           [eC_dep[r], kc_dep], [psd[4]])
                    for m in range(ncm):
                        mm(ps[5][:, 0:128], eC[r][:, m, :], ovl[:, m, :], m == 0, m == ncm - 1, [eC_dep[r], nd], [psd[5]])
                    S.op("dve", lambda e: e.tensor_scalar_max(out=rzc[:, r:r + 1], in0=ps[4][:, 64:65], scalar1=1e-30), [psd[4]], [tk, fin])
                    S.op("dve", lambda e: e.reciprocal(out=rzc[:, r:r + 1], in_=rzc[:, r:r + 1]), [tk], [tk])
                    S.op("dve", lambda e: e.tensor_copy(out=Ocs[:, r, :], in_=ps[4][:, 0:64]), [psd[4]], [Ocs_dep, fin])
                    if r == 0:
                        S.op("dve", lambda e: e.tensor_scalar(out=impacc[:], in0=ps[5][:, 0:128], scalar1=rzc[:, r:r + 1],
                                                              scalar2=None, op0=ALU.mult), [psd[5], tk], [tk])
                    else:
                        S.op("dve", lambda e: e.scalar_tensor_tensor(out=impacc[:], in0=ps[5][:, 0:128], scalar=rzc[:, r:r + 1],
                                                                     in1=impacc[:], op0=ALU.mult, op1=ALU.add), [psd[5], tk], [tk])
                S.op("dve", lambda e: e.tensor_tensor(out=score[:], in0=impacc[:], in1=selA[:, i, :], op=ALU.mult), [tk, nd], [tk])
                S.op("dve", lambda e: e.tensor_tensor(out=score[:], in0=score[:], in1=selB[:, i, :], op=ALU.add), [tk, nd], [tk])
                S.op("dve", lambda e: e.max(out=mx8[:], in_=score[:]), [tk], [tk])
                S.op("dve", lambda e: e.match_replace(out=sc2[:], in_to_replace=mx8[:], in_values=score[:], imm_value=-2.0), [tk], [tk])
                S.op("dve", lambda e: e.max(out=mx8b[:], in_=sc2[:]), [tk], [tk])
                S.op("dve", lambda e: e.tensor_scalar(out=MnegB[:], in0=score[:], scalar1=mx8b[:, 7:8], scalar2=NEGM,
                                                      op0=ALU.is_lt, op1=ALU.mult), [tk], [tk])
                S.op("pe", lambda e: e.transpose(out=psb[:, 0:128], in_=MnegB[:], identity=identb[:]), [tk, cst], [psb_dep])
                S.op("dve", lambda e: e.tensor_copy(out=MnegT[:], in_=psb[:, 0:128]), [psb_dep], [MnegT_dep])
                for r in range(4):
                    h = 4 * g + r
                    osb = 6 if r % 2 == 0 else 5
                    nidx = (i * 2 + g) * 4 + r
                    vb = nidx % 2
                    if nidx == 0:
                        nsa_prep(0)
                    if nidx + 1 < 128:
                        nsa_prep(nidx + 1)
                    k0 = max(0, 4 * i - 4)
                    steps = []
                    for gq in range(nk // 4):
                        bank = pctr[0] % 4
                        pctr[0] += 1

                        def qk(gq=gq, bank=bank, i=i, h=h, qb=qb):
                            for q in range(4):
                                k = 4 * gq + q
                                diag = k >= 4 * i
                                o = ps[bank][:, q * 128:(q + 1) * 128]
                                mm(o, KsT[:, k * 128:(k + 1) * 128], qN[qb][:, h, :], True, False, [nd, qN_dep[qb]], [psd[bank]])
                                mm(o, Rx[:, k * 128:(k + 1) * 128], MnegT[:], False, not diag, [nd, MnegT_dep], [psd[bank]])
                                if diag:
                                    mm(o, identb[:], diagm[:, k - 4 * i, :], False, True, [cst], [psd[bank]])
                            S.op("act", lambda e: e.activation(out=pT[bank][:], in_=ps[bank][:, :], func=AF.Exp), [psd[bank]], [pT_dep[bank]])

                        def pv(gq=gq, bank=bank, osb=osb, nk=nk, vb=vb):
                            for q in range(4):
                                k = 4 * gq + q
                                mm(ps[osb][:, 0:65], pT[bank][:, q * 128:(q + 1) * 128], Vsp[vb][:, k, :], k == 0, k == nk - 1,
                                   [pT_dep[bank], Vxp_dep[vb]], [psd[osb]])
                        steps.append((qk, pv))
                    for gq in range((nk - k0) // 4):
                        bank = pctr[0] % 4
                        pctr[0] += 1

                        def qk(gq=gq, bank=bank, i=i, h=h, qb=qb, k0=k0):
                            for q in range(4):
                                k = k0 + 4 * gq + q
                                wk = k - (4 * i - 4)
                                o = ps[bank][:, q * 128:(q + 1) * 128]
                                mm(o, KwT[:, k * 128:(k + 1) * 128], qN[qb][:, h, :], True, False, [nd, qN_dep[qb]], [psd[bank]])
                                mm(o, identb[:], winm[:, wk, :], False, True, [cst, nd], [psd[bank]])
                            S.op("act", lambda e: e.activation(out=pT[bank][:], in_=ps[bank][:, :], func=AF.Exp), [psd[bank]], [pT_dep[bank]])

                        def pv(gq=gq, bank=bank, k0=k0, nk=nk, vb=vb):
                            for q in range(4):
                                k = k0 + 4 * gq + q
                                mm(ps[4][:, 0:65], pT[bank][:, q * 128:(q + 1) * 128], Vwp[vb][:, k - k0, :], k == k0, k == nk - 1,
                                   [pT_dep[bank], Vxp_dep[vb]], [psd[4]])
                        steps.append((qk, pv))
                    run_pipe(steps)
                    S.op("dve", lambda e: e.tensor_scalar_max(out=coef[:, 1:2], in0=ps[osb][:, 64:65], scalar1=1e-30), [psd[osb]], [fin])
                    S.op("dve", lambda e: e.tensor_scalar_max(out=coef[:, 2:3], in0=ps[4][:, 64:65], scalar1=1e-30), [psd[4]], [fin])
                    S.op("dve", lambda e: e.reciprocal(out=coef[:, 1:3], in_=coef[:, 1:3]), [fin], [fin])
                    S.op("dve", lambda e: e.tensor_copy(out=coef[:, 0:1], in_=rzc[:, r:r + 1]), [fin, tk], [fin])
                    S.op("dve", lambda e: e.tensor_tensor(out=coef[:, 0:3], in0=coef[:, 0:3], in1=sgate[:, 3 * h:3 * h + 3], op=ALU.mult), [fin, sg_dep], [fin])
                    S.op("dve", lambda e: e.tensor_scalar(out=t1[:], in0=Ocs[:, r, :], scalar1=coef[:, 0:1], scalar2=None, op0=ALU.mult), [fin, Ocs_dep], [fin])
                    S.op("dve", lambda e: e.scalar_tensor_tensor(out=t1[:], in0=ps[osb][:, 0:64], scalar=coef[:, 1:2], in1=t1[:],
                                                                 op0=ALU.mult, op1=ALU.add), [fin, psd[osb]], [fin])
                    S.op("dve", lambda e: e.scalar_tensor_tensor(out=mon[qb][:, h * 64:(h + 1) * 64], in0=ps[4][:, 0:64], scalar=coef[:, 2:3],
                                                                 in1=t1[:], op0=ALU.mult, op1=ALU.add), [fin, psd[4]], [mon_dep[qb], fin])
            S.dma(mixS[i * 128:(i + 1) * 128, 0:512], mon[qb][:], R=[mon_dep[qb]], W=[mixS_dep])
        S.barrier()

    if stage <= 3:
        es_attn.close()
        es_all.close()
        return nc, S

    S.mute = False
    es_attn.close()
    es_tail = ExitStack()
    hres = sb(es_tail, "hres", [128, 16, D], F32)
    hres_dep = [Dep() for _ in range(16)]
    xnT = sb(es_tail, "xntok", [128, 16, D], BF16)
    xnT_dep = Dep()
    gate = sb(es_tail, "gate", [128, 16, NE], F32)
    gate_dep = Dep()
    ssc = sb(es_tail, "ssc", [128, 4], F32)
    nrm = Dep()
    sqt_box = [None]

    def rms_rstd(src, n, col, R):
        sqt = sqt_box[0]
        S.op("dve", lambda e: e.tensor_tensor(out=sqt[:, 0:n], in0=src, in1=src, op=ALU.mult), list(R) + [nrm], [nrm])
        S.op("dve", lambda e: e.reduce_sum(out=ssc[:, col:col + 1], in_=sqt[:, 0:n], axis=AX.X), [nrm], [nrm])
        S.op("act", lambda e: e.activation(out=ssc[:, col:col + 1], in_=ssc[:, col:col + 1], func=AF.Sqrt,
                                           bias=epsc[:, 0:1], scale=1.0 / n), [nrm, cst], [nrm])
        S.op("dve", lambda e: e.reciprocal(out=ssc[:, col:col + 1], in_=ssc[:, col:col + 1]), [nrm], [nrm])

    def load_w_bf16(dst, src, nchunk, ncol, stg, stg_dep, wdep, ctr):
        for dc in range(nchunk):
            k = ctr[0] % len(stg)
            ctr[0] += 1
            S.dma(stg[k][:, 0:ncol], src[dc * 128:(dc + 1) * 128, :], W=[stg_dep[k]])
            copy_on(cast_eng(), dst[:, dc, :], stg[k][:, 0:ncol], [stg_dep[k]], [wdep])

    with ExitStack() as es:
        sqt_box[0] = sb(es, "sqt4", [128, D], F32)
        woutb = sb(es, "woutb", [128, 8, D], BF16)
        stg = [sb(es, f"stg4{i}", [128, D], F32) for i in range(2)]
        stg_dep = [Dep() for _ in range(2)]
        gnb = sb(es, "gnb", [128, D], F32)
        ln2b = sb(es, "ln2b", [128, D], F32)
        wrf = sb(es, "wrf", [128, 8, NE], F32)
        brb = sb(es, "brb", [128, NE], F32)
        bdnf = sb(es, "bdnf", [NE, D], F32)
        mixb = [sb(es, f"mixb{i}", [128, D], BF16) for i in range(2)]
        mixb_dep = [Dep() for _ in range(2)]
        mixn = sb(es, "mixn", [128, D], BF16)
        mixT = sb(es, "mixT", [128, 8, 128], BF16)
        xn = sb(es, "xn", [128, D], F32)
        xnTf = sb(es, "xnTf", [128, 8, 128], F32)
        lg = sb(es, "lg", [128, NE], F32)
        ex = sb(es, "ex", [128, NE], F32)
        mxr = sb(es, "mxr", [128, 8], F32)
        gT = sb(es, "gT", [NE, 128], F32)
        wd = Dep()
        p4 = Dep()
        ctr = [0]
        load_w_bf16(woutb, wout_d, 8, D, stg, stg_dep, wd, ctr)
        S.dma(gnb[:], gn_d, W=[wd])
        S.dma(ln2b[:], ln2_d, W=[wd])
        S.dma(wrf[:], wr_d.rearrange("(c p) e -> p c e", p=128), W=[wd])
        S.dma(brb[:], br_d, W=[wd])
        S.dma(bdnf[:], bdn_d, W=[wd])
        def g4(n):
            if p4stop <= n:
                S.mute = True
        for i in range(nblk4):
            mb = i % 2
            S.mute = False
            S.dma(mixb[mb][:], mixS[i * 128:(i + 1) * 128, :], R=[mixS_dep], W=[mixb_dep[mb]])
            S.dma(hres[:, i, :], xo_d[i * 128:(i + 1) * 128, :], W=[hres_dep[i]])
            g4(1)
            for half in range(2):
                hs = slice(half * 512, (half + 1) * 512)
                rms_rstd(mixb[mb][:, hs], 512, half, [mixb_dep[mb]])
                S.op("dve", lambda e: e.scalar_tensor_tensor(out=mixn[:, hs], in0=mixb[mb][:, hs], scalar=ssc[:, half:half + 1],
                                                             in1=gnb[:, hs], op0=ALU.mult, op1=ALU.mult), [mixb_dep[mb], nrm, wd], [p4])
            g4(2)
            for c in range(8):
                S.op("pe", lambda e: e.transpose(out=psb[:, c * 128:(c + 1) * 128], in_=mixn[:, c * 128:(c + 1) * 128],
                                                 identity=identb[:]), [p4, cst], [psb_dep])
            S.op("act", lambda e: e.copy(out=mixT[:].rearrange("p c t -> p (c t)"), in_=psb[:, :]), [psb_dep], [p4])
            g4(3)
            for half in range(2):
                hs = slice(half * 512, (half + 1) * 512)
                for c in range(8):
                    mm(ps[half][:, :], mixT[:, c, :], woutb[:, c, hs], c == 0, c == 7, [p4, wd], [psd[half]])
                S.op("dve", lambda e: e.tensor_tensor(out=hres[:, i, hs], in0=ps[half][:, :], in1=hres[:, i, hs], op=ALU.add),
                     [psd[half], hres_dep[i]], [hres_dep[i]])
            rms_rstd(hres[:, i, :], D, 2, [hres_dep[i]])
            g4(4)
            S.op("dve", lambda e: e.scalar_tensor_tensor(out=xn[:], in0=hres[:, i, :], scalar=ssc[:, 2:3], in1=ln2b[:],
                                                         op0=ALU.mult, op1=ALU.mult), [hres_dep[i], nrm, wd], [p4])
            S.op("pool", lambda e: e.tensor_copy(out=xnT[:, i, :], in_=xn[:]), [p4], [xnT_dep])
            for c in range(8):
                b = 2 + c // 4
                g4(5)
                S.op("pe", lambda e: e.transpose(out=ps[b][:, (c % 4) * 128:(c % 4 + 1) * 128], in_=xn[:, c * 128:(c + 1) * 128],
                                                 identity=identf[:]), [p4, cst], [psd[b]])
            for b2 in range(2):
                S.op("act", lambda e: e.copy(out=xnTf[:, b2 * 4:(b2 + 1) * 4, :], in_=ps[2 + b2][:, :].rearrange("p (c t) -> p c t", t=128)),
                     [psd[2 + b2]], [p4])
            for c in range(8):
                mm(ps[4][:, 0:NE], xnTf[:, c, :], wrf[:, c, :], c == 0, c == 7, [p4, wd], [psd[4]])
            g4(7)
            S.op("dve", lambda e: e.tensor_tensor(out=lg[:], in0=ps[4][:, 0:NE], in1=brb[:], op=ALU.add), [psd[4], wd], [p4])
            S.op("dve", lambda e: e.max(out=mxr[:], in_=lg[:]), [p4], [p4])
            S.op("dve", lambda e: e.tensor_scalar(out=mxr[:, 4:5], in0=mxr[:, 0:1], scalar1=-1.0, scalar2=None, op0=ALU.mult), [p4], [p4])
            S.op("act", lambda e: e.activation(out=ex[:], in_=lg[:], func=AF.Exp, bias=mxr[:, 4:5], scale=1.0), [p4], [p4])
            S.op("dve", lambda e: e.scalar_tensor_tensor(out=ex[:], in0=lg[:], scalar=mxr[:, 3:4], in1=ex[:],
                                                         op0=ALU.is_ge, op1=ALU.mult), [p4], [p4])
            S.op("dve", lambda e: e.reduce_sum(out=mxr[:, 5:6], in_=ex[:], axis=AX.X), [p4], [p4])
            S.op("dve", lambda e: e.reciprocal(out=mxr[:, 5:6], in_=mxr[:, 5:6]), [p4], [p4])
            S.op("dve", lambda e: e.tensor_scalar(out=gate[:, i, :], in0=ex[:], scalar1=mxr[:, 5:6], scalar2=None, op0=ALU.mult),
                 [p4], [gate_dep])
            g4(8)
        S.mute = False
        S.barrier()

    if stage == 4:
        dbg_h = nc.dram_tensor("dbg_h", [128, 16, D], F32, kind="ExternalOutput").ap()
        dbg_g = nc.dram_tensor("dbg_g", [128, 16, NE], F32, kind="ExternalOutput").ap()
        dbg_x = nc.dram_tensor("dbg_x", [128, 16, D], BF16, kind="ExternalOutput").ap()
        S.dma(dbg_h, hres[:])
        S.dma(dbg_g, gate[:])
        S.dma(dbg_x, xnT[:])
        S.barrier()
        es_tail.close()
        es_all.close()
        return nc, S

    S.mute = skip5
    with ExitStack() as es:
        C = CAP
        NSC = C // 128
        wupb = sb(es, "wupb", [128, 8, 2048], BF16)
        wdnb = sb(es, "wdnb", [128, 8, D], BF16)
        wup_dep = Dep()
        wdn_dep = Dep()
        NSTG5 = 4
        stg = [sb(es, f"stg5{i}", [128, 512], F32) for i in range(NSTG5)]
        stg_dep = [Dep() for _ in range(NSTG5)]
        bupc = sb(es, "bupc", [128, NE, 16], F32)
        browb = sb(es, "browb", [1, D], BF16)
        brow_dep = Dep()
        bd = Dep()
        S.dma(bupc[:], bup_d, W=[bd])
        iotac = sb(es, "iotac", [128, C], F32)
        S.dma(iotac[:], iota_d, W=[bd])
        Mf = sb(es, "Mf", [128, 16, NE], F32)
        pos = sb(es, "pos", [128, 16, NE], F32)
        dsp = Dep()
        with ExitStack() as est:
            trisb = sb(est, "trisb", [128, 128], BF16)
            Mb = sb(est, "Mb", [128, 16, NE], BF16)
            tot5 = sb(est, "tot5", [128, 16, NE], F32)
            pre5 = sb(est, "pre5", [128, 16, NE], F32)
            S.op("dve", lambda g: g.tensor_tensor(out=trisb[:], in0=trif[:], in1=identf[:], op=ALU.subtract), [cst], [dsp])
            S.op("dve", lambda g: g.tensor_scalar(out=Mf[:], in0=gate[:], scalar1=0.0, scalar2=None, op0=ALU.is_gt), [gate_dep], [dsp])
            S.op("dve", lambda g: g.tensor_copy(out=Mb[:], in_=Mf[:]), [dsp], [dsp])
            mflat = Mb[:].rearrange("p a b -> p (a b)")
            mm(ps[0][:, :], trisb[:], mflat, True, True, [dsp], [psd[0]])
            mm(ps[1][:, :], onesb[:], mflat, True, True, [dsp, cst], [psd[1]])
            S.op("dve", lambda g: g.tensor_copy(out=tot5[:].rearrange("p a b -> p (a b)"), in_=ps[1][:, :]), [psd[1]], [dsp])
            S.op("dve", lambda g: g.memset(pre5[:, 0, :], 0.0), [], [dsp])
            for k in range(1, 16):
                S.op("dve", lambda g: g.tensor_tensor(out=pre5[:, k, :], in0=pre5[:, k - 1, :], in1=tot5[:, k - 1, :], op=ALU.add), [dsp], [dsp])
            S.op("dve", lambda g: g.tensor_tensor(out=pos[:].rearrange("p a b -> p (a b)"), in0=ps[0][:, :],
                                                  in1=pre5[:].rearrange("p a b -> p (a b)"), op=ALU.add), [psd[0], dsp], [dsp])
            S.barrier()

        Sel = sb(es, "Sel", [128, 16, C], BF16)
        Sel_dep = Dep()
        SelTb = [sb(es, f"SelTb{i}", [128, NSC, 128], BF16) for i in range(2)]
        SelTb_dep = [Dep() for _ in range(2)]
        XeT = sb(es, "XeT", [128, 8, C], BF16)
        XeT_dep = Dep()
        actT = sb(es, "actT5", [128, 8, C], BF16)
        actT_dep = Dep()
        assert NSC * D == 8 * C
        ye = XeT[:].rearrange("p (s two) c -> p s (two c)", two=2)
        ye_dep = XeT_dep
        gcs = [sb(es, f"gcs{i}", [128, C], F32) for i in range(1)] * 2
        sgs = [sb(es, f"sgs{i}", [128, C], F32) for i in range(1)] * 2
        lcs = [sb(es, f"lcs{i}", [128, C], F32) for i in range(1)] * 2
        gc_dep = [Dep()] * 2
        sg_dep5 = [Dep()] * 2
        lc_dep = [Dep()] * 2
        sctr = [0]

        def load_up(e):
            for dc in range(8):
                for half in range(4):
                    k = sctr[0] % NSTG5
                    sctr[0] += 1
                    S.dma(stg[k][:], wup_d[e, dc * 128:(dc + 1) * 128, half * 512:(half + 1) * 512], W=[stg_dep[k]])
                    copy_on("pool", wupb[:, dc, half * 512:(half + 1) * 512], stg[k][:], [stg_dep[k]], [wup_dep])

        def load_dn(e):
            for fc in range(8):
                for half in range(2):
                    k = sctr[0] % NSTG5
                    sctr[0] += 1
                    S.dma(stg[k][:], wdn_d[e, fc * 128:(fc + 1) * 128, half * 512:(half + 1) * 512], W=[stg_dep[k]])
                    copy_on("pool", wdnb[:, fc, half * 512:(half + 1) * 512], stg[k][:], [stg_dep[k]], [wdn_dep])

        load_up(0)
        load_dn(0)
        uc = 0
        dcn = 0
        tcn = 0
        for e_ in range(n_experts):
            for half in range(2):
                kk = sctr[0] % NSTG5
                sctr[0] += 1
                S.dma(stg[kk][0:1, :], bdn_d[e_:e_ + 1, half * 512:(half + 1) * 512], W=[stg_dep[kk]])
                S.op("act", lambda g: g.copy(out=browb[0:1, half * 512:(half + 1) * 512], in_=stg[kk][0:1, :]), [stg_dep[kk]], [brow_dep])
            for blk in range(16):
                S.op("dve", lambda g: g.tensor_scalar(out=Sel[:, blk, :], in0=iotac[:], scalar1=pos[:, blk, e_:e_ + 1],
                                                      scalar2=Mf[:, blk, e_:e_ + 1], op0=ALU.is_equal, op1=ALU.mult), [dsp, bd], [Sel_dep])
            for dc in range(8):
                bX = 4 + dcn % 3
                dcn += 1
                for blk in range(16):
                    mm(ps[bX][:, 0:C], xnT[:, blk, dc * 128:(dc + 1) * 128], Sel[:, blk, :], blk == 0, blk == 15,
                       [xnT_dep, Sel_dep], [psd[bX]])
                S.op("act", lambda g: g.copy(out=XeT[:, dc, :], in_=ps[bX][:, 0:C]), [psd[bX]], [XeT_dep])
            for fc in range(8):
                bG = (uc % 2) * 2
                bL = bG + 1
                tb = uc % 2
                uc += 1
                for dc in range(8):
                    mm(ps[bG][:, 0:C], wupb[:, dc, fc * 128:(fc + 1) * 128], XeT[:, dc, :], dc == 0, dc == 7, [wup_dep, XeT_dep], [psd[bG]])
                for dc in range(8):
                    mm(ps[bL][:, 0:C], wupb[:, dc, 1024 + fc * 128:1024 + (fc + 1) * 128], XeT[:, dc, :], dc == 0, dc == 7,
                       [wup_dep, XeT_dep], [psd[bL]])
                S.op("dve", lambda g: g.tensor_scalar(out=gcs[tb][:], in0=ps[bG][:, 0:C], scalar1=bupc[:, e_, fc:fc + 1], scalar2=7.0,
                                                      op0=ALU.add, op1=ALU.min), [psd[bG], bd], [gc_dep[tb]])
                S.op("act", lambda g: g.activation(out=sgs[tb][:], in_=gcs[tb][:], func=AF.Sigmoid, scale=1.702), [gc_dep[tb]], [sg_dep5[tb]])
                S.op("dve", lambda g: g.tensor_scalar(out=lcs[tb][:], in0=ps[bL][:, 0:C], scalar1=bupc[:, e_, 8 + fc:9 + fc], scalar2=7.0,
                                                      op0=ALU.add, op1=ALU.min), [psd[bL], bd], [lc_dep[tb]])
                S.op("dve", lambda g: g.tensor_scalar(out=lcs[tb][:], in0=lcs[tb][:], scalar1=-7.0, scalar2=1.0,
                                                      op0=ALU.max, op1=ALU.add), [lc_dep[tb]], [lc_dep[tb]])
                S.op("pool", lambda g: g.tensor_tensor(out=gcs[tb][:], in0=gcs[tb][:], in1=sgs[tb][:], op=ALU.mult), [sg_dep5[tb]], [gc_dep[tb]])
                S.op("pool", lambda g: g.tensor_tensor(out=actT[:, fc, :], in0=gcs[tb][:], in1=lcs[tb][:], op=ALU.mult),
                     [gc_dep[tb], lc_dep[tb]], [actT_dep])
            if e_ + 1 < n_experts:
                load_up(e_ + 1)
            for sc in range(NSC):
                for half in range(2):
                    hs = slice(half * 512, (half + 1) * 512)
                    bD = 4 + dcn % 3
                    dcn += 1
                    for fc in range(8):
                        mm(ps[bD][:, :], actT[:, fc, sc * 128:(sc + 1) * 128], wdnb[:, fc, hs], fc == 0, False,
                           [actT_dep, wdn_dep], [psd[bD]])
                    mm(ps[bD][:, :], onesb[0:1, :], browb[0:1, hs], False, True, [brow_dep, cst], [psd[bD]])
                    S.op("act", lambda g: g.copy(out=ye[:, sc, hs], in_=ps[bD][:, :]), [psd[bD]], [ye_dep])
            if e_ + 1 < n_experts:
                load_dn(e_ + 1)
            for blk in range(16):
                tbf = tcn % 2
                tcn += 1
                for sc in range(NSC):
                    S.op("pe", lambda g: g.transpose(out=psb[:, sc * 128:(sc + 1) * 128], in_=Sel[:, blk, sc * 128:(sc + 1) * 128],
                                                     identity=identb[:]), [Sel_dep, cst], [psb_dep])
                S.op("act", lambda g: g.copy(out=SelTb[tbf][:].rearrange("p c t -> p (c t)"), in_=psb[:, 0:NSC * 128]), [psb_dep], [SelTb_dep[tbf]])
                for half in range(2):
                    hs = slice(half * 512, (half + 1) * 512)
                    bY = 4 + dcn % 3
                    dcn += 1
                    for sc in range(NSC):
                        mm(ps[bY][:, :], SelTb[tbf][:, sc, :], ye[:, sc, hs], sc == 0, sc == NSC - 1, [SelTb_dep[tbf], ye_dep], [psd[bY]])
                    S.op("dve", lambda g: g.scalar_tensor_tensor(out=hres[:, blk, hs], in0=ps[bY][:, :], scalar=gate[:, blk, e_:e_ + 1],
                                                                 in1=hres[:, blk, hs], op0=ALU.mult, op1=ALU.add),
                         [psd[bY], gate_dep, hres_dep[blk]], [hres_dep[blk]])
        S.barrier()

    S.mute = skip6
    with ExitStack() as es:
        sqt_box[0] = sb(es, "sqt6", [128, D], F32)
        wpgb = sb(es, "wpgb", [128, 8, D], BF16)
        wpleb = sb(es, "wpleb", [128, 2, D], BF16)
        pTb = sb(es, "pTb", [128, 2, 2048], BF16)
        stg = [sb(es, f"stg6{i}", [128, 2048], F32) for i in range(2)]
        stg_dep = [Dep() for _ in range(2)]
        lnpb = sb(es, "lnpb", [128, D], F32)
        lnfb = sb(es, "lnfb", [128, D], F32)
        hn = sb(es, "hn", [128, D], BF16)
        hnT = sb(es, "hnT", [128, 8, 128], BF16)
        sig = [sb(es, f"sig{i}", [128, 512], F32) for i in range(2)]
        outt = [sb(es, f"outt{i}", [128, D], F32) for i in range(2)]
        outt_dep = [Dep() for _ in range(2)]
        wd = Dep()
        p6 = Dep()
        out_dep = Dep()
        ctr = [0]
        load_w_bf16(wpgb, wpg_d, 8, D, stg, stg_dep, wd, ctr)
        load_w_bf16(wpleb, wple_d, 2, D, stg, stg_dep, wd, ctr)
        for c2 in range(2):
            k = ctr[0] % 2
            ctr[0] += 1
            S.dma(stg[k][:], pTo_d[c2], W=[stg_dep[k]])
            copy_on(cast_eng(), pTb[:, c2, :], stg[k][:], [stg_dep[k]], [wd])
        S.dma(lnpb[:], lnp_d, W=[wd])
        S.dma(lnfb[:], lnf_d, W=[wd])
        for i in range(16):
            ob = i % 2
            rms_rstd(hres[:, i, :], D, 0, [hres_dep[i]])
            S.op("dve", lambda e: e.scalar_tensor_tensor(out=hn[:], in0=hres[:, i, :], scalar=ssc[:, 0:1], in1=lnpb[:],
                                                         op0=ALU.mult, op1=ALU.mult), [hres_dep[i], nrm, wd], [p6])
            for c in range(8):
                S.op("pe", lambda e: e.transpose(out=psb[:, c * 128:(c + 1) * 128], in_=hn[:, c * 128:(c + 1) * 128],
                                                 identity=identb[:]), [p6, cst], [psb_dep])
            S.op("act", lambda e: e.copy(out=hnT[:].rearrange("p c t -> p (c t)"), in_=psb[:, :]), [psb_dep], [p6])
            for half in range(2):
                hs = slice(half * 512, (half + 1) * 512)
                for c in range(8):
                    mm(ps[half][:, :], hnT[:, c, :], wpgb[:, c, hs], c == 0, c == 7, [p6, wd], [psd[half]])
                for c2 in range(2):
                    mm(ps[2 + half][:, :], pTb[:, c2, i * 128:(i + 1) * 128], wpleb[:, c2, hs], c2 == 0, c2 == 1, [wd], [psd[2 + half]])
                S.op("act", lambda e: e.activation(out=sig[half][:], in_=ps[half][:, :], func=AF.Exp, scale=-1.0), [psd[half]], [p6])
                S.op("dve", lambda e: e.tensor_scalar(out=sig[half][:], in0=sig[half][:], scalar1=1.0, scalar2=None, op0=ALU.add), [p6], [p6])
                S.op("dve", lambda e: e.reciprocal(out=sig[half][:], in_=sig[half][:]), [p6], [p6])
                S.op("dve", lambda e: e.tensor_tensor(out=sig[half][:], in0=ps[2 + half][:, :], in1=sig[half][:], op=ALU.mult),
                     [psd[2 + half], p6], [p6])
                S.op("dve", lambda e: e.tensor_tensor(out=hres[:, i, hs], in0=sig[half][:], in1=hres[:, i, hs], op=ALU.add),
                     [p6, hres_dep[i], nrm], [hres_dep[i]])
            rms_rstd(hres[:, i, :], D, 1, [hres_dep[i]])
            S.op("dve", lambda e: e.scalar_tensor_tensor(out=outt[ob][:], in0=hres[:, i, :], scalar=ssc[:, 1:2], in1=lnfb[:],
                                                         op0=ALU.mult, op1=ALU.mult), [hres_dep[i], nrm, wd], [outt_dep[ob]])
            S.dma(out_d[i * 128:(i + 1) * 128, :], outt[ob][:], R=[outt_dep[ob]], W=[out_dep])
        S.barrier()
    es_tail.close()

    if stage <= 3:
        dbg_f = nc.dram_tensor("dbg_ff", [128, 64 * 8 + 16 * 24], F32, kind="ExternalOutput").ap()
        S.dma(dbg_f[:, 0:512], ffall[:].rearrange("p a b -> p (a b)"))
        S.dma(dbg_f[:, 512:896], gown[:].rearrange("p a b -> p (a b)"))
        S.barrier()
        es_all.close()
        return nc, S

    es_all.close()
    return nc, S


def own_tokens(j):
    return np.concatenate([np.arange(512 * i + 128 * j, 512 * i + 128 * j + 128) for i in range(16)])


def const_tables(j):
    p = np.arange(128)
    c = {}
    c["identb"] = _bf(np.eye(128, dtype=np.float32))
    c["identf"] = np.eye(128, dtype=np.float32)
    c["trif"] = (p[:, None] <= p[None, :]).astype(np.float32)
    sl = p[:, None]
    tl = p[None, :]
    dm = np.zeros((128, 4, 128), np.float32)
    for kk in range(4):
        dist = 128 * (j - kk) + tl - sl
        dm[:, kk, :] = np.where(dist >= 0, 0.0, NEGM)
    c["diagm"] = _bf(dm)
    wm = np.zeros((128, 8, 128), np.float32)
    for wk in range(8):
        dist = 128 * (j + 4 - wk) + tl - sl
        wm[:, wk, :] = np.where((dist >= 0) & (dist < 512), 0.0, NEGM)
    c["winm"] = _bf(wm)
    cm = np.zeros((128, 5, 128), np.float32)
    for dd in range(5):
        d = dd - 4
        cond = (512 * d + 16 * sl - tl - 128 * j + 31) <= 0
        cm[:, dd, :] = np.where(cond, 0.0, NEGM)
    c["cmask"] = _bf(cm)
    slopes = np.exp2(-8.0 * np.arange(1, 9, dtype=np.float32) / 8).astype(np.float32)
    rel = np.arange(64)
    ab = slopes[None, :, None] * (p[:, None, None] - 127 - 128 * (rel[None, None, :] + j - 3))
    c["ab"] = np.ascontiguousarray(ab[:, :, ::-1]).astype(np.float32)
    dd = np.arange(16) - 15
    cab = slopes[None, :, None] * (16 * p[:, None, None] + 512 * dd[None, None, :] - 128 * j - 96)
    c["cab"] = cab.astype(np.float32)
    n = np.arange(128)
    selA = np.zeros((128, 16, 128), np.float32)
    selB = np.zeros((128, 16, 128), np.float32)
    for i in range(16):
        cur = (512 * i + 128 * j + p) // 64
        valid = n[None, :] <= cur[:, None]
        forced = valid & ((n[None, :] == 0) | (n[None, :] == cur[:, None]) | (n[None, :] == cur[:, None] - 1))
        selA[:, i, :] = (valid & ~forced)
        selB[:, i, :] = np.where(forced, 1e9, np.where(valid, 0.0, -1.0))
    c["selA"] = _bf(selA)
    c["selB"] = _bf(selB)
    ws = np.zeros((128, 16, 64), np.float32)
    for i in range(16):
        ws[:, i, :] = (np.arange(64)[None, :] <= 4 * i + j)
    c["wsel"] = ws
    s = np.arange(T)
    c["Rexp"] = _bf((n[:, None] == (s[None, :] // 64)).astype(np.float32))
    cc = np.arange(512)
    ov = ((cc[:, None] * 16 < n[None, :] * 64 + 64) & (cc[:, None] * 16 + 31 >= n[None, :] * 64)).astype(np.float32)
    ov[511, :] = 0.0
    c["ovl"] = _bf(ov.reshape(4, 128, 128).transpose(1, 0, 2))
    c["iotac"] = np.ascontiguousarray(np.broadcast_to(np.arange(CAP, dtype=np.float32)[None, :], (128, CAP)))
    return c


def make_in_maps(x, p, ln1, w_in, b_fg, w_cmp1_k, w_cmp2_k, pe_cmp_k, w_cmp1_v, w_cmp2_v, pe_cmp_v, gn_nsa, gn_fox,
                 w_out, ln2, w_router, b_router, w_up, b_up, w_down, b_down, ln_ple, w_ple, w_ple_gate, ln_f, ne=NE):
    f = lambda a: np.ascontiguousarray(np.asarray(a, dtype=np.float32))
    x = f(x); p = f(p); w = f(w_in)[0]
    q_n = w[:, 0:512]; k_c = w[:, 512:640]; v_c = w[:, 640:768]; k_s = w[:, 768:896]; v_s = w[:, 896:1024]
    k_w = w[:, 1024:1152]; v_w = w[:, 1152:1280]; g_n = w[:, 1280:1304]; q_f = w[:, 1304:1816]
    k_f = w[:, 1816:2328]; v_f = w[:, 2328:2840]; f_f = w[:, 2840:2848]
    bc = lambda v: f(np.broadcast_to(np.asarray(v, np.float32).reshape(1, -1), (128, np.asarray(v).size)))
    shared = {
        "wA": f(np.concatenate([k_f, k_s, k_w, k_c, v_c], 1)),
        "wB": f(np.concatenate([v_f, v_s, v_w, f_f, g_n], 1)),
        "wQ": f(np.concatenate([q_n, q_f], 1)),
        "ln1c": f(np.asarray(ln1, np.float32)[0].reshape(8, 128).T),
        "bfg": bc(np.asarray(b_fg)[0]),
        "gnb": bc(np.concatenate([np.asarray(gn_nsa)[0], np.asarray(gn_fox)[0]])),
        "wout": f(w_out)[0], "ln2b": bc(np.asarray(ln2)[0]), "wr": f(w_router)[0], "brb": bc(np.asarray(b_router)[0]),
        "wup": f(np.asarray(w_up)[0, :ne]), "wdn": f(np.asarray(w_down)[0, :ne]), "bdn": f(np.asarray(b_down)[0]),
        "bupc": f(np.asarray(b_up, np.float32)[0].reshape(NE, 16, 128).transpose(2, 0, 1)),
        "lnpb": bc(np.asarray(ln_ple)[0]), "wple": f(w_ple)[0], "wpg": f(w_ple_gate)[0], "lnfb": bc(np.asarray(ln_f)),
    }
    for nm, w1, w2, pe in (("k", w_cmp1_k, w_cmp2_k, pe_cmp_k), ("v", w_cmp1_v, w_cmp2_v, pe_cmp_v)):
        w1r = np.asarray(w1, np.float32)[0].reshape(32, 64, 128).transpose(1, 0, 2)
        shared["w1" + nm] = f(np.concatenate([w1r, w1r], 0))
        peT = np.asarray(pe, np.float32)[0].T
        peT = np.concatenate([peT, peT], 0)
        shared["pe" + nm] = f(np.stack([peT, peT], -1))
    w2k = np.asarray(w_cmp2_k, np.float32)[0]
    shared["w2k"] = f(np.concatenate([w2k, w2k], 1))
    shared["w2v"] = f(np.asarray(w_cmp2_v, np.float32)[0])
    maps = []
    for c in range(NCORES):
        b, j = c // 4, c % 4
        tok = own_tokens(j)
        m = dict(shared)
        m["xT"] = f(x[b].T.reshape(8, 128, T))
        m["xTo"] = f(x[b][tok].T.reshape(8, 128, 2048))
        m["xo"] = f(x[b][tok])
        m["pTo"] = f(p[0, b][tok].T.reshape(2, 128, 2048))
        m.update(const_tables(j))
        maps.append(m)
    return maps


_CACHE = {}


def kernel(**inputs):
    maps = make_in_maps(**inputs)
    if "nc" not in _CACHE:
        _CACHE["nc"] = build_program()[0]
    nc = _CACHE["nc"]
    res = run_bass_kernel_spmd(nc, maps, core_ids=list(range(NCORES)))
    out = np.zeros((2, T, D), np.float32)
    for c in range(NCORES):
        b, j = c // 4, c % 4
        out[b, own_tokens(j)] = np.asarray(res.results[c]["out"], np.float32).reshape(2048, D)
    return out
```

```python
from contextlib import ExitStack
import numpy as np
import ml_dtypes
import concourse.bass as bass
import concourse.mybir as mybir
from concourse.bass_utils import run_bass_kernel_spmd

F32 = mybir.dt.float32
BF16 = mybir.dt.bfloat16
AF = mybir.ActivationFunctionType
ALU = mybir.AluOpType
AX = mybir.AxisListType

NCORES = 8
T = 8192
D = 1024
NEGM = -30000.0
EPS = 1e-6
NE = 32
MOE_EXPERTS = 32
CAP = 512


class Dep:
    __slots__ = ("w", "r")

    def __init__(self):
        self.w = None
        self.r = []


class Sched:
    ROLL = 30000

    def __init__(self, nc, n_dma=40):
        self.nc = nc
        self.E = {"pe": nc.tensor, "act": nc.scalar, "dve": nc.vector, "pool": nc.gpsimd, "sp": nc.sync}
        self.csem = {}
        self.cnt = {}
        self.nsem = 0
        for k in ("pe", "act", "dve", "pool"):
            self._new_csem(k)
        self.seen = {k: {} for k in self.E}
        self.dsem = [nc.alloc_semaphore(name=f"dq{i}") for i in range(n_dma)]
        self.dval = [0] * n_dma
        self.dnext = 0
        self.mute = False
        self.ninst = 0

    def _new_csem(self, k):
        self.csem[k] = self.nc.alloc_semaphore(name=f"c{k}{self.nsem}")
        self.nsem += 1
        self.cnt[k] = 0

    def _collect(self, e, R, W):
        evs = []
        for d in R:
            if d.w is not None:
                evs.append(d.w)
        for d in W:
            if d.w is not None:
                evs.append(d.w)
            evs.extend(d.r)
        return evs

    def _wait(self, e, evs):
        eng = self.E[e]
        seen = self.seen[e]
        need = {}
        for (s, v, src) in evs:
            if src == "pe" and e == "pe":
                continue
            if src == e and s is self.csem.get(e) and self.cnt[e] - v >= 3:
                continue
            key = s.num
            if seen.get(key, 0) >= v:
                continue
            if key not in need or need[key][1] < v:
                need[key] = (s, v)
        for key, (s, v) in need.items():
            eng.wait_ge(s, v)
            seen[key] = v
            self.ninst += 1

    def _mark(self, ev, R, W):
        for d in R:
            d.r.append(ev)
            if len(d.r) > 64:
                d.r = d.r[-64:]
        for d in W:
            d.w = ev
            d.r = []

    def op(self, e, fn, R=(), W=()):
        if self.mute:
            return None
        self._wait(e, self._collect(e, R, W))
        ins = fn(self.E[e])
        if self.cnt[e] >= self.ROLL:
            self._new_csem(e)
        self.cnt[e] += 1
        ins.then_inc(self.csem[e], 1)
        ev = (self.csem[e], self.cnt[e], e)
        self._mark(ev, R, W)
        self.ninst += 1
        return ev

    def dma(self, out, in_, R=(), W=(), e="sp"):
        if self.mute:
            return None
        k = self.dnext
        self.dnext = (k + 1) % len(self.dsem)
        if self.dval[k] >= self.ROLL:
            self._wait(e, [(self.dsem[k], self.dval[k], "dma")])
            self.dsem[k] = self.nc.alloc_semaphore(name=f"dq{k}_{self.nsem}")
            self.nsem += 1
            self.dval[k] = 0
        s = self.dsem[k]
        evs = self._collect(e, R, W)
        if self.dval[k] > 0:
            evs.append((s, self.dval[k], "dma"))
        self._wait(e, evs)
        self.E[e].dma_start(out=out, in_=in_).then_inc(s, 16)
        self.dval[k] += 16
        ev = (s, self.dval[k], "dma")
        self._mark(ev, R, W)
        self.ninst += 1
        return ev

    def barrier(self):
        evs = [(self.csem[k], self.cnt[k], k + "_b") for k in self.csem if self.cnt[k] > 0]
        evs += [(self.dsem[k], self.dval[k], "dma") for k in range(len(self.dsem)) if self.dval[k] > 0]
        for e in self.E:
            self._wait(e, [x for x in evs])


def _bf(a):
    return np.ascontiguousarray(a).astype(ml_dtypes.bfloat16)


def build_program(stage=99, n_experts=MOE_EXPERTS, skip123=False, p4stop=99, nblk4=16, skip5=False, skip6=False):
    nc = bass.Bass("TRN2", target_bir_lowering=False)
    S = Sched(nc)

    def din(name, shape, dt=F32):
        return nc.dram_tensor(name, list(shape), dt, kind="ExternalInput").ap()

    dbg = stage < 99

    def dscr(name, shape, dt):
        return nc.dram_tensor(name, list(shape), dt, kind=("ExternalOutput" if dbg else "Internal")).ap()

    xT_d = din("xT", [8, 128, T])
    xTo_d = din("xTo", [8, 128, 2048])
    xo_d = din("xo", [2048, D])
    pTo_d = din("pTo", [2, 128, 2048])
    wA_d = din("wA", [D, 1024])
    wB_d = din("wB", [D, 800])
    wQ_d = din("wQ", [D, 1024])
    ln1c_d = din("ln1c", [128, 8])
    bfg_d = din("bfg", [128, 8])
    w1k_d = din("w1k", [128, 32, 128])
    w1v_d = din("w1v", [128, 32, 128])
    w2k_d = din("w2k", [128, 128])
    w2v_d = din("w2v", [128, 64])
    pek_d = din("pek", [128, 32, 2])
    pev_d = din("pev", [128, 32, 2])
    gn_d = din("gnb", [128, 1024])
    wout_d = din("wout", [D, D])
    ln2_d = din("ln2b", [128, D])
    wr_d = din("wr", [D, 32])
    br_d = din("brb", [128, 32])
    wup_d = din("wup", [n_experts, D, 2048])
    bup_d = din("bupc", [128, NE, 16])
    wdn_d = din("wdn", [n_experts, D, D])
    bdn_d = din("bdn", [NE, D])
    lnp_d = din("lnpb", [128, D])
    wple_d = din("wple", [256, D])
    wpg_d = din("wpg", [D, D])
    lnf_d = din("lnfb", [128, D])
    identb_d = din("identb", [128, 128], BF16)
    identf_d = din("identf", [128, 128])
    tri_d = din("trif", [128, 128])
    diagm_d = din("diagm", [128, 4, 128], BF16)
    winm_d = din("winm", [128, 8, 128], BF16)
    cmask_d = din("cmask", [128, 5, 128], BF16)
    ab_d = din("ab", [128, 8, 64])
    cab_d = din("cab", [128, 8, 16])
    selA_d = din("selA", [128, 16, 128], BF16)
    selB_d = din("selB", [128, 16, 128], BF16)
    wsel_d = din("wsel", [128, 16, 64])
    R_d = din("Rexp", [128, T], BF16)
    ovl_d = din("ovl", [128, 4, 128], BF16)
    iota_d = din("iotac", [128, CAP])

    out_d = nc.dram_tensor("out", [2048, D], F32, kind="ExternalOutput").ap()

    fmS = dscr("fmS", [8, 128, T], BF16)
    tmS = dscr("tmS", [T, 780], BF16)
    qS = dscr("qS", [16, 16, 128, 128], BF16)
    fmS_dep = [Dep() for _ in range(8)]
    tmS_dep = Dep()
    qS_dep = Dep()

    es_all = ExitStack()

    def sb(es, name, shape, dt):
        return es.enter_context(nc.sbuf_tensor("s_" + name, list(shape), dt))

    ps = [es_all.enter_context(nc.psum_tensor(f"ps{i}", [128, 512], F32)) for i in range(7)]
    psd = [Dep() for _ in range(7)]
    psb = es_all.enter_context(nc.psum_tensor("psb", [128, 1024], BF16))
    psb_dep = Dep()

    identb = sb(es_all, "identb", [128, 128], BF16)
    identf = sb(es_all, "identf", [128, 128], F32)
    trif = sb(es_all, "trif", [128, 128], F32)
    onesb = sb(es_all, "onesb", [128, 128], BF16)
    onesf = sb(es_all, "onesf", [128, 128], F32)
    epsc = sb(es_all, "epsc", [128, 1], F32)
    onec = sb(es_all, "onec", [128, 1], F32)
    cst = Dep()
    S.dma(identb[:], identb_d, W=[cst])
    S.dma(identf[:], identf_d, W=[cst])
    S.dma(trif[:], tri_d, W=[cst])
    S.op("dve", lambda e: e.memset(onesb[:], 1.0), W=[cst])
    S.op("dve", lambda e: e.memset(onesf[:], 1.0), W=[cst])
    S.op("dve", lambda e: e.memset(epsc[:], EPS), W=[cst])
    S.op("dve", lambda e: e.memset(onec[:], 1.0), W=[cst])

    es_attn = ExitStack()
    ffall = sb(es_attn, "ffall", [128, 64, 8], F32)
    ffall_dep = Dep()
    gown = sb(es_attn, "gown", [128, 16, 24], F32)
    gown_dep = Dep()

    rr = {"cast": 0, "evac": 0}

    def cast_eng():
        rr["cast"] += 1
        return ("act", "dve", "pool")[rr["cast"] % 3]

    def copy_on(e, out, in_, R, W):
        if e == "act":
            S.op("act", lambda g: g.copy(out=out, in_=in_), R, W)
        elif e == "dve":
            S.op("dve", lambda g: g.tensor_copy(out=out, in_=in_), R, W)
        else:
            S.op("pool", lambda g: g.tensor_copy(out=out, in_=in_), R, W)

    def evac_eng():
        rr["evac"] += 1
        return ("act", "dve")[rr["evac"] % 2]

    def mm(out, lhsT, rhs, start, stop, R, W):
        S.op("pe", lambda g: g.matmul(out, lhsT=lhsT, rhs=rhs, start=start, stop=stop), R, W)

    pctr = [0]
    LAG = 2

    def run_pipe(steps, LAG=2):
        n = len(steps)
        for k in range(n + LAG):
            if k < n:
                steps[k][0]()
            if k - LAG >= 0:
                steps[k - LAG][1]()

    S.mute = skip123
    with ExitStack() as es:
        wA = sb(es, "wA", [128, 8, 1024], BF16)
        wB = sb(es, "wB", [128, 8, 800], BF16)
        wQz = sb(es, "wQz", [128, 8, 16, 128], BF16)
        stg = [sb(es, f"stg{i}", [128, 1024], F32) for i in range(2)]
        stg_dep = [Dep() for _ in range(2)]
        ln1c = sb(es, "ln1c", [128, 8], F32)
        xt = [sb(es, f"xt{i}", [128, 8, 512], F32) for i in range(2)]
        xt_dep = [Dep() for _ in range(2)]
        sq = sb(es, "sq", [128, 8, 512], BF16)
        sq_dep = Dep()
        rstd = sb(es, "rstd", [128, 512], F32)
        rstd_dep = Dep()
        uT = sb(es, "uT", [128, 8, 512], BF16)
        uT_dep = Dep()
        fmo = [sb(es, f"fmo{i}", [128, 8, 512], BF16) for i in range(2)]
        fmo_dep = [Dep() for _ in range(2)]
        tmv = [sb(es, f"tmv{i}", [128, 4, 780], BF16) for i in range(2)]
        tmv_dep = [Dep() for _ in range(2)]
        qo = [sb(es, f"qo{i}", [128, 4, 16, 128], BF16) for i in range(2)]
        qo_dep = [Dep() for _ in range(2)]
        w_dep = Dep()

        S.dma(ln1c[:], ln1c_d, W=[w_dep])
        S.op("pool", lambda e: e.memset(wQz[:], 0.0), W=[w_dep])
        for k in range(2):
            S.op("pool", lambda e: e.memset(tmv[k][:], 1.0), W=[tmv_dep[k]])
        sc = 0
        for dc in range(8):
            for (src, dst, ncol) in ((wA_d, wA, 1024), (wB_d, wB, 800)):
                k = sc % 2
                sc += 1
                S.dma(stg[k][:, 0:ncol], src[dc * 128:(dc + 1) * 128, :], W=[stg_dep[k]])
                copy_on(cast_eng(), dst[:, dc, :], stg[k][:, 0:ncol], [stg_dep[k]], [w_dep])
            k = sc % 2
            sc += 1
            S.dma(stg[k][:, :], wQ_d[dc * 128:(dc + 1) * 128, :], W=[stg_dep[k]])
            copy_on(cast_eng(), wQz[:, dc, 0:4, 0:64],
                    stg[k][:, 0:256].rearrange("p (h e) -> p h e", e=64), [stg_dep[k]], [w_dep])
            copy_on(cast_eng(), wQz[:, dc, 4:8, 64:128],
                    stg[k][:, 256:512].rearrange("p (h e) -> p h e", e=64), [stg_dep[k]], [w_dep])
            fx = stg[k][:, 512:1024].rearrange("p (h two e) -> p h two e", two=2, e=64)
            wz = wQz[:, dc, 8:16, :].rearrange("p (h two) e -> p h two e", two=2)
            copy_on(cast_eng(), wz[:, :, 0, 0:64], fx[:, :, 0, :], [stg_dep[k]], [w_dep])
            copy_on(cast_eng(), wz[:, :, 1, 64:128], fx[:, :, 1, :], [stg_dep[k]], [w_dep])

        def norm_tile(src_ap, k):
            S.dma(xt[k][:], src_ap, W=[xt_dep[k]])
            S.op("act", lambda e: e.activation(out=sq[:], in_=xt[k][:], func=AF.Square), [xt_dep[k]], [sq_dep])
            for dc in range(8):
                mm(ps[0][:, :], onesb[:], sq[:, dc, :], dc == 0, dc == 7, [sq_dep, cst], [psd[0]])
            S.op("act", lambda e: e.activation(out=rstd[:], in_=ps[0][:, :], func=AF.Sqrt,
                                               bias=epsc[:, 0:1], scale=1.0 / D), [psd[0], cst], [rstd_dep])
            S.op("dve", lambda e: e.reciprocal(out=rstd[:], in_=rstd[:]), [rstd_dep], [rstd_dep])
            for dc in range(8):
                S.op("dve", lambda e: e.scalar_tensor_tensor(
                    out=uT[:, dc, :], in0=xt[k][:, dc, :], scalar=ln1c[:, dc:dc + 1], in1=rstd[:],
                    op0=ALU.mult, op1=ALU.mult), [xt_dep[k], rstd_dep, w_dep], [uT_dep])

        xT_v = xT_d.rearrange("c p s -> p c s")
        xTo_v = xTo_d.rearrange("c p s -> p c s")
        fmS_v = fmS.rearrange("o p s -> p o s")
        for Tt in range(16):
            k = Tt % 2
            norm_tile(xT_v[:, :, Tt * 512:(Tt + 1) * 512], k)
            for oc in range(8):
                b = 1 + oc % 2
                for dc in range(8):
                    mm(ps[b][:, :], wA[:, dc, oc * 128:(oc + 1) * 128], uT[:, dc, :], dc == 0, dc == 7,
                       [uT_dep, w_dep], [psd[b]])
                copy_on(evac_eng(), fmo[k][:, oc, :], ps[b][:, :], [psd[b]], [fmo_dep[k]])
            S.dma(fmS_v[:, :, Tt * 512:(Tt + 1) * 512], fmo[k][:], R=[fmo_dep[k]], W=fmS_dep)
            for sub in range(4):
                bA = 3 + (sub % 2) * 2
                bB = bA + 1
                for dc in range(8):
                    mm(ps[bA][:, 0:512], uT[:, dc, sub * 128:(sub + 1) * 128], wB[:, dc, 0:512], dc == 0, dc == 7,
                       [uT_dep, w_dep], [psd[bA]])
                for dc in range(8):
                    mm(ps[bB][:, 0:288], uT[:, dc, sub * 128:(sub + 1) * 128], wB[:, dc, 512:800], dc == 0, dc == 7,
                       [uT_dep, w_dep], [psd[bB]])
                copy_on("act", tmv[k][:, sub, 0:520].rearrange("p (h e) -> p h e", e=65)[:, :, 0:64],
                        ps[bA][:, 0:512].rearrange("p (h e) -> p h e", e=64), [psd[bA]], [tmv_dep[k]])
                copy_on("dve", tmv[k][:, sub, 520:780].rearrange("p (h e) -> p h e", e=65)[:, :, 0:64],
                        ps[bB][:, 0:256].rearrange("p (h e) -> p h e", e=64), [psd[bB]], [tmv_dep[k]])
                copy_on("dve", ffall[:, Tt * 4 + sub, :], ps[bB][:, 256:264], [psd[bB]], [ffall_dep])
            S.dma(tmS[Tt * 512:(Tt + 1) * 512, :].rearrange("(s p) c -> p s c", p=128), tmv[k][:],
                  R=[tmv_dep[k]], W=[tmS_dep])

        qS_v = qS.rearrange("i h p t -> i p h t")
        for T4 in range(4):
            norm_tile(xTo_v[:, :, T4 * 512:(T4 + 1) * 512], T4 % 2)
            k = T4 % 2
            for hd in range(16):
                b = 1 + hd % 2
                for dc in range(8):
                    mm(ps[b][:, :], wQz[:, dc, hd, :], uT[:, dc, :], dc == 0, dc == 7, [uT_dep, w_dep], [psd[b]])
                qdst = qo[k][:, :, hd, :]
                qsrc = ps[b][:, :].rearrange("p (b t) -> p b t", t=128)
                if hd % 2 == 0:
                    S.op("act", lambda e: e.activation(out=qdst, in_=qsrc, func=AF.Copy, scale=0.125), [psd[b]], [qo_dep[k]])
                else:
                    S.op("dve", lambda e: e.tensor_scalar(out=qdst, in0=qsrc, scalar1=0.125, scalar2=None, op0=ALU.mult),
                         [psd[b]], [qo_dep[k]])
            for bi in range(4):
                i = T4 * 4 + bi
                for dc in range(8):
                    mm(ps[3][:, 0:24], uT[:, dc, bi * 128:(bi + 1) * 128], wB[:, dc, 776:800], dc == 0, dc == 7,
                       [uT_dep, w_dep], [psd[3]])
                copy_on("dve", gown[:, i, :], ps[3][:, 0:24], [psd[3]], [gown_dep])
                S.dma(qS_v[i], qo[k][:, bi, :, :], R=[qo_dep[k]], W=[qS_dep])
        S.barrier()

    if stage <= 1:
        dbg_f = nc.dram_tensor("dbg_ff", [128, 64 * 8 + 16 * 24], F32, kind="ExternalOutput").ap()
        S.dma(dbg_f[:, 0:512], ffall[:].rearrange("p a b -> p (a b)"))
        S.dma(dbg_f[:, 512:896], gown[:].rearrange("p a b -> p (a b)"))
        S.barrier()
        es_attn.close()
        es_all.close()
        return nc, S

    mixS = dscr("mixS", [2048, D], BF16)
    mixS_dep = Dep()
    diagm = sb(es_attn, "diagm", [128, 4, 128], BF16)
    S.dma(diagm[:], diagm_d, W=[cst])
    pT = [sb(es_attn, f"pT{i}", [128, 512], BF16) for i in range(4)]
    pT_dep = [Dep() for _ in range(4)]
    zc = [sb(es_attn, f"zc{i}", [128, 4], F32) for i in range(2)]
    zc_dep = [Dep() for _ in range(2)]

    with ExitStack() as es:
        bfg = sb(es, "bfg", [128, 8], F32)
        wsel = sb(es, "wsel", [128, 16, 64], F32)
        lsp = sb(es, "lsp", [128, 64, 8], F32)
        cpcol = sb(es, "cpcol", [128, 64, 8], F32)
        tot = sb(es, "tot", [128, 64, 8], F32)
        pre = sb(es, "pre", [128, 64, 8], F32)
        cpref = sb(es, "cpref", [128, 16, 8], F32)
        tmpw = sb(es, "tmpw", [128, 8, 64], F32)
        cd = Dep()
        S.dma(bfg[:], bfg_d, W=[cd])
        S.dma(wsel[:], wsel_d, W=[cd])
        S.op("dve", lambda e: e.tensor_tensor(out=lsp[:], in0=ffall[:], in1=bfg[:, :].unsqueeze(1).to_broadcast([128, 64, 8]),
                                              op=ALU.add), [ffall_dep, cd], [cd])
        S.op("act", lambda e: e.activation(out=lsp[:], in_=lsp[:], func=AF.Exp, scale=-1.0), [cd], [cd])
        S.op("act", lambda e: e.activation(out=lsp[:], in_=lsp[:], func=AF.Ln, bias=onec[:, 0:1], scale=1.0), [cd, cst], [cd])
        lflat = lsp[:].rearrange("p a b -> p (a b)")
        mm(ps[0][:, :], trif[:], lflat, True, True, [cd, cst], [psd[0]])
        mm(ps[1][:, :], onesf[:], lflat, True, True, [cd, cst], [psd[1]])
        S.op("dve", lambda e: e.tensor_copy(out=tot[:].rearrange("p a b -> p (a b)"), in_=ps[1][:, :]), [psd[1]], [cd])
        S.op("dve", lambda e: e.memset(pre[:, 0, :], 0.0), [], [cd])
        for k in range(1, 64):
            S.op("dve", lambda e: e.tensor_tensor(out=pre[:, k, :], in0=pre[:, k - 1, :], in1=tot[:, k - 1, :], op=ALU.add), [cd], [cd])
        S.op("dve", lambda e: e.tensor_tensor(out=cpcol[:].rearrange("p a b -> p (a b)"), in0=ps[0][:, :],
                                              in1=pre[:].rearrange("p a b -> p (a b)"), op=ALU.add), [psd[0], cd], [cd])
        for i in range(16):
            S.op("dve", lambda e: e.tensor_tensor(out=tmpw[:], in0=tot[:].rearrange("p k h -> p h k"),
                                                  in1=wsel[:, i, :].unsqueeze(1).to_broadcast([128, 8, 64]), op=ALU.mult), [cd], [cd])
            S.op("dve", lambda e: e.reduce_sum(out=cpref[:, i, :], in_=tmpw[:], axis=AX.X), [cd], [cd])

        kT = [sb(es, f"kT{i}", [128, T], BF16) for i in range(2)]
        vP = [sb(es, f"vP{i}", [128, 64, 130], BF16) for i in range(2)]
        qP = [sb(es, f"qP{i}", [128, 16, 2, 128], BF16) for i in range(2)]
        kvq_dep = [Dep() for _ in range(2)]
        wF = [sb(es, f"wF{i}", [128, 64], F32) for i in range(2)]
        wF_dep = [Dep() for _ in range(2)]
        Vp = [sb(es, f"Vp{i}", [128, 64, 65], BF16) for i in range(2)]
        Vp_dep = [Dep() for _ in range(2)]
        mo = [sb(es, f"mo{i}", [128, 128], BF16) for i in range(2)]
        mo_dep = [Dep() for _ in range(2)]
        tmS_v = tmS.rearrange("(k p) c -> p k c", p=128)
        qS_p = qS.rearrange("i h p t -> p i h t")
        items = [(hp, i, hh) for hp in range(4) for i in range(16) for hh in range(2)]

        def fox_prep(n):
            hp, i, hh = items[n]
            kb = hp % 2
            bb = n % 2
            h = 2 * hp + hh
            nk = 4 * i + 4
            if i == 0 and hh == 0:
                S.dma(kT[kb][:], fmS[hp], R=[fmS_dep[hp]], W=[kvq_dep[kb]])
                for q4 in range(4):
                    S.dma(vP[kb][:, q4 * 16:(q4 + 1) * 16, :], tmS_v[:, q4 * 16:(q4 + 1) * 16, hp * 130:(hp + 1) * 130],
                          R=[tmS_dep], W=[kvq_dep[kb]])
                for q2 in range(2):
                    S.dma(qP[kb][:, :, q2, :], qS_p[:, :, 8 + 2 * hp + q2, :], R=[qS_dep], W=[kvq_dep[kb]])
            S.op("dve", lambda e: e.tensor_scalar(out=wF[bb][:, 0:nk], in0=cpcol[:, 0:nk, h], scalar1=cpref[:, i, h:h + 1],
                                                  scalar2=0.0, op0=ALU.subtract, op1=ALU.min), [cd], [wF_dep[bb]])
            S.op("act", lambda e: e.activation(out=wF[bb][:, 0:nk], in_=wF[bb][:, 0:nk], func=AF.Exp), [wF_dep[bb]], [wF_dep[bb]])
            eng = "dve" if n % 2 == 0 else "pool"
            S.op(eng, lambda e: e.tensor_tensor(out=Vp[bb][:, 0:nk, :], in0=vP[kb][:, 0:nk, hh * 65:(hh + 1) * 65],
                                                in1=wF[bb][:, 0:nk].unsqueeze(2).to_broadcast([128, nk, 65]), op=ALU.mult),
                 [kvq_dep[kb], wF_dep[bb]], [Vp_dep[bb]])

        def fox_run(n):
            hp, i, hh = items[n]
            kb = hp % 2
            bb = n % 2
            ob = 4 + n % 2
            mb = i % 2
            nk = 4 * i + 4
            steps = []
            for gq in range(nk // 4):
                bank = pctr[0] % 4
                pctr[0] += 1

                def qk(gq=gq, bank=bank):
                    for q in range(4):
                        k = 4 * gq + q
                        diag = k >= 4 * i
                        mm(ps[bank][:, q * 128:(q + 1) * 128], kT[kb][:, k * 128:(k + 1) * 128], qP[kb][:, i, hh, :], True, not diag,
                           [kvq_dep[kb]], [psd[bank]])
                        if diag:
                            mm(ps[bank][:, q * 128:(q + 1) * 128], identb[:], diagm[:, k - 4 * i, :], False, True, [cst], [psd[bank]])
                    S.op("act", lambda e: e.activation(out=pT[bank][:], in_=ps[bank][:, :], func=AF.Exp), [psd[bank]], [pT_dep[bank]])

                def pv(gq=gq, bank=bank):
                    for q in range(4):
                        k = 4 * gq + q
                        mm(ps[ob][:, 0:65], pT[bank][:, q * 128:(q + 1) * 128], Vp[bb][:, k, :], k == 0, k == nk - 1,
                           [pT_dep[bank], Vp_dep[bb]], [psd[ob]])
                steps.append((qk, pv))
            run_pipe(steps)
            z = zc[bb]
            S.op("dve", lambda e: e.tensor_scalar_max(out=z[:, 0:1], in0=ps[ob][:, 64:65], scalar1=1e-30), [psd[ob]], [zc_dep[bb]])
            S.op("dve", lambda e: e.reciprocal(out=z[:, 0:1], in_=z[:, 0:1]), [zc_dep[bb]], [zc_dep[bb]])
            S.op("dve", lambda e: e.tensor_scalar(out=mo[mb][:, hh * 64:(hh + 1) * 64], in0=ps[ob][:, 0:64],
                                                  scalar1=z[:, 0:1], scalar2=None, op0=ALU.mult),
                 [psd[ob], zc_dep[bb]], [mo_dep[mb]])
            if hh == 1:
                S.dma(mixS[i * 128:(i + 1) * 128, 512 + hp * 128:512 + (hp + 1) * 128], mo[mb][:], R=[mo_dep[mb]], W=[mixS_dep])

        fox_prep(0)
        for n in range(len(items)):
            if n + 1 < len(items):
                fox_prep(n + 1)
            fox_run(n)
        S.barrier()

    if stage <= 2:
        es_attn.close()
        es_all.close()
        return nc, S

    with ExitStack() as es:
        kcT = sb(es, "kcT", [128, 512], BF16)
        vc = sb(es, "vc", [128, 4, 130], BF16)
        kc_dep = Dep()
        S.op("pool", lambda e: e.memset(vc[:], 1.0), [], [kc_dep])
        KsT = sb(es, "KsT", [128, T], BF16)
        KwT = sb(es, "KwT", [128, T], BF16)
        Vs = sb(es, "Vs", [128, 64, 130], BF16)
        Vw = sb(es, "Vw", [128, 64, 130], BF16)
        Rx = sb(es, "Rx", [128, T], BF16)
        ovl = sb(es, "ovl", [128, 4, 128], BF16)
        ab = sb(es, "ab", [128, 8, 64], F32)
        cab = sb(es, "cab", [128, 8, 16], F32)
        selA = sb(es, "selA", [128, 16, 128], BF16)
        selB = sb(es, "selB", [128, 16, 128], BF16)
        cmask = sb(es, "cmask", [128, 5, 128], BF16)
        winm = sb(es, "winm", [128, 8, 128], BF16)
        nd = Dep()
        tmS_v = tmS.rearrange("(k p) c -> p k c", p=128)
        S.dma(KsT[:], fmS[4], R=[fmS_dep[4]], W=[nd])
        S.dma(KwT[:], fmS[5], R=[fmS_dep[5]], W=[nd])
        for q4 in range(4):
            S.dma(Vs[:, q4 * 16:(q4 + 1) * 16, :], tmS_v[:, q4 * 16:(q4 + 1) * 16, 520:650], R=[tmS_dep], W=[nd])
            S.dma(Vw[:, q4 * 16:(q4 + 1) * 16, :], tmS_v[:, q4 * 16:(q4 + 1) * 16, 650:780], R=[tmS_dep], W=[nd])
        for (dst, src) in ((Rx, R_d), (ovl, ovl_d), (ab, ab_d), (cab, cab_d), (selA, selA_d), (selB, selB_d),
                           (cmask, cmask_d), (winm, winm_d)):
            S.dma(dst[:], src, W=[nd])
        wab = sb(es, "wab", [128, 8, 64], F32)
        wab_dep = Dep()
        Vsp = [sb(es, f"Vsp{i}", [128, 64, 65], BF16) for i in range(2)]
        Vwp = [sb(es, f"Vwp{i}", [128, 8, 65], BF16) for i in range(2)]
        Vxp_dep = [Dep() for _ in range(2)]
        with ExitStack() as es2:
            kraw = sb(es2, "kraw", [128, T], BF16)
            w1f = sb(es2, "w1f", [128, 32, 128], F32)
            w1b = sb(es2, "w1b", [128, 32, 128], BF16)
            pef = sb(es2, "pef", [128, 32, 2], F32)
            peb = sb(es2, "peb", [128, 32, 2], BF16)
            w2f = sb(es2, "w2f", [128, 128], F32)
            w2b = sb(es2, "w2b", [128, 128], BF16)
            hx = sb(es2, "hx", [128, 512], F32)
            hu = sb(es2, "hu", [128, 512], F32)
            hidT = sb(es2, "hidT", [128, 512], BF16)
            cbias = sb(es2, "cbias", [128, 1], F32)
            cpd = Dep()
            for which in range(2):
                S.dma(kraw[:], fmS[6 + which], R=[fmS_dep[6 + which]], W=[cpd])
                S.dma(w1f[:], (w1k_d, w1v_d)[which], W=[cpd])
                S.dma(pef[:], (pek_d, pev_d)[which], W=[cpd])
                if which == 0:
                    S.dma(w2f[:, :], w2k_d, W=[cpd])
                else:
                    S.dma(w2f[:, 0:64], w2v_d, W=[cpd])
                S.op("dve", lambda e: e.tensor_copy(out=w1b[:], in_=w1f[:]), [cpd], [cpd])
                S.op("dve", lambda e: e.tensor_copy(out=peb[:], in_=pef[:]), [cpd], [cpd])
                S.op("dve", lambda e: e.tensor_copy(out=w2b[:], in_=w2f[:]), [cpd], [cpd])
                for g in range(2):
                    r0, r1 = g * 64, g * 64 + 64
                    for l in range(32):
                        mm(ps[0][:, 0:511], w1b[r0:r1, l, :], kraw[r0:r1, l:l + 16 * 510 + 1:16], l == 0, l == 31, [cpd], [psd[0]])
                    for l in range(32):
                        mm(ps[1][:, 0:2], w1b[r0:r1, l, :], peb[r0:r1, l, :], l == 0, l == 31, [cpd], [psd[1]])
                    S.op("dve", lambda e: e.tensor_copy(out=cbias[:], in_=ps[1][:, 0:1]), [psd[1]], [cpd])
                    S.op("dve", lambda e: e.memset(hx[:], 0.0), [], [cpd])
                    S.op("dve", lambda e: e.tensor_scalar(out=hx[:, 0:511], in0=ps[0][:, 0:511], scalar1=cbias[:, 0:1],
                                                          scalar2=None, op0=ALU.add), [psd[0], cpd], [cpd])
                    S.op("dve", lambda e: e.tensor_tensor(out=hu[:], in0=hx[:], in1=hx[:], op=ALU.mult), [cpd], [cpd])
                    S.op("dve", lambda e: e.tensor_scalar(out=hu[:], in0=hu[:], scalar1=0.044715, scalar2=1.0,
                                                          op0=ALU.mult, op1=ALU.add), [cpd], [cpd])
                    S.op("dve", lambda e: e.tensor_tensor(out=hu[:], in0=hu[:], in1=hx[:], op=ALU.mult), [cpd], [cpd])
                    S.op("act", lambda e: e.activation(out=hu[:], in_=hu[:], func=AF.Exp, scale=-1.5957691216057308), [cpd], [cpd])
                    S.op("dve", lambda e: e.tensor_scalar(out=hu[:], in0=hu[:], scalar1=1.0, scalar2=None, op0=ALU.add), [cpd], [cpd])
                    S.op("dve", lambda e: e.reciprocal(out=hu[:], in_=hu[:]), [cpd], [cpd])
                    S.op("dve", lambda e: e.tensor_tensor(out=hidT[:], in0=hu[:], in1=hx[:], op=ALU.mult), [cpd], [cpd])
                    if which == 0:
                        mm(ps[2][:, 0:512], w2b[:, :], hidT[:], True, True, [cpd], [psd[2]])
                        S.op("dve", lambda e: e.tensor_copy(out=kcT[r0:r1, :], in_=ps[2][r0:r1, 0:512]), [psd[2]], [kc_dep])
                    else:
                        for m in range(4):
                            mm(ps[2][:, m * 64:(m + 1) * 64], hidT[:, m * 128:(m + 1) * 128], w2b[:, 0:64], True, True, [cpd], [psd[2]])
                        S.op("dve", lambda e: e.tensor_copy(out=vc[:, :, g * 65:g * 65 + 64],
                                                            in_=ps[2][:, 0:256].rearrange("p (m e) -> p m e", e=64)), [psd[2]], [kc_dep])
            S.barrier()

        S.op("dve", lambda e: e.tensor_scalar(out=wab[:], in0=ab[:], scalar1=0.0, scalar2=None, op0=ALU.min), [nd], [wab_dep])
        S.op("act", lambda e: e.activation(out=wab[:], in_=wab[:], func=AF.Exp), [wab_dep], [wab_dep])
        qN = [sb(es, f"qN{i}", [128, 8, 128], BF16) for i in range(2)]
        qN_dep = [Dep() for _ in range(2)]
        eC = [sb(es, f"eC{i}", [128, 4, 128], BF16) for i in range(4)]
        eC_dep = [Dep() for _ in range(4)]
        Ocs = sb(es, "Ocs", [128, 4, 64], F32)
        Ocs_dep = Dep()
        rzc = sb(es, "rzc", [128, 4], F32)
        impacc = sb(es, "impacc", [128, 128], F32)
        score = sb(es, "score", [128, 128], F32)
        sc2 = sb(es, "sc2", [128, 128], F32)
        mx8 = sb(es, "mx8", [128, 8], F32)
        mx8b = sb(es, "mx8b", [128, 8], F32)
        MnegB = sb(es, "MnegB", [128, 128], BF16)
        MnegT = sb(es, "MnegT", [128, 128], BF16)
        MnegT_dep = Dep()
        tk = Dep()
        sgate = sb(es, "sgate", [128, 24], F32)
        sg_dep = Dep()
        coef = sb(es, "coef", [128, 4], F32)
        t1 = sb(es, "t1", [128, 64], F32)
        fin = Dep()
        mon = [sb(es, f"mon{i}", [128, 512], BF16) for i in range(2)]
        mon_dep = [Dep() for _ in range(2)]
        qS_p = qS.rearrange("i h p t -> i p h t")
        sctr = 0

        def nsa_prep(nidx):
            r_ = nidx % 4
            g_ = (nidx // 4) % 2
            i_ = nidx // 8
            h_ = 4 * g_ + r_
            vb_ = nidx % 2
            nk_ = 4 * i_ + 4
            k0_ = max(0, 4 * i_ - 4)
            e1, e2 = ("dve", "pool") if nidx % 2 == 0 else ("pool", "dve")
            S.op(e1, lambda e: e.tensor_tensor(out=Vsp[vb_][:, 0:nk_, :], in0=Vs[:, 0:nk_, g_ * 65:(g_ + 1) * 65],
                                               in1=wab[:, h_, 60 - 4 * i_:64].unsqueeze(2).to_broadcast([128, nk_, 65]), op=ALU.mult),
                 [nd, wab_dep], [Vxp_dep[vb_]])
            S.op(e2, lambda e: e.tensor_tensor(out=Vwp[vb_][:, 0:nk_ - k0_, :], in0=Vw[:, k0_:nk_, g_ * 65:(g_ + 1) * 65],
                                               in1=wab[:, h_, 60 - 4 * i_ + k0_:64].unsqueeze(2).to_broadcast([128, nk_ - k0_, 65]), op=ALU.mult),
                 [nd, wab_dep], [Vxp_dep[vb_]])
        for i in range(16):
            qb = i % 2
            S.dma(qN[qb][:], qS_p[i][:, 0:8, :], R=[qS_dep], W=[qN_dep[qb]])
            S.op("act", lambda e: e.activation(out=sgate[:], in_=gown[:, i, :], func=AF.Exp, scale=-1.0), [gown_dep], [sg_dep])
            S.op("dve", lambda e: e.tensor_scalar(out=sgate[:], in0=sgate[:], scalar1=1.0, scalar2=None, op0=ALU.add), [sg_dep], [sg_dep])
            S.op("dve", lambda e: e.reciprocal(out=sgate[:], in_=sgate[:]), [sg_dep], [sg_dep])
            ncm = i // 4 + 1
            nk = 4 * i + 4
            for g in range(2):
                for r in range(4):
                    h = 4 * g + r
                    for m in range(ncm):
                        d = 4 * m - i
                        partial = d >= -4
                        sbk = sctr % 4
                        sctr += 1
                        mm(ps[sbk][:, 0:128], kcT[:, m * 128:(m + 1) * 128], qN[qb][:, h, :], True, not partial,
                           [kc_dep, qN_dep[qb]], [psd[sbk]])
                        if partial:
                            mm(ps[sbk][:, 0:128], identb[:], cmask[:, d + 4, :], False, True, [cst, nd], [psd[sbk]])
                        S.op("act", lambda e: e.activation(out=eC[r][:, m, :], in_=ps[sbk][:, 0:128], func=AF.Exp,
                                                           bias=cab[:, h, d + 15:d + 16], scale=1.0), [psd[sbk], nd], [eC_dep[r]])
                    for m in range(ncm):
                        mm(ps[4][:, 0:65], eC[r][:, m, :], vc[:, m, g * 65:(g + 1) * 65], m == 0, m == ncm - 1,
                           [eC_dep[r], kc_dep], [psd[4]])
                    for m in range(ncm):
                        mm(ps[5][:, 0:128], eC[r][:, m, :], ovl[:, m, :], m == 0, m == ncm - 1, [eC_dep[r], nd], [psd[5]])
                    S.op("dve", lambda e: e.tensor_scalar_max(out=rzc[:, r:r + 1], in0=ps[4][:, 64:65], scalar1=1e-30), [psd[4]], [tk, fin])
                    S.op("dve", lambda e: e.reciprocal(out=rzc[:, r:r + 1], in_=rzc[:, r:r + 1]), [tk], [tk])
                    S.op("dve", lambda e: e.tensor_copy(out=Ocs[:, r, :], in_=ps[4][:, 0:64]), [psd[4]], [Ocs_dep, fin])
                    if r == 0:
                        S.op("dve", lambda e: e.tensor_scalar(out=impacc[:], in0=ps[5][:, 0:128], scalar1=rzc[:, r:r + 1],
                                                              scalar2=None, op0=ALU.mult), [psd[5], tk], [tk])
                    else:
                        S.op("dve", lambda e: e.scalar_tensor_tensor(out=impacc[:], in0=ps[5][:, 0:128], scalar=rzc[:, r:r + 1],
                                                                     in1=impacc[:], op0=ALU.mult, op1=ALU.add), [psd[5], tk], [tk])
                S.op("dve", lambda e: e.tensor_tensor(out=score[:], in0=impacc[:], in1=selA[:, i, :], op=ALU.mult), [tk, nd], [tk])
                S.op("dve", lambda e: e.tensor_tensor(out=score[:], in0=score[:], in1=selB[:, i, :], op=ALU.add), [tk, nd], [tk])
                S.op("dve", lambda e: e.max(out=mx8[:], in_=score[:]), [tk], [tk])
                S.op("dve", lambda e: e.match_replace(out=sc2[:], in_to_replace=mx8[:], in_values=score[:], imm_value=-2.0), [tk], [tk])
                S.op("dve", lambda e: e.max(out=mx8b[:], in_=sc2[:]), [tk], [tk])
                S.op("dve", lambda e: e.tensor_scalar(out=MnegB[:], in0=score[:], scalar1=mx8b[:, 7:8], scalar2=NEGM,
                                                      op0=ALU.is_lt, op1=ALU.mult), [tk], [tk])
                S.op("pe", lambda e: e.transpose(out=psb[:, 0:128], in_=MnegB[:], identity=identb[:]), [tk, cst], [psb_dep])
                S.op("dve", lambda e: e.tensor_copy(out=MnegT[:], in_=psb[:, 0:128]), [psb_dep], [MnegT_dep])
                for r in range(4):
                    h = 4 * g + r
                    osb = 6 if r % 2 == 0 else 5
                    nidx = (i * 2 + g) * 4 + r
                    vb = nidx % 2
                    if nidx == 0:
                        nsa_prep(0)
                    if nidx + 1 < 128:
                        nsa_prep(nidx + 1)
                    k0 = max(0, 4 * i - 4)
                    steps = []
                    for gq in range(nk // 4):
                        bank = pctr[0] % 4
                        pctr[0] += 1

                        def qk(gq=gq, bank=bank, i=i, h=h, qb=qb):
                            for q in range(4):
                                k = 4 * gq + q
                                diag = k >= 4 * i
                                o = ps[bank][:, q * 128:(q + 1) * 128]
                                mm(o, KsT[:, k * 128:(k + 1) * 128], qN[qb][:, h, :], True, False, [nd, qN_dep[qb]], [psd[bank]])
                                mm(o, Rx[:, k * 128:(k + 1) * 128], MnegT[:], False, not diag, [nd, MnegT_dep], [psd[bank]])
                                if diag:
                                    mm(o, identb[:], diagm[:, k - 4 * i, :], False, True, [cst], [psd[bank]])
                            S.op("act", lambda e: e.activation(out=pT[bank][:], in_=ps[bank][:, :], func=AF.Exp), [psd[bank]], [pT_dep[bank]])

                        def pv(gq=gq, bank=bank, osb=osb, nk=nk, vb=vb):
                            for q in range(4):
                                k = 4 * gq + q
                                mm(ps[osb][:, 0:65], pT[bank][:, q * 128:(q + 1) * 128], Vsp[vb][:, k, :], k == 0, k == nk - 1,
                                   [pT_dep[bank], Vxp_dep[vb]], [psd[osb]])
                        steps.append((qk, pv))
                    for gq in range((nk - k0) // 4):
                        bank = pctr[0] % 4
                        pctr[0] += 1

                        def qk(gq=gq, bank=bank, i=i, h=h, qb=qb, k0=k0):
                            for q in range(4):
                                k = k0 + 4 * gq + q
                                wk = k - (4 * i - 4)
                                o = ps[bank][:, q * 128:(q + 1) * 128]
                                mm(o, KwT[:, k * 128:(k + 1) * 128], qN[qb][:, h, :], True, False, [nd, qN_dep[qb]], [psd[bank]])
                                mm(o, identb[:], winm[:, wk, :], False, True, [cst, nd], [psd[bank]])
                            S.op("act", lambda e: e.activation(out=pT[bank][:], in_=ps[bank][:, :], func=AF.Exp), [psd[bank]], [pT_dep[bank]])

                        def pv(gq=gq, bank=bank, k0=k0, nk=nk, vb=vb):
                            for q in range(4):
                                k = k0 + 4 * gq + q
                                mm(ps[4][:, 0:65], pT[bank][:, q * 128:(q + 1) * 128], Vwp[vb][:, k - k0, :], k == k0, k == nk - 1,
                                   [pT_dep[bank], Vxp_dep[vb]], [psd[4]])
                        steps.append((qk, pv))
                    run_pipe(steps)
                    S.op("dve", lambda e: e.tensor_scalar_max(out=coef[:, 1:2], in0=ps[osb][:, 64:65], scalar1=1e-30), [psd[osb]], [fin])
                    S.op("dve", lambda e: e.tensor_scalar_max(out=coef[:, 2:3], in0=ps[4][:, 64:65], scalar1=1e-30), [psd[4]], [fin])
                    S.op("dve", lambda e: e.reciprocal(out=coef[:, 1:3], in_=coef[:, 1:3]), [fin], [fin])
                    S.op("dve", lambda e: e.tensor_copy(out=coef[:, 0:1], in_=rzc[:, r:r + 1]), [fin, tk], [fin])
                    S.op("dve", lambda e: e.tensor_tensor(out=coef[:, 0:3], in0=coef[:, 0:3], in1=sgate[:, 3 * h:3 * h + 3], op=ALU.mult), [fin, sg_dep], [fin])
                    S.op("dve", lambda e: e.tensor_scalar(out=t1[:], in0=Ocs[:, r, :], scalar1=coef[:, 0:1], scalar2=None, op0=ALU.mult), [fin, Ocs_dep], [fin])
                    S.op("dve", lambda e: e.scalar_tensor_tensor(out=t1[:], in0=ps[osb][:, 0:64], scalar=coef[:, 1:2], in1=t1[:],
                                                                 op0=ALU.mult, op1=ALU.add), [fin, psd[osb]], [fin])
                    S.op("dve", lambda e: e.scalar_tensor_tensor(out=mon[qb][:, h * 64:(h + 1) * 64], in0=ps[4][:, 0:64], scalar=coef[:, 2:3],
                                                                 in1=t1[:], op0=ALU.mult, op1=ALU.add), [fin, psd[4]], [mon_dep[qb], fin])
            S.dma(mixS[i * 128:(i + 1) * 128, 0:512], mon[qb][:], R=[mon_dep[qb]], W=[mixS_dep])
        S.barrier()

    if stage <= 3:
        es_attn.close()
        es_all.close()
        return nc, S

    S.mute = False
    es_attn.close()
    es_tail = ExitStack()
    hres = sb(es_tail, "hres", [128, 16, D], F32)
    hres_dep = [Dep() for _ in range(16)]
    xnT = sb(es_tail, "xntok", [128, 16, D], BF16)
    xnT_dep = Dep()
    gate = sb(es_tail, "gate", [128, 16, NE], F32)
    gate_dep = Dep()
    ssc = sb(es_tail, "ssc", [128, 4], F32)
    nrm = Dep()
    sqt_box = [None]

    def rms_rstd(src, n, col, R):
        sqt = sqt_box[0]
        S.op("dve", lambda e: e.tensor_tensor(out=sqt[:, 0:n], in0=src, in1=src, op=ALU.mult), list(R) + [nrm], [nrm])
        S.op("dve", lambda e: e.reduce_sum(out=ssc[:, col:col + 1], in_=sqt[:, 0:n], axis=AX.X), [nrm], [nrm])
        S.op("act", lambda e: e.activation(out=ssc[:, col:col + 1], in_=ssc[:, col:col + 1], func=AF.Sqrt,
                                           bias=epsc[:, 0:1], scale=1.0 / n), [nrm, cst], [nrm])
        S.op("dve", lambda e: e.reciprocal(out=ssc[:, col:col + 1], in_=ssc[:, col:col + 1]), [nrm], [nrm])

    def load_w_bf16(dst, src, nchunk, ncol, stg, stg_dep, wdep, ctr):
        for dc in range(nchunk):
            k = ctr[0] % len(stg)
            ctr[0] += 1
            S.dma(stg[k][:, 0:ncol], src[dc * 128:(dc + 1) * 128, :], W=[stg_dep[k]])
            copy_on(cast_eng(), dst[:, dc, :], stg[k][:, 0:ncol], [stg_dep[k]], [wdep])

    with ExitStack() as es:
        sqt_box[0] = sb(es, "sqt4", [128, D], F32)
        woutb = sb(es, "woutb", [128, 8, D], BF16)
        stg = [sb(es, f"stg4{i}", [128, D], F32) for i in range(2)]
        stg_dep = [Dep() for _ in range(2)]
        gnb = sb(es, "gnb", [128, D], F32)
        ln2b = sb(es, "ln2b", [128, D], F32)
        wrf = sb(es, "wrf", [128, 8, NE], F32)
        brb = sb(es, "brb", [128, NE], F32)
        bdnf = sb(es, "bdnf", [NE, D], F32)
        mixb = [sb(es, f"mixb{i}", [128, D], BF16) for i in range(2)]
        mixb_dep = [Dep() for _ in range(2)]
        mixn = sb(es, "mixn", [128, D], BF16)
        mixT = sb(es, "mixT", [128, 8, 128], BF16)
        xn = sb(es, "xn", [128, D], F32)
        xnTf = sb(es, "xnTf", [128, 8, 128], F32)
        lg = sb(es, "lg", [128, NE], F32)
        ex = sb(es, "ex", [128, NE], F32)
        mxr = sb(es, "mxr", [128, 8], F32)
        gT = sb(es, "gT", [NE, 128], F32)
        wd = Dep()
        p4 = Dep()
        ctr = [0]
        load_w_bf16(woutb, wout_d, 8, D, stg, stg_dep, wd, ctr)
        S.dma(gnb[:], gn_d, W=[wd])
        S.dma(ln2b[:], ln2_d, W=[wd])
        S.dma(wrf[:], wr_d.rearrange("(c p) e -> p c e", p=128), W=[wd])
        S.dma(brb[:], br_d, W=[wd])
        S.dma(bdnf[:], bdn_d, W=[wd])
        def g4(n):
            if p4stop <= n:
                S.mute = True
        for i in range(nblk4):
            mb = i % 2
            S.mute = False
            S.dma(mixb[mb][:], mixS[i * 128:(i + 1) * 128, :], R=[mixS_dep], W=[mixb_dep[mb]])
            S.dma(hres[:, i, :], xo_d[i * 128:(i + 1) * 128, :], W=[hres_dep[i]])
            g4(1)
            for half in range(2):
                hs = slice(half * 512, (half + 1) * 512)
                rms_rstd(mixb[mb][:, hs], 512, half, [mixb_dep[mb]])
                S.op("dve", lambda e: e.scalar_tensor_tensor(out=mixn[:, hs], in0=mixb[mb][:, hs], scalar=ssc[:, half:half + 1],
                                                             in1=gnb[:, hs], op0=ALU.mult, op1=ALU.mult), [mixb_dep[mb], nrm, wd], [p4])
            g4(2)
            for c in range(8):
                S.op("pe", lambda e: e.transpose(out=psb[:, c * 128:(c + 1) * 128], in_=mixn[:, c * 128:(c + 1) * 128],
                                                 identity=identb[:]), [p4, cst], [psb_dep])
            S.op("act", lambda e: e.copy(out=mixT[:].rearrange("p c t -> p (c t)"), in_=psb[:, :]), [psb_dep], [p4])
            g4(3)
            for half in range(2):
                hs = slice(half * 512, (half + 1) * 512)
                for c in range(8):
                    mm(ps[half][:, :], mixT[:, c, :], woutb[:, c, hs], c == 0, c == 7, [p4, wd], [psd[half]])
                S.op("dve", lambda e: e.tensor_tensor(out=hres[:, i, hs], in0=ps[half][:, :], in1=hres[:, i, hs], op=ALU.add),
                     [psd[half], hres_dep[i]], [hres_dep[i]])
            rms_rstd(hres[:, i, :], D, 2, [hres_dep[i]])
            g4(4)
            S.op("dve", lambda e: e.scalar_tensor_tensor(out=xn[:], in0=hres[:, i, :], scalar=ssc[:, 2:3], in1=ln2b[:],
                                                         op0=ALU.mult, op1=ALU.mult), [hres_dep[i], nrm, wd], [p4])
            S.op("pool", lambda e: e.tensor_copy(out=xnT[:, i, :], in_=xn[:]), [p4], [xnT_dep])
            for c in range(8):
                b = 2 + c // 4
                g4(5)
                S.op("pe", lambda e: e.transpose(out=ps[b][:, (c % 4) * 128:(c % 4 + 1) * 128], in_=xn[:, c * 128:(c + 1) * 128],
                                                 identity=identf[:]), [p4, cst], [psd[b]])
            for b2 in range(2):
                S.op("act", lambda e: e.copy(out=xnTf[:, b2 * 4:(b2 + 1) * 4, :], in_=ps[2 + b2][:, :].rearrange("p (c t) -> p c t", t=128)),
                     [psd[2 + b2]], [p4])
            for c in range(8):
                mm(ps[4][:, 0:NE], xnTf[:, c, :], wrf[:, c, :], c == 0, c == 7, [p4, wd], [psd[4]])
            g4(7)
            S.op("dve", lambda e: e.tensor_tensor(out=lg[:], in0=ps[4][:, 0:NE], in1=brb[:], op=ALU.add), [psd[4], wd], [p4])
            S.op("dve", lambda e: e.max(out=mxr[:], in_=lg[:]), [p4], [p4])
            S.op("dve", lambda e: e.tensor_scalar(out=mxr[:, 4:5], in0=mxr[:, 0:1], scalar1=-1.0, scalar2=None, op0=ALU.mult), [p4], [p4])
            S.op("act", lambda e: e.activation(out=ex[:], in_=lg[:], func=AF.Exp, bias=mxr[:, 4:5], scale=1.0), [p4], [p4])
            S.op("dve", lambda e: e.scalar_tensor_tensor(out=ex[:], in0=lg[:], scalar=mxr[:, 3:4], in1=ex[:],
                                                         op0=ALU.is_ge, op1=ALU.mult), [p4], [p4])
            S.op("dve", lambda e: e.reduce_sum(out=mxr[:, 5:6], in_=ex[:], axis=AX.X), [p4], [p4])
            S.op("dve", lambda e: e.reciprocal(out=mxr[:, 5:6], in_=mxr[:, 5:6]), [p4], [p4])
            S.op("dve", lambda e: e.tensor_scalar(out=gate[:, i, :], in0=ex[:], scalar1=mxr[:, 5:6], scalar2=None, op0=ALU.mult),
                 [p4], [gate_dep])
            g4(8)
        S.mute = False
        S.barrier()

    if stage == 4:
        dbg_h = nc.dram_tensor("dbg_h", [128, 16, D], F32, kind="ExternalOutput").ap()
        dbg_g = nc.dram_tensor("dbg_g", [128, 16, NE], F32, kind="ExternalOutput").ap()
        dbg_x = nc.dram_tensor("dbg_x", [128, 16, D], BF16, kind="ExternalOutput").ap()
        S.dma(dbg_h, hres[:])
        S.dma(dbg_g, gate[:])
        S.dma(dbg_x, xnT[:])
        S.barrier()
        es_tail.close()
        es_all.close()
        return nc, S

    S.mute = skip5
    with ExitStack() as es:
        C = CAP
        NSC = C // 128
        wupb = sb(es, "wupb", [128, 8, 2048], BF16)
        wdnb = sb(es, "wdnb", [128, 8, D], BF16)
        wup_dep = Dep()
        wdn_dep = Dep()
        NSTG5 = 4
        stg = [sb(es, f"stg5{i}", [128, 512], F32) for i in range(NSTG5)]
        stg_dep = [Dep() for _ in range(NSTG5)]
        bupc = sb(es, "bupc", [128, NE, 16], F32)
        browb = sb(es, "browb", [1, D], BF16)
        brow_dep = Dep()
        bd = Dep()
        S.dma(bupc[:], bup_d, W=[bd])
        iotac = sb(es, "iotac", [128, C], F32)
        S.dma(iotac[:], iota_d, W=[bd])
        Mf = sb(es, "Mf", [128, 16, NE], F32)
        pos = sb(es, "pos", [128, 16, NE], F32)
        dsp = Dep()
        with ExitStack() as est:
            trisb = sb(est, "trisb", [128, 128], BF16)
            Mb = sb(est, "Mb", [128, 16, NE], BF16)
            tot5 = sb(est, "tot5", [128, 16, NE], F32)
            pre5 = sb(est, "pre5", [128, 16, NE], F32)
            S.op("dve", lambda g: g.tensor_tensor(out=trisb[:], in0=trif[:], in1=identf[:], op=ALU.subtract), [cst], [dsp])
            S.op("dve", lambda g: g.tensor_scalar(out=Mf[:], in0=gate[:], scalar1=0.0, scalar2=None, op0=ALU.is_gt), [gate_dep], [dsp])
            S.op("dve", lambda g: g.tensor_copy(out=Mb[:], in_=Mf[:]), [dsp], [dsp])
            mflat = Mb[:].rearrange("p a b -> p (a b)")
            mm(ps[0][:, :], trisb[:], mflat, True, True, [dsp], [psd[0]])
            mm(ps[1][:, :], onesb[:], mflat, True, True, [dsp, cst], [psd[1]])
            S.op("dve", lambda g: g.tensor_copy(out=tot5[:].rearrange("p a b -> p (a b)"), in_=ps[1][:, :]), [psd[1]], [dsp])
            S.op("dve", lambda g: g.memset(pre5[:, 0, :], 0.0), [], [dsp])
            for k in range(1, 16):
                S.op("dve", lambda g: g.tensor_tensor(out=pre5[:, k, :], in0=pre5[:, k - 1, :], in1=tot5[:, k - 1, :], op=ALU.add), [dsp], [dsp])
            S.op("dve", lambda g: g.tensor_tensor(out=pos[:].rearrange("p a b -> p (a b)"), in0=ps[0][:, :],
                                                  in1=pre5[:].rearrange("p a b -> p (a b)"), op=ALU.add), [psd[0], dsp], [dsp])
            S.barrier()

        Sel = sb(es, "Sel", [128, 16, C], BF16)
        Sel_dep = Dep()
        SelTb = [sb(es, f"SelTb{i}", [128, NSC, 128], BF16) for i in range(2)]
        SelTb_dep = [Dep() for _ in range(2)]
        XeT = sb(es, "XeT", [128, 8, C], BF16)
        XeT_dep = Dep()
        actT = sb(es, "actT5", [128, 8, C], BF16)
        actT_dep = Dep()
        assert NSC * D == 8 * C
        ye = XeT[:].rearrange("p (s two) c -> p s (two c)", two=2)
        ye_dep = XeT_dep
        gcs = [sb(es, f"gcs{i}", [128, C], F32) for i in range(1)] * 2
        sgs = [sb(es, f"sgs{i}", [128, C], F32) for i in range(1)] * 2
        lcs = [sb(es, f"lcs{i}", [128, C], F32) for i in range(1)] * 2
        gc_dep = [Dep()] * 2
        sg_dep5 = [Dep()] * 2
        lc_dep = [Dep()] * 2
        sctr = [0]

        def load_up(e):
            for dc in range(8):
                for half in range(4):
                    k = sctr[0] % NSTG5
                    sctr[0] += 1
                    S.dma(stg[k][:], wup_d[e, dc * 128:(dc + 1) * 128, half * 512:(half + 1) * 512], W=[stg_dep[k]])
                    copy_on("pool", wupb[:, dc, half * 512:(half + 1) * 512], stg[k][:], [stg_dep[k]], [wup_dep])

        def load_dn(e):
            for fc in range(8):
                for half in range(2):
                    k = sctr[0] % NSTG5
                    sctr[0] += 1
                    S.dma(stg[k][:], wdn_d[e, fc * 128:(fc + 1) * 128, half * 512:(half + 1) * 512], W=[stg_dep[k]])
                    copy_on("pool", wdnb[:, fc, half * 512:(half + 1) * 512], stg[k][:], [stg_dep[k]], [wdn_dep])

        load_up(0)
        load_dn(0)
        uc = 0
        dcn = 0
        tcn = 0
        for e_ in range(n_experts):
            for half in range(2):
                kk = sctr[0] % NSTG5
                sctr[0] += 1
                S.dma(stg[kk][0:1, :], bdn_d[e_:e_ + 1, half * 512:(half + 1) * 512], W=[stg_dep[kk]])
                S.op("act", lambda g: g.copy(out=browb[0:1, half * 512:(half + 1) * 512], in_=stg[kk][0:1, :]), [stg_dep[kk]], [brow_dep])
            for blk in range(16):
                S.op("dve", lambda g: g.tensor_scalar(out=Sel[:, blk, :], in0=iotac[:], scalar1=pos[:, blk, e_:e_ + 1],
                                                      scalar2=Mf[:, blk, e_:e_ + 1], op0=ALU.is_equal, op1=ALU.mult), [dsp, bd], [Sel_dep])
            for dc in range(8):
                bX = 4 + dcn % 3
                dcn += 1
                for blk in range(16):
                    mm(ps[bX][:, 0:C], xnT[:, blk, dc * 128:(dc + 1) * 128], Sel[:, blk, :], blk == 0, blk == 15,
                       [xnT_dep, Sel_dep], [psd[bX]])
                S.op("act", lambda g: g.copy(out=XeT[:, dc, :], in_=ps[bX][:, 0:C]), [psd[bX]], [XeT_dep])
            for fc in range(8):
                bG = (uc % 2) * 2
                bL = bG + 1
                tb = uc % 2
                uc += 1
                for dc in range(8):
                    mm(ps[bG][:, 0:C], wupb[:, dc, fc * 128:(fc + 1) * 128], XeT[:, dc, :], dc == 0, dc == 7, [wup_dep, XeT_dep], [psd[bG]])
                for dc in range(8):
                    mm(ps[bL][:, 0:C], wupb[:, dc, 1024 + fc * 128:1024 + (fc + 1) * 128], XeT[:, dc, :], dc == 0, dc == 7,
                       [wup_dep, XeT_dep], [psd[bL]])
                S.op("dve", lambda g: g.tensor_scalar(out=gcs[tb][:], in0=ps[bG][:, 0:C], scalar1=bupc[:, e_, fc:fc + 1], scalar2=7.0,
                                                      op0=ALU.add, op1=ALU.min), [psd[bG], bd], [gc_dep[tb]])
                S.op("act", lambda g: g.activation(out=sgs[tb][:], in_=gcs[tb][:], func=AF.Sigmoid, scale=1.702), [gc_dep[tb]], [sg_dep5[tb]])
                S.op("dve", lambda g: g.tensor_scalar(out=lcs[tb][:], in0=ps[bL][:, 0:C], scalar1=bupc[:, e_, 8 + fc:9 + fc], scalar2=7.0,
                                                      op0=ALU.add, op1=ALU.min), [psd[bL], bd], [lc_dep[tb]])
                S.op("dve", lambda g: g.tensor_scalar(out=lcs[tb][:], in0=lcs[tb][:], scalar1=-7.0, scalar2=1.0,
                                                      op0=ALU.max, op1=ALU.add), [lc_dep[tb]], [lc_dep[tb]])
                S.op("pool", lambda g: g.tensor_tensor(out=gcs[tb][:], in0=gcs[tb][:], in1=sgs[tb][:], op=ALU.mult), [sg_dep5[tb]], [gc_dep[tb]])
                S.op("pool", lambda g: g.tensor_tensor(out=actT[:, fc, :], in0=gcs[tb][:], in1=lcs[tb][:], op=ALU.mult),
                     [gc_dep[tb], lc_dep[tb]], [actT_dep])
            if e_ + 1 < n_experts:
                load_up(e_ + 1)
            for sc in range(NSC):
                for half in range(2):
                    hs = slice(half * 512, (half + 1) * 512)
                    bD = 4 + dcn % 3
                    dcn += 1
                    for fc in range(8):
                        mm(ps[bD][:, :], actT[:, fc, sc * 128:(sc + 1) * 128], wdnb[:, fc, hs], fc == 0, False,
                           [actT_dep, wdn_dep], [psd[bD]])
                    mm(ps[bD][:, :], onesb[0:1, :], browb[0:1, hs], False, True, [brow_dep, cst], [psd[bD]])
                    S.op("act", lambda g: g.copy(out=ye[:, sc, hs], in_=ps[bD][:, :]), [psd[bD]], [ye_dep])
            if e_ + 1 < n_experts:
                load_dn(e_ + 1)
            for blk in range(16):
                tbf = tcn % 2
                tcn += 1
                for sc in range(NSC):
                    S.op("pe", lambda g: g.transpose(out=psb[:, sc * 128:(sc + 1) * 128], in_=Sel[:, blk, sc * 128:(sc + 1) * 128],
                                                     identity=identb[:]), [Sel_dep, cst], [psb_dep])
                S.op("act", lambda g: g.copy(out=SelTb[tbf][:].rearrange("p c t -> p (c t)"), in_=psb[:, 0:NSC * 128]), [psb_dep], [SelTb_dep[tbf]])
                for half in range(2):
                    hs = slice(half * 512, (half + 1) * 512)
                    bY = 4 + dcn % 3
                    dcn += 1
                    for sc in range(NSC):
                        mm(ps[bY][:, :], SelTb[tbf][:, sc, :], ye[:, sc, hs], sc == 0, sc == NSC - 1, [SelTb_dep[tbf], ye_dep], [psd[bY]])
                    S.op("dve", lambda g: g.scalar_tensor_tensor(out=hres[:, blk, hs], in0=ps[bY][:, :], scalar=gate[:, blk, e_:e_ + 1],
                                                                 in1=hres[:, blk, hs], op0=ALU.mult, op1=ALU.add),
                         [psd[bY], gate_dep, hres_dep[blk]], [hres_dep[blk]])
        S.barrier()

    S.mute = skip6
    with ExitStack() as es:
        sqt_box[0] = sb(es, "sqt6", [128, D], F32)
        wpgb = sb(es, "wpgb", [128, 8, D], BF16)
        wpleb = sb(es, "wpleb", [128, 2, D], BF16)
        pTb = sb(es, "pTb", [128, 2, 2048], BF16)
        stg = [sb(es, f"stg6{i}", [128, 2048], F32) for i in range(2)]
        stg_dep = [Dep() for _ in range(2)]
        lnpb = sb(es, "lnpb", [128, D], F32)
        lnfb = sb(es, "lnfb", [128, D], F32)
        hn = sb(es, "hn", [128, D], BF16)
        hnT = sb(es, "hnT", [128, 8, 128], BF16)
        sig = [sb(es, f"sig{i}", [128, 512], F32) for i in range(2)]
        outt = [sb(es, f"outt{i}", [128, D], F32) for i in range(2)]
        outt_dep = [Dep() for _ in range(2)]
        wd = Dep()
        p6 = Dep()
        out_dep = Dep()
        ctr = [0]
        load_w_bf16(wpgb, wpg_d, 8, D, stg, stg_dep, wd, ctr)
        load_w_bf16(wpleb, wple_d, 2, D, stg, stg_dep, wd, ctr)
        for c2 in range(2):
            k = ctr[0] % 2
            ctr[0] += 1
            S.dma(stg[k][:], pTo_d[c2], W=[stg_dep[k]])
            copy_on(cast_eng(), pTb[:, c2, :], stg[k][:], [stg_dep[k]], [wd])
        S.dma(lnpb[:], lnp_d, W=[wd])
        S.dma(lnfb[:], lnf_d, W=[wd])
        for i in range(16):
            ob = i % 2
            rms_rstd(hres[:, i, :], D, 0, [hres_dep[i]])
            S.op("dve", lambda e: e.scalar_tensor_tensor(out=hn[:], in0=hres[:, i, :], scalar=ssc[:, 0:1], in1=lnpb[:],
                                                         op0=ALU.mult, op1=ALU.mult), [hres_dep[i], nrm, wd], [p6])
            for c in range(8):
                S.op("pe", lambda e: e.transpose(out=psb[:, c * 128:(c + 1) * 128], in_=hn[:, c * 128:(c + 1) * 128],
                                                 identity=identb[:]), [p6, cst], [psb_dep])
            S.op("act", lambda e: e.copy(out=hnT[:].rearrange("p c t -> p (c t)"), in_=psb[:, :]), [psb_dep], [p6])
            for half in range(2):
                hs = slice(half * 512, (half + 1) * 512)
                for c in range(8):
                    mm(ps[half][:, :], hnT[:, c, :], wpgb[:, c, hs], c == 0, c == 7, [p6, wd], [psd[half]])
                for c2 in range(2):
                    mm(ps[2 + half][:, :], pTb[:, c2, i * 128:(i + 1) * 128], wpleb[:, c2, hs], c2 == 0, c2 == 1, [wd], [psd[2 + half]])
                S.op("act", lambda e: e.activation(out=sig[half][:], in_=ps[half][:, :], func=AF.Exp, scale=-1.0), [psd[half]], [p6])
                S.op("dve", lambda e: e.tensor_scalar(out=sig[half][:], in0=sig[half][:], scalar1=1.0, scalar2=None, op0=ALU.add), [p6], [p6])
                S.op("dve", lambda e: e.reciprocal(out=sig[half][:], in_=sig[half][:]), [p6], [p6])
                S.op("dve", lambda e: e.tensor_tensor(out=sig[half][:], in0=ps[2 + half][:, :], in1=sig[half][:], op=ALU.mult),
                     [psd[2 + half], p6], [p6])
                S.op("dve", lambda e: e.tensor_tensor(out=hres[:, i, hs], in0=sig[half][:], in1=hres[:, i, hs], op=ALU.add),
                     [p6, hres_dep[i], nrm], [hres_dep[i]])
            rms_rstd(hres[:, i, :], D, 1, [hres_dep[i]])
            S.op("dve", lambda e: e.scalar_tensor_tensor(out=outt[ob][:], in0=hres[:, i, :], scalar=ssc[:, 1:2], in1=lnfb[:],
                                                         op0=ALU.mult, op1=ALU.mult), [hres_dep[i], nrm, wd], [outt_dep[ob]])
            S.dma(out_d[i * 128:(i + 1) * 128, :], outt[ob][:], R=[outt_dep[ob]], W=[out_dep])
        S.barrier()
    es_tail.close()

    if stage <= 3:
        dbg_f = nc.dram_tensor("dbg_ff", [128, 64 * 8 + 16 * 24], F32, kind="ExternalOutput").ap()
        S.dma(dbg_f[:, 0:512], ffall[:].rearrange("p a b -> p (a b)"))
        S.dma(dbg_f[:, 512:896], gown[:].rearrange("p a b -> p (a b)"))
        S.barrier()
        es_all.close()
        return nc, S

    es_all.close()
    return nc, S


def own_tokens(j):
    return np.concatenate([np.arange(512 * i + 128 * j, 512 * i + 128 * j + 128) for i in range(16)])


def const_tables(j):
    p = np.arange(128)
    c = {}
    c["identb"] = _bf(np.eye(128, dtype=np.float32))
    c["identf"] = np.eye(128, dtype=np.float32)
    c["trif"] = (p[:, None] <= p[None, :]).astype(np.float32)
    sl = p[:, None]
    tl = p[None, :]
    dm = np.zeros((128, 4, 128), np.float32)
    for kk in range(4):
        dist = 128 * (j - kk) + tl - sl
        dm[:, kk, :] = np.where(dist >= 0, 0.0, NEGM)
    c["diagm"] = _bf(dm)
    wm = np.zeros((128, 8, 128), np.float32)
    for wk in range(8):
        dist = 128 * (j + 4 - wk) + tl - sl
        wm[:, wk, :] = np.where((dist >= 0) & (dist < 512), 0.0, NEGM)
    c["winm"] = _bf(wm)
    cm = np.zeros((128, 5, 128), np.float32)
    for dd in range(5):
        d = dd - 4
        cond = (512 * d + 16 * sl - tl - 128 * j + 31) <= 0
        cm[:, dd, :] = np.where(cond, 0.0, NEGM)
    c["cmask"] = _bf(cm)
    slopes = np.exp2(-8.0 * np.arange(1, 9, dtype=np.float32) / 8).astype(np.float32)
    rel = np.arange(64)
    ab = slopes[None, :, None] * (p[:, None, None] - 127 - 128 * (rel[None, None, :] + j - 3))
    c["ab"] = np.ascontiguousarray(ab[:, :, ::-1]).astype(np.float32)
    dd = np.arange(16) - 15
    cab = slopes[None, :, None] * (16 * p[:, None, None] + 512 * dd[None, None, :] - 128 * j - 96)
    c["cab"] = cab.astype(np.float32)
    n = np.arange(128)
    selA = np.zeros((128, 16, 128), np.float32)
    selB = np.zeros((128, 16, 128), np.float32)
    for i in range(16):
        cur = (512 * i + 128 * j + p) // 64
        valid = n[None, :] <= cur[:, None]
        forced = valid & ((n[None, :] == 0) | (n[None, :] == cur[:, None]) | (n[None, :] == cur[:, None] - 1))
        selA[:, i, :] = (valid & ~forced)
        selB[:, i, :] = np.where(forced, 1e9, np.where(valid, 0.0, -1.0))
    c["selA"] = _bf(selA)
    c["selB"] = _bf(selB)
    ws = np.zeros((128, 16, 64), np.float32)
    for i in range(16):
        ws[:, i, :] = (np.arange(64)[None, :] <= 4 * i + j)
    c["wsel"] = ws
    s = np.arange(T)
    c["Rexp"] = _bf((n[:, None] == (s[None, :] // 64)).astype(np.float32))
    cc = np.arange(512)
    ov = ((cc[:, None] * 16 < n[None, :] * 64 + 64) & (cc[:, None] * 16 + 31 >= n[None, :] * 64)).astype(np.float32)
    ov[511, :] = 0.0
    c["ovl"] = _bf(ov.reshape(4, 128, 128).transpose(1, 0, 2))
    c["iotac"] = np.ascontiguousarray(np.broadcast_to(np.arange(CAP, dtype=np.float32)[None, :], (128, CAP)))
    return c


def make_in_maps(x, p, ln1, w_in, b_fg, w_cmp1_k, w_cmp2_k, pe_cmp_k, w_cmp1_v, w_cmp2_v, pe_cmp_v, gn_nsa, gn_fox,
                 w_out, ln2, w_router, b_router, w_up, b_up, w_down, b_down, ln_ple, w_ple, w_ple_gate, ln_f, ne=NE):
    f = lambda a: np.ascontiguousarray(np.asarray(a, dtype=np.float32))
    x = f(x); p = f(p); w = f(w_in)[0]
    q_n = w[:, 0:512]; k_c = w[:, 512:640]; v_c = w[:, 640:768]; k_s = w[:, 768:896]; v_s = w[:, 896:1024]
    k_w = w[:, 1024:1152]; v_w = w[:, 1152:1280]; g_n = w[:, 1280:1304]; q_f = w[:, 1304:1816]
    k_f = w[:, 1816:2328]; v_f = w[:, 2328:2840]; f_f = w[:, 2840:2848]
    bc = lambda v: f(np.broadcast_to(np.asarray(v, np.float32).reshape(1, -1), (128, np.asarray(v).size)))
    shared = {
        "wA": f(np.concatenate([k_f, k_s, k_w, k_c, v_c], 1)),
        "wB": f(np.concatenate([v_f, v_s, v_w, f_f, g_n], 1)),
        "wQ": f(np.concatenate([q_n, q_f], 1)),
        "ln1c": f(np.asarray(ln1, np.float32)[0].reshape(8, 128).T),
        "bfg": bc(np.asarray(b_fg)[0]),
        "gnb": bc(np.concatenate([np.asarray(gn_nsa)[0], np.asarray(gn_fox)[0]])),
        "wout": f(w_out)[0], "ln2b": bc(np.asarray(ln2)[0]), "wr": f(w_router)[0], "brb": bc(np.asarray(b_router)[0]),
        "wup": f(np.asarray(w_up)[0, :ne]), "wdn": f(np.asarray(w_down)[0, :ne]), "bdn": f(np.asarray(b_down)[0]),
        "bupc": f(np.asarray(b_up, np.float32)[0].reshape(NE, 16, 128).transpose(2, 0, 1)),
        "lnpb": bc(np.asarray(ln_ple)[0]), "wple": f(w_ple)[0], "wpg": f(w_ple_gate)[0], "lnfb": bc(np.asarray(ln_f)),
    }
    for nm, w1, w2, pe in (("k", w_cmp1_k, w_cmp2_k, pe_cmp_k), ("v", w_cmp1_v, w_cmp2_v, pe_cmp_v)):
        w1r = np.asarray(w1, np.float32)[0].reshape(32, 64, 128).transpose(1, 0, 2)
        shared["w1" + nm] = f(np.concatenate([w1r, w1r], 0))
        peT = np.asarray(pe, np.float32)[0].T
        peT = np.concatenate([peT, peT], 0)
        shared["pe" + nm] = f(np.stack([peT, peT], -1))
    w2k = np.asarray(w_cmp2_k, np.float32)[0]
    shared["w2k"] = f(np.concatenate([w2k, w2k], 1))
    shared["w2v"] = f(np.asarray(w_cmp2_v, np.float32)[0])
    maps = []
    for c in range(NCORES):
        b, j = c // 4, c % 4
        tok = own_tokens(j)
        m = dict(shared)
        m["xT"] = f(x[b].T.reshape(8, 128, T))
        m["xTo"] = f(x[b][tok].T.reshape(8, 128, 2048))
        m["xo"] = f(x[b][tok])
        m["pTo"] = f(p[0, b][tok].T.reshape(2, 128, 2048))
        m.update(const_tables(j))
        maps.append(m)
    return maps


_CACHE = {}


def kernel(**inputs):
    maps = make_in_maps(**inputs)
    if "nc" not in _CACHE:
        _CACHE["nc"] = build_program()[0]
    nc = _CACHE["nc"]
    res = run_bass_kernel_spmd(nc, maps, core_ids=list(range(NCORES)))
    out = np.zeros((2, T, D), np.float32)
    for c in range(NCORES):
        b, j = c // 4, c % 4
        out[b, own_tokens(j)] = np.asarray(res.results[c]["out"], np.float32).reshape(2048, D)
    return out
```

```python
from contextlib import ExitStack
import numpy as np
import ml_dtypes
import concourse.bass as bass
import concourse.mybir as mybir
from concourse.bass_utils import run_bass_kernel_spmd

F32 = mybir.dt.float32
BF16 = mybir.dt.bfloat16
AF = mybir.ActivationFunctionType
ALU = mybir.AluOpType
AX = mybir.AxisListType

NCORES = 8
T = 8192
D = 1024
NEGM = -30000.0
EPS = 1e-6
NE = 32
MOE_EXPERTS = 32
CAP = 512


class Dep:
    __slots__ = ("w", "r")

    def __init__(self):
        self.w = None
        self.r = []


class Sched:
    ROLL = 30000

    def __init__(self, nc, n_dma=40):
        self.nc = nc
        self.E = {"pe": nc.tensor, "act": nc.scalar, "dve": nc.vector, "pool": nc.gpsimd, "sp": nc.sync}
        self.csem = {}
        self.cnt = {}
        self.nsem = 0
        for k in ("pe", "act", "dve", "pool"):
            self._new_csem(k)
        self.seen = {k: {} for k in self.E}
        self.dsem = [nc.alloc_semaphore(name=f"dq{i}") for i in range(n_dma)]
        self.dval = [0] * n_dma
        self.dnext = 0
        self.mute = False
        self.ninst = 0

    def _new_csem(self, k):
        self.csem[k] = self.nc.alloc_semaphore(name=f"c{k}{self.nsem}")
        self.nsem += 1
        self.cnt[k] = 0

    def _collect(self, e, R, W):
        evs = []
        for d in R:
            if d.w is not None:
                evs.append(d.w)
        for d in W:
            if d.w is not None:
                evs.append(d.w)
            evs.extend(d.r)
        return evs

    def _wait(self, e, evs):
        eng = self.E[e]
        seen = self.seen[e]
        need = {}
        for (s, v, src) in evs:
            if src == "pe" and e == "pe":
                continue
            if src == e and s is self.csem.get(e) and self.cnt[e] - v >= 3:
                continue
            key = s.num
            if seen.get(key, 0) >= v:
                continue
            if key not in need or need[key][1] < v:
                need[key] = (s, v)
        for key, (s, v) in need.items():
            eng.wait_ge(s, v)
            seen[key] = v
            self.ninst += 1

    def _mark(self, ev, R, W):
        for d in R:
            d.r.append(ev)
            if len(d.r) > 64:
                d.r = d.r[-64:]
        for d in W:
            d.w = ev
            d.r = []

    def op(self, e, fn, R=(), W=()):
        if self.mute:
            return None
        self._wait(e, self._collect(e, R, W))
        ins = fn(self.E[e])
        if self.cnt[e] >= self.ROLL:
            self._new_csem(e)
        self.cnt[e] += 1
        ins.then_inc(self.csem[e], 1)
        ev = (self.csem[e], self.cnt[e], e)
        self._mark(ev, R, W)
        self.ninst += 1
        return ev

    def dma(self, out, in_, R=(), W=(), e="sp"):
        if self.mute:
            return None
        k = self.dnext
        self.dnext = (k + 1) % len(self.dsem)
        if self.dval[k] >= self.ROLL:
            self._wait(e, [(self.dsem[k], self.dval[k], "dma")])
            self.dsem[k] = self.nc.alloc_semaphore(name=f"dq{k}_{self.nsem}")
            self.nsem += 1
            self.dval[k] = 0
        s = self.dsem[k]
        evs = self._collect(e, R, W)
        if self.dval[k] > 0:
            evs.append((s, self.dval[k], "dma"))
        self._wait(e, evs)
        self.E[e].dma_start(out=out, in_=in_).then_inc(s, 16)
        self.dval[k] += 16
        ev = (s, self.dval[k], "dma")
        self._mark(ev, R, W)
        self.ninst += 1
        return ev

    def barrier(self):
        evs = [(self.csem[k], self.cnt[k], k + "_b") for k in self.csem if self.cnt[k] > 0]
        evs += [(self.dsem[k], self.dval[k], "dma") for k in range(len(self.dsem)) if self.dval[k] > 0]
        for e in self.E:
            self._wait(e, [x for x in evs])


def _bf(a):
    return np.ascontiguousarray(a).astype(ml_dtypes.bfloat16)


def build_program(stage=99, n_experts=MOE_EXPERTS, skip123=False, p4stop=99, nblk4=16, skip5=False, skip6=False):
    nc = bass.Bass("TRN2", target_bir_lowering=False)
    S = Sched(nc)

    def din(name, shape, dt=F32):
        return nc.dram_tensor(name, list(shape), dt, kind="ExternalInput").ap()

    dbg = stage < 99

    def dscr(name, shape, dt):
        return nc.dram_tensor(name, list(shape), dt, kind=("ExternalOutput" if dbg else "Internal")).ap()

    xT_d = din("xT", [8, 128, T])
    xTo_d = din("xTo", [8, 128, 2048])
    xo_d = din("xo", [2048, D])
    pTo_d = din("pTo", [2, 128, 2048])
    wA_d = din("wA", [D, 1024])
    wB_d = din("wB", [D, 800])
    wQ_d = din("wQ", [D, 1024])
    ln1c_d = din("ln1c", [128, 8])
    bfg_d = din("bfg", [128, 8])
    w1k_d = din("w1k", [128, 32, 128])
    w1v_d = din("w1v", [128, 32, 128])
    w2k_d = din("w2k", [128, 128])
    w2v_d = din("w2v", [128, 64])
    pek_d = din("pek", [128, 32, 2])
    pev_d = din("pev", [128, 32, 2])
    gn_d = din("gnb", [128, 1024])
    wout_d = din("wout", [D, D])
    ln2_d = din("ln2b", [128, D])
    wr_d = din("wr", [D, 32])
    br_d = din("brb", [128, 32])
    wup_d = din("wup", [n_experts, D, 2048])
    bup_d = din("bupc", [128, NE, 16])
    wdn_d = din("wdn", [n_experts, D, D])
    bdn_d = din("bdn", [NE, D])
    lnp_d = din("lnpb", [128, D])
    wple_d = din("wple", [256, D])
    wpg_d = din("wpg", [D, D])
    lnf_d = din("lnfb", [128, D])
    identb_d = din("identb", [128, 128], BF16)
    identf_d = din("identf", [128, 128])
    tri_d = din("trif", [128, 128])
    diagm_d = din("diagm", [128, 4, 128], BF16)
    winm_d = din("winm", [128, 8, 128], BF16)
    cmask_d = din("cmask", [128, 5, 128], BF16)
    ab_d = din("ab", [128, 8, 64])
    cab_d = din("cab", [128, 8, 16])
    selA_d = din("selA", [128, 16, 128], BF16)
    selB_d = din("selB", [128, 16, 128], BF16)
    wsel_d = din("wsel", [128, 16, 64])
    R_d = din("Rexp", [128, T], BF16)
    ovl_d = din("ovl", [128, 4, 128], BF16)
    iota_d = din("iotac", [128, CAP])

    out_d = nc.dram_tensor("out", [2048, D], F32, kind="ExternalOutput").ap()

    fmS = dscr("fmS", [8, 128, T], BF16)
    tmS = dscr("tmS", [T, 780], BF16)
    qS = dscr("qS", [16, 16, 128, 128], BF16)
    fmS_dep = [Dep() for _ in range(8)]
    tmS_dep = Dep()
    qS_dep = Dep()

    es_all = ExitStack()

    def sb(es, name, shape, dt):
        return es.enter_context(nc.sbuf_tensor("s_" + name, list(shape), dt))

    ps = [es_all.enter_context(nc.psum_tensor(f"ps{i}", [128, 512], F32)) for i in range(7)]
    psd = [Dep() for _ in range(7)]
    psb = es_all.enter_context(nc.psum_tensor("psb", [128, 1024], BF16))
    psb_dep = Dep()

    identb = sb(es_all, "identb", [128, 128], BF16)
    identf = sb(es_all, "identf", [128, 128], F32)
    trif = sb(es_all, "trif", [128, 128], F32)
    onesb = sb(es_all, "onesb", [128, 128], BF16)
    onesf = sb(es_all, "onesf", [128, 128], F32)
    epsc = sb(es_all, "epsc", [128, 1], F32)
    onec = sb(es_all, "onec", [128, 1], F32)
    cst = Dep()
    S.dma(identb[:], identb_d, W=[cst])
    S.dma(identf[:], identf_d, W=[cst])
    S.dma(trif[:], tri_d, W=[cst])
    S.op("dve", lambda e: e.memset(onesb[:], 1.0), W=[cst])
    S.op("dve", lambda e: e.memset(onesf[:], 1.0), W=[cst])
    S.op("dve", lambda e: e.memset(epsc[:], EPS), W=[cst])
    S.op("dve", lambda e: e.memset(onec[:], 1.0), W=[cst])

    es_attn = ExitStack()
    ffall = sb(es_attn, "ffall", [128, 64, 8], F32)
    ffall_dep = Dep()
    gown = sb(es_attn, "gown", [128, 16, 24], F32)
    gown_dep = Dep()

    rr = {"cast": 0, "evac": 0}

    def cast_eng():
        rr["cast"] += 1
        return ("act", "dve", "pool")[rr["cast"] % 3]

    def copy_on(e, out, in_, R, W):
        if e == "act":
            S.op("act", lambda g: g.copy(out=out, in_=in_), R, W)
        elif e == "dve":
            S.op("dve", lambda g: g.tensor_copy(out=out, in_=in_), R, W)
        else:
            S.op("pool", lambda g: g.tensor_copy(out=out, in_=in_), R, W)

    def evac_eng():
        rr["evac"] += 1
        return ("act", "dve")[rr["evac"] % 2]

    def mm(out, lhsT, rhs, start, stop, R, W):
        S.op("pe", lambda g: g.matmul(out, lhsT=lhsT, rhs=rhs, start=start, stop=stop), R, W)

    pctr = [0]
    LAG = 2

    def run_pipe(steps, LAG=3):
        n = len(steps)
        for k in range(n + LAG):
            if k < n:
                steps[k][0]()
            if k - LAG >= 0:
                steps[k - LAG][1]()

    S.mute = skip123
    with ExitStack() as es:
        wA = sb(es, "wA", [128, 8, 1024], BF16)
        wB = sb(es, "wB", [128, 8, 800], BF16)
        wQz = sb(es, "wQz", [128, 8, 16, 128], BF16)
        stg = [sb(es, f"stg{i}", [128, 1024], F32) for i in range(2)]
        stg_dep = [Dep() for _ in range(2)]
        ln1c = sb(es, "ln1c", [128, 8], F32)
        xt = [sb(es, f"xt{i}", [128, 8, 512], F32) for i in range(2)]
        xt_dep = [Dep() for _ in range(2)]
        sq = sb(es, "sq", [128, 8, 512], BF16)
        sq_dep = Dep()
        rstd = sb(es, "rstd", [128, 512], F32)
        rstd_dep = Dep()
        uT = sb(es, "uT", [128, 8, 512], BF16)
        uT_dep = Dep()
        fmo = [sb(es, f"fmo{i}", [128, 8, 512], BF16) for i in range(2)]
        fmo_dep = [Dep() for _ in range(2)]
        tmv = [sb(es, f"tmv{i}", [128, 4, 780], BF16) for i in range(2)]
        tmv_dep = [Dep() for _ in range(2)]
        qo = [sb(es, f"qo{i}", [128, 4, 16, 128], BF16) for i in range(2)]
        qo_dep = [Dep() for _ in range(2)]
        w_dep = Dep()

        S.dma(ln1c[:], ln1c_d, W=[w_dep])
        S.op("pool", lambda e: e.memset(wQz[:], 0.0), W=[w_dep])
        for k in range(2):
            S.op("pool", lambda e: e.memset(tmv[k][:], 1.0), W=[tmv_dep[k]])
        sc = 0
        for dc in range(8):
            for (src, dst, ncol) in ((wA_d, wA, 1024), (wB_d, wB, 800)):
                k = sc % 2
                sc += 1
                S.dma(stg[k][:, 0:ncol], src[dc * 128:(dc + 1) * 128, :], W=[stg_dep[k]])
                copy_on(cast_eng(), dst[:, dc, :], stg[k][:, 0:ncol], [stg_dep[k]], [w_dep])
            k = sc % 2
            sc += 1
            S.dma(stg[k][:, :], wQ_d[dc * 128:(dc + 1) * 128, :], W=[stg_dep[k]])
            copy_on(cast_eng(), wQz[:, dc, 0:4, 0:64],
                    stg[k][:, 0:256].rearrange("p (h e) -> p h e", e=64), [stg_dep[k]], [w_dep])
            copy_on(cast_eng(), wQz[:, dc, 4:8, 64:128],
                    stg[k][:, 256:512].rearrange("p (h e) -> p h e", e=64), [stg_dep[k]], [w_dep])
            fx = stg[k][:, 512:1024].rearrange("p (h two e) -> p h two e", two=2, e=64)
            wz = wQz[:, dc, 8:16, :].rearrange("p (h two) e -> p h two e", two=2)
            copy_on(cast_eng(), wz[:, :, 0, 0:64], fx[:, :, 0, :], [stg_dep[k]], [w_dep])
            copy_on(cast_eng(), wz[:, :, 1, 64:128], fx[:, :, 1, :], [stg_dep[k]], [w_dep])

        def norm_tile(src_ap, k):
            S.dma(xt[k][:], src_ap, W=[xt_dep[k]])
            S.op("act", lambda e: e.activation(out=sq[:], in_=xt[k][:], func=AF.Square), [xt_dep[k]], [sq_dep])
            for dc in range(8):
                mm(ps[0][:, :], onesb[:], sq[:, dc, :], dc == 0, dc == 7, [sq_dep, cst], [psd[0]])
            S.op("act", lambda e: e.activation(out=rstd[:], in_=ps[0][:, :], func=AF.Sqrt,
                                               bias=epsc[:, 0:1], scale=1.0 / D), [psd[0], cst], [rstd_dep])
            S.op("dve", lambda e: e.reciprocal(out=rstd[:], in_=rstd[:]), [rstd_dep], [rstd_dep])
            for dc in range(8):
                S.op("dve", lambda e: e.scalar_tensor_tensor(
                    out=uT[:, dc, :], in0=xt[k][:, dc, :], scalar=ln1c[:, dc:dc + 1], in1=rstd[:],
                    op0=ALU.mult, op1=ALU.mult), [xt_dep[k], rstd_dep, w_dep], [uT_dep])

        xT_v = xT_d.rearrange("c p s -> p c s")
        xTo_v = xTo_d.rearrange("c p s -> p c s")
        fmS_v = fmS.rearrange("o p s -> p o s")
        for Tt in range(16):
            k = Tt % 2
            norm_tile(xT_v[:, :, Tt * 512:(Tt + 1) * 512], k)
            for oc in range(8):
                b = 1 + oc % 2
                for dc in range(8):
                    mm(ps[b][:, :], wA[:, dc, oc * 128:(oc + 1) * 128], uT[:, dc, :], dc == 0, dc == 7,
                       [uT_dep, w_dep], [psd[b]])
                copy_on(evac_eng(), fmo[k][:, oc, :], ps[b][:, :], [psd[b]], [fmo_dep[k]])
            S.dma(fmS_v[:, :, Tt * 512:(Tt + 1) * 512], fmo[k][:], R=[fmo_dep[k]], W=fmS_dep)
            for sub in range(4):
                bA = 3 + (sub % 2) * 2
                bB = bA + 1
                for dc in range(8):
                    mm(ps[bA][:, 0:512], uT[:, dc, sub * 128:(sub + 1) * 128], wB[:, dc, 0:512], dc == 0, dc == 7,
                       [uT_dep, w_dep], [psd[bA]])
                for dc in range(8):
                    mm(ps[bB][:, 0:288], uT[:, dc, sub * 128:(sub + 1) * 128], wB[:, dc, 512:800], dc == 0, dc == 7,
                       [uT_dep, w_dep], [psd[bB]])
                copy_on("act", tmv[k][:, sub, 0:520].rearrange("p (h e) -> p h e", e=65)[:, :, 0:64],
                        ps[bA][:, 0:512].rearrange("p (h e) -> p h e", e=64), [psd[bA]], [tmv_dep[k]])
                copy_on("dve", tmv[k][:, sub, 520:780].rearrange("p (h e) -> p h e", e=65)[:, :, 0:64],
                        ps[bB][:, 0:256].rearrange("p (h e) -> p h e", e=64), [psd[bB]], [tmv_dep[k]])
                copy_on("dve", ffall[:, Tt * 4 + sub, :], ps[bB][:, 256:264], [psd[bB]], [ffall_dep])
            S.dma(tmS[Tt * 512:(Tt + 1) * 512, :].rearrange("(s p) c -> p s c", p=128), tmv[k][:],
                  R=[tmv_dep[k]], W=[tmS_dep])

        qS_v = qS.rearrange("i h p t -> i p h t")
        for T4 in range(4):
            norm_tile(xTo_v[:, :, T4 * 512:(T4 + 1) * 512], T4 % 2)
            k = T4 % 2
            for hd in range(16):
                b = 1 + hd % 2
                for dc in range(8):
                    mm(ps[b][:, :], wQz[:, dc, hd, :], uT[:, dc, :], dc == 0, dc == 7, [uT_dep, w_dep], [psd[b]])
                qdst = qo[k][:, :, hd, :]
                qsrc = ps[b][:, :].rearrange("p (b t) -> p b t", t=128)
                if hd % 2 == 0:
                    S.op("act", lambda e: e.activation(out=qdst, in_=qsrc, func=AF.Copy, scale=0.125), [psd[b]], [qo_dep[k]])
                else:
                    S.op("dve", lambda e: e.tensor_scalar(out=qdst, in0=qsrc, scalar1=0.125, scalar2=None, op0=ALU.mult),
                         [psd[b]], [qo_dep[k]])
            for bi in range(4):
                i = T4 * 4 + bi
                for dc in range(8):
                    mm(ps[3][:, 0:24], uT[:, dc, bi * 128:(bi + 1) * 128], wB[:, dc, 776:800], dc == 0, dc == 7,
                       [uT_dep, w_dep], [psd[3]])
                copy_on("dve", gown[:, i, :], ps[3][:, 0:24], [psd[3]], [gown_dep])
                S.dma(qS_v[i], qo[k][:, bi, :, :], R=[qo_dep[k]], W=[qS_dep])
        S.barrier()

    if stage <= 1:
        dbg_f = nc.dram_tensor("dbg_ff", [128, 64 * 8 + 16 * 24], F32, kind="ExternalOutput").ap()
        S.dma(dbg_f[:, 0:512], ffall[:].rearrange("p a b -> p (a b)"))
        S.dma(dbg_f[:, 512:896], gown[:].rearrange("p a b -> p (a b)"))
        S.barrier()
        es_attn.close()
        es_all.close()
        return nc, S

    mixS = dscr("mixS", [2048, D], BF16)
    mixS_dep = Dep()
    diagm = sb(es_attn, "diagm", [128, 4, 128], BF16)
    S.dma(diagm[:], diagm_d, W=[cst])
    pT = [sb(es_attn, f"pT{i}", [128, 512], BF16) for i in range(4)]
    pT_dep = [Dep() for _ in range(4)]
    zc = [sb(es_attn, f"zc{i}", [128, 4], F32) for i in range(2)]
    zc_dep = [Dep() for _ in range(2)]

    with ExitStack() as es:
        bfg = sb(es, "bfg", [128, 8], F32)
        wsel = sb(es, "wsel", [128, 16, 64], F32)
        lsp = sb(es, "lsp", [128, 64, 8], F32)
        cpcol = sb(es, "cpcol", [128, 64, 8], F32)
        tot = sb(es, "tot", [128, 64, 8], F32)
        pre = sb(es, "pre", [128, 64, 8], F32)
        cpref = sb(es, "cpref", [128, 16, 8], F32)
        tmpw = sb(es, "tmpw", [128, 8, 64], F32)
        cd = Dep()
        S.dma(bfg[:], bfg_d, W=[cd])
        S.dma(wsel[:], wsel_d, W=[cd])
        S.op("dve", lambda e: e.tensor_tensor(out=lsp[:], in0=ffall[:], in1=bfg[:, :].unsqueeze(1).to_broadcast([128, 64, 8]),
                                              op=ALU.add), [ffall_dep, cd], [cd])
        S.op("act", lambda e: e.activation(out=lsp[:], in_=lsp[:], func=AF.Exp, scale=-1.0), [cd], [cd])
        S.op("act", lambda e: e.activation(out=lsp[:], in_=lsp[:], func=AF.Ln, bias=onec[:, 0:1], scale=1.0), [cd, cst], [cd])
        lflat = lsp[:].rearrange("p a b -> p (a b)")
        mm(ps[0][:, :], trif[:], lflat, True, True, [cd, cst], [psd[0]])
        mm(ps[1][:, :], onesf[:], lflat, True, True, [cd, cst], [psd[1]])
        S.op("dve", lambda e: e.tensor_copy(out=tot[:].rearrange("p a b -> p (a b)"), in_=ps[1][:, :]), [psd[1]], [cd])
        S.op("dve", lambda e: e.memset(pre[:, 0, :], 0.0), [], [cd])
        for k in range(1, 64):
            S.op("dve", lambda e: e.tensor_tensor(out=pre[:, k, :], in0=pre[:, k - 1, :], in1=tot[:, k - 1, :], op=ALU.add), [cd], [cd])
        S.op("dve", lambda e: e.tensor_tensor(out=cpcol[:].rearrange("p a b -> p (a b)"), in0=ps[0][:, :],
                                              in1=pre[:].rearrange("p a b -> p (a b)"), op=ALU.add), [psd[0], cd], [cd])
        for i in range(16):
            S.op("dve", lambda e: e.tensor_tensor(out=tmpw[:], in0=tot[:].rearrange("p k h -> p h k"),
                                                  in1=wsel[:, i, :].unsqueeze(1).to_broadcast([128, 8, 64]), op=ALU.mult), [cd], [cd])
            S.op("dve", lambda e: e.reduce_sum(out=cpref[:, i, :], in_=tmpw[:], axis=AX.X), [cd], [cd])

        kT = [sb(es, f"kT{i}", [128, T], BF16) for i in range(2)]
        vP = [sb(es, f"vP{i}", [128, 64, 130], BF16) for i in range(2)]
        qP = [sb(es, f"qP{i}", [128, 16, 2, 128], BF16) for i in range(2)]
        kvq_dep = [Dep() for _ in range(2)]
        wF = [sb(es, f"wF{i}", [128, 64], F32) for i in range(2)]
        wF_dep = [Dep() for _ in range(2)]
        Vp = [sb(es, f"Vp{i}", [128, 64, 65], BF16) for i in range(2)]
        Vp_dep = [Dep() for _ in range(2)]
        mo = [sb(es, f"mo{i}", [128, 128], BF16) for i in range(2)]
        mo_dep = [Dep() for _ in range(2)]
        tmS_v = tmS.rearrange("(k p) c -> p k c", p=128)
        qS_p = qS.rearrange("i h p t -> p i h t")
        items = [(hp, i, hh) for hp in range(4) for i in range(16) for hh in range(2)]

        def fox_prep(n):
            hp, i, hh = items[n]
            kb = hp % 2
            bb = n % 2
            h = 2 * hp + hh
            nk = 4 * i + 4
            if i == 0 and hh == 0:
                S.dma(kT[kb][:], fmS[hp], R=[fmS_dep[hp]], W=[kvq_dep[kb]])
                for q4 in range(4):
                    S.dma(vP[kb][:, q4 * 16:(q4 + 1) * 16, :], tmS_v[:, q4 * 16:(q4 + 1) * 16, hp * 130:(hp + 1) * 130],
                          R=[tmS_dep], W=[kvq_dep[kb]])
                for q2 in range(2):
                    S.dma(qP[kb][:, :, q2, :], qS_p[:, :, 8 + 2 * hp + q2, :], R=[qS_dep], W=[kvq_dep[kb]])
            S.op("dve", lambda e: e.tensor_scalar(out=wF[bb][:, 0:nk], in0=cpcol[:, 0:nk, h], scalar1=cpref[:, i, h:h + 1],
                                                  scalar2=0.0, op0=ALU.subtract, op1=ALU.min), [cd], [wF_dep[bb]])
            S.op("act", lambda e: e.activation(out=wF[bb][:, 0:nk], in_=wF[bb][:, 0:nk], func=AF.Exp), [wF_dep[bb]], [wF_dep[bb]])
            eng = "dve" if n % 2 == 0 else "pool"
            S.op(eng, lambda e: e.tensor_tensor(out=Vp[bb][:, 0:nk, :], in0=vP[kb][:, 0:nk, hh * 65:(hh + 1) * 65],
                                                in1=wF[bb][:, 0:nk].unsqueeze(2).to_broadcast([128, nk, 65]), op=ALU.mult),
                 [kvq_dep[kb], wF_dep[bb]], [Vp_dep[bb]])

        def fox_run(n):
            hp, i, hh = items[n]
            kb = hp % 2
            bb = n % 2
            ob = 4 + n % 2
            mb = i % 2
            nk = 4 * i + 4
            steps = []
            for gq in range(nk // 4):
                bank = pctr[0] % 4
                pctr[0] += 1

                def qk(gq=gq, bank=bank):
                    for q in range(4):
                        k = 4 * gq + q
                        diag = k >= 4 * i
                        mm(ps[bank][:, q * 128:(q + 1) * 128], kT[kb][:, k * 128:(k + 1) * 128], qP[kb][:, i, hh, :], True, not diag,
                           [kvq_dep[kb]], [psd[bank]])
                        if diag:
                            mm(ps[bank][:, q * 128:(q + 1) * 128], identb[:], diagm[:, k - 4 * i, :], False, True, [cst], [psd[bank]])
                    S.op("act", lambda e: e.activation(out=pT[bank][:], in_=ps[bank][:, :], func=AF.Exp), [psd[bank]], [pT_dep[bank]])

                def pv(gq=gq, bank=bank):
                    for q in range(4):
                        k = 4 * gq + q
                        mm(ps[ob][:, 0:65], pT[bank][:, q * 128:(q + 1) * 128], Vp[bb][:, k, :], k == 0, k == nk - 1,
                           [pT_dep[bank], Vp_dep[bb]], [psd[ob]])
                steps.append((qk, pv))
            run_pipe(steps)
            z = zc[bb]
            S.op("dve", lambda e: e.tensor_scalar_max(out=z[:, 0:1], in0=ps[ob][:, 64:65], scalar1=1e-30), [psd[ob]], [zc_dep[bb]])
            S.op("dve", lambda e: e.reciprocal(out=z[:, 0:1], in_=z[:, 0:1]), [zc_dep[bb]], [zc_dep[bb]])
            S.op("dve", lambda e: e.tensor_scalar(out=mo[mb][:, hh * 64:(hh + 1) * 64], in0=ps[ob][:, 0:64],
                                                  scalar1=z[:, 0:1], scalar2=None, op0=ALU.mult),
                 [psd[ob], zc_dep[bb]], [mo_dep[mb]])
            if hh == 1:
                S.dma(mixS[i * 128:(i + 1) * 128, 512 + hp * 128:512 + (hp + 1) * 128], mo[mb][:], R=[mo_dep[mb]], W=[mixS_dep])

        fox_prep(0)
        for n in range(len(items)):
            if n + 1 < len(items):
                fox_prep(n + 1)
            fox_run(n)
        S.barrier()

    if stage <= 2:
        es_attn.close()
        es_all.close()
        return nc, S

    with ExitStack() as es:
        kcT = sb(es, "kcT", [128, 512], BF16)
        vc = sb(es, "vc", [128, 4, 130], BF16)
        kc_dep = Dep()
        S.op("pool", lambda e: e.memset(vc[:], 1.0), [], [kc_dep])
        KsT = sb(es, "KsT", [128, T], BF16)
        KwT = sb(es, "KwT", [128, T], BF16)
        Vs = sb(es, "Vs", [128, 64, 130], BF16)
        Vw = sb(es, "Vw", [128, 64, 130], BF16)
        Rx = sb(es, "Rx", [128, T], BF16)
        ovl = sb(es, "ovl", [128, 4, 128], BF16)
        ab = sb(es, "ab", [128, 8, 64], F32)
        cab = sb(es, "cab", [128, 8, 16], F32)
        selA = sb(es, "selA", [128, 16, 128], BF16)
        selB = sb(es, "selB", [128, 16, 128], BF16)
        cmask = sb(es, "cmask", [128, 5, 128], BF16)
        winm = sb(es, "winm", [128, 8, 128], BF16)
        nd = Dep()
        tmS_v = tmS.rearrange("(k p) c -> p k c", p=128)
        S.dma(KsT[:], fmS[4], R=[fmS_dep[4]], W=[nd])
        S.dma(KwT[:], fmS[5], R=[fmS_dep[5]], W=[nd])
        for q4 in range(4):
            S.dma(Vs[:, q4 * 16:(q4 + 1) * 16, :], tmS_v[:, q4 * 16:(q4 + 1) * 16, 520:650], R=[tmS_dep], W=[nd])
            S.dma(Vw[:, q4 * 16:(q4 + 1) * 16, :], tmS_v[:, q4 * 16:(q4 + 1) * 16, 650:780], R=[tmS_dep], W=[nd])
        for (dst, src) in ((Rx, R_d), (ovl, ovl_d), (ab, ab_d), (cab, cab_d), (selA, selA_d), (selB, selB_d),
                           (cmask, cmask_d), (winm, winm_d)):
            S.dma(dst[:], src, W=[nd])
        wab = sb(es, "wab", [128, 8, 64], F32)
        wab_dep = Dep()
        Vsp = [sb(es, f"Vsp{i}", [128, 64, 65], BF16) for i in range(2)]
        Vwp = [sb(es, f"Vwp{i}", [128, 8, 65], BF16) for i in range(2)]
        Vxp_dep = [Dep() for _ in range(2)]
        with ExitStack() as es2:
            kraw = sb(es2, "kraw", [128, T], BF16)
            w1f = sb(es2, "w1f", [128, 32, 128], F32)
            w1b = sb(es2, "w1b", [128, 32, 128], BF16)
            pef = sb(es2, "pef", [128, 32, 2], F32)
            peb = sb(es2, "peb", [128, 32, 2], BF16)
            w2f = sb(es2, "w2f", [128, 128], F32)
            w2b = sb(es2, "w2b", [128, 128], BF16)
            hx = sb(es2, "hx", [128, 512], F32)
            hu = sb(es2, "hu", [128, 512], F32)
            hidT = sb(es2, "hidT", [128, 512], BF16)
            cbias = sb(es2, "cbias", [128, 1], F32)
            cpd = Dep()
            for which in range(2):
                S.dma(kraw[:], fmS[6 + which], R=[fmS_dep[6 + which]], W=[cpd])
                S.dma(w1f[:], (w1k_d, w1v_d)[which], W=[cpd])
                S.dma(pef[:], (pek_d, pev_d)[which], W=[cpd])
                if which == 0:
                    S.dma(w2f[:, :], w2k_d, W=[cpd])
                else:
                    S.dma(w2f[:, 0:64], w2v_d, W=[cpd])
                S.op("dve", lambda e: e.tensor_copy(out=w1b[:], in_=w1f[:]), [cpd], [cpd])
                S.op("dve", lambda e: e.tensor_copy(out=peb[:], in_=pef[:]), [cpd], [cpd])
                S.op("dve", lambda e: e.tensor_copy(out=w2b[:], in_=w2f[:]), [cpd], [cpd])
                for g in range(2):
                    r0, r1 = g * 64, g * 64 + 64
                    for l in range(32):
                        mm(ps[0][:, 0:511], w1b[r0:r1, l, :], kraw[r0:r1, l:l + 16 * 510 + 1:16], l == 0, l == 31, [cpd], [psd[0]])
                    for l in range(32):
                        mm(ps[1][:, 0:2], w1b[r0:r1, l, :], peb[r0:r1, l, :], l == 0, l == 31, [cpd], [psd[1]])
                    S.op("dve", lambda e: e.tensor_copy(out=cbias[:], in_=ps[1][:, 0:1]), [psd[1]], [cpd])
                    S.op("dve", lambda e: e.memset(hx[:], 0.0), [], [cpd])
                    S.op("dve", lambda e: e.tensor_scalar(out=hx[:, 0:511], in0=ps[0][:, 0:511], scalar1=cbias[:, 0:1],
                                                          scalar2=None, op0=ALU.add), [psd[0], cpd], [cpd])
                    S.op("dve", lambda e: e.tensor_tensor(out=hu[:], in0=hx[:], in1=hx[:], op=ALU.mult), [cpd], [cpd])
                    S.op("dve", lambda e: e.tensor_scalar(out=hu[:], in0=hu[:], scalar1=0.044715, scalar2=1.0,
                                                          op0=ALU.mult, op1=ALU.add), [cpd], [cpd])
                    S.op("dve", lambda e: e.tensor_tensor(out=hu[:], in0=hu[:], in1=hx[:], op=ALU.mult), [cpd], [cpd])
                    S.op("act", lambda e: e.activation(out=hu[:], in_=hu[:], func=AF.Exp, scale=-1.5957691216057308), [cpd], [cpd])
                    S.op("dve", lambda e: e.tensor_scalar(out=hu[:], in0=hu[:], scalar1=1.0, scalar2=None, op0=ALU.add), [cpd], [cpd])
                    S.op("dve", lambda e: e.reciprocal(out=hu[:], in_=hu[:]), [cpd], [cpd])
                    S.op("dve", lambda e: e.tensor_tensor(out=hidT[:], in0=hu[:], in1=hx[:], op=ALU.mult), [cpd], [cpd])
                    if which == 0:
                        mm(ps[2][:, 0:512], w2b[:, :], hidT[:], True, True, [cpd], [psd[2]])
                        S.op("dve", lambda e: e.tensor_copy(out=kcT[r0:r1, :], in_=ps[2][r0:r1, 0:512]), [psd[2]], [kc_dep])
                    else:
                        for m in range(4):
                            mm(ps[2][:, m * 64:(m + 1) * 64], hidT[:, m * 128:(m + 1) * 128], w2b[:, 0:64], True, True, [cpd], [psd[2]])
                        S.op("dve", lambda e: e.tensor_copy(out=vc[:, :, g * 65:g * 65 + 64],
                                                            in_=ps[2][:, 0:256].rearrange("p (m e) -> p m e", e=64)), [psd[2]], [kc_dep])
            S.barrier()

        S.op("dve", lambda e: e.tensor_scalar(out=wab[:], in0=ab[:], scalar1=0.0, scalar2=None, op0=ALU.min), [nd], [wab_dep])
        S.op("act", lambda e: e.activation(out=wab[:], in_=wab[:], func=AF.Exp), [wab_dep], [wab_dep])
        qN = [sb(es, f"qN{i}", [128, 8, 128], BF16) for i in range(2)]
        qN_dep = [Dep() for _ in range(2)]
        eC = [sb(es, f"eC{i}", [128, 4, 128], BF16) for i in range(4)]
        eC_dep = [Dep() for _ in range(4)]
        Ocs = sb(es, "Ocs", [128, 4, 64], F32)
        Ocs_dep = Dep()
        rzc = sb(es, "rzc", [128, 4], F32)
        impacc = sb(es, "impacc", [128, 128], F32)
        score = sb(es, "score", [128, 128], F32)
        sc2 = sb(es, "sc2", [128, 128], F32)
        mx8 = sb(es, "mx8", [128, 8], F32)
        mx8b = sb(es, "mx8b", [128, 8], F32)
        MnegB = sb(es, "MnegB", [128, 128], BF16)
        MnegT = sb(es, "MnegT", [128, 128], BF16)
        MnegT_dep = Dep()
        tk = Dep()
        sgate = sb(es, "sgate", [128, 24], F32)
        sg_dep = Dep()
        coef = sb(es, "coef", [128, 4], F32)
        t1 = sb(es, "t1", [128, 64], F32)
        fin = Dep()
        mon = [sb(es, f"mon{i}", [128, 512], BF16) for i in range(2)]
        mon_dep = [Dep() for _ in range(2)]
        qS_p = qS.rearrange("i h p t -> i p h t")
        sctr = 0

        def nsa_prep(nidx):
            r_ = nidx % 4
            g_ = (nidx // 4) % 2
            i_ = nidx // 8
            h_ = 4 * g_ + r_
            vb_ = nidx % 2
            nk_ = 4 * i_ + 4
            k0_ = max(0, 4 * i_ - 4)
            e1, e2 = ("dve", "pool") if nidx % 2 == 0 else ("pool", "dve")
            S.op(e1, lambda e: e.tensor_tensor(out=Vsp[vb_][:, 0:nk_, :], in0=Vs[:, 0:nk_, g_ * 65:(g_ + 1) * 65],
                                               in1=wab[:, h_, 60 - 4 * i_:64].unsqueeze(2).to_broadcast([128, nk_, 65]), op=ALU.mult),
                 [nd, wab_dep], [Vxp_dep[vb_]])
            S.op(e2, lambda e: e.tensor_tensor(out=Vwp[vb_][:, 0:nk_ - k0_, :], in0=Vw[:, k0_:nk_, g_ * 65:(g_ + 1) * 65],
                                               in1=wab[:, h_, 60 - 4 * i_ + k0_:64].unsqueeze(2).to_broadcast([128, nk_ - k0_, 65]), op=ALU.mult),
                 [nd, wab_dep], [Vxp_dep[vb_]])
        for i in range(16):
            qb = i % 2
            S.dma(qN[qb][:], qS_p[i][:, 0:8, :], R=[qS_dep], W=[qN_dep[qb]])
            S.op("act", lambda e: e.activation(out=sgate[:], in_=gown[:, i, :], func=AF.Exp, scale=-1.0), [gown_dep], [sg_dep])
            S.op("dve", lambda e: e.tensor_scalar(out=sgate[:], in0=sgate[:], scalar1=1.0, scalar2=None, op0=ALU.add), [sg_dep], [sg_dep])
            S.op("dve", lambda e: e.reciprocal(out=sgate[:], in_=sgate[:]), [sg_dep], [sg_dep])
            ncm = i // 4 + 1
            nk = 4 * i + 4
            for g in range(2):
                for r in range(4):
                    h = 4 * g + r
                    for m in range(ncm):
                        d = 4 * m - i
                        partial = d >= -4
                        sbk = sctr % 4
                        sctr += 1
                        mm(ps[sbk][:, 0:128], kcT[:, m * 128:(m + 1) * 128], qN[qb][:, h, :], True, not partial,
                           [kc_dep, qN_dep[qb]], [psd[sbk]])
                        if partial:
                            mm(ps[sbk][:, 0:128], identb[:], cmask[:, d + 4, :], False, True, [cst, nd], [psd[sbk]])
                        S.op("act", lambda e: e.activation(out=eC[r][:, m, :], in_=ps[sbk][:, 0:128], func=AF.Exp,
                                                           bias=cab[:, h, d + 15:d + 16], scale=1.0), [psd[sbk], nd], [eC_dep[r]])
                    for m in range(ncm):
                        mm(ps[4][:, 0:65], eC[r][:, m, :], vc[:, m, g * 65:(g + 1) * 65], m == 0, m == ncm - 1,
                           [eC_dep[r], kc_dep], [psd[4]])
                    for m in range(ncm):
                        mm(ps[5][:, 0:128], eC[r][:, m, :], ovl[:, m, :], m == 0, m == ncm - 1, [eC_dep[r], nd], [psd[5]])
                    S.op("dve", lambda e: e.tensor_scalar_max(out=rzc[:, r:r + 1], in0=ps[4][:, 64:65], scalar1=1e-30), [psd[4]], [tk, fin])
                    S.op("dve", lambda e: e.reciprocal(out=rzc[:, r:r + 1], in_=rzc[:, r:r + 1]), [tk], [tk])
                    S.op("dve", lambda e: e.tensor_copy(out=Ocs[:, r, :], in_=ps[4][:, 0:64]), [psd[4]], [Ocs_dep, fin])
                    if r == 0:
                        S.op("dve", lambda e: e.tensor_scalar(out=impacc[:], in0=ps[5][:, 0:128], scalar1=rzc[:, r:r + 1],
                                                              scalar2=None, op0=ALU.mult), [psd[5], tk], [tk])
                    else:
                        S.op("dve", lambda e: e.scalar_tensor_tensor(out=impacc[:], in0=ps[5][:, 0:128], scalar=rzc[:, r:r + 1],
                                                                     in1=impacc[:], op0=ALU.mult, op1=ALU.add), [psd[5], tk], [tk])
                S.op("dve", lambda e: e.tensor_tensor(out=score[:], in0=impacc[:], in1=selA[:, i, :], op=ALU.mult), [tk, nd], [tk])
                S.op("dve", lambda e: e.tensor_tensor(out=score[:], in0=score[:], in1=selB[:, i, :], op=ALU.add), [tk, nd], [tk])
                S.op("dve", lambda e: e.max(out=mx8[:], in_=score[:]), [tk], [tk])
                S.op("dve", lambda e: e.match_replace(out=sc2[:], in_to_replace=mx8[:], in_values=score[:], imm_value=-2.0), [tk], [tk])
                S.op("dve", lambda e: e.max(out=mx8b[:], in_=sc2[:]), [tk], [tk])
                S.op("dve", lambda e: e.tensor_scalar(out=MnegB[:], in0=score[:], scalar1=mx8b[:, 7:8], scalar2=NEGM,
                                                      op0=ALU.is_lt, op1=ALU.mult), [tk], [tk])
                S.op("pe", lambda e: e.transpose(out=psb[:, 0:128], in_=MnegB[:], identity=identb[:]), [tk, cst], [psb_dep])
                S.op("dve", lambda e: e.tensor_copy(out=MnegT[:], in_=psb[:, 0:128]), [psb_dep], [MnegT_dep])
                for r in range(4):
                    h = 4 * g + r
                    osb = 6 if r % 2 == 0 else 5
                    nidx = (i * 2 + g) * 4 + r
                    vb = nidx % 2
                    if nidx == 0:
                        nsa_prep(0)
                    if nidx + 1 < 128:
                        nsa_prep(nidx + 1)
                    k0 = max(0, 4 * i - 4)
                    steps = []
                    for gq in range(nk // 4):
                        bank = pctr[0] % 4
                        pctr[0] += 1

                        def qk(gq=gq, bank=bank, i=i, h=h, qb=qb):
                            for q in range(4):
                                k = 4 * gq + q
                                diag = k >= 4 * i
                                o = ps[bank][:, q * 128:(q + 1) * 128]
                                mm(o, KsT[:, k * 128:(k + 1) * 128], qN[qb][:, h, :], True, False, [nd, qN_dep[qb]], [psd[bank]])
                                mm(o, Rx[:, k * 128:(k + 1) * 128], MnegT[:], False, not diag, [nd, MnegT_dep], [psd[bank]])
                                if diag:
                                    mm(o, identb[:], diagm[:, k - 4 * i, :], False, True, [cst], [psd[bank]])
                            S.op("act", lambda e: e.activation(out=pT[bank][:], in_=ps[bank][:, :], func=AF.Exp), [psd[bank]], [pT_dep[bank]])

                        def pv(gq=gq, bank=bank, osb=osb, nk=nk, vb=vb):
                            for q in range(4):
                                k = 4 * gq + q
                                mm(ps[osb][:, 0:65], pT[bank][:, q * 128:(q + 1) * 128], Vsp[vb][:, k, :], k == 0, k == nk - 1,
                                   [pT_dep[bank], Vxp_dep[vb]], [psd[osb]])
                        steps.append((qk, pv))
                    for gq in range((nk - k0) // 4):
                        bank = pctr[0] % 4
                        pctr[0] += 1

                        def qk(gq=gq, bank=bank, i=i, h=h, qb=qb, k0=k0):
                            for q in range(4):
                                k = k0 + 4 * gq + q
                                wk = k - (4 * i - 4)
                                o = ps[bank][:, q * 128:(q + 1) * 128]
                                mm(o, KwT[:, k * 128:(k + 1) * 128], qN[qb][:, h, :], True, False, [nd, qN_dep[qb]], [psd[bank]])
                                mm(o, identb[:], winm[:, wk, :], False, True, [cst, nd], [psd[bank]])
                            S.op("act", lambda e: e.activation(out=pT[bank][:], in_=ps[bank][:, :], func=AF.Exp), [psd[bank]], [pT_dep[bank]])

                        def pv(gq=gq, bank=bank, k0=k0, nk=nk, vb=vb):
                            for q in range(4):
                                k = k0 + 4 * gq + q
                                mm(ps[4][:, 0:65], pT[bank][:, q * 128:(q + 1) * 128], Vwp[vb][:, k - k0, :], k == k0, k == nk - 1,
                                   [pT_dep[bank], Vxp_dep[vb]], [psd[4]])
                        steps.append((qk, pv))
                    run_pipe(steps)
                    S.op("dve", lambda e: e.tensor_scalar_max(out=coef[:, 1:2], in0=ps[osb][:, 64:65], scalar1=1e-30), [psd[osb]], [fin])
                    S.op("dve", lambda e: e.tensor_scalar_max(out=coef[:, 2:3], in0=ps[4][:, 64:65], scalar1=1e-30), [psd[4]], [fin])
                    S.op("dve", lambda e: e.reciprocal(out=coef[:, 1:3], in_=coef[:, 1:3]), [fin], [fin])
                    S.op("dve", lambda e: e.tensor_copy(out=coef[:, 0:1], in_=rzc[:, r:r + 1]), [fin, tk], [fin])
                    S.op("dve", lambda e: e.tensor_tensor(out=coef[:, 0:3], in0=coef[:, 0:3], in1=sgate[:, 3 * h:3 * h + 3], op=ALU.mult), [fin, sg_dep], [fin])
                    S.op("dve", lambda e: e.tensor_scalar(out=t1[:], in0=Ocs[:, r, :], scalar1=coef[:, 0:1], scalar2=None, op0=ALU.mult), [fin, Ocs_dep], [fin])
                    S.op("dve", lambda e: e.scalar_tensor_tensor(out=t1[:], in0=ps[osb][:, 0:64], scalar=coef[:, 1:2], in1=t1[:],
                                                                 op0=ALU.mult, op1=ALU.add), [fin, psd[osb]], [fin])
                    S.op("dve", lambda e: e.scalar_tensor_tensor(out=mon[qb][:, h * 64:(h + 1) * 64], in0=ps[4][:, 0:64], scalar=coef[:, 2:3],
                                                                 in1=t1[:], op0=ALU.mult, op1=ALU.add), [fin, psd[4]], [mon_dep[qb], fin])
            S.dma(mixS[i * 128:(i + 1) * 128, 0:512], mon[qb][:], R=[mon_dep[qb]], W=[mixS_dep])
        S.barrier()

    if stage <= 3:
        es_attn.close()
        es_all.close()
        return nc, S

    S.mute = False
    es_attn.close()
    es_tail = ExitStack()
    hres = sb(es_tail, "hres", [128, 16, D], F32)
    hres_dep = [Dep() for _ in range(16)]
    xnT = sb(es_tail, "xntok", [128, 16, D], BF16)
    xnT_dep = Dep()
    gate = sb(es_tail, "gate", [128, 16, NE], F32)
    gate_dep = Dep()
    ssc = sb(es_tail, "ssc", [128, 4], F32)
    nrm = Dep()
    sqt_box = [None]

    def rms_rstd(src, n, col, R):
        sqt = sqt_box[0]
        S.op("dve", lambda e: e.tensor_tensor(out=sqt[:, 0:n], in0=src, in1=src, op=ALU.mult), list(R) + [nrm], [nrm])
        S.op("dve", lambda e: e.reduce_sum(out=ssc[:, col:col + 1], in_=sqt[:, 0:n], axis=AX.X), [nrm], [nrm])
        S.op("act", lambda e: e.activation(out=ssc[:, col:col + 1], in_=ssc[:, col:col + 1], func=AF.Sqrt,
                                           bias=epsc[:, 0:1], scale=1.0 / n), [nrm, cst], [nrm])
        S.op("dve", lambda e: e.reciprocal(out=ssc[:, col:col + 1], in_=ssc[:, col:col + 1]), [nrm], [nrm])

    def load_w_bf16(dst, src, nchunk, ncol, stg, stg_dep, wdep, ctr):
        for dc in range(nchunk):
            k = ctr[0] % len(stg)
            ctr[0] += 1
            S.dma(stg[k][:, 0:ncol], src[dc * 128:(dc + 1) * 128, :], W=[stg_dep[k]])
            copy_on(cast_eng(), dst[:, dc, :], stg[k][:, 0:ncol], [stg_dep[k]], [wdep])

    with ExitStack() as es:
        sqt_box[0] = sb(es, "sqt4", [128, D], F32)
        woutb = sb(es, "woutb", [128, 8, D], BF16)
        stg = [sb(es, f"stg4{i}", [128, D], F32) for i in range(2)]
        stg_dep = [Dep() for _ in range(2)]
        gnb = sb(es, "gnb", [128, D], F32)
        ln2b = sb(es, "ln2b", [128, D], F32)
        wrf = sb(es, "wrf", [128, 8, NE], F32)
        brb = sb(es, "brb", [128, NE], F32)
        bdnf = sb(es, "bdnf", [NE, D], F32)
        mixb = [sb(es, f"mixb{i}", [128, D], BF16) for i in range(2)]
        mixb_dep = [Dep() for _ in range(2)]
        mixn = sb(es, "mixn", [128, D], BF16)
        mixT = sb(es, "mixT", [128, 8, 128], BF16)
        xn = sb(es, "xn", [128, D], F32)
        xnTf = sb(es, "xnTf", [128, 8, 128], F32)
        lg = sb(es, "lg", [128, NE], F32)
        ex = sb(es, "ex", [128, NE], F32)
        mxr = sb(es, "mxr", [128, 8], F32)
        gT = sb(es, "gT", [NE, 128], F32)
        wd = Dep()
        p4 = Dep()
        ctr = [0]
        load_w_bf16(woutb, wout_d, 8, D, stg, stg_dep, wd, ctr)
        S.dma(gnb[:], gn_d, W=[wd])
        S.dma(ln2b[:], ln2_d, W=[wd])
        S.dma(wrf[:], wr_d.rearrange("(c p) e -> p c e", p=128), W=[wd])
        S.dma(brb[:], br_d, W=[wd])
        S.dma(bdnf[:], bdn_d, W=[wd])
        def g4(n):
            if p4stop <= n:
                S.mute = True
        for i in range(nblk4):
            mb = i % 2
            S.mute = False
            S.dma(mixb[mb][:], mixS[i * 128:(i + 1) * 128, :], R=[mixS_dep], W=[mixb_dep[mb]])
            S.dma(hres[:, i, :], xo_d[i * 128:(i + 1) * 128, :], W=[hres_dep[i]])
            g4(1)
            for half in range(2):
                hs = slice(half * 512, (half + 1) * 512)
                rms_rstd(mixb[mb][:, hs], 512, half, [mixb_dep[mb]])
                S.op("dve", lambda e: e.scalar_tensor_tensor(out=mixn[:, hs], in0=mixb[mb][:, hs], scalar=ssc[:, half:half + 1],
                                                             in1=gnb[:, hs], op0=ALU.mult, op1=ALU.mult), [mixb_dep[mb], nrm, wd], [p4])
            g4(2)
            for c in range(8):
                S.op("pe", lambda e: e.transpose(out=psb[:, c * 128:(c + 1) * 128], in_=mixn[:, c * 128:(c + 1) * 128],
                                                 identity=identb[:]), [p4, cst], [psb_dep])
            S.op("act", lambda e: e.copy(out=mixT[:].rearrange("p c t -> p (c t)"), in_=psb[:, :]), [psb_dep], [p4])
            g4(3)
            for half in range(2):
                hs = slice(half * 512, (half + 1) * 512)
                for c in range(8):
                    mm(ps[half][:, :], mixT[:, c, :], woutb[:, c, hs], c == 0, c == 7, [p4, wd], [psd[half]])
                S.op("dve", lambda e: e.tensor_tensor(out=hres[:, i, hs], in0=ps[half][:, :], in1=hres[:, i, hs], op=ALU.add),
                     [psd[half], hres_dep[i]], [hres_dep[i]])
            rms_rstd(hres[:, i, :], D, 2, [hres_dep[i]])
            g4(4)
            S.op("dve", lambda e: e.scalar_tensor_tensor(out=xn[:], in0=hres[:, i, :], scalar=ssc[:, 2:3], in1=ln2b[:],
                                                         op0=ALU.mult, op1=ALU.mult), [hres_dep[i], nrm, wd], [p4])
            S.op("pool", lambda e: e.tensor_copy(out=xnT[:, i, :], in_=xn[:]), [p4], [xnT_dep])
            for c in range(8):
                b = 2 + c // 4
                g4(5)
                S.op("pe", lambda e: e.transpose(out=ps[b][:, (c % 4) * 128:(c % 4 + 1) * 128], in_=xn[:, c * 128:(c + 1) * 128],
                                                 identity=identf[:]), [p4, cst], [psd[b]])
            for b2 in range(2):
                S.op("act", lambda e: e.copy(out=xnTf[:, b2 * 4:(b2 + 1) * 4, :], in_=ps[2 + b2][:, :].rearrange("p (c t) -> p c t", t=128)),
                     [psd[2 + b2]], [p4])
            for c in range(8):
                mm(ps[4][:, 0:NE], xnTf[:, c, :], wrf[:, c, :], c == 0, c == 7, [p4, wd], [psd[4]])
            g4(7)
            S.op("dve", lambda e: e.tensor_tensor(out=lg[:], in0=ps[4][:, 0:NE], in1=brb[:], op=ALU.add), [psd[4], wd], [p4])
            S.op("dve", lambda e: e.max(out=mxr[:], in_=lg[:]), [p4], [p4])
            S.op("dve", lambda e: e.tensor_scalar(out=mxr[:, 4:5], in0=mxr[:, 0:1], scalar1=-1.0, scalar2=None, op0=ALU.mult), [p4], [p4])
            S.op("act", lambda e: e.activation(out=ex[:], in_=lg[:], func=AF.Exp, bias=mxr[:, 4:5], scale=1.0), [p4], [p4])
            S.op("dve", lambda e: e.scalar_tensor_tensor(out=ex[:], in0=lg[:], scalar=mxr[:, 3:4], in1=ex[:],
                                                         op0=ALU.is_ge, op1=ALU.mult), [p4], [p4])
            S.op("dve", lambda e: e.reduce_sum(out=mxr[:, 5:6], in_=ex[:], axis=AX.X), [p4], [p4])
            S.op("dve", lambda e: e.reciprocal(out=mxr[:, 5:6], in_=mxr[:, 5:6]), [p4], [p4])
            S.op("dve", lambda e: e.tensor_scalar(out=gate[:, i, :], in0=ex[:], scalar1=mxr[:, 5:6], scalar2=None, op0=ALU.mult),
                 [p4], [gate_dep])
            g4(8)
        S.mute = False
        S.barrier()

    if stage == 4:
        dbg_h = nc.dram_tensor("dbg_h", [128, 16, D], F32, kind="ExternalOutput").ap()
        dbg_g = nc.dram_tensor("dbg_g", [128, 16, NE], F32, kind="ExternalOutput").ap()
        dbg_x = nc.dram_tensor("dbg_x", [128, 16, D], BF16, kind="ExternalOutput").ap()
        S.dma(dbg_h, hres[:])
        S.dma(dbg_g, gate[:])
        S.dma(dbg_x, xnT[:])
        S.barrier()
        es_tail.close()
        es_all.close()
        return nc, S

    S.mute = skip5
    with ExitStack() as es:
        C = CAP
        NSC = C // 128
        wupb = sb(es, "wupb", [128, 8, 2048], BF16)
        wdnb = sb(es, "wdnb", [128, 8, D], BF16)
        wup_dep = Dep()
        wdn_dep = Dep()
        NSTG5 = 4
        stg = [sb(es, f"stg5{i}", [128, 512], F32) for i in range(NSTG5)]
        stg_dep = [Dep() for _ in range(NSTG5)]
        bupc = sb(es, "bupc", [128, NE, 16], F32)
        browb = sb(es, "browb", [1, D], BF16)
        brow_dep = Dep()
        bd = Dep()
        S.dma(bupc[:], bup_d, W=[bd])
        iotac = sb(es, "iotac", [128, C], F32)
        S.dma(iotac[:], iota_d, W=[bd])
        Mf = sb(es, "Mf", [128, 16, NE], F32)
        pos = sb(es, "pos", [128, 16, NE], F32)
        dsp = Dep()
        with ExitStack() as est:
            trisb = sb(est, "trisb", [128, 128], BF16)
            Mb = sb(est, "Mb", [128, 16, NE], BF16)
            tot5 = sb(est, "tot5", [128, 16, NE], F32)
            pre5 = sb(est, "pre5", [128, 16, NE], F32)
            S.op("dve", lambda g: g.tensor_tensor(out=trisb[:], in0=trif[:], in1=identf[:], op=ALU.subtract), [cst], [dsp])
            S.op("dve", lambda g: g.tensor_scalar(out=Mf[:], in0=gate[:], scalar1=0.0, scalar2=None, op0=ALU.is_gt), [gate_dep], [dsp])
            S.op("dve", lambda g: g.tensor_copy(out=Mb[:], in_=Mf[:]), [dsp], [dsp])
            mflat = Mb[:].rearrange("p a b -> p (a b)")
            mm(ps[0][:, :], trisb[:], mflat, True, True, [dsp], [psd[0]])
            mm(ps[1][:, :], onesb[:], mflat, True, True, [dsp, cst], [psd[1]])
            S.op("dve", lambda g: g.tensor_copy(out=tot5[:].rearrange("p a b -> p (a b)"), in_=ps[1][:, :]), [psd[1]], [dsp])
            S.op("dve", lambda g: g.memset(pre5[:, 0, :], 0.0), [], [dsp])
            for k in range(1, 16):
                S.op("dve", lambda g: g.tensor_tensor(out=pre5[:, k, :], in0=pre5[:, k - 1, :], in1=tot5[:, k - 1, :], op=ALU.add), [dsp], [dsp])
            S.op("dve", lambda g: g.tensor_tensor(out=pos[:].rearrange("p a b -> p (a b)"), in0=ps[0][:, :],
                                                  in1=pre5[:].rearrange("p a b -> p (a b)"), op=ALU.add), [psd[0], dsp], [dsp])
            S.barrier()

        Sel = sb(es, "Sel", [128, 16, C], BF16)
        Sel_dep = Dep()
        SelTb = [sb(es, f"SelTb{i}", [128, NSC, 128], BF16) for i in range(2)]
        SelTb_dep = [Dep() for _ in range(2)]
        XeT = sb(es, "XeT", [128, 8, C], BF16)
        XeT_dep = Dep()
        actT = sb(es, "actT5", [128, 8, C], BF16)
        actT_dep = Dep()
        assert NSC * D == 8 * C
        ye = XeT[:].rearrange("p (s two) c -> p s (two c)", two=2)
        ye_dep = XeT_dep
        gcs = [sb(es, f"gcs{i}", [128, C], F32) for i in range(1)] * 2
        sgs = [sb(es, f"sgs{i}", [128, C], F32) for i in range(1)] * 2
        lcs = [sb(es, f"lcs{i}", [128, C], F32) for i in range(1)] * 2
        gc_dep = [Dep()] * 2
        sg_dep5 = [Dep()] * 2
        lc_dep = [Dep()] * 2
        sctr = [0]

        def load_up(e):
            for dc in range(8):
                for half in range(4):
                    k = sctr[0] % NSTG5
                    sctr[0] += 1
                    S.dma(stg[k][:], wup_d[e, dc * 128:(dc + 1) * 128, half * 512:(half + 1) * 512], W=[stg_dep[k]])
                    copy_on("pool", wupb[:, dc, half * 512:(half + 1) * 512], stg[k][:], [stg_dep[k]], [wup_dep])

        def load_dn(e):
            for fc in range(8):
                for half in range(2):
                    k = sctr[0] % NSTG5
                    sctr[0] += 1
                    S.dma(stg[k][:], wdn_d[e, fc * 128:(fc + 1) * 128, half * 512:(half + 1) * 512], W=[stg_dep[k]])
                    copy_on("pool", wdnb[:, fc, half * 512:(half + 1) * 512], stg[k][:], [stg_dep[k]], [wdn_dep])

        load_up(0)
        load_dn(0)
        uc = 0
        dcn = 0
        tcn = 0
        for e_ in range(n_experts):
            for half in range(2):
                kk = sctr[0] % NSTG5
                sctr[0] += 1
                S.dma(stg[kk][0:1, :], bdn_d[e_:e_ + 1, half * 512:(half + 1) * 512], W=[stg_dep[kk]])
                S.op("act", lambda g: g.copy(out=browb[0:1, half * 512:(half + 1) * 512], in_=stg[kk][0:1, :]), [stg_dep[kk]], [brow_dep])
            for blk in range(16):
                S.op("dve", lambda g: g.tensor_scalar(out=Sel[:, blk, :], in0=iotac[:], scalar1=pos[:, blk, e_:e_ + 1],
                                                      scalar2=Mf[:, blk, e_:e_ + 1], op0=ALU.is_equal, op1=ALU.mult), [dsp, bd], [Sel_dep])
            for dc in range(8):
                bX = 4 + dcn % 3
                dcn += 1
                for blk in range(16):
                    mm(ps[bX][:, 0:C], xnT[:, blk, dc * 128:(dc + 1) * 128], Sel[:, blk, :], blk == 0, blk == 15,
                       [xnT_dep, Sel_dep], [psd[bX]])
                S.op("act", lambda g: g.copy(out=XeT[:, dc, :], in_=ps[bX][:, 0:C]), [psd[bX]], [XeT_dep])
            for fc in range(8):
                bG = (uc % 2) * 2
                bL = bG + 1
                tb = uc % 2
                uc += 1
                for dc in range(8):
                    mm(ps[bG][:, 0:C], wupb[:, dc, fc * 128:(fc + 1) * 128], XeT[:, dc, :], dc == 0, dc == 7, [wup_dep, XeT_dep], [psd[bG]])
                for dc in range(8):
                    mm(ps[bL][:, 0:C], wupb[:, dc, 1024 + fc * 128:1024 + (fc + 1) * 128], XeT[:, dc, :], dc == 0, dc == 7,
                       [wup_dep, XeT_dep], [psd[bL]])
                S.op("dve", lambda g: g.tensor_scalar(out=gcs[tb][:], in0=ps[bG][:, 0:C], scalar1=bupc[:, e_, fc:fc + 1], scalar2=7.0,
                                                      op0=ALU.add, op1=ALU.min), [psd[bG], bd], [gc_dep[tb]])
                S.op("act", lambda g: g.activation(out=sgs[tb][:], in_=gcs[tb][:], func=AF.Sigmoid, scale=1.702), [gc_dep[tb]], [sg_dep5[tb]])
                S.op("dve", lambda g: g.tensor_scalar(out=lcs[tb][:], in0=ps[bL][:, 0:C], scalar1=bupc[:, e_, 8 + fc:9 + fc], scalar2=7.0,
                                                      op0=ALU.add, op1=ALU.min), [psd[bL], bd], [lc_dep[tb]])
                S.op("dve", lambda g: g.tensor_scalar(out=lcs[tb][:], in0=lcs[tb][:], scalar1=-7.0, scalar2=1.0,
                                                      op0=ALU.max, op1=ALU.add), [lc_dep[tb]], [lc_dep[tb]])
                S.op("pool", lambda g: g.tensor_tensor(out=gcs[tb][:], in0=gcs[tb][:], in1=sgs[tb][:], op=ALU.mult), [sg_dep5[tb]], [gc_dep[tb]])
                S.op("pool", lambda g: g.tensor_tensor(out=actT[:, fc, :], in0=gcs[tb][:], in1=lcs[tb][:], op=ALU.mult),
                     [gc_dep[tb], lc_dep[tb]], [actT_dep])
            if e_ + 1 < n_experts:
                load_up(e_ + 1)
            for sc in range(NSC):
                for half in range(2):
                    hs = slice(half * 512, (half + 1) * 512)
                    bD = 4 + dcn % 3
                    dcn += 1
                    for fc in range(8):
                        mm(ps[bD][:, :], actT[:, fc, sc * 128:(sc + 1) * 128], wdnb[:, fc, hs], fc == 0, False,
                           [actT_dep, wdn_dep], [psd[bD]])
                    mm(ps[bD][:, :], onesb[0:1, :], browb[0:1, hs], False, True, [brow_dep, cst], [psd[bD]])
                    S.op("act", lambda g: g.copy(out=ye[:, sc, hs], in_=ps[bD][:, :]), [psd[bD]], [ye_dep])
            if e_ + 1 < n_experts:
                load_dn(e_ + 1)
            for blk in range(16):
                tbf = tcn % 2
                tcn += 1
                for sc in range(NSC):
                    S.op("pe", lambda g: g.transpose(out=psb[:, sc * 128:(sc + 1) * 128], in_=Sel[:, blk, sc * 128:(sc + 1) * 128],
                                                     identity=identb[:]), [Sel_dep, cst], [psb_dep])
                S.op("act", lambda g: g.copy(out=SelTb[tbf][:].rearrange("p c t -> p (c t)"), in_=psb[:, 0:NSC * 128]), [psb_dep], [SelTb_dep[tbf]])
                for half in range(2):
                    hs = slice(half * 512, (half + 1) * 512)
                    bY = 4 + dcn % 3
                    dcn += 1
                    for sc in range(NSC):
                        mm(ps[bY][:, :], SelTb[tbf][:, sc, :], ye[:, sc, hs], sc == 0, sc == NSC - 1, [SelTb_dep[tbf], ye_dep], [psd[bY]])
                    S.op("dve", lambda g: g.scalar_tensor_tensor(out=hres[:, blk, hs], in0=ps[bY][:, :], scalar=gate[:, blk, e_:e_ + 1],
                                                                 in1=hres[:, blk, hs], op0=ALU.mult, op1=ALU.add),
                         [psd[bY], gate_dep, hres_dep[blk]], [hres_dep[blk]])
        S.barrier()

    S.mute = skip6
    with ExitStack() as es:
        sqt_box[0] = sb(es, "sqt6", [128, D], F32)
        wpgb = sb(es, "wpgb", [128, 8, D], BF16)
        wpleb = sb(es, "wpleb", [128, 2, D], BF16)
        pTb = sb(es, "pTb", [128, 2, 2048], BF16)
        stg = [sb(es, f"stg6{i}", [128, 2048], F32) for i in range(2)]
        stg_dep = [Dep() for _ in range(2)]
        lnpb = sb(es, "lnpb", [128, D], F32)
        lnfb = sb(es, "lnfb", [128, D], F32)
        hn = sb(es, "hn", [128, D], BF16)
        hnT = sb(es, "hnT", [128, 8, 128], BF16)
        sig = [sb(es, f"sig{i}", [128, 512], F32) for i in range(2)]
        outt = [sb(es, f"outt{i}", [128, D], F32) for i in range(2)]
        outt_dep = [Dep() for _ in range(2)]
        wd = Dep()
        p6 = Dep()
        out_dep = Dep()
        ctr = [0]
        load_w_bf16(wpgb, wpg_d, 8, D, stg, stg_dep, wd, ctr)
        load_w_bf16(wpleb, wple_d, 2, D, stg, stg_dep, wd, ctr)
        for c2 in range(2):
            k = ctr[0] % 2
            ctr[0] += 1
            S.dma(stg[k][:], pTo_d[c2], W=[stg_dep[k]])
            copy_on(cast_eng(), pTb[:, c2, :], stg[k][:], [stg_dep[k]], [wd])
        S.dma(lnpb[:], lnp_d, W=[wd])
        S.dma(lnfb[:], lnf_d, W=[wd])
        for i in range(16):
            ob = i % 2
            rms_rstd(hres[:, i, :], D, 0, [hres_dep[i]])
            S.op("dve", lambda e: e.scalar_tensor_tensor(out=hn[:], in0=hres[:, i, :], scalar=ssc[:, 0:1], in1=lnpb[:],
                                                         op0=ALU.mult, op1=ALU.mult), [hres_dep[i], nrm, wd], [p6])
            for c in range(8):
                S.op("pe", lambda e: e.transpose(out=psb[:, c * 128:(c + 1) * 128], in_=hn[:, c * 128:(c + 1) * 128],
                                                 identity=identb[:]), [p6, cst], [psb_dep])
            S.op("act", lambda e: e.copy(out=hnT[:].rearrange("p c t -> p (c t)"), in_=psb[:, :]), [psb_dep], [p6])
            for half in range(2):
                hs = slice(half * 512, (half + 1) * 512)
                for c in range(8):
                    mm(ps[half][:, :], hnT[:, c, :], wpgb[:, c, hs], c == 0, c == 7, [p6, wd], [psd[half]])
                for c2 in range(2):
                    mm(ps[2 + half][:, :], pTb[:, c2, i * 128:(i + 1) * 128], wpleb[:, c2, hs], c2 == 0, c2 == 1, [wd], [psd[2 + half]])
                S.op("act", lambda e: e.activation(out=sig[half][:], in_=ps[half][:, :], func=AF.Exp, scale=-1.0), [psd[half]], [p6])
                S.op("dve", lambda e: e.tensor_scalar(out=sig[half][:], in0=sig[half][:], scalar1=1.0, scalar2=None, op0=ALU.add), [p6], [p6])
                S.op("dve", lambda e: e.reciprocal(out=sig[half][:], in_=sig[half][:]), [p6], [p6])
                S.op("dve", lambda e: e.tensor_tensor(out=sig[half][:], in0=ps[2 + half][:, :], in1=sig[half][:], op=ALU.mult),
                     [psd[2 + half], p6], [p6])
                S.op("dve", lambda e: e.tensor_tensor(out=hres[:, i, hs], in0=sig[half][:], in1=hres[:, i, hs], op=ALU.add),
                     [p6, hres_dep[i], nrm], [hres_dep[i]])
            rms_rstd(hres[:, i, :], D, 1, [hres_dep[i]])
            S.op("dve", lambda e: e.scalar_tensor_tensor(out=outt[ob][:], in0=hres[:, i, :], scalar=ssc[:, 1:2], in1=lnfb[:],
                                                         op0=ALU.mult, op1=ALU.mult), [hres_dep[i], nrm, wd], [outt_dep[ob]])
            S.dma(out_d[i * 128:(i + 1) * 128, :], outt[ob][:], R=[outt_dep[ob]], W=[out_dep])
        S.barrier()
    es_tail.close()

    if stage <= 3:
        dbg_f = nc.dram_tensor("dbg_ff", [128, 64 * 8 + 16 * 24], F32, kind="ExternalOutput").ap()
        S.dma(dbg_f[:, 0:512], ffall[:].rearrange("p a b -> p (a b)"))
        S.dma(dbg_f[:, 512:896], gown[:].rearrange("p a b -> p (a b)"))
        S.barrier()
        es_all.close()
        return nc, S

    es_all.close()
    return nc, S


def own_tokens(j):
    return np.concatenate([np.arange(512 * i + 128 * j, 512 * i + 128 * j + 128) for i in range(16)])


def const_tables(j):
    p = np.arange(128)
    c = {}
    c["identb"] = _bf(np.eye(128, dtype=np.float32))
    c["identf"] = np.eye(128, dtype=np.float32)
    c["trif"] = (p[:, None] <= p[None, :]).astype(np.float32)
    sl = p[:, None]
    tl = p[None, :]
    dm = np.zeros((128, 4, 128), np.float32)
    for kk in range(4):
        dist = 128 * (j - kk) + tl - sl
        dm[:, kk, :] = np.where(dist >= 0, 0.0, NEGM)
    c["diagm"] = _bf(dm)
    wm = np.zeros((128, 8, 128), np.float32)
    for wk in range(8):
        dist = 128 * (j + 4 - wk) + tl - sl
        wm[:, wk, :] = np.where((dist >= 0) & (dist < 512), 0.0, NEGM)
    c["winm"] = _bf(wm)
    cm = np.zeros((128, 5, 128), np.float32)
    for dd in range(5):
        d = dd - 4
        cond = (512 * d + 16 * sl - tl - 128 * j + 31) <= 0
        cm[:, dd, :] = np.where(cond, 0.0, NEGM)
    c["cmask"] = _bf(cm)
    slopes = np.exp2(-8.0 * np.arange(1, 9, dtype=np.float32) / 8).astype(np.float32)
    rel = np.arange(64)
    ab = slopes[None, :, None] * (p[:, None, None] - 127 - 128 * (rel[None, None, :] + j - 3))
    c["ab"] = np.ascontiguousarray(ab[:, :, ::-1]).astype(np.float32)
    dd = np.arange(16) - 15
    cab = slopes[None, :, None] * (16 * p[:, None, None] + 512 * dd[None, None, :] - 128 * j - 96)
    c["cab"] = cab.astype(np.float32)
    n = np.arange(128)
    selA = np.zeros((128, 16, 128), np.float32)
    selB = np.zeros((128, 16, 128), np.float32)
    for i in range(16):
        cur = (512 * i + 128 * j + p) // 64
        valid = n[None, :] <= cur[:, None]
        forced = valid & ((n[None, :] == 0) | (n[None, :] == cur[:, None]) | (n[None, :] == cur[:, None] - 1))
        selA[:, i, :] = (valid & ~forced)
        selB[:, i, :] = np.where(forced, 1e9, np.where(valid, 0.0, -1.0))
    c["selA"] = _bf(selA)
    c["selB"] = _bf(selB)
    ws = np.zeros((128, 16, 64), np.float32)
    for i in range(16):
        ws[:, i, :] = (np.arange(64)[None, :] <= 4 * i + j)
    c["wsel"] = ws
    s = np.arange(T)
    c["Rexp"] = _bf((n[:, None] == (s[None, :] // 64)).astype(np.float32))
    cc = np.arange(512)
    ov = ((cc[:, None] * 16 < n[None, :] * 64 + 64) & (cc[:, None] * 16 + 31 >= n[None, :] * 64)).astype(np.float32)
    ov[511, :] = 0.0
    c["ovl"] = _bf(ov.reshape(4, 128, 128).transpose(1, 0, 2))
    c["iotac"] = np.ascontiguousarray(np.broadcast_to(np.arange(CAP, dtype=np.float32)[None, :], (128, CAP)))
    return c


def make_in_maps(x, p, ln1, w_in, b_fg, w_cmp1_k, w_cmp2_k, pe_cmp_k, w_cmp1_v, w_cmp2_v, pe_cmp_v, gn_nsa, gn_fox,
                 w_out, ln2, w_router, b_router, w_up, b_up, w_down, b_down, ln_ple, w_ple, w_ple_gate, ln_f, ne=NE):
    f = lambda a: np.ascontiguousarray(np.asarray(a, dtype=np.float32))
    x = f(x); p = f(p); w = f(w_in)[0]
    q_n = w[:, 0:512]; k_c = w[:, 512:640]; v_c = w[:, 640:768]; k_s = w[:, 768:896]; v_s = w[:, 896:1024]
    k_w = w[:, 1024:1152]; v_w = w[:, 1152:1280]; g_n = w[:, 1280:1304]; q_f = w[:, 1304:1816]
    k_f = w[:, 1816:2328]; v_f = w[:, 2328:2840]; f_f = w[:, 2840:2848]
    bc = lambda v: f(np.broadcast_to(np.asarray(v, np.float32).reshape(1, -1), (128, np.asarray(v).size)))
    shared = {
        "wA": f(np.concatenate([k_f, k_s, k_w, k_c, v_c], 1)),
        "wB": f(np.concatenate([v_f, v_s, v_w, f_f, g_n], 1)),
        "wQ": f(np.concatenate([q_n, q_f], 1)),
        "ln1c": f(np.asarray(ln1, np.float32)[0].reshape(8, 128).T),
        "bfg": bc(np.asarray(b_fg)[0]),
        "gnb": bc(np.concatenate([np.asarray(gn_nsa)[0], np.asarray(gn_fox)[0]])),
        "wout": f(w_out)[0], "ln2b": bc(np.asarray(ln2)[0]), "wr": f(w_router)[0], "brb": bc(np.asarray(b_router)[0]),
        "wup": f(np.asarray(w_up)[0, :ne]), "wdn": f(np.asarray(w_down)[0, :ne]), "bdn": f(np.asarray(b_down)[0]),
        "bupc": f(np.asarray(b_up, np.float32)[0].reshape(NE, 16, 128).transpose(2, 0, 1)),
        "lnpb": bc(np.asarray(ln_ple)[0]), "wple": f(w_ple)[0], "wpg": f(w_ple_gate)[0], "lnfb": bc(np.asarray(ln_f)),
    }
    for nm, w1, w2, pe in (("k", w_cmp1_k, w_cmp2_k, pe_cmp_k), ("v", w_cmp1_v, w_cmp2_v, pe_cmp_v)):
        w1r = np.asarray(w1, np.float32)[0].reshape(32, 64, 128).transpose(1, 0, 2)
        shared["w1" + nm] = f(np.concatenate([w1r, w1r], 0))
        peT = np.asarray(pe, np.float32)[0].T
        peT = np.concatenate([peT, peT], 0)
        shared["pe" + nm] = f(np.stack([peT, peT], -1))
    w2k = np.asarray(w_cmp2_k, np.float32)[0]
    shared["w2k"] = f(np.concatenate([w2k, w2k], 1))
    shared["w2v"] = f(np.asarray(w_cmp2_v, np.float32)[0])
    maps = []
    for c in range(NCORES):
        b, j = c // 4, c % 4
        tok = own_tokens(j)
        m = dict(shared)
        m["xT"] = f(x[b].T.reshape(8, 128, T))
        m["xTo"] = f(x[b][tok].T.reshape(8, 128, 2048))
        m["xo"] = f(x[b][tok])
        m["pTo"] = f(p[0, b][tok].T.reshape(2, 128, 2048))
        m.update(const_tables(j))
        maps.append(m)
    return maps


_CACHE = {}


def kernel(**inputs):
    maps = make_in_maps(**inputs)
    if "nc" not in _CACHE:
        _CACHE["nc"] = build_program()[0]
    nc = _CACHE["nc"]
    res = run_bass_kernel_spmd(nc, maps, core_ids=list(range(NCORES)))
    out = np.zeros((2, T, D), np.float32)
    for c in range(NCORES):
        b, j = c // 4, c % 4
        out[b, own_tokens(j)] = np.asarray(res.results[c]["out"], np.float32).reshape(2048, D)
    return out
```

```python
from contextlib import ExitStack
import numpy as np
import ml_dtypes
import concourse.bass as bass
import concourse.mybir as mybir
from concourse.bass_utils import run_bass_kernel_spmd

F32 = mybir.dt.float32
BF16 = mybir.dt.bfloat16
AF = mybir.ActivationFunctionType
ALU = mybir.AluOpType
AX = mybir.AxisListType

NCORES = 8
T = 8192
D = 1024
NEGM = -30000.0
EPS = 1e-6
NE = 32
MOE_EXPERTS = 32
CAP = 512


class Dep:
    __slots__ = ("w", "r")

    def __init__(self):
        self.w = None
        self.r = []


class Sched:
    ROLL = 30000

    def __init__(self, nc, n_dma=40):
        self.nc = nc
        self.E = {"pe": nc.tensor, "act": nc.scalar, "dve": nc.vector, "pool": nc.gpsimd, "sp": nc.sync}
        self.csem = {}
        self.cnt = {}
        self.nsem = 0
        for k in ("pe", "act", "dve", "pool"):
            self._new_csem(k)
        self.seen = {k: {} for k in self.E}
        self.dsem = [nc.alloc_semaphore(name=f"dq{i}") for i in range(n_dma)]
        self.dval = [0] * n_dma
        self.dnext = 0
        self.mute = False
        self.ninst = 0

    def _new_csem(self, k):
        self.csem[k] = self.nc.alloc_semaphore(name=f"c{k}{self.nsem}")
        self.nsem += 1
        self.cnt[k] = 0

    def _collect(self, e, R, W):
        evs = []
        for d in R:
            if d.w is not None:
                evs.append(d.w)
        for d in W:
            if d.w is not None:
                evs.append(d.w)
            evs.extend(d.r)
        return evs

    def _wait(self, e, evs):
        eng = self.E[e]
        seen = self.seen[e]
        need = {}
        for (s, v, src) in evs:
            if src == "pe" and e == "pe":
                continue
            if src == e and s is self.csem.get(e) and self.cnt[e] - v >= 3:
                continue
            key = s.num
            if seen.get(key, 0) >= v:
                continue
            if key not in need or need[key][1] < v:
                need[key] = (s, v)
        for key, (s, v) in need.items():
            eng.wait_ge(s, v)
            seen[key] = v
            self.ninst += 1

    def _mark(self, ev, R, W):
        for d in R:
            d.r.append(ev)
            if len(d.r) > 64:
                d.r = d.r[-64:]
        for d in W:
            d.w = ev
            d.r = []

    def op(self, e, fn, R=(), W=()):
        if self.mute:
            return None
        self._wait(e, self._collect(e, R, W))
        ins = fn(self.E[e])
        if self.cnt[e] >= self.ROLL:
            self._new_csem(e)
        self.cnt[e] += 1
        ins.then_inc(self.csem[e], 1)
        ev = (self.csem[e], self.cnt[e], e)
        self._mark(ev, R, W)
        self.ninst += 1
        return ev

    def dma(self, out, in_, R=(), W=(), e="sp"):
        if self.mute:
            return None
        k = self.dnext
        self.dnext = (k + 1) % len(self.dsem)
        if self.dval[k] >= self.ROLL:
            self._wait(e, [(self.dsem[k], self.dval[k], "dma")])
            self.dsem[k] = self.nc.alloc_semaphore(name=f"dq{k}_{self.nsem}")
            self.nsem += 1
            self.dval[k] = 0
        s = self.dsem[k]
        evs = self._collect(e, R, W)
        if self.dval[k] > 0:
            evs.append((s, self.dval[k], "dma"))
        self._wait(e, evs)
        self.E[e].dma_start(out=out, in_=in_).then_inc(s, 16)
        self.dval[k] += 16
        ev = (s, self.dval[k], "dma")
        self._mark(ev, R, W)
        self.ninst += 1
        return ev

    def barrier(self):
        evs = [(self.csem[k], self.cnt[k], k + "_b") for k in self.csem if self.cnt[k] > 0]
        evs += [(self.dsem[k], self.dval[k], "dma") for k in range(len(self.dsem)) if self.dval[k] > 0]
        for e in self.E:
            self._wait(e, [x for x in evs])


def _bf(a):
    return np.ascontiguousarray(a).astype(ml_dtypes.bfloat16)


def build_program(stage=99, n_experts=MOE_EXPERTS, skip123=False, p4stop=99, nblk4=16, skip5=False, skip6=False):
    nc = bass.Bass("TRN2", target_bir_lowering=False)
    S = Sched(nc)

    def din(name, shape, dt=F32):
        return nc.dram_tensor(name, list(shape), dt, kind="ExternalInput").ap()

    dbg = stage < 99

    def dscr(name, shape, dt):
        return nc.dram_tensor(name, list(shape), dt, kind=("ExternalOutput" if dbg else "Internal")).ap()

    xT_d = din("xT", [8, 128, T])
    xTo_d = din("xTo", [8, 128, 2048])
    xo_d = din("xo", [2048, D])
    pTo_d = din("pTo", [2, 128, 2048])
    wA_d = din("wA", [D, 1024])
    wB_d = din("wB", [D, 800])
    wQ_d = din("wQ", [D, 1024])
    ln1c_d = din("ln1c", [128, 8])
    bfg_d = din("bfg", [128, 8])
    w1k_d = din("w1k", [128, 32, 128])
    w1v_d = din("w1v", [128, 32, 128])
    w2k_d = din("w2k", [128, 128])
    w2v_d = din("w2v", [128, 64])
    pek_d = din("pek", [128, 32, 2])
    pev_d = din("pev", [128, 32, 2])
    gn_d = din("gnb", [128, 1024])
    wout_d = din("wout", [D, D])
    ln2_d = din("ln2b", [128, D])
    wr_d = din("wr", [D, 32])
    br_d = din("brb", [128, 32])
    wup_d = din("wup", [n_experts, D, 2048])
    bup_d = din("bupc", [128, NE, 16])
    wdn_d = din("wdn", [n_experts, D, D])
    bdn_d = din("bdn", [NE, D])
    lnp_d = din("lnpb", [128, D])
    wple_d = din("wple", [256, D])
    wpg_d = din("wpg", [D, D])
    lnf_d = din("lnfb", [128, D])
    identb_d = din("identb", [128, 128], BF16)
    identf_d = din("identf", [128, 128])
    tri_d = din("trif", [128, 128])
    diagm_d = din("diagm", [128, 4, 128], BF16)
    winm_d = din("winm", [128, 8, 128], BF16)
    cmask_d = din("cmask", [128, 5, 128], BF16)
    ab_d = din("ab", [128, 8, 64])
    cab_d = din("cab", [128, 8, 16])
    selA_d = din("selA", [128, 16, 128], BF16)
    selB_d = din("selB", [128, 16, 128], BF16)
    wsel_d = din("wsel", [128, 16, 64])
    R_d = din("Rexp", [128, T], BF16)
    ovl_d = din("ovl", [128, 4, 128], BF16)
    iota_d = din("iotac", [128, CAP])

    out_d = nc.dram_tensor("out", [2048, D], F32, kind="ExternalOutput").ap()

    fmS = dscr("fmS", [8, 128, T], BF16)
    tmS = dscr("tmS", [T, 780], BF16)
    qS = dscr("qS", [16, 16, 128, 128], BF16)
    fmS_dep = [Dep() for _ in range(8)]
    tmS_dep = Dep()
    qS_dep = Dep()

    es_all = ExitStack()

    def sb(es, name, shape, dt):
        return es.enter_context(nc.sbuf_tensor("s_" + name, list(shape), dt))

    ps = [es_all.enter_context(nc.psum_tensor(f"ps{i}", [128, 512], F32)) for i in range(7)]
    psd = [Dep() for _ in range(7)]
    psb = es_all.enter_context(nc.psum_tensor("psb", [128, 1024], BF16))
    psb_dep = Dep()

    identb = sb(es_all, "identb", [128, 128], BF16)
    identf = sb(es_all, "identf", [128, 128], F32)
    trif = sb(es_all, "trif", [128, 128], F32)
    onesb = sb(es_all, "onesb", [128, 128], BF16)
    onesf = sb(es_all, "onesf", [128, 128], F32)
    epsc = sb(es_all, "epsc", [128, 1], F32)
    onec = sb(es_all, "onec", [128, 1], F32)
    cst = Dep()
    S.dma(identb[:], identb_d, W=[cst])
    S.dma(identf[:], identf_d, W=[cst])
    S.dma(trif[:], tri_d, W=[cst])
    S.op("dve", lambda e: e.memset(onesb[:], 1.0), W=[cst])
    S.op("dve", lambda e: e.memset(onesf[:], 1.0), W=[cst])
    S.op("dve", lambda e: e.memset(epsc[:], EPS), W=[cst])
    S.op("dve", lambda e: e.memset(onec[:], 1.0), W=[cst])

    es_attn = ExitStack()
    ffall = sb(es_attn, "ffall", [128, 64, 8], F32)
    ffall_dep = Dep()
    gown = sb(es_attn, "gown", [128, 16, 24], F32)
    gown_dep = Dep()

    rr = {"cast": 0, "evac": 0}

    def cast_eng():
        rr["cast"] += 1
        return ("act", "dve", "pool")[rr["cast"] % 3]

    def copy_on(e, out, in_, R, W):
        if e == "act":
            S.op("act", lambda g: g.copy(out=out, in_=in_), R, W)
        elif e == "dve":
            S.op("dve", lambda g: g.tensor_copy(out=out, in_=in_), R, W)
        else:
            S.op("pool", lambda g: g.tensor_copy(out=out, in_=in_), R, W)

    def evac_eng():
        rr["evac"] += 1
        return ("act", "dve")[rr["evac"] % 2]

    def mm(out, lhsT, rhs, start, stop, R, W):
        S.op("pe", lambda g: g.matmul(out, lhsT=lhsT, rhs=rhs, start=start, stop=stop), R, W)

    pctr = [0]
    LAG = 2

    def run_pipe(steps, LAG=3):
        n = len(steps)
        for k in range(n + LAG):
            if k < n:
                steps[k][0]()
            if k - LAG >= 0:
                steps[k - LAG][1]()

    S.mute = skip123
    with ExitStack() as es:
        wA = sb(es, "wA", [128, 8, 1024], BF16)
        wB = sb(es, "wB", [128, 8, 800], BF16)
        wQz = sb(es, "wQz", [128, 8, 16, 128], BF16)
        stg = [sb(es, f"stg{i}", [128, 1024], F32) for i in range(2)]
        stg_dep = [Dep() for _ in range(2)]
        ln1c = sb(es, "ln1c", [128, 8], F32)
        xt = [sb(es, f"xt{i}", [128, 8, 512], F32) for i in range(2)]
        xt_dep = [Dep() for _ in range(2)]
        sq = sb(es, "sq", [128, 8, 512], BF16)
        sq_dep = Dep()
        rstd = sb(es, "rstd", [128, 512], F32)
        rstd_dep = Dep()
        uT = [sb(es, f"uT{i}", [128, 8, 512], BF16) for i in range(2)]
        uT_dep = [Dep() for _ in range(2)]
        fmo = [sb(es, f"fmo{i}", [128, 8, 512], BF16) for i in range(2)]
        fmo_dep = [Dep() for _ in range(2)]
        tmv = [sb(es, f"tmv{i}", [128, 4, 780], BF16) for i in range(2)]
        tmv_dep = [Dep() for _ in range(2)]
        qo = [sb(es, f"qo{i}", [128, 4, 16, 128], BF16) for i in range(2)]
        qo_dep = [Dep() for _ in range(2)]
        w_dep = Dep()

        S.dma(ln1c[:], ln1c_d, W=[w_dep])
        S.op("pool", lambda e: e.memset(wQz[:], 0.0), W=[w_dep])
        for k in range(2):
            S.op("pool", lambda e: e.memset(tmv[k][:], 1.0), W=[tmv_dep[k]])
        sc = 0
        for dc in range(8):
            for (src, dst, ncol) in ((wA_d, wA, 1024), (wB_d, wB, 800)):
                k = sc % 2
                sc += 1
                S.dma(stg[k][:, 0:ncol], src[dc * 128:(dc + 1) * 128, :], W=[stg_dep[k]])
                copy_on(cast_eng(), dst[:, dc, :], stg[k][:, 0:ncol], [stg_dep[k]], [w_dep])
            k = sc % 2
            sc += 1
            S.dma(stg[k][:, :], wQ_d[dc * 128:(dc + 1) * 128, :], W=[stg_dep[k]])
            copy_on(cast_eng(), wQz[:, dc, 0:4, 0:64],
                    stg[k][:, 0:256].rearrange("p (h e) -> p h e", e=64), [stg_dep[k]], [w_dep])
            copy_on(cast_eng(), wQz[:, dc, 4:8, 64:128],
                    stg[k][:, 256:512].rearrange("p (h e) -> p h e", e=64), [stg_dep[k]], [w_dep])
            fx = stg[k][:, 512:1024].rearrange("p (h two e) -> p h two e", two=2, e=64)
            wz = wQz[:, dc, 8:16, :].rearrange("p (h two) e -> p h two e", two=2)
            copy_on(cast_eng(), wz[:, :, 0, 0:64], fx[:, :, 0, :], [stg_dep[k]], [w_dep])
            copy_on(cast_eng(), wz[:, :, 1, 64:128], fx[:, :, 1, :], [stg_dep[k]], [w_dep])

        def norm_tile(src_ap, k):
            S.dma(xt[k][:], src_ap, W=[xt_dep[k]])
            S.op("act", lambda e: e.activation(out=sq[:], in_=xt[k][:], func=AF.Square), [xt_dep[k]], [sq_dep])
            for dc in range(8):
                mm(ps[0][:, :], onesb[:], sq[:, dc, :], dc == 0, dc == 7, [sq_dep, cst], [psd[0]])
            S.op("act", lambda e: e.activation(out=rstd[:], in_=ps[0][:, :], func=AF.Sqrt,
                                               bias=epsc[:, 0:1], scale=1.0 / D), [psd[0], cst], [rstd_dep])
            S.op("dve", lambda e: e.reciprocal(out=rstd[:], in_=rstd[:]), [rstd_dep], [rstd_dep])
            for dc in range(8):
                S.op("dve", lambda e: e.scalar_tensor_tensor(
                    out=uT[k][:, dc, :], in0=xt[k][:, dc, :], scalar=ln1c[:, dc:dc + 1], in1=rstd[:],
                    op0=ALU.mult, op1=ALU.mult), [xt_dep[k], rstd_dep, w_dep], [uT_dep[k]])

        xT_v = xT_d.rearrange("c p s -> p c s")
        xTo_v = xTo_d.rearrange("c p s -> p c s")
        fmS_v = fmS.rearrange("o p s -> p o s")
        for Tt in range(16):
            k = Tt % 2
            norm_tile(xT_v[:, :, Tt * 512:(Tt + 1) * 512], k)
            for oc in range(8):
                b = 1 + oc % 2
                for dc in range(8):
                    mm(ps[b][:, :], wA[:, dc, oc * 128:(oc + 1) * 128], uT[k][:, dc, :], dc == 0, dc == 7,
                       [uT_dep[k], w_dep], [psd[b]])
                copy_on(evac_eng(), fmo[k][:, oc, :], ps[b][:, :], [psd[b]], [fmo_dep[k]])
            S.dma(fmS_v[:, :, Tt * 512:(Tt + 1) * 512], fmo[k][:], R=[fmo_dep[k]], W=fmS_dep)
            for sub in range(4):
                bA = 3 + (sub % 2) * 2
                bB = bA + 1
                for dc in range(8):
                    mm(ps[bA][:, 0:512], uT[k][:, dc, sub * 128:(sub + 1) * 128], wB[:, dc, 0:512], dc == 0, dc == 7,
                       [uT_dep[k], w_dep], [psd[bA]])
                for dc in range(8):
                    mm(ps[bB][:, 0:288], uT[k][:, dc, sub * 128:(sub + 1) * 128], wB[:, dc, 512:800], dc == 0, dc == 7,
                       [uT_dep[k], w_dep], [psd[bB]])
                copy_on("act", tmv[k][:, sub, 0:520].rearrange("p (h e) -> p h e", e=65)[:, :, 0:64],
                        ps[bA][:, 0:512].rearrange("p (h e) -> p h e", e=64), [psd[bA]], [tmv_dep[k]])
                copy_on("dve", tmv[k][:, sub, 520:780].rearrange("p (h e) -> p h e", e=65)[:, :, 0:64],
                        ps[bB][:, 0:256].rearrange("p (h e) -> p h e", e=64), [psd[bB]], [tmv_dep[k]])
                copy_on("dve", ffall[:, Tt * 4 + sub, :], ps[bB][:, 256:264], [psd[bB]], [ffall_dep])
            S.dma(tmS[Tt * 512:(Tt + 1) * 512, :].rearrange("(s p) c -> p s c", p=128), tmv[k][:],
                  R=[tmv_dep[k]], W=[tmS_dep])

        qS_v = qS.rearrange("i h p t -> i p h t")
        for T4 in range(4):
            norm_tile(xTo_v[:, :, T4 * 512:(T4 + 1) * 512], T4 % 2)
            k = T4 % 2
            for hd in range(16):
                b = 1 + hd % 2
                for dc in range(8):
                    mm(ps[b][:, :], wQz[:, dc, hd, :], uT[k][:, dc, :], dc == 0, dc == 7, [uT_dep[k], w_dep], [psd[b]])
                qdst = qo[k][:, :, hd, :]
                qsrc = ps[b][:, :].rearrange("p (b t) -> p b t", t=128)
                if hd % 2 == 0:
                    S.op("act", lambda e: e.activation(out=qdst, in_=qsrc, func=AF.Copy, scale=0.125), [psd[b]], [qo_dep[k]])
                else:
                    S.op("dve", lambda e: e.tensor_scalar(out=qdst, in0=qsrc, scalar1=0.125, scalar2=None, op0=ALU.mult),
                         [psd[b]], [qo_dep[k]])
            for bi in range(4):
                i = T4 * 4 + bi
                for dc in range(8):
                    mm(ps[3][:, 0:24], uT[k][:, dc, bi * 128:(bi + 1) * 128], wB[:, dc, 776:800], dc == 0, dc == 7,
                       [uT_dep[k], w_dep], [psd[3]])
                copy_on("dve", gown[:, i, :], ps[3][:, 0:24], [psd[3]], [gown_dep])
                S.dma(qS_v[i], qo[k][:, bi, :, :], R=[qo_dep[k]], W=[qS_dep])
        S.barrier()

    if stage <= 1:
        dbg_f = nc.dram_tensor("dbg_ff", [128, 64 * 8 + 16 * 24], F32, kind="ExternalOutput").ap()
        S.dma(dbg_f[:, 0:512], ffall[:].rearrange("p a b -> p (a b)"))
        S.dma(dbg_f[:, 512:896], gown[:].rearrange("p a b -> p (a b)"))
        S.barrier()
        es_attn.close()
        es_all.close()
        return nc, S

    mixS = dscr("mixS", [2048, D], BF16)
    mixS_dep = Dep()
    diagm = sb(es_attn, "diagm", [128, 4, 128], BF16)
    S.dma(diagm[:], diagm_d, W=[cst])
    pT = [sb(es_attn, f"pT{i}", [128, 512], BF16) for i in range(4)]
    pT_dep = [Dep() for _ in range(4)]
    zc = [sb(es_attn, f"zc{i}", [128, 4], F32) for i in range(2)]
    zc_dep = [Dep() for _ in range(2)]

    with ExitStack() as es:
        bfg = sb(es, "bfg", [128, 8], F32)
        wsel = sb(es, "wsel", [128, 16, 64], F32)
        lsp = sb(es, "lsp", [128, 64, 8], F32)
        cpcol = sb(es, "cpcol", [128, 64, 8], F32)
        tot = sb(es, "tot", [128, 64, 8], F32)
        pre = sb(es, "pre", [128, 64, 8], F32)
        cpref = sb(es, "cpref", [128, 16, 8], F32)
        tmpw = sb(es, "tmpw", [128, 8, 64], F32)
        cd = Dep()
        S.dma(bfg[:], bfg_d, W=[cd])
        S.dma(wsel[:], wsel_d, W=[cd])
        S.op("dve", lambda e: e.tensor_tensor(out=lsp[:], in0=ffall[:], in1=bfg[:, :].unsqueeze(1).to_broadcast([128, 64, 8]),
                                              op=ALU.add), [ffall_dep, cd], [cd])
        S.op("act", lambda e: e.activation(out=lsp[:], in_=lsp[:], func=AF.Exp, scale=-1.0), [cd], [cd])
        S.op("act", lambda e: e.activation(out=lsp[:], in_=lsp[:], func=AF.Ln, bias=onec[:, 0:1], scale=1.0), [cd, cst], [cd])
        lflat = lsp[:].rearrange("p a b -> p (a b)")
        mm(ps[0][:, :], trif[:], lflat, True, True, [cd, cst], [psd[0]])
        mm(ps[1][:, :], onesf[:], lflat, True, True, [cd, cst], [psd[1]])
        S.op("dve", lambda e: e.tensor_copy(out=tot[:].rearrange("p a b -> p (a b)"), in_=ps[1][:, :]), [psd[1]], [cd])
        S.op("dve", lambda e: e.memset(pre[:, 0, :], 0.0), [], [cd])
        for k in range(1, 64):
            S.op("dve", lambda e: e.tensor_tensor(out=pre[:, k, :], in0=pre[:, k - 1, :], in1=tot[:, k - 1, :], op=ALU.add), [cd], [cd])
        S.op("dve", lambda e: e.tensor_tensor(out=cpcol[:].rearrange("p a b -> p (a b)"), in0=ps[0][:, :],
                                              in1=pre[:].rearrange("p a b -> p (a b)"), op=ALU.add), [psd[0], cd], [cd])
        for i in range(16):
            S.op("dve", lambda e: e.tensor_tensor(out=tmpw[:], in0=tot[:].rearrange("p k h -> p h k"),
                                                  in1=wsel[:, i, :].unsqueeze(1).to_broadcast([128, 8, 64]), op=ALU.mult), [cd], [cd])
            S.op("dve", lambda e: e.reduce_sum(out=cpref[:, i, :], in_=tmpw[:], axis=AX.X), [cd], [cd])

        kT = [sb(es, f"kT{i}", [128, T], BF16) for i in range(2)]
        vP = [sb(es, f"vP{i}", [128, 64, 130], BF16) for i in range(2)]
        qP = [sb(es, f"qP{i}", [128, 16, 2, 128], BF16) for i in range(2)]
        kvq_dep = [Dep() for _ in range(2)]
        wF = [sb(es, f"wF{i}", [128, 64], F32) for i in range(2)]
        wF_dep = [Dep() for _ in range(2)]
        Vp = [sb(es, f"Vp{i}", [128, 64, 65], BF16) for i in range(2)]
        Vp_dep = [Dep() for _ in range(2)]
        mo = [sb(es, f"mo{i}", [128, 128], BF16) for i in range(2)]
        mo_dep = [Dep() for _ in range(2)]
        tmS_v = tmS.rearrange("(k p) c -> p k c", p=128)
        qS_p = qS.rearrange("i h p t -> p i h t")
        items = [(hp, i, hh) for hp in range(4) for i in range(16) for hh in range(2)]

        def fox_prep(n):
            hp, i, hh = items[n]
            kb = hp % 2
            bb = n % 2
            h = 2 * hp + hh
            nk = 4 * i + 4
            if i == 0 and hh == 0:
                S.dma(kT[kb][:], fmS[hp], R=[fmS_dep[hp]], W=[kvq_dep[kb]])
                for q4 in range(4):
                    S.dma(vP[kb][:, q4 * 16:(q4 + 1) * 16, :], tmS_v[:, q4 * 16:(q4 + 1) * 16, hp * 130:(hp + 1) * 130],
                          R=[tmS_dep], W=[kvq_dep[kb]])
                for q2 in range(2):
                    S.dma(qP[kb][:, :, q2, :], qS_p[:, :, 8 + 2 * hp + q2, :], R=[qS_dep], W=[kvq_dep[kb]])
            S.op("dve", lambda e: e.tensor_scalar(out=wF[bb][:, 0:nk], in0=cpcol[:, 0:nk, h], scalar1=cpref[:, i, h:h + 1],
                                                  scalar2=0.0, op0=ALU.subtract, op1=ALU.min), [cd], [wF_dep[bb]])
            S.op("act", lambda e: e.activation(out=wF[bb][:, 0:nk], in_=wF[bb][:, 0:nk], func=AF.Exp), [wF_dep[bb]], [wF_dep[bb]])
            eng = "dve" if n % 2 == 0 else "pool"
            S.op(eng, lambda e: e.tensor_tensor(out=Vp[bb][:, 0:nk, :], in0=vP[kb][:, 0:nk, hh * 65:(hh + 1) * 65],
                                                in1=wF[bb][:, 0:nk].unsqueeze(2).to_broadcast([128, nk, 65]), op=ALU.mult),
                 [kvq_dep[kb], wF_dep[bb]], [Vp_dep[bb]])

        def fox_run(n):
            hp, i, hh = items[n]
            kb = hp % 2
            bb = n % 2
            ob = 4 + n % 2
            mb = i % 2
            nk = 4 * i + 4
            steps = []
            for gq in range(nk // 4):
                bank = pctr[0] % 4
                pctr[0] += 1

                def qk(gq=gq, bank=bank):
                    for q in range(4):
                        k = 4 * gq + q
                        diag = k >= 4 * i
                        mm(ps[bank][:, q * 128:(q + 1) * 128], kT[kb][:, k * 128:(k + 1) * 128], qP[kb][:, i, hh, :], True, not diag,
                           [kvq_dep[kb]], [psd[bank]])
                        if diag:
                            mm(ps[bank][:, q * 128:(q + 1) * 128], identb[:], diagm[:, k - 4 * i, :], False, True, [cst], [psd[bank]])
                    S.op("act", lambda e: e.activation(out=pT[bank][:], in_=ps[bank][:, :], func=AF.Exp), [psd[bank]], [pT_dep[bank]])

                def pv(gq=gq, bank=bank):
                    for q in range(4):
                        k = 4 * gq + q
                        mm(ps[ob][:, 0:65], pT[bank][:, q * 128:(q + 1) * 128], Vp[bb][:, k, :], k == 0, k == nk - 1,
                           [pT_dep[bank], Vp_dep[bb]], [psd[ob]])
                steps.append((qk, pv))
            run_pipe(steps)
            z = zc[bb]
            S.op("dve", lambda e: e.tensor_scalar_max(out=z[:, 0:1], in0=ps[ob][:, 64:65], scalar1=1e-30), [psd[ob]], [zc_dep[bb]])
            S.op("dve", lambda e: e.reciprocal(out=z[:, 0:1], in_=z[:, 0:1]), [zc_dep[bb]], [zc_dep[bb]])
            S.op("dve", lambda e: e.tensor_scalar(out=mo[mb][:, hh * 64:(hh + 1) * 64], in0=ps[ob][:, 0:64],
                                                  scalar1=z[:, 0:1], scalar2=None, op0=ALU.mult),
                 [psd[ob], zc_dep[bb]], [mo_dep[mb]])
            if hh == 1:
                S.dma(mixS[i * 128:(i + 1) * 128, 512 + hp * 128:512 + (hp + 1) * 128], mo[mb][:], R=[mo_dep[mb]], W=[mixS_dep])

        fox_prep(0)
        for n in range(len(items)):
            if n + 1 < len(items):
                fox_prep(n + 1)
            fox_run(n)
        S.barrier()

    if stage <= 2:
        es_attn.close()
        es_all.close()
        return nc, S

    with ExitStack() as es:
        kcT = sb(es, "kcT", [128, 512], BF16)
        vc = sb(es, "vc", [128, 4, 130], BF16)
        kc_dep = Dep()
        S.op("pool", lambda e: e.memset(vc[:], 1.0), [], [kc_dep])
        KsT = sb(es, "KsT", [128, T], BF16)
        KwT = sb(es, "KwT", [128, T], BF16)
        Vs = sb(es, "Vs", [128, 64, 130], BF16)
        Vw = sb(es, "Vw", [128, 64, 130], BF16)
        Rx = sb(es, "Rx", [128, T], BF16)
        ovl = sb(es, "ovl", [128, 4, 128], BF16)
        ab = sb(es, "ab", [128, 8, 64], F32)
        cab = sb(es, "cab", [128, 8, 16], F32)
        selA = sb(es, "selA", [128, 16, 128], BF16)
        selB = sb(es, "selB", [128, 16, 128], BF16)
        cmask = sb(es, "cmask", [128, 5, 128], BF16)
        winm = sb(es, "winm", [128, 8, 128], BF16)
        nd = Dep()
        tmS_v = tmS.rearrange("(k p) c -> p k c", p=128)
        S.dma(KsT[:], fmS[4], R=[fmS_dep[4]], W=[nd])
        S.dma(KwT[:], fmS[5], R=[fmS_dep[5]], W=[nd])
        for q4 in range(4):
            S.dma(Vs[:, q4 * 16:(q4 + 1) * 16, :], tmS_v[:, q4 * 16:(q4 + 1) * 16, 520:650], R=[tmS_dep], W=[nd])
            S.dma(Vw[:, q4 * 16:(q4 + 1) * 16, :], tmS_v[:, q4 * 16:(q4 + 1) * 16, 650:780], R=[tmS_dep], W=[nd])
        for (dst, src) in ((Rx, R_d), (ovl, ovl_d), (ab, ab_d), (cab, cab_d), (selA, selA_d), (selB, selB_d),
                           (cmask, cmask_d), (winm, winm_d)):
            S.dma(dst[:], src, W=[nd])
        wab = sb(es, "wab", [128, 8, 64], F32)
        wab_dep = Dep()
        Vsp = [sb(es, f"Vsp{i}", [128, 64, 65], BF16) for i in range(2)]
        Vwp = [sb(es, f"Vwp{i}", [128, 8, 65], BF16) for i in range(2)]
        Vxp_dep = [Dep() for _ in range(2)]
        with ExitStack() as es2:
            kraw = sb(es2, "kraw", [128, T], BF16)
            w1f = sb(es2, "w1f", [128, 32, 128], F32)
            w1b = sb(es2, "w1b", [128, 32, 128], BF16)
            pef = sb(es2, "pef", [128, 32, 2], F32)
            peb = sb(es2, "peb", [128, 32, 2], BF16)
            w2f = sb(es2, "w2f", [128, 128], F32)
            w2b = sb(es2, "w2b", [128, 128], BF16)
            hx = sb(es2, "hx", [128, 512], F32)
            hu = sb(es2, "hu", [128, 512], F32)
            hidT = sb(es2, "hidT", [128, 512], BF16)
            cbias = sb(es2, "cbias", [128, 1], F32)
            cpd = Dep()
            for which in range(2):
                S.dma(kraw[:], fmS[6 + which], R=[fmS_dep[6 + which]], W=[cpd])
                S.dma(w1f[:], (w1k_d, w1v_d)[which], W=[cpd])
                S.dma(pef[:], (pek_d, pev_d)[which], W=[cpd])
                if which == 0:
                    S.dma(w2f[:, :], w2k_d, W=[cpd])
                else:
                    S.dma(w2f[:, 0:64], w2v_d, W=[cpd])
                S.op("dve", lambda e: e.tensor_copy(out=w1b[:], in_=w1f[:]), [cpd], [cpd])
                S.op("dve", lambda e: e.tensor_copy(out=peb[:], in_=pef[:]), [cpd], [cpd])
                S.op("dve", lambda e: e.tensor_copy(out=w2b[:], in_=w2f[:]), [cpd], [cpd])
                for g in range(2):
                    r0, r1 = g * 64, g * 64 + 64
                    for l in range(32):
                        mm(ps[0][:, 0:511], w1b[r0:r1, l, :], kraw[r0:r1, l:l + 16 * 510 + 1:16], l == 0, l == 31, [cpd], [psd[0]])
                    for l in range(32):
                        mm(ps[1][:, 0:2], w1b[r0:r1, l, :], peb[r0:r1, l, :], l == 0, l == 31, [cpd], [psd[1]])
                    S.op("dve", lambda e: e.tensor_copy(out=cbias[:], in_=ps[1][:, 0:1]), [psd[1]], [cpd])
                    S.op("dve", lambda e: e.memset(hx[:], 0.0), [], [cpd])
                    S.op("dve", lambda e: e.tensor_scalar(out=hx[:, 0:511], in0=ps[0][:, 0:511], scalar1=cbias[:, 0:1],
                                                          scalar2=None, op0=ALU.add), [psd[0], cpd], [cpd])
                    S.op("dve", lambda e: e.tensor_tensor(out=hu[:], in0=hx[:], in1=hx[:], op=ALU.mult), [cpd], [cpd])
                    S.op("dve", lambda e: e.tensor_scalar(out=hu[:], in0=hu[:], scalar1=0.044715, scalar2=1.0,
                                                          op0=ALU.mult, op1=ALU.add), [cpd], [cpd])
                    S.op("dve", lambda e: e.tensor_tensor(out=hu[:], in0=hu[:], in1=hx[:], op=ALU.mult), [cpd], [cpd])
                    S.op("act", lambda e: e.activation(out=hu[:], in_=hu[:], func=AF.Exp, scale=-1.5957691216057308), [cpd], [cpd])
                    S.op("dve", lambda e: e.tensor_scalar(out=hu[:], in0=hu[:], scalar1=1.0, scalar2=None, op0=ALU.add), [cpd], [cpd])
                    S.op("dve", lambda e: e.reciprocal(out=hu[:], in_=hu[:]), [cpd], [cpd])
                    S.op("dve", lambda e: e.tensor_tensor(out=hidT[:], in0=hu[:], in1=hx[:], op=ALU.mult), [cpd], [cpd])
                    if which == 0:
                        mm(ps[2][:, 0:512], w2b[:, :], hidT[:], True, True, [cpd], [psd[2]])
                        S.op("dve", lambda e: e.tensor_copy(out=kcT[r0:r1, :], in_=ps[2][r0:r1, 0:512]), [psd[2]], [kc_dep])
                    else:
                        for m in range(4):
                            mm(ps[2][:, m * 64:(m + 1) * 64], hidT[:, m * 128:(m + 1) * 128], w2b[:, 0:64], True, True, [cpd], [psd[2]])
                        S.op("dve", lambda e: e.tensor_copy(out=vc[:, :, g * 65:g * 65 + 64],
                                                            in_=ps[2][:, 0:256].rearrange("p (m e) -> p m e", e=64)), [psd[2]], [kc_dep])
            S.barrier()

        S.op("dve", lambda e: e.tensor_scalar(out=wab[:], in0=ab[:], scalar1=0.0, scalar2=None, op0=ALU.min), [nd], [wab_dep])
        S.op("act", lambda e: e.activation(out=wab[:], in_=wab[:], func=AF.Exp), [wab_dep], [wab_dep])
        qN = [sb(es, f"qN{i}", [128, 8, 128], BF16) for i in range(2)]
        qN_dep = [Dep() for _ in range(2)]
        eC = [sb(es, f"eC{i}", [128, 4, 128], BF16) for i in range(4)]
        eC_dep = [Dep() for _ in range(4)]
        Ocs = sb(es, "Ocs", [128, 4, 64], F32)
        Ocs_dep = Dep()
        rzc = sb(es, "rzc", [128, 4], F32)
        impacc = sb(es, "impacc", [128, 128], F32)
        score = sb(es, "score", [128, 128], F32)
        sc2 = sb(es, "sc2", [128, 128], F32)
        mx8 = sb(es, "mx8", [128, 8], F32)
        mx8b = sb(es, "mx8b", [128, 8], F32)
        MnegB = sb(es, "MnegB", [128, 128], BF16)
        MnegT = sb(es, "MnegT", [128, 128], BF16)
        MnegT_dep = Dep()
        tk = Dep()
        sgate = sb(es, "sgate", [128, 24], F32)
        sg_dep = Dep()
        coef = sb(es, "coef", [128, 4], F32)
        t1 = sb(es, "t1", [128, 64], F32)
        fin = Dep()
        mon = [sb(es, f"mon{i}", [128, 512], BF16) for i in range(2)]
        mon_dep = [Dep() for _ in range(2)]
        qS_p = qS.rearrange("i h p t -> i p h t")
        sctr = 0

        def nsa_prep(nidx):
            r_ = nidx % 4
            g_ = (nidx // 4) % 2
            i_ = nidx // 8
            h_ = 4 * g_ + r_
            vb_ = nidx % 2
            nk_ = 4 * i_ + 4
            k0_ = max(0, 4 * i_ - 4)
            e1, e2 = ("dve", "pool") if nidx % 2 == 0 else ("pool", "dve")
            S.op(e1, lambda e: e.tensor_tensor(out=Vsp[vb_][:, 0:nk_, :], in0=Vs[:, 0:nk_, g_ * 65:(g_ + 1) * 65],
                                               in1=wab[:, h_, 60 - 4 * i_:64].unsqueeze(2).to_broadcast([128, nk_, 65]), op=ALU.mult),
                 [nd, wab_dep], [Vxp_dep[vb_]])
            S.op(e2, lambda e: e.tensor_tensor(out=Vwp[vb_][:, 0:nk_ - k0_, :], in0=Vw[:, k0_:nk_, g_ * 65:(g_ + 1) * 65],
                                               in1=wab[:, h_, 60 - 4 * i_ + k0_:64].unsqueeze(2).to_broadcast([128, nk_ - k0_, 65]), op=ALU.mult),
                 [nd, wab_dep], [Vxp_dep[vb_]])
        for i in range(16):
            qb = i % 2
            S.dma(qN[qb][:], qS_p[i][:, 0:8, :], R=[qS_dep], W=[qN_dep[qb]])
            S.op("act", lambda e: e.activation(out=sgate[:], in_=gown[:, i, :], func=AF.Exp, scale=-1.0), [gown_dep], [sg_dep])
            S.op("dve", lambda e: e.tensor_scalar(out=sgate[:], in0=sgate[:], scalar1=1.0, scalar2=None, op0=ALU.add), [sg_dep], [sg_dep])
            S.op("dve", lambda e: e.reciprocal(out=sgate[:], in_=sgate[:]), [sg_dep], [sg_dep])
            ncm = i // 4 + 1
            nk = 4 * i + 4
            for g in range(2):
                for r in range(4):
                    h = 4 * g + r
                    for m in range(ncm):
                        d = 4 * m - i
                        partial = d >= -4
                        sbk = sctr % 4
                        sctr += 1
                        mm(ps[sbk][:, 0:128], kcT[:, m * 128:(m + 1) * 128], qN[qb][:, h, :], True, not partial,
                           [kc_dep, qN_dep[qb]], [psd[sbk]])
                        if partial:
                            mm(ps[sbk][:, 0:128], identb[:], cmask[:, d + 4, :], False, True, [cst, nd], [psd[sbk]])
                        S.op("act", lambda e: e.activation(out=eC[r][:, m, :], in_=ps[sbk][:, 0:128], func=AF.Exp,
                                                           bias=cab[:, h, d + 15:d + 16], scale=1.0), [psd[sbk], nd], [eC_dep[r]])
                    for m in range(ncm):
                        mm(ps[4][:, 0:65], eC[r][:, m, :], vc[:, m, g * 65:(g + 1) * 65], m == 0, m == ncm - 1,
                           [eC_dep[r], kc_dep], [psd[4]])
                    for m in range(ncm):
                        mm(ps[5][:, 0:128], eC[r][:, m, :], ovl[:, m, :], m == 0, m == ncm - 1, [eC_dep[r], nd], [psd[5]])
                    S.op("dve", lambda e: e.tensor_scalar_max(out=rzc[:, r:r + 1], in0=ps[4][:, 64:65], scalar1=1e-30), [psd[4]], [tk, fin])
                    S.op("dve", lambda e: e.reciprocal(out=rzc[:, r:r + 1], in_=rzc[:, r:r + 1]), [tk], [tk])
                    S.op("dve", lambda e: e.tensor_copy(out=Ocs[:, r, :], in_=ps[4][:, 0:64]), [psd[4]], [Ocs_dep, fin])
                    if r == 0:
                        S.op("dve", lambda e: e.tensor_scalar(out=impacc[:], in0=ps[5][:, 0:128], scalar1=rzc[:, r:r + 1],
                                                              scalar2=None, op0=ALU.mult), [psd[5], tk], [tk])
                    else:
                        S.op("dve", lambda e: e.scalar_tensor_tensor(out=impacc[:], in0=ps[5][:, 0:128], scalar=rzc[:, r:r + 1],
                                                                     in1=impacc[:], op0=ALU.mult, op1=ALU.add), [psd[5], tk], [tk])
                S.op("dve", lambda e: e.tensor_tensor(out=score[:], in0=impacc[:], in1=selA[:, i, :], op=ALU.mult), [tk, nd], [tk])
                S.op("dve", lambda e: e.tensor_tensor(out=score[:], in0=score[:], in1=selB[:, i, :], op=ALU.add), [tk, nd], [tk])
                S.op("dve", lambda e: e.max(out=mx8[:], in_=score[:]), [tk], [tk])
                S.op("dve", lambda e: e.match_replace(out=sc2[:], in_to_replace=mx8[:], in_values=score[:], imm_value=-2.0), [tk], [tk])
                S.op("dve", lambda e: e.max(out=mx8b[:], in_=sc2[:]), [tk], [tk])
                S.op("dve", lambda e: e.tensor_scalar(out=MnegB[:], in0=score[:], scalar1=mx8b[:, 7:8], scalar2=NEGM,
                                                      op0=ALU.is_lt, op1=ALU.mult), [tk], [tk])
                S.op("pe", lambda e: e.transpose(out=psb[:, 0:128], in_=MnegB[:], identity=identb[:]), [tk, cst], [psb_dep])
                S.op("dve", lambda e: e.tensor_copy(out=MnegT[:], in_=psb[:, 0:128]), [psb_dep], [MnegT_dep])
                for r in range(4):
                    h = 4 * g + r
                    osb = 6 if r % 2 == 0 else 5
                    nidx = (i * 2 + g) * 4 + r
                    vb = nidx % 2
                    if nidx == 0:
                        nsa_prep(0)
                    if nidx + 1 < 128:
                        nsa_prep(nidx + 1)
                    k0 = max(0, 4 * i - 4)
                    steps = []
                    for gq in range(nk // 4):
                        bank = pctr[0] % 4
                        pctr[0] += 1

                        def qk(gq=gq, bank=bank, i=i, h=h, qb=qb):
                            for q in range(4):
                                k = 4 * gq + q
                                diag = k >= 4 * i
                                o = ps[bank][:, q * 128:(q + 1) * 128]
                                mm(o, KsT[:, k * 128:(k + 1) * 128], qN[qb][:, h, :], True, False, [nd, qN_dep[qb]], [psd[bank]])
                                mm(o, Rx[:, k * 128:(k + 1) * 128], MnegT[:], False, not diag, [nd, MnegT_dep], [psd[bank]])
                                if diag:
                                    mm(o, identb[:], diagm[:, k - 4 * i, :], False, True, [cst], [psd[bank]])
                            S.op("act", lambda e: e.activation(out=pT[bank][:], in_=ps[bank][:, :], func=AF.Exp), [psd[bank]], [pT_dep[bank]])

                        def pv(gq=gq, bank=bank, osb=osb, nk=nk, vb=vb):
                            for q in range(4):
                                k = 4 * gq + q
                                mm(ps[osb][:, 0:65], pT[bank][:, q * 128:(q + 1) * 128], Vsp[vb][:, k, :], k == 0, k == nk - 1,
                                   [pT_dep[bank], Vxp_dep[vb]], [psd[osb]])
                        steps.append((qk, pv))
                    for gq in range((nk - k0) // 4):
                        bank = pctr[0] % 4
                        pctr[0] += 1

                        def qk(gq=gq, bank=bank, i=i, h=h, qb=qb, k0=k0):
                            for q in range(4):
                                k = k0 + 4 * gq + q
                                wk = k - (4 * i - 4)
                                o = ps[bank][:, q * 128:(q + 1) * 128]
                                mm(o, KwT[:, k * 128:(k + 1) * 128], qN[qb][:, h, :], True, False, [nd, qN_dep[qb]], [psd[bank]])
                                mm(o, identb[:], winm[:, wk, :], False, True, [cst, nd], [psd[bank]])
                            S.op("act", lambda e: e.activation(out=pT[bank][:], in_=ps[bank][:, :], func=AF.Exp), [psd[bank]], [pT_dep[bank]])

                        def pv(gq=gq, bank=bank, k0=k0, nk=nk, vb=vb):
                            for q in range(4):
                                k = k0 + 4 * gq + q
                                mm(ps[4][:, 0:65], pT[bank][:, q * 128:(q + 1) * 128], Vwp[vb][:, k - k0, :], k == k0, k == nk - 1,
                                   [pT_dep[bank], Vxp_dep[vb]], [psd[4]])
                        steps.append((qk, pv))
                    run_pipe(steps)
                    S.op("dve", lambda e: e.tensor_scalar_max(out=coef[:, 1:2], in0=ps[osb][:, 64:65], scalar1=1e-30), [psd[osb]], [fin])
                    S.op("dve", lambda e: e.tensor_scalar_max(out=coef[:, 2:3], in0=ps[4][:, 64:65], scalar1=1e-30), [psd[4]], [fin])
                    S.op("dve", lambda e: e.reciprocal(out=coef[:, 1:3], in_=coef[:, 1:3]), [fin], [fin])
                    S.op("dve", lambda e: e.tensor_copy(out=coef[:, 0:1], in_=rzc[:, r:r + 1]), [fin, tk], [fin])
                    S.op("dve", lambda e: e.tensor_tensor(out=coef[:, 0:3], in0=coef[:, 0:3], in1=sgate[:, 3 * h:3 * h + 3], op=ALU.mult), [fin, sg_dep], [fin])
                    S.op("dve", lambda e: e.tensor_scalar(out=t1[:], in0=Ocs[:, r, :], scalar1=coef[:, 0:1], scalar2=None, op0=ALU.mult), [fin, Ocs_dep], [fin])
                    S.op("dve", lambda e: e.scalar_tensor_tensor(out=t1[:], in0=ps[osb][:, 0:64], scalar=coef[:, 1:2], in1=t1[:],
                                                                 op0=ALU.mult, op1=ALU.add), [fin, psd[osb]], [fin])
                    S.op("dve", lambda e: e.scalar_tensor_tensor(out=mon[qb][:, h * 64:(h + 1) * 64], in0=ps[4][:, 0:64], scalar=coef[:, 2:3],
                                                                 in1=t1[:], op0=ALU.mult, op1=ALU.add), [fin, psd[4]], [mon_dep[qb], fin])
            S.dma(mixS[i * 128:(i + 1) * 128, 0:512], mon[qb][:], R=[mon_dep[qb]], W=[mixS_dep])
        S.barrier()

    if stage <= 3:
        es_attn.close()
        es_all.close()
        return nc, S

    S.mute = False
    es_attn.close()
    es_tail = ExitStack()
    hres = sb(es_tail, "hres", [128, 16, D], F32)
    hres_dep = [Dep() for _ in range(16)]
    xnT = sb(es_tail, "xntok", [128, 16, D], BF16)
    xnT_dep = Dep()
    gate = sb(es_tail, "gate", [128, 16, NE], F32)
    gate_dep = Dep()
    ssc = sb(es_tail, "ssc", [128, 4], F32)
    nrm = Dep()
    sqt_box = [None]

    def rms_rstd(src, n, col, R):
        sqt = sqt_box[0]
        S.op("dve", lambda e: e.tensor_tensor(out=sqt[:, 0:n], in0=src, in1=src, op=ALU.mult), list(R) + [nrm], [nrm])
        S.op("dve", lambda e: e.reduce_sum(out=ssc[:, col:col + 1], in_=sqt[:, 0:n], axis=AX.X), [nrm], [nrm])
        S.op("act", lambda e: e.activation(out=ssc[:, col:col + 1], in_=ssc[:, col:col + 1], func=AF.Sqrt,
                                           bias=epsc[:, 0:1], scale=1.0 / n), [nrm, cst], [nrm])
        S.op("dve", lambda e: e.reciprocal(out=ssc[:, col:col + 1], in_=ssc[:, col:col + 1]), [nrm], [nrm])

    def load_w_bf16(dst, src, nchunk, ncol, stg, stg_dep, wdep, ctr):
        for dc in range(nchunk):
            k = ctr[0] % len(stg)
            ctr[0] += 1
            S.dma(stg[k][:, 0:ncol], src[dc * 128:(dc + 1) * 128, :], W=[stg_dep[k]])
            copy_on(cast_eng(), dst[:, dc, :], stg[k][:, 0:ncol], [stg_dep[k]], [wdep])

    with ExitStack() as es:
        sqt_box[0] = sb(es, "sqt4", [128, D], F32)
        woutb = sb(es, "woutb", [128, 8, D], BF16)
        stg = [sb(es, f"stg4{i}", [128, D], F32) for i in range(2)]
        stg_dep = [Dep() for _ in range(2)]
        gnb = sb(es, "gnb", [128, D], F32)
        ln2b = sb(es, "ln2b", [128, D], F32)
        wrf = sb(es, "wrf", [128, 8, NE], F32)
        brb = sb(es, "brb", [128, NE], F32)
        bdnf = sb(es, "bdnf", [NE, D], F32)
        mixb = [sb(es, f"mixb{i}", [128, D], BF16) for i in range(2)]
        mixb_dep = [Dep() for _ in range(2)]
        mixn = sb(es, "mixn", [128, D], BF16)
        mixT = sb(es, "mixT", [128, 8, 128], BF16)
        xn = sb(es, "xn", [128, D], F32)
        xnTf = sb(es, "xnTf", [128, 8, 128], F32)
        lg = sb(es, "lg", [128, NE], F32)
        ex = sb(es, "ex", [128, NE], F32)
        mxr = sb(es, "mxr", [128, 8], F32)
        gT = sb(es, "gT", [NE, 128], F32)
        wd = Dep()
        p4 = Dep()
        ctr = [0]
        load_w_bf16(woutb, wout_d, 8, D, stg, stg_dep, wd, ctr)
        S.dma(gnb[:], gn_d, W=[wd])
        S.dma(ln2b[:], ln2_d, W=[wd])
        S.dma(wrf[:], wr_d.rearrange("(c p) e -> p c e", p=128), W=[wd])
        S.dma(brb[:], br_d, W=[wd])
        S.dma(bdnf[:], bdn_d, W=[wd])
        def g4(n):
            if p4stop <= n:
                S.mute = True
        for i in range(nblk4):
            mb = i % 2
            S.mute = False
            S.dma(mixb[mb][:], mixS[i * 128:(i + 1) * 128, :], R=[mixS_dep], W=[mixb_dep[mb]])
            S.dma(hres[:, i, :], xo_d[i * 128:(i + 1) * 128, :], W=[hres_dep[i]])
            g4(1)
            for half in range(2):
                hs = slice(half * 512, (half + 1) * 512)
                rms_rstd(mixb[mb][:, hs], 512, half, [mixb_dep[mb]])
                S.op("dve", lambda e: e.scalar_tensor_tensor(out=mixn[:, hs], in0=mixb[mb][:, hs], scalar=ssc[:, half:half + 1],
                                                             in1=gnb[:, hs], op0=ALU.mult, op1=ALU.mult), [mixb_dep[mb], nrm, wd], [p4])
            g4(2)
            for c in range(8):
                S.op("pe", lambda e: e.transpose(out=psb[:, c * 128:(c + 1) * 128], in_=mixn[:, c * 128:(c + 1) * 128],
                                                 identity=identb[:]), [p4, cst], [psb_dep])
            S.op("act", lambda e: e.copy(out=mixT[:].rearrange("p c t -> p (c t)"), in_=psb[:, :]), [psb_dep], [p4])
            g4(3)
            for half in range(2):
                hs = slice(half * 512, (half + 1) * 512)
                for c in range(8):
                    mm(ps[half][:, :], mixT[:, c, :], woutb[:, c, hs], c == 0, c == 7, [p4, wd], [psd[half]])
                S.op("dve", lambda e: e.tensor_tensor(out=hres[:, i, hs], in0=ps[half][:, :], in1=hres[:, i, hs], op=ALU.add),
                     [psd[half], hres_dep[i]], [hres_dep[i]])
            rms_rstd(hres[:, i, :], D, 2, [hres_dep[i]])
            g4(4)
            S.op("dve", lambda e: e.scalar_tensor_tensor(out=xn[:], in0=hres[:, i, :], scalar=ssc[:, 2:3], in1=ln2b[:],
                                                         op0=ALU.mult, op1=ALU.mult), [hres_dep[i], nrm, wd], [p4])
            S.op("pool", lambda e: e.tensor_copy(out=xnT[:, i, :], in_=xn[:]), [p4], [xnT_dep])
            for c in range(8):
                b = 2 + c // 4
                g4(5)
                S.op("pe", lambda e: e.transpose(out=ps[b][:, (c % 4) * 128:(c % 4 + 1) * 128], in_=xn[:, c * 128:(c + 1) * 128],
                                                 identity=identf[:]), [p4, cst], [psd[b]])
            for b2 in range(2):
                S.op("act", lambda e: e.copy(out=xnTf[:, b2 * 4:(b2 + 1) * 4, :], in_=ps[2 + b2][:, :].rearrange("p (c t) -> p c t", t=128)),
                     [psd[2 + b2]], [p4])
            for c in range(8):
                mm(ps[4][:, 0:NE], xnTf[:, c, :], wrf[:, c, :], c == 0, c == 7, [p4, wd], [psd[4]])
            g4(7)
            S.op("dve", lambda e: e.tensor_tensor(out=lg[:], in0=ps[4][:, 0:NE], in1=brb[:], op=ALU.add), [psd[4], wd], [p4])
            S.op("dve", lambda e: e.max(out=mxr[:], in_=lg[:]), [p4], [p4])
            S.op("dve", lambda e: e.tensor_scalar(out=mxr[:, 4:5], in0=mxr[:, 0:1], scalar1=-1.0, scalar2=None, op0=ALU.mult), [p4], [p4])
            S.op("act", lambda e: e.activation(out=ex[:], in_=lg[:], func=AF.Exp, bias=mxr[:, 4:5], scale=1.0), [p4], [p4])
            S.op("dve", lambda e: e.scalar_tensor_tensor(out=ex[:], in0=lg[:], scalar=mxr[:, 3:4], in1=ex[:],
                                                         op0=ALU.is_ge, op1=ALU.mult), [p4], [p4])
            S.op("dve", lambda e: e.reduce_sum(out=mxr[:, 5:6], in_=ex[:], axis=AX.X), [p4], [p4])
            S.op("dve", lambda e: e.reciprocal(out=mxr[:, 5:6], in_=mxr[:, 5:6]), [p4], [p4])
            S.op("dve", lambda e: e.tensor_scalar(out=gate[:, i, :], in0=ex[:], scalar1=mxr[:, 5:6], scalar2=None, op0=ALU.mult),
                 [p4], [gate_dep])
            g4(8)
        S.mute = False
        S.barrier()

    if stage == 4:
        dbg_h = nc.dram_tensor("dbg_h", [128, 16, D], F32, kind="ExternalOutput").ap()
        dbg_g = nc.dram_tensor("dbg_g", [128, 16, NE], F32, kind="ExternalOutput").ap()
        dbg_x = nc.dram_tensor("dbg_x", [128, 16, D], BF16, kind="ExternalOutput").ap()
        S.dma(dbg_h, hres[:])
        S.dma(dbg_g, gate[:])
        S.dma(dbg_x, xnT[:])
        S.barrier()
        es_tail.close()
        es_all.close()
        return nc, S

    S.mute = skip5
    with ExitStack() as es:
        C = CAP
        NSC = C // 128
        wupb = sb(es, "wupb", [128, 8, 2048], BF16)
        wdnb = sb(es, "wdnb", [128, 8, D], BF16)
        wup_dep = Dep()
        wdn_dep = Dep()
        NSTG5 = 4
        stg = [sb(es, f"stg5{i}", [128, 512], F32) for i in range(NSTG5)]
        stg_dep = [Dep() for _ in range(NSTG5)]
        bupc = sb(es, "bupc", [128, NE, 16], F32)
        browb = sb(es, "browb", [1, D], BF16)
        brow_dep = Dep()
        bd = Dep()
        S.dma(bupc[:], bup_d, W=[bd])
        iotac = sb(es, "iotac", [128, C], F32)
        S.dma(iotac[:], iota_d, W=[bd])
        Mf = sb(es, "Mf", [128, 16, NE], F32)
        pos = sb(es, "pos", [128, 16, NE], F32)
        dsp = Dep()
        with ExitStack() as est:
            trisb = sb(est, "trisb", [128, 128], BF16)
            Mb = sb(est, "Mb", [128, 16, NE], BF16)
            tot5 = sb(est, "tot5", [128, 16, NE], F32)
            pre5 = sb(est, "pre5", [128, 16, NE], F32)
            S.op("dve", lambda g: g.tensor_tensor(out=trisb[:], in0=trif[:], in1=identf[:], op=ALU.subtract), [cst], [dsp])
            S.op("dve", lambda g: g.tensor_scalar(out=Mf[:], in0=gate[:], scalar1=0.0, scalar2=None, op0=ALU.is_gt), [gate_dep], [dsp])
            S.op("dve", lambda g: g.tensor_copy(out=Mb[:], in_=Mf[:]), [dsp], [dsp])
            mflat = Mb[:].rearrange("p a b -> p (a b)")
            mm(ps[0][:, :], trisb[:], mflat, True, True, [dsp], [psd[0]])
            mm(ps[1][:, :], onesb[:], mflat, True, True, [dsp, cst], [psd[1]])
            S.op("dve", lambda g: g.tensor_copy(out=tot5[:].rearrange("p a b -> p (a b)"), in_=ps[1][:, :]), [psd[1]], [dsp])
            S.op("dve", lambda g: g.memset(pre5[:, 0, :], 0.0), [], [dsp])
            for k in range(1, 16):
                S.op("dve", lambda g: g.tensor_tensor(out=pre5[:, k, :], in0=pre5[:, k - 1, :], in1=tot5[:, k - 1, :], op=ALU.add), [dsp], [dsp])
            S.op("dve", lambda g: g.tensor_tensor(out=pos[:].rearrange("p a b -> p (a b)"), in0=ps[0][:, :],
                                                  in1=pre5[:].rearrange("p a b -> p (a b)"), op=ALU.add), [psd[0], dsp], [dsp])
            S.barrier()

        Sel = sb(es, "Sel", [128, 16, C], BF16)
        Sel_dep = Dep()
        SelTb = [sb(es, f"SelTb{i}", [128, NSC, 128], BF16) for i in range(2)]
        SelTb_dep = [Dep() for _ in range(2)]
        XeT = sb(es, "XeT", [128, 8, C], BF16)
        XeT_dep = Dep()
        actT = sb(es, "actT5", [128, 8, C], BF16)
        actT_dep = Dep()
        assert NSC * D == 8 * C
        ye = XeT[:].rearrange("p (s two) c -> p s (two c)", two=2)
        ye_dep = XeT_dep
        gcs = [sb(es, f"gcs{i}", [128, C], F32) for i in range(1)] * 2
        sgs = [sb(es, f"sgs{i}", [128, C], F32) for i in range(1)] * 2
        lcs = [sb(es, f"lcs{i}", [128, C], F32) for i in range(1)] * 2
        gc_dep = [Dep()] * 2
        sg_dep5 = [Dep()] * 2
        lc_dep = [Dep()] * 2
        sctr = [0]

        def load_up(e):
            for dc in range(8):
                for half in range(4):
                    k = sctr[0] % NSTG5
                    sctr[0] += 1
                    S.dma(stg[k][:], wup_d[e, dc * 128:(dc + 1) * 128, half * 512:(half + 1) * 512], W=[stg_dep[k]])
                    copy_on("pool", wupb[:, dc, half * 512:(half + 1) * 512], stg[k][:], [stg_dep[k]], [wup_dep])

        def load_dn(e):
            for fc in range(8):
                for half in range(2):
                    k = sctr[0] % NSTG5
                    sctr[0] += 1
                    S.dma(stg[k][:], wdn_d[e, fc * 128:(fc + 1) * 128, half * 512:(half + 1) * 512], W=[stg_dep[k]])
                    copy_on("pool", wdnb[:, fc, half * 512:(half + 1) * 512], stg[k][:], [stg_dep[k]], [wdn_dep])

        load_up(0)
        load_dn(0)
        uc = 0
        dcn = 0
        tcn = 0
        for e_ in range(n_experts):
            for half in range(2):
                kk = sctr[0] % NSTG5
                sctr[0] += 1
                S.dma(stg[kk][0:1, :], bdn_d[e_:e_ + 1, half * 512:(half + 1) * 512], W=[stg_dep[kk]])
                S.op("act", lambda g: g.copy(out=browb[0:1, half * 512:(half + 1) * 512], in_=stg[kk][0:1, :]), [stg_dep[kk]], [brow_dep])
            for blk in range(16):
                S.op("dve", lambda g: g.tensor_scalar(out=Sel[:, blk, :], in0=iotac[:], scalar1=pos[:, blk, e_:e_ + 1],
                                                      scalar2=Mf[:, blk, e_:e_ + 1], op0=ALU.is_equal, op1=ALU.mult), [dsp, bd], [Sel_dep])
            for dc in range(8):
                bX = 4 + dcn % 3
                dcn += 1
                for blk in range(16):
                    mm(ps[bX][:, 0:C], xnT[:, blk, dc * 128:(dc + 1) * 128], Sel[:, blk, :], blk == 0, blk == 15,
                       [xnT_dep, Sel_dep], [psd[bX]])
                S.op("act", lambda g: g.copy(out=XeT[:, dc, :], in_=ps[bX][:, 0:C]), [psd[bX]], [XeT_dep])
            for fc in range(8):
                bG = (uc % 2) * 2
                bL = bG + 1
                tb = uc % 2
                uc += 1
                for dc in range(8):
                    mm(ps[bG][:, 0:C], wupb[:, dc, fc * 128:(fc + 1) * 128], XeT[:, dc, :], dc == 0, dc == 7, [wup_dep, XeT_dep], [psd[bG]])
                for dc in range(8):
                    mm(ps[bL][:, 0:C], wupb[:, dc, 1024 + fc * 128:1024 + (fc + 1) * 128], XeT[:, dc, :], dc == 0, dc == 7,
                       [wup_dep, XeT_dep], [psd[bL]])
                S.op("dve", lambda g: g.tensor_scalar(out=gcs[tb][:], in0=ps[bG][:, 0:C], scalar1=bupc[:, e_, fc:fc + 1], scalar2=7.0,
                                                      op0=ALU.add, op1=ALU.min), [psd[bG], bd], [gc_dep[tb]])
                S.op("act", lambda g: g.activation(out=sgs[tb][:], in_=gcs[tb][:], func=AF.Sigmoid, scale=1.702), [gc_dep[tb]], [sg_dep5[tb]])
                S.op("dve", lambda g: g.tensor_scalar(out=lcs[tb][:], in0=ps[bL][:, 0:C], scalar1=bupc[:, e_, 8 + fc:9 + fc], scalar2=7.0,
                                                      op0=ALU.add, op1=ALU.min), [psd[bL], bd], [lc_dep[tb]])
                S.op("dve", lambda g: g.tensor_scalar(out=lcs[tb][:], in0=lcs[tb][:], scalar1=-7.0, scalar2=1.0,
                                                      op0=ALU.max, op1=ALU.add), [lc_dep[tb]], [lc_dep[tb]])
                S.op("pool", lambda g: g.tensor_tensor(out=gcs[tb][:], in0=gcs[tb][:], in1=sgs[tb][:], op=ALU.mult), [sg_dep5[tb]], [gc_dep[tb]])
                S.op("pool", lambda g: g.tensor_tensor(out=actT[:, fc, :], in0=gcs[tb][:], in1=lcs[tb][:], op=ALU.mult),
                     [gc_dep[tb], lc_dep[tb]], [actT_dep])
            if e_ + 1 < n_experts:
                load_up(e_ + 1)
            for sc in range(NSC):
                for half in range(2):
                    hs = slice(half * 512, (half + 1) * 512)
                    bD = 4 + dcn % 3
                    dcn += 1
                    for fc in range(8):
                        mm(ps[bD][:, :], actT[:, fc, sc * 128:(sc + 1) * 128], wdnb[:, fc, hs], fc == 0, False,
                           [actT_dep, wdn_dep], [psd[bD]])
                    mm(ps[bD][:, :], onesb[0:1, :], browb[0:1, hs], False, True, [brow_dep, cst], [psd[bD]])
                    S.op("act", lambda g: g.copy(out=ye[:, sc, hs], in_=ps[bD][:, :]), [psd[bD]], [ye_dep])
            if e_ + 1 < n_experts:
                load_dn(e_ + 1)
            for blk in range(16):
                tbf = tcn % 2
                tcn += 1
                for sc in range(NSC):
                    S.op("pe", lambda g: g.transpose(out=psb[:, sc * 128:(sc + 1) * 128], in_=Sel[:, blk, sc * 128:(sc + 1) * 128],
                                                     identity=identb[:]), [Sel_dep, cst], [psb_dep])
                S.op("act", lambda g: g.copy(out=SelTb[tbf][:].rearrange("p c t -> p (c t)"), in_=psb[:, 0:NSC * 128]), [psb_dep], [SelTb_dep[tbf]])
                for half in range(2):
                    hs = slice(half * 512, (half + 1) * 512)
                    bY = 4 + dcn % 3
                    dcn += 1
                    for sc in range(NSC):
                        mm(ps[bY][:, :], SelTb[tbf][:, sc, :], ye[:, sc, hs], sc == 0, sc == NSC - 1, [SelTb_dep[tbf], ye_dep], [psd[bY]])
                    S.op("dve", lambda g: g.scalar_tensor_tensor(out=hres[:, blk, hs], in0=ps[bY][:, :], scalar=gate[:, blk, e_:e_ + 1],
                                                                 in1=hres[:, blk, hs], op0=ALU.mult, op1=ALU.add),
                         [psd[bY], gate_dep, hres_dep[blk]], [hres_dep[blk]])
        S.barrier()

    S.mute = skip6
    with ExitStack() as es:
        sqt_box[0] = sb(es, "sqt6", [128, D], F32)
        wpgb = sb(es, "wpgb", [128, 8, D], BF16)
        wpleb = sb(es, "wpleb", [128, 2, D], BF16)
        pTb = sb(es, "pTb", [128, 2, 2048], BF16)
        stg = [sb(es, f"stg6{i}", [128, 2048], F32) for i in range(2)]
        stg_dep = [Dep() for _ in range(2)]
        lnpb = sb(es, "lnpb", [128, D], F32)
        lnfb = sb(es, "lnfb", [128, D], F32)
        hn = sb(es, "hn", [128, D], BF16)
        hnT = sb(es, "hnT", [128, 8, 128], BF16)
        sig = [sb(es, f"sig{i}", [128, 512], F32) for i in range(2)]
        outt = [sb(es, f"outt{i}", [128, D], F32) for i in range(2)]
        outt_dep = [Dep() for _ in range(2)]
        wd = Dep()
        p6 = Dep()
        out_dep = Dep()
        ctr = [0]
        load_w_bf16(wpgb, wpg_d, 8, D, stg, stg_dep, wd, ctr)
        load_w_bf16(wpleb, wple_d, 2, D, stg, stg_dep, wd, ctr)
        for c2 in range(2):
            k = ctr[0] % 2
            ctr[0] += 1
            S.dma(stg[k][:], pTo_d[c2], W=[stg_dep[k]])
            copy_on(cast_eng(), pTb[:, c2, :], stg[k][:], [stg_dep[k]], [wd])
        S.dma(lnpb[:], lnp_d, W=[wd])
        S.dma(lnfb[:], lnf_d, W=[wd])
        for i in range(16):
            ob = i % 2
            rms_rstd(hres[:, i, :], D, 0, [hres_dep[i]])
            S.op("dve", lambda e: e.scalar_tensor_tensor(out=hn[:], in0=hres[:, i, :], scalar=ssc[:, 0:1], in1=lnpb[:],
                                                         op0=ALU.mult, op1=ALU.mult), [hres_dep[i], nrm, wd], [p6])
            for c in range(8):
                S.op("pe", lambda e: e.transpose(out=psb[:, c * 128:(c + 1) * 128], in_=hn[:, c * 128:(c + 1) * 128],
                                                 identity=identb[:]), [p6, cst], [psb_dep])
            S.op("act", lambda e: e.copy(out=hnT[:].rearrange("p c t -> p (c t)"), in_=psb[:, :]), [psb_dep], [p6])
            for half in range(2):
                hs = slice(half * 512, (half + 1) * 512)
                for c in range(8):
                    mm(ps[half][:, :], hnT[:, c, :], wpgb[:, c, hs], c == 0, c == 7, [p6, wd], [psd[half]])
                for c2 in range(2):
                    mm(ps[2 + half][:, :], pTb[:, c2, i * 128:(i + 1) * 128], wpleb[:, c2, hs], c2 == 0, c2 == 1, [wd], [psd[2 + half]])
                S.op("act", lambda e: e.activation(out=sig[half][:], in_=ps[half][:, :], func=AF.Exp, scale=-1.0), [psd[half]], [p6])
                S.op("dve", lambda e: e.tensor_scalar(out=sig[half][:], in0=sig[half][:], scalar1=1.0, scalar2=None, op0=ALU.add), [p6], [p6])
                S.op("dve", lambda e: e.reciprocal(out=sig[half][:], in_=sig[half][:]), [p6], [p6])
                S.op("dve", lambda e: e.tensor_tensor(out=sig[half][:], in0=ps[2 + half][:, :], in1=sig[half][:], op=ALU.mult),
                     [psd[2 + half], p6], [p6])
                S.op("dve", lambda e: e.tensor_tensor(out=hres[:, i, hs], in0=sig[half][:], in1=hres[:, i, hs], op=ALU.add),
                     [p6, hres_dep[i], nrm], [hres_dep[i]])
            rms_rstd(hres[:, i, :], D, 1, [hres_dep[i]])
            S.op("dve", lambda e: e.scalar_tensor_tensor(out=outt[ob][:], in0=hres[:, i, :], scalar=ssc[:, 1:2], in1=lnfb[:],
                                                         op0=ALU.mult, op1=ALU.mult), [hres_dep[i], nrm, wd], [outt_dep[ob]])
            S.dma(out_d[i * 128:(i + 1) * 128, :], outt[ob][:], R=[outt_dep[ob]], W=[out_dep])
        S.barrier()
    es_tail.close()

    if stage <= 3:
        dbg_f = nc.dram_tensor("dbg_ff", [128, 64 * 8 + 16 * 24], F32, kind="ExternalOutput").ap()
        S.dma(dbg_f[:, 0:512], ffall[:].rearrange("p a b -> p (a b)"))
        S.dma(dbg_f[:, 512:896], gown[:].rearrange("p a b -> p (a b)"))
        S.barrier()
        es_all.close()
        return nc, S

    es_all.close()
    return nc, S


def own_tokens(j):
    return np.concatenate([np.arange(512 * i + 128 * j, 512 * i + 128 * j + 128) for i in range(16)])


def const_tables(j):
    p = np.arange(128)
    c = {}
    c["identb"] = _bf(np.eye(128, dtype=np.float32))
    c["identf"] = np.eye(128, dtype=np.float32)
    c["trif"] = (p[:, None] <= p[None, :]).astype(np.float32)
    sl = p[:, None]
    tl = p[None, :]
    dm = np.zeros((128, 4, 128), np.float32)
    for kk in range(4):
        dist = 128 * (j - kk) + tl - sl
        dm[:, kk, :] = np.where(dist >= 0, 0.0, NEGM)
    c["diagm"] = _bf(dm)
    wm = np.zeros((128, 8, 128), np.float32)
    for wk in range(8):
        dist = 128 * (j + 4 - wk) + tl - sl
        wm[:, wk, :] = np.where((dist >= 0) & (dist < 512), 0.0, NEGM)
    c["winm"] = _bf(wm)
    cm = np.zeros((128, 5, 128), np.float32)
    for dd in range(5):
        d = dd - 4
        cond = (512 * d + 16 * sl - tl - 128 * j + 31) <= 0
        cm[:, dd, :] = np.where(cond, 0.0, NEGM)
    c["cmask"] = _bf(cm)
    slopes = np.exp2(-8.0 * np.arange(1, 9, dtype=np.float32) / 8).astype(np.float32)
    rel = np.arange(64)
    ab = slopes[None, :, None] * (p[:, None, None] - 127 - 128 * (rel[None, None, :] + j - 3))
    c["ab"] = np.ascontiguousarray(ab[:, :, ::-1]).astype(np.float32)
    dd = np.arange(16) - 15
    cab = slopes[None, :, None] * (16 * p[:, None, None] + 512 * dd[None, None, :] - 128 * j - 96)
    c["cab"] = cab.astype(np.float32)
    n = np.arange(128)
    selA = np.zeros((128, 16, 128), np.float32)
    selB = np.zeros((128, 16, 128), np.float32)
    for i in range(16):
        cur = (512 * i + 128 * j + p) // 64
        valid = n[None, :] <= cur[:, None]
        forced = valid & ((n[None, :] == 0) | (n[None, :] == cur[:, None]) | (n[None, :] == cur[:, None] - 1))
        selA[:, i, :] = (valid & ~forced)
        selB[:, i, :] = np.where(forced, 1e9, np.where(valid, 0.0, -1.0))
    c["selA"] = _bf(selA)
    c["selB"] = _bf(selB)
    ws = np.zeros((128, 16, 64), np.float32)
    for i in range(16):
        ws[:, i, :] = (np.arange(64)[None, :] <= 4 * i + j)
    c["wsel"] = ws
    s = np.arange(T)
    c["Rexp"] = _bf((n[:, None] == (s[None, :] // 64)).astype(np.float32))
    cc = np.arange(512)
    ov = ((cc[:, None] * 16 < n[None, :] * 64 + 64) & (cc[:, None] * 16 + 31 >= n[None, :] * 64)).astype(np.float32)
    ov[511, :] = 0.0
    c["ovl"] = _bf(ov.reshape(4, 128, 128).transpose(1, 0, 2))
    c["iotac"] = np.ascontiguousarray(np.broadcast_to(np.arange(CAP, dtype=np.float32)[None, :], (128, CAP)))
    return c


def make_in_maps(x, p, ln1, w_in, b_fg, w_cmp1_k, w_cmp2_k, pe_cmp_k, w_cmp1_v, w_cmp2_v, pe_cmp_v, gn_nsa, gn_fox,
                 w_out, ln2, w_router, b_router, w_up, b_up, w_down, b_down, ln_ple, w_ple, w_ple_gate, ln_f, ne=NE):
    f = lambda a: np.ascontiguousarray(np.asarray(a, dtype=np.float32))
    x = f(x); p = f(p); w = f(w_in)[0]
    q_n = w[:, 0:512]; k_c = w[:, 512:640]; v_c = w[:, 640:768]; k_s = w[:, 768:896]; v_s = w[:, 896:1024]
    k_w = w[:, 1024:1152]; v_w = w[:, 1152:1280]; g_n = w[:, 1280:1304]; q_f = w[:, 1304:1816]
    k_f = w[:, 1816:2328]; v_f = w[:, 2328:2840]; f_f = w[:, 2840:2848]
    bc = lambda v: f(np.broadcast_to(np.asarray(v, np.float32).reshape(1, -1), (128, np.asarray(v).size)))
    shared = {
        "wA": f(np.concatenate([k_f, k_s, k_w, k_c, v_c], 1)),
        "wB": f(np.concatenate([v_f, v_s, v_w, f_f, g_n], 1)),
        "wQ": f(np.concatenate([q_n, q_f], 1)),
        "ln1c": f(np.asarray(ln1, np.float32)[0].reshape(8, 128).T),
        "bfg": bc(np.asarray(b_fg)[0]),
        "gnb": bc(np.concatenate([np.asarray(gn_nsa)[0], np.asarray(gn_fox)[0]])),
        "wout": f(w_out)[0], "ln2b": bc(np.asarray(ln2)[0]), "wr": f(w_router)[0], "brb": bc(np.asarray(b_router)[0]),
        "wup": f(np.asarray(w_up)[0, :ne]), "wdn": f(np.asarray(w_down)[0, :ne]), "bdn": f(np.asarray(b_down)[0]),
        "bupc": f(np.asarray(b_up, np.float32)[0].reshape(NE, 16, 128).transpose(2, 0, 1)),
        "lnpb": bc(np.asarray(ln_ple)[0]), "wple": f(w_ple)[0], "wpg": f(w_ple_gate)[0], "lnfb": bc(np.asarray(ln_f)),
    }
    for nm, w1, w2, pe in (("k", w_cmp1_k, w_cmp2_k, pe_cmp_k), ("v", w_cmp1_v, w_cmp2_v, pe_cmp_v)):
        w1r = np.asarray(w1, np.float32)[0].reshape(32, 64, 128).transpose(1, 0, 2)
        shared["w1" + nm] = f(np.concatenate([w1r, w1r], 0))
        peT = np.asarray(pe, np.float32)[0].T
        peT = np.concatenate([peT, peT], 0)
        shared["pe" + nm] = f(np.stack([peT, peT], -1))
    w2k = np.asarray(w_cmp2_k, np.float32)[0]
    shared["w2k"] = f(np.concatenate([w2k, w2k], 1))
    shared["w2v"] = f(np.asarray(w_cmp2_v, np.float32)[0])
    maps = []
    for c in range(NCORES):
        b, j = c // 4, c % 4
        tok = own_tokens(j)
        m = dict(shared)
        m["xT"] = f(x[b].T.reshape(8, 128, T))
        m["xTo"] = f(x[b][tok].T.reshape(8, 128, 2048))
        m["xo"] = f(x[b][tok])
        m["pTo"] = f(p[0, b][tok].T.reshape(2, 128, 2048))
        m.update(const_tables(j))
        maps.append(m)
    return maps


_CACHE = {}


def kernel(**inputs):
    maps = make_in_maps(**inputs)
    if "nc" not in _CACHE:
        _CACHE["nc"] = build_program()[0]
    nc = _CACHE["nc"]
    res = run_bass_kernel_spmd(nc, maps, core_ids=list(range(NCORES)))
    out = np.zeros((2, T, D), np.float32)
    for c in range(NCORES):
        b, j = c // 4, c % 4
        out[b, own_tokens(j)] = np.asarray(res.results[c]["out"], np.float32).reshape(2048, D)
    return out
```
